# Optimizing a Trainium2 kernel written in Bass

```python
import math
import jax
import jax.numpy as jnp
from jax import lax
import numpy as np

D_MODEL = 1024
BATCH = 8
SEQ = 8192
DEPTH = 2

CHUNK = 64
Q_BLOCK = 128
EPS = 1e-6
RET_HEADS = 4
RET_DK = 64
RET_DV = 64
RET_THETA = 10000.0
DIFF_HEADS = 4
DIFF_DK = 64
DIFF_DV = 2 * DIFF_DK
ROPE_THETA = 500000.0
ROPE_DIMS = DIFF_DK // 4
GLA_HEADS = 4
GLA_DK = 32
GLA_DV = 64
GLA_RANK = 16
GLA_TAU = 16.0
RET_W = RET_HEADS * RET_DV
DIFF_W = DIFF_HEADS * DIFF_DV
GLA_W = GLA_HEADS * GLA_DV
MIX_WIDTH = RET_W + DIFF_W + GLA_W
IN_SPLITS = (RET_HEADS * RET_DK, RET_HEADS * RET_DK, RET_W, RET_W,
             DIFF_HEADS * 2 * DIFF_DK, DIFF_HEADS * 2 * DIFF_DK, DIFF_W,
             GLA_HEADS * GLA_DK, GLA_HEADS * GLA_DK, GLA_W, GLA_RANK, GLA_W)
IN_WIDTH = sum(IN_SPLITS)
N_EXPERTS = 32
N_GROUPS = 8
EXPERTS_PER_GROUP = N_EXPERTS // N_GROUPS
TOP_K = 2
GROUP_SCORE_K = 2
D_EXPERT = 512
MOE_BLOCK = 256

kernel_name = "hybrid_ret_diff_gla_grouped_moe_adaln"


def rms_norm(x, g):
    x32 = x.astype(jnp.float32)
    y = x32 * lax.rsqrt(jnp.mean(x32 * x32, axis=-1, keepdims=True) + EPS)
    return (y * g.astype(jnp.float32)).astype(x.dtype)


def split_cols(p, sizes):
    idx = np.cumsum(np.array(sizes))[:-1].tolist()
    return jnp.split(p, idx, axis=-1)


def apply_rotary(x, positions, theta, rot_dims):
    half = rot_dims // 2
    inv_freq = 1.0 / (theta ** (jnp.arange(half, dtype=jnp.float32) / half))
    ang = positions.astype(jnp.float32)[..., None] * inv_freq
    cos = jnp.cos(ang)[:, :, None, :]
    sin = jnp.sin(ang)[:, :, None, :]
    xr = x[..., :rot_dims].astype(jnp.float32)
    x1, x2 = xr[..., :half], xr[..., half:]
    rot = jnp.concatenate([x1 * cos - x2 * sin, x1 * sin + x2 * cos], axis=-1).astype(x.dtype)
    return jnp.concatenate([rot, x[..., rot_dims:]], axis=-1)


def retention(q, k, v, positions):
    B_, S_, H, dk = q.shape
    dv = v.shape[-1]
    n_c = S_ // CHUNK
    q = apply_rotary(q, positions, RET_THETA, dk)
    k = apply_rotary(k, positions, RET_THETA, dk) * (dk ** -0.5)
    log_g = jnp.log(1.0 - 2.0 ** (-5.0 - jnp.arange(H, dtype=jnp.float32)))
    j = jnp.arange(CHUNK, dtype=jnp.float32)
    intra_decay = jnp.exp(log_g[:, None, None] * jnp.abs(j[:, None] - j[None, :]))
    q_decay = jnp.exp(log_g[:, None] * (j + 1.0))
    k_decay = jnp.exp(log_g[:, None] * (CHUNK - 1.0 - j))
    chunk_decay = jnp.exp(log_g * CHUNK)
    qc = q.reshape(B_, n_c, CHUNK, H, dk)
    kc = k.reshape(B_, n_c, CHUNK, H, dk)
    vc = v.reshape(B_, n_c, CHUNK, H, dv)
    scores = jnp.einsum('bnthd,bnshd->bnhts', qc, kc) * intra_decay
    intra = jnp.einsum('bnhts,bnshe->bnthe', scores, vc)
    kv = jnp.einsum('bnshd,hs,bnshe->bnhde', kc, k_decay, vc)

    def step(state, kv_i):
        return state * chunk_decay[None, :, None, None] + kv_i, state

    init = jnp.zeros((B_, H, dk, dv), kv.dtype)
    _, prev = lax.scan(step, init, jnp.moveaxis(kv, 1, 0))
    prev = jnp.moveaxis(prev, 0, 1)
    cross = jnp.einsum('bnthd,ht,bnhde->bnthe', qc, q_decay, prev)
    return (intra + cross).reshape(B_, S_, H, dv)


def diff_attention(q, k, v, positions, q_g, k_g, lq1, lk1, lq2, lk2, sub_g, lam_init):
    B_, S_, H, _, dk = q.shape
    dv = v.shape[-1]
    q = rms_norm(q, q_g)
    k = rms_norm(k, k_g)
    q = apply_rotary(q.reshape(B_, S_, 2 * H, dk), positions, ROPE_THETA, ROPE_DIMS)
    q = q.reshape(B_, S_, H, 2, dk) * (dk ** -0.5)
    k = apply_rotary(k.reshape(B_, S_, 2 * H, dk), positions, ROPE_THETA, ROPE_DIMS)
    k = k.reshape(B_, S_, H, 2, dk)
    lam = (jnp.exp(jnp.sum((lq1 * lk1).astype(jnp.float32)))
           - jnp.exp(jnp.sum((lq2 * lk2).astype(jnp.float32))) + lam_init)
    n_q = S_ // Q_BLOCK
    key_chunk = jnp.arange(S_) // CHUNK
    qb = jnp.swapaxes(q.reshape(B_, n_q, Q_BLOCK, H, 2, dk), 0, 1)

    def block(args):
        qi, bi = args
        s = jnp.einsum('bqhcd,bkhcd->bhcqk', qi, k).astype(jnp.float32)
        q_chunk = (bi * Q_BLOCK + jnp.arange(Q_BLOCK)) // CHUNK
        mask = key_chunk[None, :] <= q_chunk[:, None]
        p = jax.nn.softmax(jnp.where(mask, s, -jnp.inf), axis=-1)
        a = p[:, :, 0] - lam * p[:, :, 1]
        return jnp.einsum('bhqk,bkhe->bqhe', a.astype(v.dtype), v)

    o = lax.map(block, (qb, jnp.arange(n_q)))
    o = jnp.swapaxes(o, 0, 1).reshape(B_, S_, H, dv)
    return rms_norm(o, sub_g) * (1.0 - lam_init)


def gated_linear_attention(q, k, v, log_a):
    B_, S_, H, dk = q.shape
    dv = v.shape[-1]
    n_c = S_ // CHUNK
    q = q * (dk ** -0.5)
    to_chunks = lambda t: jnp.swapaxes(t.reshape(B_, n_c, CHUNK, H, t.shape[-1]), 0, 1)

    def step(state, inp):
        qi, ki, vi, la = inp
        g = jnp.cumsum(la.astype(jnp.float32), axis=1)
        dmat = jnp.exp(-jnp.abs(g[:, :, None] - g[:, None, :]))
        a = jnp.einsum('bthd,bshd,btshd->bhts', qi, ki, dmat)
        o = (jnp.einsum('bhts,bshe->bthe', a, vi)
             + jnp.einsum('bthd,bhde->bthe', qi * jnp.exp(g), state))
        g_last = g[:, -1]
        k_dec = ki * jnp.exp(g_last[:, None] - g)
        state = jnp.exp(g_last)[..., None] * state + jnp.einsum('bshd,bshe->bhde', k_dec, vi)
        return state, o

    init = jnp.zeros((B_, H, dk, dv), jnp.float32)
    _, o = lax.scan(step, init, (to_chunks(q), to_chunks(k), to_chunks(v), to_chunks(log_a)))
    return jnp.swapaxes(o, 0, 1).reshape(B_, S_, H, dv)


def route(h, router_w, router_b):
    t = h.shape[0]
    scores = jax.nn.sigmoid((h @ router_w).astype(jnp.float32))
    biased = scores + router_b.astype(jnp.float32)
    grp = biased.reshape(t, N_GROUPS, EXPERTS_PER_GROUP)
    group_score = jnp.sum(lax.top_k(grp, GROUP_SCORE_K)[0], axis=-1)
    g_sel = jnp.argmax(group_score, axis=-1)
    in_group = (jnp.arange(N_EXPERTS) // EXPERTS_PER_GROUP)[None, :] == g_sel[:, None]
    _, idx = lax.top_k(jnp.where(in_group, biased, -jnp.inf), TOP_K)
    w = jnp.take_along_axis(scores, idx, axis=-1)
    return idx, w / jnp.sum(w, axis=-1, keepdims=True)


def moe(h, idx, gates, w_gate, w_up, w_down):
    t, d = h.shape
    n_a = t * TOP_K
    e_flat = idx.reshape(n_a)
    tok_flat = jnp.repeat(jnp.arange(t, dtype=jnp.int32), TOP_K)
    g_flat = gates.reshape(n_a)
    order = jnp.argsort(e_flat)
    e_s, tok_s, g_s = e_flat[order], tok_flat[order], g_flat[order]
    counts = jnp.bincount(e_flat, length=N_EXPERTS)
    starts = jnp.cumsum(counts) - counts
    padded = ((counts + MOE_BLOCK - 1) // MOE_BLOCK) * MOE_BLOCK
    pad_ends = jnp.cumsum(padded)
    pad_starts = pad_ends - padded
    dest = pad_starts[e_s] + jnp.arange(n_a) - starts[e_s]
    n_blocks = -(-n_a // MOE_BLOCK) + N_EXPERTS
    p_len = n_blocks * MOE_BLOCK
    tok_buf = jnp.full((p_len,), t, jnp.int32).at[dest].set(tok_s)
    gate_buf = jnp.zeros((p_len,), g_s.dtype).at[dest].set(g_s)
    block_expert = jnp.minimum(
        jnp.searchsorted(pad_ends, jnp.arange(n_blocks) * MOE_BLOCK, side='right'), N_EXPERTS - 1)
    h_pad = jnp.concatenate([h, jnp.zeros((1, d), h.dtype)], axis=0)

    def expert_block(args):
        toks, gts, e = args
        xb = h_pad[toks]
        u = jax.nn.silu(xb @ w_gate[e]) * (xb @ w_up[e])
        return (u @ w_down[e]) * gts[:, None].astype(xb.dtype)

    y = lax.map(expert_block, (tok_buf.reshape(n_blocks, MOE_BLOCK),
                               gate_buf.reshape(n_blocks, MOE_BLOCK), block_expert))
    out = jnp.zeros((t + 1, d), y.dtype).at[tok_buf].add(y.reshape(p_len, d))
    return out[:t]


def setup_inputs(seed: int = 0) -> dict:
    key = jax.random.key(seed)
    ks = jax.random.split(key, 26)
    f32 = jnp.float32
    nrm = lambda k, shape, s: jax.random.normal(k, shape, f32) * s
    gain = lambda k, shape: 1.0 + 0.02 * jax.random.normal(k, shape, f32)
    x = jax.random.normal(ks[0], (BATCH, SEQ, D_MODEL), f32)
    c = jax.random.normal(ks[1], (BATCH, D_MODEL), f32)
    offsets = jax.random.randint(ks[2], (BATCH, 1), 0, 1024) * CHUNK
    positions = (offsets + jnp.arange(SEQ, dtype=jnp.int32)[None, :]).astype(jnp.int32)
    return {
        "x": x,
        "c": c,
        "positions": positions,
        "w_mod": nrm(ks[3], (DEPTH, D_MODEL, 6 * D_MODEL), 0.01),
        "b_mod": nrm(ks[4], (DEPTH, 6 * D_MODEL), 0.02),
        "norm1_g": gain(ks[5], (DEPTH, D_MODEL)),
        "norm2_g": gain(ks[6], (DEPTH, D_MODEL)),
        "w_in": nrm(ks[7], (DEPTH, D_MODEL, IN_WIDTH), D_MODEL ** -0.5),
        "ret_norm_g": gain(ks[8], (DEPTH, RET_DV)),
        "diff_q_g": gain(ks[9], (DEPTH, DIFF_DK)),
        "diff_k_g": gain(ks[10], (DEPTH, DIFF_DK)),
        "lam_q1": nrm(ks[11], (DEPTH, DIFF_DK), 0.1),
        "lam_k1": nrm(ks[12], (DEPTH, DIFF_DK), 0.1),
        "lam_q2": nrm(ks[13], (DEPTH, DIFF_DK), 0.1),
        "lam_k2": nrm(ks[14], (DEPTH, DIFF_DK), 0.1),
        "diff_sub_g": gain(ks[15], (DEPTH, DIFF_DV)),
        "gla_w_a2": nrm(ks[16], (DEPTH, GLA_RANK, GLA_HEADS * GLA_DK), GLA_RANK ** -0.5),
        "gla_b_a": nrm(ks[17], (DEPTH, GLA_HEADS * GLA_DK), 0.1),
        "gla_norm_g": gain(ks[18], (DEPTH, GLA_DV)),
        "w_out": nrm(ks[19], (DEPTH, MIX_WIDTH, D_MODEL), MIX_WIDTH ** -0.5),
        "router_w": nrm(ks[20], (D_MODEL, N_EXPERTS), D_MODEL ** -0.5),
        "router_b": nrm(ks[21], (N_EXPERTS,), 0.01),
        "w_gate": nrm(ks[22], (DEPTH, N_EXPERTS, D_MODEL, D_EXPERT), D_MODEL ** -0.5),
        "w_up": nrm(ks[23], (DEPTH, N_EXPERTS, D_MODEL, D_EXPERT), D_MODEL ** -0.5),
        "w_down": nrm(ks[24], (DEPTH, N_EXPERTS, D_EXPERT, D_MODEL), D_EXPERT ** -0.5),
    }


def reference(x, c, positions, w_mod, b_mod, norm1_g, norm2_g, w_in, ret_norm_g,
              diff_q_g, diff_k_g, lam_q1, lam_k1, lam_q2, lam_k2, diff_sub_g,
              gla_w_a2, gla_b_a, gla_norm_g, w_out, router_w, router_b,
              w_gate, w_up, w_down):
    B_, S_, D = x.shape
    heads = lambda t, n: t.reshape(B_, S_, n, -1)
    c_act = jax.nn.silu(c)
    for l in range(DEPTH):
        lam_init = 0.8 - 0.6 * math.exp(-0.3 * l)
        mod = c_act @ w_mod[l] + b_mod[l]
        sh1, sc1, gt1, sh2, sc2, gt2 = jnp.split(mod[:, None, :], 6, axis=-1)
        h = rms_norm(x, norm1_g[l]) * (1.0 + sc1) + sh1
        (rq, rk, rv, rg, dq, dk_, dv_, gq, gk, gv, ga, gr) = split_cols(h @ w_in[l], IN_SPLITS)
        out_r = rms_norm(retention(heads(rq, RET_HEADS), heads(rk, RET_HEADS),
                                   heads(rv, RET_HEADS), positions),
                         ret_norm_g[l]) * jax.nn.silu(heads(rg, RET_HEADS))
        out_d = diff_attention(dq.reshape(B_, S_, DIFF_HEADS, 2, DIFF_DK),
                               dk_.reshape(B_, S_, DIFF_HEADS, 2, DIFF_DK),
                               heads(dv_, DIFF_HEADS), positions, diff_q_g[l], diff_k_g[l],
                               lam_q1[l], lam_k1[l], lam_q2[l], lam_k2[l], diff_sub_g[l],
                               lam_init)
        log_a = jax.nn.log_sigmoid((ga @ gla_w_a2[l] + gla_b_a[l]).astype(jnp.float32)) / GLA_TAU
        out_g = rms_norm(gated_linear_attention(heads(gq, GLA_HEADS), heads(gk, GLA_HEADS),
                                                heads(gv, GLA_HEADS), heads(log_a, GLA_HEADS)),
                         gla_norm_g[l]) * jax.nn.silu(heads(gr, GLA_HEADS))
        mixed = jnp.concatenate([out_r.reshape(B_, S_, RET_W), out_d.reshape(B_, S_, DIFF_W),
                                 out_g.reshape(B_, S_, GLA_W)], axis=-1)
        x = x + gt1 * (mixed @ w_out[l])
        h2 = (rms_norm(x, norm2_g[l]) * (1.0 + sc2) + sh2).reshape(B_ * S_, D)
        idx, gates = route(h2, router_w, router_b)
        y = moe(h2, idx, gates, w_gate[l], w_up[l], w_down[l]).reshape(B_, S_, D)
        x = x + gt2 * y
    return x
```

```python
import math
import contextlib
import numpy as np
import concourse.bass as bass
import concourse.mybir as mybir
from concourse.bass_utils import run_bass_kernel_spmd

F32 = mybir.dt.float32
BF16 = mybir.dt.bfloat16
I32 = mybir.dt.int32
ALU = mybir.AluOpType
AF = mybir.ActivationFunctionType
AX = mybir.AxisListType

S = 8192
D = 1024
NT = S // 128
DEPTH = 2
EPS = 1e-6
NE = 32
DE = 512
BLK = 384
NSUB = BLK // 128
NB = -(-(2 * S) // BLK) + NE
NPAD = NB * BLK
SEM_LIMIT = 30000
WINC = 3456


class Buf:
    __slots__ = ("name", "w", "r", "excl")

    def __init__(self, name="", excl=False):
        self.name = name
        self.w = {}
        self.r = {}
        self.excl = excl


class V:
    __slots__ = ("ap", "b")

    def __init__(self, ap, b):
        self.ap = ap
        self.b = b

    def __getitem__(self, k):
        return V(self.ap[k], self.b)

    def rearrange(self, s, **kw):
        return V(self.ap.rearrange(s, **kw), self.b)

    def unsqueeze(self, a):
        return V(self.ap.unsqueeze(a), self.b)

    def bc(self, shape):
        return V(self.ap.broadcast_to(list(shape)), self.b)

    def pbc(self, n):
        return V(self.ap.partition_broadcast(n), self.b)

    def bitcast(self, dt):
        return V(self.ap.bitcast(dt), self.b)


class Tile:
    def __init__(self, t, name):
        self.t = t
        self.b = Buf(name)

    def __getitem__(self, k):
        return V(self.t[k], self.b)

    @property
    def v(self):
        return self[:]


class EngS:
    def __init__(self, name, handle):
        self.name = name
        self.h = handle
        self.sem = None
        self.n = 0
        self.seen = {}
        self.dma_sems = []
        self.dma_cnt = []
        self.dma_i = 0


class FW:
    def __init__(self, nc, ndma=6, same=True):
        self.nc = nc
        self.nsem = 0
        self.same = same
        self.E = {"pe": EngS("pe", nc.tensor), "dve": EngS("dve", nc.vector),
                  "act": EngS("act", nc.scalar), "pool": EngS("pool", nc.gpsimd),
                  "sp": EngS("sp", nc.sync)}
        self.ndma = ndma
        self.owner = {}
        self.nwaits = 0
        self.nops = 0

    def new_sem(self):
        self.nsem += 1
        return self.nc.alloc_semaphore(f"fs{self.nsem}")

    def _tok(self, E):
        if E.sem is None or E.n >= SEM_LIMIT:
            E.sem = self.new_sem()
            E.n = 0
            self.owner[E.sem] = E.name
        E.n += 1
        return (E.sem, E.n)

    def _wait(self, E, deps):
        for sem, val in deps.items():
            if E.seen.get(sem, 0) >= val:
                continue
            E.h.wait_ge(sem, val)
            E.seen[sem] = val
            self.nwaits += 1

    def _deps(self, E, reads, writes, skip_own):
        deps = {}
        for b in reads:
            for s, v in b.w.items():
                if deps.get(s, 0) < v:
                    deps[s] = v
        for b in writes:
            for d in (b.w, b.r):
                for s, v in d.items():
                    if deps.get(s, 0) < v:
                        deps[s] = v
        if skip_own:
            for s in list(deps):
                if self.owner.get(s) == E.name:
                    del deps[s]
        self._wait(E, deps)

    def _commit(self, tok, reads, writes):
        s, v = tok
        for b in reads:
            if b.r.get(s, 0) < v:
                b.r[s] = v
        for b in writes:
            if b.w.get(s, 0) < v:
                b.w[s] = v
            b.r = {}

    def op(self, eng, fn, reads=(), writes=()):
        E = self.E[eng]
        if any(b.excl for b in reads):
            writes = list(writes) + [b for b in reads if b.excl]
            reads = [b for b in reads if not b.excl]
        self._deps(E, reads, writes, (eng == "pe") or (not self.same))
        ins = fn()
        tok = self._tok(E)
        ins.then_inc(tok[0], 1)
        self._commit(tok, reads, writes)
        self.nops += 1
        return ins

    def dma(self, q, fn, reads=(), writes=()):
        E = self.E[q]
        if not E.dma_sems:
            E.dma_sems = [self.new_sem() for _ in range(self.ndma)]
            E.dma_cnt = [0] * self.ndma
        k = E.dma_i % self.ndma
        E.dma_i += 1
        if E.dma_cnt[k] * 16 >= SEM_LIMIT:
            self._wait(E, {E.dma_sems[k]: E.dma_cnt[k] * 16})
            E.dma_sems[k] = self.new_sem()
            E.dma_cnt[k] = 0
        sem = E.dma_sems[k]
        if E.dma_cnt[k] > 0:
            self._wait(E, {sem: E.dma_cnt[k] * 16})
        self._deps(E, reads, writes, False)
        ins = fn()
        E.dma_cnt[k] += 1
        ins.then_inc(sem, 16)
        self._commit((sem, E.dma_cnt[k] * 16), reads, writes)
        self.nops += 1
        return ins

    def barrier(self):
        deps = {}
        for E in self.E.values():
            if E.sem is not None and E.n > 0:
                deps[E.sem] = E.n
            for s, c in zip(E.dma_sems, E.dma_cnt):
                if c > 0:
                    deps[s] = c * 16
        for E in self.E.values():
            self._wait(E, dict(deps))


def _const_tables():
    c = {}
    p = np.arange(128)
    c["ident"] = np.eye(128, dtype=np.float32)
    gam = 1.0 - 2.0 ** (-5.0 - np.arange(4))
    same = (p[:, None] // 64) == (p[None, :] // 64)
    dr = np.zeros((128, 4, 128), np.float32)
    for h in range(4):
        dr[:, h, :] = np.where(same, gam[h] ** np.abs(p[:, None] - p[None, :]), 0.0)
    c["dr"] = dr.reshape(128, 512)
    j = p % 64
    qd = np.stack([gam[h] ** (j + 1.0) for h in range(4)], 1)
    kd = np.stack([gam[h] ** (63.0 - j) for h in range(4)], 1) / 8.0
    c["qd"] = np.repeat(qd, 64, axis=1)
    c["kd0"] = np.repeat(kd * (p[:, None] < 64), 64, axis=1)
    c["kd1"] = np.repeat(kd * (p[:, None] >= 64), 64, axis=1)
    c["cd"] = np.broadcast_to(np.repeat(gam ** 64.0, 64)[None, :], (128, 256)).copy()
    ml = (same & (p[:, None] <= p[None, :])).astype(np.float32)
    mu = (same & (p[:, None] > p[None, :])).astype(np.float32)
    c["ml4"] = np.tile(ml, (1, 4))
    c["mu4"] = np.tile(mu, (1, 4))
    c["tri"] = ml
    c["chk"] = np.stack([(p < 64), (p >= 64)], 1).astype(np.float32)
    c["m0"] = (p < 64).astype(np.float32)[:, None]
    c["m1"] = (p >= 64).astype(np.float32)[:, None]
    c["ifr"] = np.broadcast_to((1.0 / (10000.0 ** (np.arange(32, dtype=np.float32) / 32)))[None, :], (128, 32)).copy()
    c["ifd"] = np.broadcast_to((1.0 / (500000.0 ** (np.arange(8, dtype=np.float32) / 8)))[None, :], (128, 8)).copy()
    c["lts"] = (p[:, None] < p[None, :]).astype(np.float32)
    c["ones"] = np.ones((128, 128), np.float32)
    c["jb"] = np.broadcast_to((np.arange(NB, dtype=np.float32) * BLK)[None, :], (128, NB)).copy()
    coff = np.zeros((128, 12), np.float32)
    for cc in range(12):
        coff[:, cc] = (cc if cc < 8 else cc - 8) * 128 + p
    c["coff"] = coff
    c["gid"] = np.broadcast_to((np.arange(32) // 4).astype(np.float32)[None, :], (128, 32)).copy()
    off = {}
    cur = 0
    arrs = []
    for k, v in c.items():
        v = np.ascontiguousarray(v, dtype=np.float32).reshape(128, -1)
        off[k] = (cur, v.shape[1])
        cur += v.shape[1]
        arrs.append(v)
    return np.concatenate(arrs, axis=1), off


CONST_NP, COFF = _const_tables()
NCONST = CONST_NP.shape[1]


class Prog:
    def __init__(self, stop_after=None, debug=False):
        self.nc = nc = bass.Bass("TRN2", target_bir_lowering=False)
        import os
        self.fw = FW(nc, same=(os.environ.get("SAMEENG", "1") == "1"))
        self.stop_after = stop_after
        self.debug = debug
        self.dram = {}

    def din(self, name, shape, dt=F32):
        t = self.nc.dram_tensor(name, list(shape), dt, kind="ExternalInput")
        T = Tile(t.ap(), name)
        self.dram[name] = T
        return T

    def dout(self, name, shape, dt=F32):
        t = self.nc.dram_tensor(name, list(shape), dt, kind="ExternalOutput")
        T = Tile(t.ap(), name)
        self.dram[name] = T
        return T

    def dint(self, name, shape, dt, dbg=False):
        kind = "ExternalOutput" if (dbg and self.debug) else "Internal"
        t = self.nc.dram_tensor(name, list(shape), dt, kind=kind)
        T = Tile(t.ap(), name)
        self.dram[name] = T
        return T

    def dump(self, name, v, shape, dt=F32):
        if not self.debug:
            return
        T = self.dout(self._uname("dbg_" + name), shape, dt)
        self.dma("sp", T.v, v)

    def _uname(self, name):
        self._uid = getattr(self, "_uid", 0) + 1
        return f"{name}_u{self._uid}"

    def sb(self, st, name, shape, dt):
        name = self._uname(name)
        return Tile(st.enter_context(self.nc.sbuf_tensor(name, list(shape), dt)), name)

    def ps(self, st, name, shape, dt):
        name = self._uname(name)
        T = Tile(st.enter_context(self.nc.psum_tensor(name, list(shape), dt)), name)
        T.b.excl = True
        return T

    def _eng(self, e):
        return {"dve": self.nc.vector, "pool": self.nc.gpsimd, "act": self.nc.scalar}[e]

    def _pe(self, e):
        import os
        if e == "pool" and os.environ.get("NOPOOL", "0") == "1":
            return "dve"
        return e

    def dma(self, q, out, in_, **kw):
        h = {"sp": self.nc.sync, "act": self.nc.scalar, "pool": self.nc.gpsimd}[q]
        return self.fw.dma(q, lambda: h.dma_start(out=out.ap, in_=in_.ap, **kw), reads=[in_.b], writes=[out.b])

    def gather(self, out, table, idx):
        return self.fw.dma("pool", lambda: self.nc.gpsimd.indirect_dma_start(
            out=out.ap, out_offset=None, in_=table.ap,
            in_offset=bass.IndirectOffsetOnAxis(ap=idx.ap, axis=0)), reads=[table.b, idx.b], writes=[out.b])

    def scatter(self, table, in_, idx):
        return self.fw.dma("pool", lambda: self.nc.gpsimd.indirect_dma_start(
            out=table.ap, out_offset=bass.IndirectOffsetOnAxis(ap=idx.ap, axis=0),
            in_=in_.ap, in_offset=None), reads=[in_.b, idx.b], writes=[table.b])

    def mm(self, out, lhsT, rhs, start=True, stop=True):
        return self.fw.op("pe", lambda: self.nc.tensor.matmul(out=out.ap, lhsT=lhsT.ap, rhs=rhs.ap, start=start, stop=stop),
                          reads=[lhsT.b, rhs.b], writes=[out.b])

    def tr(self, out, in_, ident):
        return self.fw.op("pe", lambda: self.nc.tensor.transpose(out=out.ap, in_=in_.ap, identity=ident.ap),
                          reads=[in_.b, ident.b], writes=[out.b])

    def act(self, out, in_, func, scale=1.0, bias=0.0, accum=None):
        rd = [in_.b]
        kw = {}
        if isinstance(scale, V):
            rd.append(scale.b)
            kw["scale"] = scale.ap
        else:
            kw["scale"] = float(scale)
        if isinstance(bias, V):
            rd.append(bias.b)
            kw["bias"] = bias.ap
        else:
            kw["bias"] = float(bias)
        wr = [out.b]
        if accum is not None:
            kw["accum_out"] = accum.ap
            wr.append(accum.b)
        return self.fw.op("act", lambda: self.nc.scalar.activation(out=out.ap, in_=in_.ap, func=func, **kw), reads=rd, writes=wr)

    def tt(self, e, out, a, b, op):
        e = self._pe(e)
        return self.fw.op(e, lambda: self._eng(e).tensor_tensor(out=out.ap, in0=a.ap, in1=b.ap, op=op), reads=[a.b, b.b], writes=[out.b])

    def ts(self, e, out, a, s1, s2=None, op0=ALU.mult, op1=None):
        e = self._pe(e)
        rd = [a.b]
        s1a = s1
        s2a = s2
        if isinstance(s1, V):
            rd.append(s1.b)
            s1a = s1.ap
        if isinstance(s2, V):
            rd.append(s2.b)
            s2a = s2.ap
        kw = {}
        if op1 is not None:
            kw["op1"] = op1
        return self.fw.op(e, lambda: self._eng(e).tensor_scalar(out=out.ap, in0=a.ap, scalar1=s1a, scalar2=s2a, op0=op0, **kw), reads=rd, writes=[out.b])

    def stt(self, e, out, a, s, b, op0, op1):
        e = self._pe(e)
        rd = [a.b, b.b]
        sa = s
        if isinstance(s, V):
            rd.append(s.b)
            sa = s.ap
        return self.fw.op(e, lambda: self._eng(e).scalar_tensor_tensor(out=out.ap, in0=a.ap, scalar=sa, in1=b.ap, op0=op0, op1=op1), reads=rd, writes=[out.b])

    def cp(self, e, out, in_):
        e = self._pe(e)
        if e == "act":
            return self.act(out, in_, AF.Copy)
        return self.fw.op(e, lambda: self._eng(e).tensor_copy(out=out.ap, in_=in_.ap), reads=[in_.b], writes=[out.b])

    def red(self, e, out, in_, op=ALU.add, axis=AX.X):
        return self.fw.op(e, lambda: self._eng(e).tensor_reduce(out=out.ap, in_=in_.ap, axis=axis, op=op), reads=[in_.b], writes=[out.b])

    def recip(self, out, in_):
        return self.fw.op("dve", lambda: self.nc.vector.reciprocal(out=out.ap, in_=in_.ap), reads=[in_.b], writes=[out.b])

    def memset(self, e, out, val):
        e = self._pe(e)
        return self.fw.op(e, lambda: self._eng(e).memset(out.ap, val), writes=[out.b])

    def rsqrt(self, out, in_, scale, eps):
        self.act(out, in_, AF.Sqrt, scale=scale, bias=self.epsv(eps, out))
        self.recip(out, out)

    def epsv(self, eps, like):
        n = like.ap.shape[0]
        return self.C_eps[0:n, 0:1] if eps == EPS else 0.0

    def build(self):
        nc = self.nc
        with contextlib.ExitStack() as gst:
            self.gst = gst
            self.declare_io()
            self.load_consts(gst)
            for l in range(DEPTH):
                self.layer(l)
                if self.stop_after is not None and self.stop_after[0] == l and self.stop_after[1] != "all":
                    break
            self.finish()
        return nc

    def declare_io(self):
        d = self.din
        self.x_in = d("x", [S, D])
        self.c_in = d("c", [128, 8])
        self.pos_in = d("pos", [128, NT], I32)
        self.consts_in = d("consts", [128, NCONST])
        self.w_mod = d("w_mod", [DEPTH, D, 6 * D])
        self.b_mod = d("b_mod", [DEPTH, 6 * D])
        self.norm1_g = d("norm1_g", [DEPTH, D])
        self.norm2_g = d("norm2_g", [DEPTH, D])
        self.w_in = d("w_in", [DEPTH, D, 3344])
        self.ret_norm_g = d("ret_norm_g", [DEPTH, 64])
        self.diff_q_g = d("diff_q_g", [DEPTH, 64])
        self.diff_k_g = d("diff_k_g", [DEPTH, 64])
        self.lam_q1 = d("lam_q1", [DEPTH, 64])
        self.lam_k1 = d("lam_k1", [DEPTH, 64])
        self.lam_q2 = d("lam_q2", [DEPTH, 64])
        self.lam_k2 = d("lam_k2", [DEPTH, 64])
        self.diff_sub_g = d("diff_sub_g", [DEPTH, 128])
        self.gla_w_a2 = d("gla_w_a2", [DEPTH, 16, 128])
        self.gla_b_a = d("gla_b_a", [DEPTH, 128])
        self.gla_norm_g = d("gla_norm_g", [DEPTH, 64])
        self.w_out = d("w_out", [DEPTH, D, D])
        self.router_w = d("router_w", [D, NE])
        self.router_b = d("router_b", [NE])
        self.w_gate = [d(f"w_gate{l}", [NE * D, DE]) for l in range(DEPTH)]
        self.w_up = [d(f"w_up{l}", [NE * D, DE]) for l in range(DEPTH)]
        self.w_down = [d(f"w_down{l}", [NE * DE, D]) for l in range(DEPTH)]
        self.wgb = [self.dint(f"wgb{l}", [NE * D, DE], BF16) for l in range(DEPTH)]
        self.wub = [self.dint(f"wub{l}", [NE * D, DE], BF16) for l in range(DEPTH)]
        self.wdb = [self.dint(f"wdb{l}", [NE * DE, D], BF16) for l in range(DEPTH)]
        self.bg = []
        self.out = self.dout("out", [S, D])
        self.out_b = [Buf(f"out{i}") for i in range(NT)]
        if self.debug:
            self.ydbg = self.dout("ydbg", [S, D])
        self.qT_s = self.dint("qT_s", [4, 128, S], BF16, dbg=True)
        self.kT_s = self.dint("kT_s", [4, 128, S], BF16, dbg=True)
        self.v_s = self.dint("v_s", [S, 512], BF16, dbg=True)
        self.mixT_s = self.dint("mixT_s", [D, S], BF16, dbg=True)
        self.h2_s = self.dint("h2_s", [S, D], BF16, dbg=True)
        self.xs_s = self.dint("xs_s", [NPAD, D], BF16)
        self.ys_s = self.dint("ys_s", [NPAD, D], F32)

    def load_consts(self, st):
        self.CT = self.sb(st, "consts", [128, NCONST], F32)
        self.dma("sp", self.CT.v, self.consts_in.v)
        self.C_eps = self.sb(st, "c_eps", [128, 1], F32)
        self.memset("dve", self.C_eps.v, EPS)
        self.C_npi = self.sb(st, "c_npi", [128, 1], F32)
        self.memset("dve", self.C_npi.v, -math.pi)
        self.C_one = self.sb(st, "c_one", [128, 1], F32)
        self.memset("dve", self.C_one.v, 1.0)
        posi = self.sb(st, "posi", [128, NT], I32)
        self.dma("sp", posi.v, self.pos_in.v)
        self.POSF = self.sb(st, "posf", [128, NT], F32)
        self.cp("dve", self.POSF.v, posi.v)
        self.identb = self.sb(st, "identb", [128, 128], BF16)
        self.cp("dve", self.identb.v, self.cst("ident"))
        self.rope_tables(st)
        self.onesb = self.sb(st, "onesb", [128, 128], BF16)
        self.cp("dve", self.onesb.v, self.cst("ones"))

    def rope_tables(self, gst):
        self.COS = self.sb(gst, "COS", [128, NT, 40], F32)
        self.SIN = self.sb(gst, "SIN", [128, NT, 40], F32)
        o, _ = COFF["ifr"]
        if40 = self.CT[:, o:o + 40]
        C1 = 6.28125
        C2 = 2.0 * math.pi - C1
        with contextlib.ExitStack() as st:
            ANG = self.sb(st, "ANG", [128, NT, 40], F32)
            KI = self.sb(st, "KI", [128, NT, 40], I32)
            KF = self.sb(st, "KF", [128, NT, 40], F32)
            MK = self.sb(st, "MK", [128, NT, 40], F32)
            self.tt("dve", ANG.v, self.POSF.v.unsqueeze(2).bc([128, NT, 40]), if40.unsqueeze(1).bc([128, NT, 40]), ALU.mult)
            self.ts("dve", KF.v, ANG.v, 1.0 / (2.0 * math.pi))
            self.cp("dve", KI.v, KF.v)
            self.cp("dve", KF.v, KI.v)
            self.stt("dve", ANG.v, KF.v, -C1, ANG.v, ALU.mult, ALU.add)
            self.stt("dve", ANG.v, KF.v, -C2, ANG.v, ALU.mult, ALU.add)
            self.ts("dve", MK.v, ANG.v, math.pi, None, op0=ALU.is_gt)
            self.stt("dve", self.SIN.v, MK.v, -2.0 * math.pi, ANG.v, ALU.mult, ALU.add)
            self.ts("dve", MK.v, self.SIN.v, -math.pi, None, op0=ALU.is_lt)
            self.stt("dve", self.SIN.v, MK.v, 2.0 * math.pi, self.SIN.v, ALU.mult, ALU.add)
            self.ts("dve", self.COS.v, self.SIN.v, 0.5 * math.pi, None, op0=ALU.add)
            self.ts("dve", MK.v, self.COS.v, math.pi, None, op0=ALU.is_gt)
            self.stt("dve", self.COS.v, MK.v, -2.0 * math.pi, self.COS.v, ALU.mult, ALU.add)
            self.act(self.SIN.v, self.SIN.v, AF.Sin)
            self.act(self.COS.v, self.COS.v, AF.Sin)
        self.fw.barrier()

    def out_tile(self, i):
        return V(self.out.t[i * 128:(i + 1) * 128, :], self.out_b[i])

    def cst(self, name):
        o, n = COFF[name]
        return self.CT[:, o:o + n]

    def finish(self):
        fw = self.fw
        fw.barrier()

    def layer(self, l):
        x_src = self.x_in if l == 0 else self.out
        sa = self.stop_after
        with contextlib.ExitStack() as lst:
            self.phase_mod(l, lst)
            if sa == (l, "mod"):
                return
            self.queue_weight_casts(l)
            self.phase1(l, x_src)
            self.run_bg(10 ** 9)
            self.fw.barrier()
            if sa == (l, "p1"):
                return
            self.phase2(l)
            self.fw.barrier()
            if sa == (l, "p2"):
                return
            with contextlib.ExitStack() as st3:
                self.LOG = self.sb(st3, f"log{l}", [128, NT, NE], F32)
                self.phase3(l, x_src)
                self.fw.barrier()
                if sa == (l, "p3"):
                    return
                self.phase_moe(l)
                self.fw.barrier()
                if sa == (l, "moe"):
                    return

    def queue_weight_casts(self, l):
        for e in range(NE):
            for src, dst, rows in ((self.w_gate[l], self.wgb[l], D), (self.w_up[l], self.wub[l], D), (self.w_down[l], self.wdb[l], DE)):
                def task(src=src, dst=dst, rows=rows, e=e):
                    self.dma("pool", dst[e * rows:(e + 1) * rows, :], src[e * rows:(e + 1) * rows, :])
                self.bg.append(task)

    def run_bg(self, n):
        while n > 0 and self.bg:
            self.bg.pop(0)()
            n -= 1

    def phase_mod(self, l, lst):
        MODB = self.sb(lst, f"modb{l}", [128, 6 * D], F32)
        self.MODB = MODB
        with contextlib.ExitStack() as st:
            cT = self.sb(st, "cT", [128, 8], F32)
            self.dma("sp", cT.v, self.c_in.v)
            ca = self.sb(st, "ca", [128, 8], F32)
            self.act(ca.v, cT.v, AF.Silu)
            cb = self.sb(st, "cb", [128, 8, 128], F32)
            for k in range(8):
                self.cp("dve", cb[:, k, :], ca[:, k:k + 1].bc([128, 128]))
            bm = self.sb(st, "bm", [1, 6 * D], F32)
            self.dma("sp", bm.v, self.b_mod[l:l + 1, :])
            ones_f = self.cst("ones")
            wm = [self.sb(st, f"wm{i}", [128, 8, 512], F32) for i in range(2)]
            pm = [self.ps(st, f"pm{i}", [128, 512], F32) for i in range(2)]
            for n in range(12):
                w = wm[n % 2]
                p = pm[n % 2]
                self.dma("sp" if n % 2 == 0 else "act", w.v,
                         self.w_mod[l, :, n * 512:(n + 1) * 512].rearrange("(k p) n -> p k n", p=128))
                for k in range(8):
                    self.mm(p.v, cb[:, k, :], w[:, k, :], start=(k == 0), stop=False)
                self.mm(p.v, ones_f[0:1, :], bm[0:1, n * 512:(n + 1) * 512], start=False, stop=True)
                self.cp("dve" if n % 2 == 0 else "act", MODB[:, n * 512:(n + 1) * 512], p.v)
        self.dump(f"mod{l}", MODB[0:2, :], [2, 6 * D])
        self.fw.barrier()

    def mod(self, j):
        return self.MODB[:, j * D:(j + 1) * D]

    def phase1(self, l, x_src):
        nc = self.nc
        with contextlib.ExitStack() as st:
            sb = lambda n, s, d: self.sb(st, n, s, d)
            ps = lambda n, s, d: self.ps(st, n, s, d)
            WIN = sb("WIN", [128, 8, WINC], BF16)
            for k in range(8):
                rows = self.w_in[l, k * 128:(k + 1) * 128, :]
                self.dma("pool", WIN[:, k, 0:1536], rows[:, 0:1536])
                self.dma("pool", WIN[:, k, 1536:3072], rows[:, 1536:3072])
                self.dma("pool", WIN[:, k, 3200:3456], rows[:, 3088:3344])
            import os
            setup = int(os.environ.get("P1SETUP", "9"))
            if setup < 1:
                return
            wga = sb("wga", [128, 8, 16], F32)
            self.dma("sp", wga.v, self.w_in[l, :, 3072:3088].rearrange("(k p) r -> p k r", p=128))
            wa2 = sb("wa2", [16, 128], F32)
            self.dma("sp", wa2.v, self.gla_w_a2[l])
            identf = self.cst("ident")
            PJ = [ps(f"PJ{i}", [128, 512], F32) for i in range(2)]
            TP = [ps(f"TP{i}", [128, 1024], BF16) for i in range(2)]
            SC = ps("SC", [128, 512], F32)
            SC2 = ps("SC2", [128, 512], F32)
            OT = ps("OT", [64, 512], F32)
            MS = ps("MS", [128, 512], F32)
            gaT = sb("gaT", [16, 128], F32)
            for k in range(8):
                self.tr(SC[0:16, 0:128], wga[:, k, :], identf)
                self.cp("dve", gaT.v, SC[0:16, 0:128])
                self.mm(SC2[:, 0:128], gaT.v, wa2.v)
                self.cp("dve", WIN[:, k, 3072:3200], SC2[:, 0:128])
            if setup < 2:
                return
            G1 = sb("G1", [128, D], F32)
            self.dma("sp", G1.v, self.norm1_g[l].pbc(128))
            self.stt("dve", G1.v, self.mod(1), 1.0, G1.v, ALU.add, ALU.mult)
            SH1 = self.mod(0)
            GQ = sb("GQ", [128, 64], F32)
            GK = sb("GK", [128, 64], F32)
            self.dma("sp", GQ.v, self.diff_q_g[l].pbc(128))
            self.dma("sp", GK.v, self.diff_k_g[l].pbc(128))
            self.ts("dve", GQ.v, GQ.v, 0.125)
            BA = sb("BA", [128, 128], F32)
            self.dma("sp", BA.v, self.gla_b_a[l].pbc(128))
            GRN = sb("GRN", [64, 1], F32)
            GGN = sb("GGN", [64, 1], F32)
            self.dma("sp", GRN.v, self.ret_norm_g[l].unsqueeze(1))
            self.dma("sp", GGN.v, self.gla_norm_g[l].unsqueeze(1))
            onesf = self.cst("ones")
            if setup < 3:
                return
            SR = sb("SR", [64, 256], F32)
            SRb = sb("SRb", [64, 256], BF16)
            SG = sb("SG", [32, 256], F32)
            SGb = sb("SGb", [32, 256], BF16)
            for t_ in (SR, SRb, SG, SGb):
                self.memset("dve", t_.v, 0.0)
            XT = [sb(f"XT{i}", [128, D], F32) for i in range(2)]
            ssq = sb("ssq", [128, 1], F32)
            HF = sb("HF", [128, D], F32)
            HB = sb("HB", [128, D], BF16)
            junk = HB
            HT = sb("HT", [128, 8, 128], BF16)
            R1 = sb("R1", [128, 8, 32], F32)
            R2 = sb("R2", [128, 8, 32], F32)
            ROT = sb("ROT", [128, 8, 64], F32)
            QB = sb("QB", [128, 256], BF16)
            QDB = sb("QDB", [128, 256], BF16)
            KB = sb("KB", [128, 256], BF16)
            KD0 = sb("KD0", [128, 256], BF16)
            KD1 = sb("KD1", [128, 256], BF16)
            VR = sb("VR", [128, 256], BF16)
            SGR = sb("SGR", [128, 256], BF16)
            RT = sb("RT", [64, 12, 128], BF16)
            SGT = sb("SGT", [64, 8, 128], BF16)
            DSQ = sb("DSQ", [128, 512], F32)
            DSS = sb("DSS", [128, 8], F32)
            DXN = sb("DXN", [128, 8, 64], F32)
            DQB = sb("DQB", [128, 512], BF16)
            DKB = sb("DKB", [128, 512], BF16)
            DT = sb("DT", [128, 8, 128], BF16)
            DVB = sb("DVB", [128, 512], BF16)
            D1 = sb("D1", [128, 8, 8], F32)
            D2 = sb("D2", [128, 8, 8], F32)
            AT = sb("AT", [128, 512], BF16)
            ZT = sb("ZT", [128, 128], F32)
            SP = sb("SP", [128, 128], F32)
            EPt = sb("EPt", [128, 128], F32)
            EMt = sb("EMt", [128, 128], F32)
            GQP = sb("GQP", [128, 128], BF16)
            GQM = sb("GQM", [128, 128], BF16)
            GKP = sb("GKP", [128, 128], BF16)
            GKM = sb("GKM", [128, 128], BF16)
            GKM0 = sb("GKM0", [128, 128], BF16)
            GKM1 = sb("GKM1", [128, 128], BF16)
            GVB = sb("GVB", [128, 256], BF16)
            SGG = sb("SGG", [128, 256], BF16)
            GT = sb("GT", [32, 16, 128], BF16)
            EGL = sb("EGL", [32, 8], F32)
            TMP1 = sb("TMP1", [128, 512], F32)
            OY = sb("OY", [64, 512], F32)
            MXR = sb("MXR", [64, 512], BF16)
            MXG = sb("MXG", [64, 512], BF16)
            KVT = sb("KVT", [64, 256], F32)

            ifr = self.cst("ifr")
            ifd = self.cst("ifd")
            TWO_PI = 2.0 * math.pi

            def post_norm(OTv, G, SGTv, MX):
                OSQ_ = TMP1[0:64, :]
                ORS_ = DSQ[0:64, :]
                self.act(OSQ_, OTv, AF.Square)
                self.mm(MS[0:64, :], onesf[0:64, 0:64], OSQ_)
                self.rsqrt(ORS_, MS[0:64, :], 1.0 / 64, EPS)
                self.tt("dve", OY.v, OTv, ORS_, ALU.mult)
                self.stt("dve", MX.v, OY.v, G[:, 0:1], SGTv, ALU.mult, ALU.mult)

            import os
            cut = int(os.environ.get("P1CUT", "9"))
            ntl = int(os.environ.get("P1TILES", str(NT)))
            TPA, TPB = TP[0], TP[1]
            PROJ = [sb(f"PROJ{i}", [128, WINC], F32) for i in range(2)]

            class _PV:
                def __init__(self, tile, off, w):
                    self.tile, self.off, self.w = tile, off, w

                def __getitem__(self, k):
                    rows, cols = k
                    return self.tile[rows, self.off + cols.start:self.off + cols.stop]

                @property
                def v(self):
                    return self.tile[:, self.off:self.off + self.w]

            def stageA(i):
                xt = XT[i % 2]
                self.dma("sp", xt.v, x_src[i * 128:(i + 1) * 128, :] if l == 0 else self.out_tile(i))
                self.act(junk.v, xt.v, AF.Square, accum=ssq.v)
                self.rsqrt(ssq.v, ssq.v, 1.0 / D, EPS)
                self.stt("dve", HF.v, xt.v, ssq[:, 0:1], G1.v, ALU.mult, ALU.mult)
                self.tt("pool", HB.v, HF.v, SH1, ALU.add)
                tp = TPA
                for k in range(8):
                    self.tr(tp[:, k * 128:(k + 1) * 128], HB[:, k * 128:(k + 1) * 128], self.identb.v)
                self.cp("act", HT[:, 0:4, :], tp[:, 0:512].rearrange("p (k t) -> p k t", k=4))
                self.cp("dve", HT[:, 4:8, :], tp[:, 512:1024].rearrange("p (k t) -> p k t", k=4))
                yield
                P_ = PROJ[i % 2]
                for n, width in ((0, 512), (1, 512), (2, 512), (3, 512), (4, 512), (6, 384), (5, 512)):
                    p = PJ[n % 2]
                    for k in range(8):
                        self.mm(p[:, 0:width], HT[:, k, :], WIN[:, k, n * 512:n * 512 + width], start=(k == 0), stop=(k == 7))
                    self.cp("act" if n % 2 == 0 else "dve", P_[:, n * 512:n * 512 + width], p[:, 0:width])
                    yield

            def stageB(i):
                if cut < 1:
                    return

                def proj(n, width):
                    return _PV(PROJ[i % 2], n * 512, width)

                cosr = self.COS[:, i, 0:32].unsqueeze(1).bc([128, 8, 32])
                sinr = self.SIN[:, i, 0:32].unsqueeze(1).bc([128, 8, 32])
                cosd = self.COS[:, i, 32:40].unsqueeze(1).bc([128, 8, 8])
                sind = self.SIN[:, i, 32:40].unsqueeze(1).bc([128, 8, 8])


                yield
                p0 = proj(0, 512)
                pv = p0.v.rearrange("p (g h w) -> p g h w", g=8, h=2)
                X1 = pv[:, :, 0, :]
                X2 = pv[:, :, 1, :]
                rv_ = ROT.v.rearrange("p g (h w) -> p g h w", h=2)
                self.tt("dve", R1.v, X1, cosr, ALU.mult)
                self.tt("dve", R2.v, X2, sinr, ALU.mult)
                self.tt("pool", rv_[:, :, 0, :], R1.v, R2.v, ALU.subtract)
                self.tt("dve", R1.v, X1, sinr, ALU.mult)
                self.tt("dve", R2.v, X2, cosr, ALU.mult)
                self.tt("pool", rv_[:, :, 1, :], R1.v, R2.v, ALU.add)
                rq = ROT[:, 0:4, :].rearrange("p g w -> p (g w)")
                rk = ROT[:, 4:8, :].rearrange("p g w -> p (g w)")
                self.cp("act", QB.v, rq)
                self.tt("pool", QDB.v, rq, self.cst("qd"), ALU.mult)
                self.act(KB.v, rk, AF.Copy, scale=0.125)
                self.tt("pool", KD0.v, rk, self.cst("kd0"), ALU.mult)
                self.tt("pool", KD1.v, rk, self.cst("kd1"), ALU.mult)
                if cut < 2:
                    return
                yield
                p1 = proj(1, 512)
                self.cp("act", VR.v, p1[:, 0:256])
                self.act(SGR.v, p1[:, 256:512], AF.Silu)
                if cut == 2 and os.environ.get("SUB", "9") == "0":
                    return
                tp = TPB
                for h in range(4):
                    self.tr(tp[0:64, h * 128:(h + 1) * 128], QB[:, h * 64:(h + 1) * 64], self.identb.v)
                    self.tr(tp[0:64, (4 + h) * 128:(5 + h) * 128], QDB[:, h * 64:(h + 1) * 64], self.identb.v)
                self.cp("dve", RT[:, 0:8, :], tp[0:64, :].rearrange("p (k t) -> p k t", k=8))
                if cut == 2 and os.environ.get("SUB", "9") == "1":
                    return
                tp = TPB
                for h in range(4):
                    self.tr(tp[0:64, h * 128:(h + 1) * 128], KB[:, h * 64:(h + 1) * 64], self.identb.v)
                    self.tr(tp[0:64, (4 + h) * 128:(5 + h) * 128], SGR[:, h * 64:(h + 1) * 64], self.identb.v)
                self.cp(os.environ.get("GRP2", "act"), RT[:, 8:12, :], tp[0:64, 0:512].rearrange("p (k t) -> p k t", k=4))
                self.cp("dve", SGT[:, 0:4, :], tp[0:64, 512:1024].rearrange("p (k t) -> p k t", k=4))
                if cut < 3:
                    return
                yield
                for h in range(4):
                    self.mm(SC[:, h * 128:(h + 1) * 128], RT[:, 8 + h, :], RT[:, h, :])
                self.tt("dve", AT.v, SC.v, self.cst("dr"), ALU.mult)
                yield
                for h in range(4):
                    o = OT[:, h * 128:(h + 1) * 128]
                    self.mm(o, VR[:, h * 64:(h + 1) * 64], AT[:, h * 128:(h + 1) * 128], start=(h == 0), stop=False)
                    self.mm(OT[:, h * 128:h * 128 + 64], SRb[:, h * 64:(h + 1) * 64], RT[:, 4 + h, 0:64], start=False, stop=False)
                for h in range(4):
                    self.mm(MS[0:64, h * 64:(h + 1) * 64], KD0[:, h * 64:(h + 1) * 64], VR[:, h * 64:(h + 1) * 64])
                    self.mm(MS[0:64, 256 + h * 64:256 + (h + 1) * 64], KD1[:, h * 64:(h + 1) * 64], VR[:, h * 64:(h + 1) * 64])
                self.tt("dve", KVT.v, SR.v, self.cst("cd")[0:64, :], ALU.mult)
                self.tt("dve", SR.v, KVT.v, MS[0:64, 0:256], ALU.add)
                self.cp("act", SRb.v, SR.v)
                yield
                for h in range(4):
                    self.mm(OT[:, h * 128 + 64:(h + 1) * 128], SRb[:, h * 64:(h + 1) * 64], RT[:, 4 + h, 64:128], start=False, stop=True)
                self.tt("dve", KVT.v, SR.v, self.cst("cd")[0:64, :], ALU.mult)
                self.tt("dve", SR.v, KVT.v, MS[0:64, 256:512], ALU.add)
                self.cp("act", SRb.v, SR.v)
                yield
                post_norm(OT.v, GRN, SGT[:, 0:4, :].rearrange("p k t -> p (k t)"), MXR)
                yield
                self.dma("act", self.mixT_s[0:256, i * 128:(i + 1) * 128].rearrange("(h p) t -> p h t", p=64),
                         MXR.v.rearrange("p (h t) -> p h t", h=4))

                if cut < 4:
                    return
                yield
                for (n, G, OUTB, dst) in ((2, GQ, DQB, self.qT_s), (3, GK, DKB, self.kT_s)):
                    pq = proj(n, 512)
                    self.act(DSQ.v, pq.v, AF.Square)
                    self.red("dve", DSS.v, DSQ.v.rearrange("p (g w) -> p g w", g=8))
                    self.rsqrt(DSS.v, DSS.v, 1.0 / 64, EPS)
                    self.tt("dve", DXN.v, pq.v.rearrange("p (g w) -> p g w", g=8), DSS.v.unsqueeze(2).bc([128, 8, 64]), ALU.mult)
                    self.tt("pool", DXN.v, DXN.v, G.v.unsqueeze(1).bc([128, 8, 64]), ALU.mult)
                    ob = OUTB.v.rearrange("p (g w) -> p g w", g=8)
                    self.cp("act", ob[:, :, 16:64], DXN[:, :, 16:64])
                    x1 = DXN[:, :, 0:8]
                    x2 = DXN[:, :, 8:16]
                    self.tt("dve", D1.v, x1, cosd, ALU.mult)
                    self.tt("pool", D2.v, x2, sind, ALU.mult)
                    self.tt("dve", ob[:, :, 0:8], D1.v, D2.v, ALU.subtract)
                    self.tt("dve", D1.v, x1, sind, ALU.mult)
                    self.tt("pool", D2.v, x2, cosd, ALU.mult)
                    self.tt("dve", ob[:, :, 8:16], D1.v, D2.v, ALU.add)
                    tp = TPB
                    for h in range(4):
                        self.tr(tp[:, h * 128:(h + 1) * 128], OUTB[:, h * 128:(h + 1) * 128], self.identb.v)
                    dtv = DT[:, 0:4, :] if n == 2 else DT[:, 4:8, :]
                    self.cp("act" if n == 2 else "dve", dtv, tp[:, 0:512].rearrange("p (k t) -> p k t", k=4))
                    self.dma("sp", dst[:, :, i * 128:(i + 1) * 128].rearrange("h p t -> p h t"), dtv)
                yield
                p4 = proj(4, 512)
                self.cp("act", DVB.v, p4.v)
                self.dma("act", self.v_s[i * 128:(i + 1) * 128, :], DVB.v)

                if cut < 5:
                    return
                yield
                p6 = proj(6, 384)
                self.tt("dve", ZT.v, p6[:, 0:128], BA.v, ALU.add)
                self.act(SGG.v, p6[:, 128:384], AF.Silu)
                self.act(SP.v, ZT.v, AF.Exp, scale=-1.0)
                self.act(SP.v, SP.v, AF.Ln, bias=self.C_one[:, 0:1])
                self.mm(SC2[:, 0:128], self.cst("tri"), SP.v)
                self.act(EPt.v, SC2[:, 0:128], AF.Exp, scale=-1.0 / 16)
                self.act(EMt.v, SC2[:, 0:128], AF.Exp, scale=1.0 / 16)
                for h in range(4):
                    self.mm(MS[0:32, 2 * h:2 * h + 2], SP[:, h * 32:(h + 1) * 32], self.cst("chk"))
                self.act(EGL.v, MS[0:32, 0:8], AF.Exp, scale=-1.0 / 16)
                p5 = proj(5, 512)
                qs = 32.0 ** -0.5
                self.stt("dve", GQP.v, p5[:, 0:128], qs, EPt.v, ALU.mult, ALU.mult)
                self.stt("dve", GQM.v, p5[:, 0:128], qs, EMt.v, ALU.mult, ALU.mult)
                self.tt("dve", GKP.v, p5[:, 128:256], EPt.v, ALU.mult)
                self.tt("dve", GKM.v, p5[:, 128:256], EMt.v, ALU.mult)
                self.cp("act", GVB.v, p5[:, 256:512])
                self.ts("pool", GKM0.v, GKM.v, self.cst("m0")[:, 0:1], None, op0=ALU.mult)
                self.ts("pool", GKM1.v, GKM.v, self.cst("m1")[:, 0:1], None, op0=ALU.mult)
                tp = TPB
                for h in range(4):
                    self.tr(tp[0:32, h * 128:(h + 1) * 128], GQP[:, h * 32:(h + 1) * 32], self.identb.v)
                    self.tr(tp[0:32, (4 + h) * 128:(5 + h) * 128], GQM[:, h * 32:(h + 1) * 32], self.identb.v)
                self.cp("act", GT[:, 0:8, :], tp[0:32, :].rearrange("p (k t) -> p k t", k=8))
                tp = TPB
                for h in range(4):
                    self.tr(tp[0:32, h * 128:(h + 1) * 128], GKM[:, h * 32:(h + 1) * 32], self.identb.v)
                    self.tr(tp[0:32, (4 + h) * 128:(5 + h) * 128], GKP[:, h * 32:(h + 1) * 32], self.identb.v)
                self.cp("dve", GT[:, 8:16, :], tp[0:32, :].rearrange("p (k t) -> p k t", k=8))
                tp = TPB
                for h in range(4):
                    self.tr(tp[0:64, h * 128:(h + 1) * 128], SGG[:, h * 64:(h + 1) * 64], self.identb.v)
                self.cp("act", SGT[:, 4:8, :], tp[0:64, 0:512].rearrange("p (k t) -> p k t", k=4))
                for h in range(4):
                    self.mm(SC[:, h * 128:(h + 1) * 128], GT[:, 8 + h, :], GT[:, h, :])
                    self.mm(SC2[:, h * 128:(h + 1) * 128], GT[:, 12 + h, :], GT[:, 4 + h, :])
                self.tt("dve", TMP1.v, SC.v, self.cst("ml4"), ALU.mult)
                self.tt("dve", DSQ.v, SC2.v, self.cst("mu4"), ALU.mult)
                self.tt("pool", AT.v, TMP1.v, DSQ.v, ALU.add)
                yield
                for h in range(4):
                    self.mm(OT[:, h * 128:(h + 1) * 128], GVB[:, h * 64:(h + 1) * 64], AT[:, h * 128:(h + 1) * 128], start=(h == 0), stop=False)
                    self.mm(OT[:, h * 128:h * 128 + 64], SGb[:, h * 64:(h + 1) * 64], GT[:, h, 0:64], start=False, stop=False)
                for h in range(4):
                    self.mm(MS[0:32, h * 64:(h + 1) * 64], GKM0[:, h * 32:(h + 1) * 32], GVB[:, h * 64:(h + 1) * 64])
                    self.mm(MS[0:32, 256 + h * 64:256 + (h + 1) * 64], GKM1[:, h * 32:(h + 1) * 32], GVB[:, h * 64:(h + 1) * 64])
                eg = EGL.v.rearrange("p (h c) -> p h c", c=2)
                sgv = SG.v.rearrange("p (h w) -> p h w", h=4)
                kv3 = KVT[0:32, :].rearrange("p (h w) -> p h w", h=4)
                self.tt("dve", KVT[0:32, :], SG.v, MS[0:32, 0:256], ALU.add)
                self.tt("dve", sgv, kv3, eg[:, :, 0:1].bc([32, 4, 64]), ALU.mult)
                self.cp("act", SGb.v, SG.v)
                yield
                for h in range(4):
                    self.mm(OT[:, h * 128 + 64:(h + 1) * 128], SGb[:, h * 64:(h + 1) * 64], GT[:, h, 64:128], start=False, stop=True)
                self.tt("dve", KVT[0:32, :], SG.v, MS[0:32, 256:512], ALU.add)
                self.tt("dve", sgv, kv3, eg[:, :, 1:2].bc([32, 4, 64]), ALU.mult)
                self.cp("act", SGb.v, SG.v)
                yield
                post_norm(OT.v, GGN, SGT[:, 4:8, :].rearrange("p k t -> p (k t)"), MXG)
                yield
                self.dma("act", self.mixT_s[768:1024, i * 128:(i + 1) * 128].rearrange("(h p) t -> p h t", p=64),
                         MXG.v.rearrange("p (h t) -> p h t", h=4))


            RB = int(os.environ.get("P1RB", "3"))
            for _ in stageA(0):
                pass
            for i in range(ntl):
                self.run_bg(2)
                gb = stageB(i)
                ga = stageA(i + 1) if i + 1 < ntl else iter(())
                done_a = done_b = False
                while not (done_a and done_b):
                    for _ in range(RB):
                        if not done_b:
                            try:
                                next(gb)
                            except StopIteration:
                                done_b = True
                    if not done_a:
                        try:
                            next(ga)
                        except StopIteration:
                            done_a = True

    def phase2(self, l):
        lam_init = 0.8 - 0.6 * math.exp(-0.3 * l)
        with contextlib.ExitStack() as st:
            sb = lambda n, s, d: self.sb(st, n, s, d)
            ps = lambda n, s, d: self.ps(st, n, s, d)
            KT = sb("KT", [128, 2, S], BF16)
            VV = sb("VV", [128, NT, 256], BF16)
            vsrc = self.v_s.v.rearrange("(i p) c -> p i c", p=128)

            def load_group(hg):
                for hh in range(2):
                    for j in range(4):
                        self.dma("sp" if (hh + j) % 2 == 0 else "act", KT[:, hh, j * 2048:(j + 1) * 2048], self.kT_s[2 * hg + hh, :, j * 2048:(j + 1) * 2048])
                for j in range(8):
                    self.dma("sp" if j % 2 == 0 else "act", VV[:, j * 8:(j + 1) * 8, :], vsrc[:, j * 8:(j + 1) * 8, hg * 256:(hg + 1) * 256])
            lq = sb("lq", [1, 4, 64], F32)
            for j, src in enumerate((self.lam_q1, self.lam_k1, self.lam_q2, self.lam_k2)):
                self.dma("sp", lq[:, j, :], src[l:l + 1, :])
            lp = sb("lp", [1, 2, 64], F32)
            self.tt("dve", lp[:, 0, :], lq[:, 0, :], lq[:, 1, :], ALU.mult)
            self.tt("dve", lp[:, 1, :], lq[:, 2, :], lq[:, 3, :], ALU.mult)
            ls = sb("ls", [1, 2], F32)
            self.red("dve", ls.v, lp.v)
            self.act(ls.v, ls.v, AF.Exp)
            lam1 = sb("lam1", [1, 1], F32)
            self.tt("dve", lam1.v, ls[:, 0:1], ls[:, 1:2], ALU.subtract)
            self.ts("dve", lam1.v, lam1.v, lam_init, -1.0, op0=ALU.add, op1=ALU.mult)
            NSB = 4
            SPS = [ps(f"SPS{i}", [128, 512], F32) for i in range(NSB)]
            OP = [ps(f"OP{i}", [128, 512], F32) for i in range(2)]
            MSP = ps("MSP", [128, 512], F32)
            MSP2 = ps("MSP2", [128, 512], F32)
            NLAM = sb("NLAM", [128, 1], F32)
            self.mm(MSP[:, 0:1], self.cst("ones")[0:1, :], lam1.v)
            self.cp("dve", NLAM.v, MSP[:, 0:1])
            SUBG = sb("SUBG", [128, 1], F32)
            self.dma("sp", SUBG.v, self.diff_sub_g[l].unsqueeze(1))
            self.ts("dve", SUBG.v, SUBG.v, 1.0 - lam_init)
            QT = [sb(f"QT{i}", [128, 512], BF16) for i in range(2)]
            PT = [sb(f"PT{i}", [128, 512], BF16) for i in range(4)]
            ACC = [[sb(f"ACC{p_}{c}", [128, 512], F32) for c in range(2)] for p_ in range(2)]
            OS = [sb(f"OS{c}", [128, 512], F32) for c in range(2)]
            R0 = sb("R0", [128, 512], F32)
            R1 = sb("R1", [128, 512], F32)
            ACB = [sb(f"ACB{c}", [128, 512], BF16) for c in range(2)]
            T0 = sb("T0", [128, 512], F32)
            T1 = sb("T1", [128, 512], F32)
            OSQ = sb("OSQ2", [128, 512], BF16)
            ORS = sb("ORS2", [128, 512], F32)
            OB = [sb(f"OB{i}", [128, 512], BF16) for i in range(2)]
            onesf = self.cst("ones")
            import os
            nqt = int(os.environ.get("P2QT", str(S // 512)))
            groups = [(a, b_, c_) for a in range(2) for b_ in range(nqt) for c_ in range(2)]
            units = []
            for gi, (hg, qt, hh) in enumerate(groups):
                nk = 4 * qt + 4
                for kt in range(nk):
                    for c in range(2):
                        units.append((gi, c, kt, nk))
            LOOK = 2
            qloaded = set()

            def load_q(gi):
                if gi >= len(groups) or gi in qloaded:
                    return
                qloaded.add(gi)
                hg, qt, hh = groups[gi]
                if qt == 0 and hh == 0:
                    load_group(hg)
                self.dma("sp", QT[gi % 2].v, self.qT_s[2 * hg + hh, :, qt * 512:(qt + 1) * 512])

            def c0_of(gi, kt):
                qt = groups[gi][1]
                m = kt - 4 * qt
                return (128 * m if m > 0 else 0), m

            def issue_qk(u):
                gi, c, kt, nk = units[u]
                load_q(gi)
                hh = groups[gi][2]
                c0, m = c0_of(gi, kt)
                self.mm(SPS[u % NSB][:, c0:512], KT[64 * c:64 * c + 64, hh, kt * 128:(kt + 1) * 128], QT[gi % 2][64 * c:64 * c + 64, c0:512])

            def finish(gi):
                hg, qt, hh = groups[gi]
                h = 2 * hg + hh
                acc = ACC[gi % 2]
                self.cp("act", OS[0].v, OP[0].v)
                self.cp("act", OS[1].v, OP[1].v)
                self.cp("dve", ACB[0].v, acc[0].v)
                self.cp("dve", ACB[1].v, acc[1].v)
                self.mm(MSP.v, self.onesb.v, ACB[0].v)
                self.mm(MSP2.v, self.onesb.v, ACB[1].v)
                self.act(R0.v, MSP.v, AF.Ln)
                self.act(R0.v, R0.v, AF.Exp, scale=-1.0)
                self.act(R1.v, MSP2.v, AF.Ln)
                self.act(R1.v, R1.v, AF.Exp, scale=-1.0)
                self.tt("dve", T0.v, OS[0].v, R0.v, ALU.mult)
                self.tt("dve", T1.v, OS[1].v, R1.v, ALU.mult)
                self.stt("dve", T0.v, T1.v, NLAM[:, 0:1], T0.v, ALU.mult, ALU.add)
                self.act(OSQ.v, T0.v, AF.Square)
                self.mm(MSP.v, self.onesb.v, OSQ.v)
                self.act(ORS.v, MSP.v, AF.Ln, scale=1.0 / 128, bias=self.C_eps[:, 0:1])
                self.act(ORS.v, ORS.v, AF.Exp, scale=-0.5)
                ob = OB[gi % 2]
                self.stt("dve", ob.v, T0.v, SUBG[:, 0:1], ORS.v, ALU.mult, ALU.mult)
                self.dma("act", self.mixT_s[256 + 128 * h:256 + 128 * (h + 1), qt * 512:(qt + 1) * 512], ob.v)

            def hg_of(u):
                return groups[units[u][0]][0]

            for u, (gi, c, kt, nk) in enumerate(units):
                if u == 0 or hg_of(u - 1) != hg_of(u):
                    for u2 in range(u, min(u + LOOK, len(units))):
                        if hg_of(u2) == hg_of(u):
                            issue_qk(u2)
                if u % 2 == 0:
                    for u2 in (u + LOOK, u + LOOK + 1):
                        if u2 < len(units) and hg_of(u2) == hg_of(u):
                            issue_qk(u2)
                hh = groups[gi][2]
                c0, m = c0_of(gi, kt)
                pt = PT[u % 4]
                if kt == 0 and c == 0 and gi + 1 < len(groups) and groups[gi + 1][0] == groups[gi][0]:
                    load_q(gi + 1)
                self.act(pt[:, c0:512], SPS[u % NSB][:, c0:512], AF.Exp)
                if m >= 0:
                    self.memset("pool", pt[64:128, c0:c0 + 64], 0.0)
                self.mm(OP[c][:, c0:512], VV[:, kt, hh * 128:(hh + 1) * 128], pt[:, c0:512], start=(kt == 0), stop=(kt == nk - 1))
                a = ACC[gi % 2][c]
                if kt == 0:
                    self.cp("dve", a.v, pt.v)
                else:
                    self.tt("dve", a[:, c0:512], a[:, c0:512], pt[:, c0:512], ALU.add)
                if kt == nk - 1 and c == 1:
                    finish(gi)

    def phase3(self, l, x_src):
        with contextlib.ExitStack() as st:
            sb = lambda n, s, d: self.sb(st, n, s, d)
            ps = lambda n, s, d: self.ps(st, n, s, d)
            WO = sb("WO", [128, 8, D], BF16)
            for k in range(8):
                self.dma("pool", WO[:, k, :], self.w_out[l, k * 128:(k + 1) * 128, :])
            G2 = sb("G2", [128, D], F32)
            self.dma("sp", G2.v, self.norm2_g[l].pbc(128))
            self.stt("dve", G2.v, self.mod(4), 1.0, G2.v, ALU.add, ALU.mult)
            SH2 = self.mod(3)
            GT1 = self.mod(2)
            RW = sb("RW", [128, 8, NE], F32)
            self.dma("sp", RW.v, self.router_w.v.rearrange("(k p) e -> p k e", p=128))
            MT = [sb(f"MT{i}", [128, 8, 512], BF16) for i in range(2)]
            XT = [sb(f"X3{i}", [128, D], F32) for i in range(2)]
            X1 = [sb(f"X1{i}", [128, D], F32) for i in range(2)]
            TM = sb("TM3", [128, D], F32)
            junk = sb("junk3", [128, D], BF16)
            ssq = sb("ssq3", [128, 1], F32)
            H2F = sb("H2F", [128, D], F32)
            H2B = [sb(f"H2B{i}", [128, D], BF16) for i in range(2)]
            H2T = sb("H2T", [128, D], F32)
            PO = [ps(f"PO{i}", [128, 512], F32) for i in range(2)]
            PTR = [ps(f"PTR{i}", [128, 512], F32) for i in range(2)]
            PL = ps("PL", [128, NE], F32)
            identf = self.cst("ident")
            mview = self.mixT_s.v.rearrange("(k p) t -> p k t", p=128)
            for i in range(NT):
                if i % 4 == 0:
                    mt = MT[(i // 4) % 2]
                    self.dma("sp", mt.v, mview[:, :, i * 128:i * 128 + 512])
                xt = XT[i % 2]
                x1 = X1[i % 2]
                self.dma("act", xt.v, x_src[i * 128:(i + 1) * 128, :] if l == 0 else self.out_tile(i))
                tcol = (i % 4) * 128
                for hf in range(2):
                    for k in range(8):
                        self.mm(PO[hf].v, mt[:, k, tcol:tcol + 128], WO[:, k, hf * 512:(hf + 1) * 512], start=(k == 0), stop=(k == 7))
                    self.tt("dve", TM[:, hf * 512:(hf + 1) * 512], PO[hf].v, GT1[:, hf * 512:(hf + 1) * 512], ALU.mult)
                self.tt("pool", x1.v, TM.v, xt.v, ALU.add)
                self.dma("sp", self.out_tile(i), x1.v)
                self.act(junk.v, x1.v, AF.Square, accum=ssq.v)
                self.rsqrt(ssq.v, ssq.v, 1.0 / D, EPS)
                self.stt("dve", H2F.v, x1.v, ssq[:, 0:1], G2.v, ALU.mult, ALU.mult)
                self.tt("pool", H2F.v, H2F.v, SH2, ALU.add)
                hb = H2B[i % 2]
                self.cp("act", hb.v, H2F.v)
                self.dma("act", self.h2_s[i * 128:(i + 1) * 128, :], hb.v)
                for hf in range(2):
                    for k in range(4):
                        kk = hf * 4 + k
                        self.tr(PTR[hf][:, k * 128:(k + 1) * 128], H2F[:, kk * 128:(kk + 1) * 128], identf)
                    self.cp("dve" if hf == 0 else "act", H2T[:, hf * 512:(hf + 1) * 512], PTR[hf].v)
                for k in range(8):
                    self.mm(PL.v, H2T[:, k * 128:(k + 1) * 128], RW[:, k, :], start=(k == 0), stop=(k == 7))
                self.cp("dve", self.LOG[:, i, :], PL.v)
            self.dump(f"log{l}", self.LOG.v, [128, NT, NE])

    def phase_moe(self, l):
        import os
        BIG = 1.0e30
        NTE = NT * NE
        with contextlib.ExitStack() as rst:
            rsb = lambda n, s_, d: self.sb(rst, n, s_, d)
            RG1 = rsb("RG1", [128, NT], F32)
            RG2 = rsb("RG2", [128, NT], F32)
            D1I = rsb("D1I", [128, NT], I32)
            D2I = rsb("D2I", [128, NT], I32)
            IDXW = rsb("IDXW", [128, NB, 12], I32)
            with contextlib.ExitStack() as st:
                sb = lambda n, s_, d: self.sb(st, n, s_, d)
                ps = lambda n, s_, d: self.ps(st, n, s_, d)
                RB = sb("RB", [128, NE], F32)
                self.dma("sp", RB.v, self.router_b.v.pbc(128))
                SCO = sb("SCO", [128, NT, NE], F32)
                BIA = sb("BIA", [128, NT, NE], F32)
                TA = sb("TA", [128, NT, NE], F32)
                TB = sb("TB", [128, NT, NE], F32)
                OH1 = sb("OH1", [128, NT, NE], F32)
                OH2 = sb("OH2", [128, NT, NE], F32)
                M1 = sb("M1", [128, NT * 8], F32)
                M2 = sb("M2", [128, NT * 8], F32)
                GM = sb("GM", [128, NT], F32)
                V1 = sb("V1", [128, NT], F32)
                W1 = sb("W1", [128, NT], F32)
                W2 = sb("W2", [128, NT], F32)
                self.act(SCO.v, self.LOG.v, AF.Sigmoid)
                self.tt("dve", BIA.v, SCO.v, RB.v.unsqueeze(1).bc([128, NT, NE]), ALU.add)
                b4 = BIA.v.rearrange("p i (g k) -> p (i g) k", k=4)
                self.red("dve", M1.v, b4, op=ALU.max)
                ta4 = TA.v.rearrange("p i (g k) -> p (i g) k", k=4)
                self.tt("dve", ta4, b4, M1.v.unsqueeze(2).bc([128, NT * 8, 4]), ALU.is_equal)
                self.stt("dve", TA.v, TA.v, -BIG, BIA.v, ALU.mult, ALU.add)
                self.red("dve", M2.v, ta4, op=ALU.max)
                self.tt("dve", M1.v, M1.v, M2.v, ALU.add)
                gs3 = M1.v.rearrange("p (i g) -> p i g", g=8)
                self.red("dve", GM.v, gs3, op=ALU.max)
                self.tt("dve", M2.v.rearrange("p (i g) -> p i g", g=8), gs3, GM.v.unsqueeze(2).bc([128, NT, 8]), ALU.is_equal)
                self.ts("dve", ta4, M2.v.unsqueeze(2).bc([128, NT * 8, 4]), BIG, -BIG, op0=ALU.mult, op1=ALU.add)
                self.tt("dve", TA.v, TA.v, BIA.v, ALU.add)
                self.red("dve", V1.v, TA.v, op=ALU.max)
                self.tt("dve", OH1.v, TA.v, V1.v.unsqueeze(2).bc([128, NT, NE]), ALU.is_equal)
                self.stt("dve", TB.v, OH1.v, -BIG, TA.v, ALU.mult, ALU.add)
                self.red("dve", V1.v, TB.v, op=ALU.max)
                self.tt("dve", OH2.v, TB.v, V1.v.unsqueeze(2).bc([128, NT, NE]), ALU.is_equal)
                self.tt("dve", TA.v, SCO.v, OH1.v, ALU.mult)
                self.red("dve", W1.v, TA.v)
                self.tt("dve", TA.v, SCO.v, OH2.v, ALU.mult)
                self.red("dve", W2.v, TA.v)
                self.tt("dve", V1.v, W1.v, W2.v, ALU.add)
                self.recip(V1.v, V1.v)
                self.tt("dve", RG1.v, W1.v, V1.v, ALU.mult)
                self.tt("dve", RG2.v, W2.v, V1.v, ALU.mult)
                MS_ = sb("MSEL", [128, NT, NE], BF16)
                self.tt("dve", MS_.v, OH1.v, OH2.v, ALU.add)
                LTSb = sb("LTSb", [128, 128], BF16)
                self.cp("dve", LTSb.v, self.cst("lts"))
                PRE = sb("PRE", [128, NT, NE], F32)
                TOT = sb("TOT", [128, NT, NE], F32)
                PP = [ps(f"PP{i}", [128, 512], F32) for i in range(2)]
                msf = MS_.v.rearrange("p i e -> p (i e)")
                pre_f = PRE.v.rearrange("p i e -> p (i e)")
                tot_f = TOT.v.rearrange("p i e -> p (i e)")
                for c in range(NTE // 512):
                    self.mm(PP[0].v, LTSb.v, msf[:, c * 512:(c + 1) * 512])
                    self.cp("dve", pre_f[:, c * 512:(c + 1) * 512], PP[0].v)
                    self.mm(PP[1].v, self.onesb.v, msf[:, c * 512:(c + 1) * 512])
                    self.cp("act", tot_f[:, c * 512:(c + 1) * 512], PP[1].v)
                A_, B_ = TA, TB
                self.cp("dve", A_.v, TOT.v)
                k = 1
                while k < NT:
                    self.tt("dve", B_[:, k:, :], A_[:, k:, :], A_[:, :NT - k, :], ALU.add)
                    self.cp("dve", B_[:, :k, :], A_[:, :k, :])
                    A_, B_ = B_, A_
                    k *= 2
                INC = A_
                OFF = B_
                self.tt("dve", OFF.v, INC.v, TOT.v, ALU.subtract)
                CNT = INC[:, NT - 1, :]
                CMP = sb("CMP", [128, NE, 32], F32)
                self.tt("dve", CMP.v, CNT.unsqueeze(2).bc([128, NE, 32]), self.cst("jb")[:, 0:32].unsqueeze(1).bc([128, NE, 32]), ALU.is_gt)
                PADE = sb("PADE", [128, NE], F32)
                self.red("dve", PADE.v, CMP.v)
                self.ts("dve", PADE.v, PADE.v, float(BLK))
                EA = sb("EA", [128, NE], F32)
                EB = sb("EB", [128, NE], F32)
                self.cp("dve", EA.v, PADE.v)
                a_, b_ = EA, EB
                k = 1
                while k < NE:
                    self.tt("dve", b_[:, k:], a_[:, k:], a_[:, :NE - k], ALU.add)
                    self.cp("dve", b_[:, :k], a_[:, :k])
                    a_, b_ = b_, a_
                    k *= 2
                PEND = a_
                PST = b_
                self.tt("dve", PST.v, PEND.v, PADE.v, ALU.subtract)
                self.tt("dve", PRE.v, PRE.v, OFF.v, ALU.add)
                self.tt("dve", PRE.v, PRE.v, PST.v.unsqueeze(1).bc([128, NT, NE]), ALU.add)
                self.tt("dve", TOT.v, PRE.v, OH1.v, ALU.mult)
                self.red("dve", W1.v, TOT.v)
                self.tt("dve", TOT.v, PRE.v, OH2.v, ALU.mult)
                self.red("dve", W2.v, TOT.v)
                self.cp("dve", D1I.v, W1.v)
                self.cp("dve", D2I.v, W2.v)
                CM2 = sb("CM2", [128, NB, NE], F32)
                self.tt("dve", CM2.v, PEND.v.unsqueeze(1).bc([128, NB, NE]), self.cst("jb").unsqueeze(2).bc([128, NB, NE]), ALU.is_le)
                BE = sb("BE", [128, NB], F32)
                self.red("dve", BE.v, CM2.v)
                self.ts("dve", BE.v, BE.v, float(NE - 1), None, op0=ALU.min)
                IDXF = sb("IDXF", [128, NB, 12], F32)
                coff = self.cst("coff")
                self.stt("dve", IDXF[:, :, 0:8], BE.v.unsqueeze(2).bc([128, NB, 8]), float(D), coff[:, 0:8].unsqueeze(1).bc([128, NB, 8]), ALU.mult, ALU.add)
                self.stt("dve", IDXF[:, :, 8:12], BE.v.unsqueeze(2).bc([128, NB, 4]), float(DE), coff[:, 8:12].unsqueeze(1).bc([128, NB, 4]), ALU.mult, ALU.add)
                self.cp("dve", IDXW.v, IDXF.v)
                if self.debug:
                    self.dump(f"d1_{l}", W1.v, [128, NT])
                    self.dump(f"d2_{l}", W2.v, [128, NT])
                    self.dump(f"g1_{l}", RG1.v, [128, NT])
                    self.dump(f"g2_{l}", RG2.v, [128, NT])
                    self.dump(f"be_{l}", BE.v, [128, NB])
            self.fw.barrier()
            if os.environ.get("MOECUT", "9") == "0":
                return
            with contextlib.ExitStack() as st:
                sb = lambda n, s_, d: self.sb(st, n, s_, d)
                HB = [sb(f"HBm{i}", [128, D], BF16) for i in range(3)]
                for i in range(NT):
                    hb = HB[i % 3]
                    self.dma("sp", hb.v, self.h2_s[i * 128:(i + 1) * 128, :])
                    self.scatter(self.xs_s.v, hb.v, D1I[:, i:i + 1])
                    self.scatter(self.xs_s.v, hb.v, D2I[:, i:i + 1])
            self.fw.barrier()
            if os.environ.get("MOECUT", "9") == "1":
                return
            with contextlib.ExitStack() as st:
                sb = lambda n, s_, d: self.sb(st, n, s_, d)
                ps = lambda n, s_, d: self.ps(st, n, s_, d)
                WG = [sb(f"WG{i}", [128, 8, DE], BF16) for i in range(2)]
                WU = [sb(f"WU{i}", [128, 8, DE], BF16) for i in range(2)]
                WD = [sb(f"WD{i}", [128, 4, D], BF16) for i in range(2)]
                XB = [sb(f"XB{i}", [128, NSUB, D], BF16) for i in range(2)]
                XTB = [sb(f"XTB{i}", [128, 8, BLK], BF16) for i in range(2)]
                SGt = sb("SGt", [128, BLK], F32)
                UT = sb("UT", [128, 4, BLK], BF16)
                YB = [sb(f"YB{i}", [128, D], F32) for i in range(2)]
                TPX = ps("TPX", [128, 1024], BF16)
                PG = [ps(f"PG{i}", [128, BLK], F32) for i in range(2)]
                PU = [ps(f"PU{i}", [128, BLK], F32) for i in range(2)]
                PD = [ps(f"PD{i}", [128, 512], F32) for i in range(2)]
                wg_t = self.wgb[l].v
                wu_t = self.wub[l].v
                wd_t = self.wdb[l].v
                nblk = int(os.environ.get("MOEBLK", str(NB)))
                yi = 0
                for j in range(nblk):
                    wg, wu, wd, xb, xtb = WG[j % 2], WU[j % 2], WD[j % 2], XB[j % 2], XTB[j % 2]
                    for c in range(8):
                        self.gather(wg[:, c, :], wg_t, IDXW[:, j, c:c + 1])
                        self.gather(wu[:, c, :], wu_t, IDXW[:, j, c:c + 1])
                    for c in range(4):
                        self.gather(wd[:, c, :], wd_t, IDXW[:, j, 8 + c:9 + c])
                    self.dma("sp", xb.v, self.xs_s[j * BLK:(j + 1) * BLK, :].rearrange("(s p) d -> p s d", p=128))
                    for sub in range(NSUB):
                        for k in range(8):
                            self.tr(TPX[:, k * 128:(k + 1) * 128], xb[:, sub, k * 128:(k + 1) * 128], self.identb.v)
                        self.cp("dve" if sub % 2 == 0 else "act", xtb[:, :, sub * 128:(sub + 1) * 128], TPX.v.rearrange("p (k t) -> p k t", k=8))
                    for fc in range(4):
                        pg, pu = PG[fc % 2], PU[fc % 2]
                        for k in range(8):
                            self.mm(pg.v, wg[:, k, fc * 128:(fc + 1) * 128], xtb[:, k, :], start=(k == 0), stop=(k == 7))
                        for k in range(8):
                            self.mm(pu.v, wu[:, k, fc * 128:(fc + 1) * 128], xtb[:, k, :], start=(k == 0), stop=(k == 7))
                        self.act(SGt.v, pg.v, AF.Silu)
                        self.tt("dve", UT[:, fc, :], SGt.v, pu.v, ALU.mult)
                    for sub in range(NSUB):
                        yb = YB[yi % 2]
                        yi += 1
                        for hf in range(2):
                            pd = PD[hf]
                            for fc in range(4):
                                self.mm(pd.v, UT[:, fc, sub * 128:(sub + 1) * 128], wd[:, fc, hf * 512:(hf + 1) * 512], start=(fc == 0), stop=(fc == 3))
                            self.cp("act" if hf == 0 else "dve", yb[:, hf * 512:(hf + 1) * 512], pd.v)
                        self.dma("act", self.ys_s[j * BLK + sub * 128:j * BLK + (sub + 1) * 128, :], yb.v)
            self.fw.barrier()
            if os.environ.get("MOECUT", "9") == "2":
                return
            with contextlib.ExitStack() as st:
                sb = lambda n, s_, d: self.sb(st, n, s_, d)
                GT2 = self.mod(5)
                Y1 = [sb(f"Y1{i}", [128, D], F32) for i in range(2)]
                Y2 = [sb(f"Y2{i}", [128, D], F32) for i in range(2)]
                XX = [sb(f"XX{i}", [128, D], F32) for i in range(2)]
                TT = [sb(f"TT{i}", [128, D], F32) for i in range(2)]
                for i in range(NT):
                    y1, y2, xx, tt_ = Y1[i % 2], Y2[i % 2], XX[i % 2], TT[i % 2]
                    self.gather(y1.v, self.ys_s.v, D1I[:, i:i + 1])
                    self.gather(y2.v, self.ys_s.v, D2I[:, i:i + 1])
                    self.dma("sp", xx.v, self.out_tile(i))
                    self.ts("dve", tt_.v, y1.v, RG1[:, i:i + 1], None, op0=ALU.mult)
                    self.stt("dve", tt_.v, y2.v, RG2[:, i:i + 1], tt_.v, ALU.mult, ALU.add)
                    if self.debug and l == 0:
                        self.dma("act", self.ydbg[i * 128:(i + 1) * 128, :], tt_.v)
                    self.tt("pool", tt_.v, tt_.v, GT2, ALU.mult)
                    self.tt("pool", xx.v, xx.v, tt_.v, ALU.add)
                    self.dma("sp", self.out_tile(i), xx.v)


_CACHE = {}


def _get_prog(stop_after=None, debug=False):
    key = (stop_after, debug)
    if key not in _CACHE:
        p = Prog(stop_after=stop_after, debug=debug)
        p.build()
        _CACHE[key] = p
    return _CACHE[key]


def make_in_map(inputs, b):
    f = lambda a: np.ascontiguousarray(a, dtype=np.float32)
    m = {
        "x": f(inputs["x"][b]),
        "c": f(np.asarray(inputs["c"][b]).reshape(8, 128).T),
        "pos": np.ascontiguousarray(np.asarray(inputs["positions"][b]).reshape(NT, 128).T.astype(np.int32)),
        "consts": CONST_NP,
    }
    for k in ("w_mod", "b_mod", "norm1_g", "norm2_g", "w_in", "ret_norm_g", "diff_q_g", "diff_k_g", "lam_q1", "lam_k1",
              "lam_q2", "lam_k2", "diff_sub_g", "gla_w_a2", "gla_b_a", "gla_norm_g", "w_out", "router_w", "router_b"):
        m[k] = f(inputs[k])
    for l in range(DEPTH):
        m[f"w_gate{l}"] = f(inputs["w_gate"][l]).reshape(NE * D, DE)
        m[f"w_up{l}"] = f(inputs["w_up"][l]).reshape(NE * D, DE)
        m[f"w_down{l}"] = f(inputs["w_down"][l]).reshape(NE * DE, D)
    return m


def kernel(**inputs):
    prog = _get_prog()
    shared = make_in_map(inputs, 0)
    in_maps = []
    for b in range(8):
        m = dict(shared)
        m["x"] = np.ascontiguousarray(inputs["x"][b], dtype=np.float32)
        m["c"] = np.ascontiguousarray(np.asarray(inputs["c"][b], dtype=np.float32).reshape(8, 128).T)
        m["pos"] = np.ascontiguousarray(np.asarray(inputs["positions"][b]).reshape(NT, 128).T.astype(np.int32))
        in_maps.append(m)
    res = run_bass_kernel_spmd(prog.nc, in_maps, core_ids=list(range(8)))
    return np.stack([np.asarray(r["out"], dtype=np.float32) for r in res.results], axis=0)
```

```python
import math
import contextlib
import numpy as np
import concourse.bass as bass
import concourse.mybir as mybir
from concourse.bass_utils import run_bass_kernel_spmd

F32 = mybir.dt.float32
BF16 = mybir.dt.bfloat16
I32 = mybir.dt.int32
ALU = mybir.AluOpType
AF = mybir.ActivationFunctionType
AX = mybir.AxisListType

S = 8192
D = 1024
NT = S // 128
DEPTH = 2
EPS = 1e-6
NE = 32
DE = 512
BLK = 384
NSUB = BLK // 128
NB = -(-(2 * S) // BLK) + NE
NPAD = NB * BLK
SEM_LIMIT = 30000
WINC = 3456


class Buf:
    __slots__ = ("name", "w", "r", "excl", "nowaw")

    def __init__(self, name="", excl=False):
        self.name = name
        self.w = {}
        self.r = {}
        self.excl = excl
        self.nowaw = False


class V:
    __slots__ = ("ap", "b")

    def __init__(self, ap, b):
        self.ap = ap
        self.b = b

    def __getitem__(self, k):
        return V(self.ap[k], self.b)

    def rearrange(self, s, **kw):
        return V(self.ap.rearrange(s, **kw), self.b)

    def unsqueeze(self, a):
        return V(self.ap.unsqueeze(a), self.b)

    def bc(self, shape):
        return V(self.ap.broadcast_to(list(shape)), self.b)

    def pbc(self, n):
        return V(self.ap.partition_broadcast(n), self.b)

    def bitcast(self, dt):
        return V(self.ap.bitcast(dt), self.b)


class Tile:
    def __init__(self, t, name):
        self.t = t
        self.b = Buf(name)

    def __getitem__(self, k):
        return V(self.t[k], self.b)

    @property
    def v(self):
        return self[:]


class EngS:
    def __init__(self, name, handle):
        self.name = name
        self.h = handle
        self.sem = None
        self.n = 0
        self.seen = {}
        self.dma_sems = []
        self.dma_cnt = []
        self.dma_i = 0


class FW:
    def __init__(self, nc, ndma=10, same=True):
        self.nc = nc
        self.nsem = 0
        self.same = same
        self.E = {"pe": EngS("pe", nc.tensor), "dve": EngS("dve", nc.vector),
                  "act": EngS("act", nc.scalar), "pool": EngS("pool", nc.gpsimd),
                  "sp": EngS("sp", nc.sync)}
        self.ndma = ndma
        self.owner = {}
        self.nwaits = 0
        self.nops = 0

    def new_sem(self):
        self.nsem += 1
        return self.nc.alloc_semaphore(f"fs{self.nsem}")

    def _tok(self, E):
        if E.sem is None or E.n >= SEM_LIMIT:
            E.sem = self.new_sem()
            E.n = 0
            self.owner[E.sem] = E.name
        E.n += 1
        return (E.sem, E.n)

    def _wait(self, E, deps):
        for sem, val in deps.items():
            if E.seen.get(sem, 0) >= val:
                continue
            E.h.wait_ge(sem, val)
            E.seen[sem] = val
            self.nwaits += 1

    def _deps(self, E, reads, writes, skip_own):
        deps = {}
        for b in reads:
            for s, v in b.w.items():
                if deps.get(s, 0) < v:
                    deps[s] = v
        for b in writes:
            for d in ((b.r,) if b.nowaw else (b.w, b.r)):
                for s, v in d.items():
                    if deps.get(s, 0) < v:
                        deps[s] = v
        if skip_own:
            for s in list(deps):
                if self.owner.get(s) == E.name:
                    del deps[s]
        self._wait(E, deps)

    def _commit(self, tok, reads, writes):
        s, v = tok
        for b in reads:
            if b.r.get(s, 0) < v:
                b.r[s] = v
        for b in writes:
            if b.w.get(s, 0) < v:
                b.w[s] = v
            b.r = {}

    def op(self, eng, fn, reads=(), writes=()):
        E = self.E[eng]
        if any(b.excl for b in reads):
            writes = list(writes) + [b for b in reads if b.excl]
            reads = [b for b in reads if not b.excl]
        self._deps(E, reads, writes, (eng == "pe") or (not self.same))
        ins = fn()
        tok = self._tok(E)
        ins.then_inc(tok[0], 1)
        self._commit(tok, reads, writes)
        self.nops += 1
        return ins

    def dma(self, q, fn, reads=(), writes=()):
        E = self.E[q]
        if not E.dma_sems:
            E.dma_sems = [self.new_sem() for _ in range(self.ndma)]
            E.dma_cnt = [0] * self.ndma
        k = E.dma_i % self.ndma
        E.dma_i += 1
        if E.dma_cnt[k] * 16 >= SEM_LIMIT:
            self._wait(E, {E.dma_sems[k]: E.dma_cnt[k] * 16})
            E.dma_sems[k] = self.new_sem()
            E.dma_cnt[k] = 0
        sem = E.dma_sems[k]
        if E.dma_cnt[k] > 0:
            self._wait(E, {sem: E.dma_cnt[k] * 16})
        self._deps(E, reads, writes, False)
        ins = fn()
        E.dma_cnt[k] += 1
        ins.then_inc(sem, 16)
        self._commit((sem, E.dma_cnt[k] * 16), reads, writes)
        self.nops += 1
        return ins

    def barrier(self):
        deps = {}
        for E in self.E.values():
            if E.sem is not None and E.n > 0:
                deps[E.sem] = E.n
            for s, c in zip(E.dma_sems, E.dma_cnt):
                if c > 0:
                    deps[s] = c * 16
        for E in self.E.values():
            self._wait(E, dict(deps))


def _const_tables():
    c = {}
    p = np.arange(128)
    c["ident"] = np.eye(128, dtype=np.float32)
    gam = 1.0 - 2.0 ** (-5.0 - np.arange(4))
    same = (p[:, None] // 64) == (p[None, :] // 64)
    dr = np.zeros((128, 4, 128), np.float32)
    for h in range(4):
        dr[:, h, :] = np.where(same, gam[h] ** np.abs(p[:, None] - p[None, :]), 0.0)
    c["dr"] = dr.reshape(128, 512)
    j = p % 64
    qd = np.stack([gam[h] ** (j + 1.0) for h in range(4)], 1)
    kd = np.stack([gam[h] ** (63.0 - j) for h in range(4)], 1) / 8.0
    c["qd"] = np.repeat(qd, 64, axis=1)
    c["kd0"] = np.repeat(kd * (p[:, None] < 64), 64, axis=1)
    c["kd1"] = np.repeat(kd * (p[:, None] >= 64), 64, axis=1)
    c["cd"] = np.broadcast_to(np.repeat(gam ** 64.0, 64)[None, :], (128, 256)).copy()
    ml = (same & (p[:, None] <= p[None, :])).astype(np.float32)
    mu = (same & (p[:, None] > p[None, :])).astype(np.float32)
    c["ml4"] = np.tile(ml, (1, 4))
    c["mu4"] = np.tile(mu, (1, 4))
    c["tri"] = ml
    c["chk"] = np.stack([(p < 64), (p >= 64)], 1).astype(np.float32)
    c["m0"] = (p < 64).astype(np.float32)[:, None]
    c["m1"] = (p >= 64).astype(np.float32)[:, None]
    c["ifr"] = np.broadcast_to((1.0 / (10000.0 ** (np.arange(32, dtype=np.float32) / 32)))[None, :], (128, 32)).copy()
    c["ifd"] = np.broadcast_to((1.0 / (500000.0 ** (np.arange(8, dtype=np.float32) / 8)))[None, :], (128, 8)).copy()
    c["lts"] = (p[:, None] < p[None, :]).astype(np.float32)
    c["ones"] = np.ones((128, 128), np.float32)
    c["jb"] = np.broadcast_to((np.arange(NB, dtype=np.float32) * BLK)[None, :], (128, NB)).copy()
    coff = np.zeros((128, 12), np.float32)
    for cc in range(12):
        coff[:, cc] = (cc if cc < 8 else cc - 8) * 128 + p
    c["coff"] = coff
    c["gid"] = np.broadcast_to((np.arange(32) // 4).astype(np.float32)[None, :], (128, 32)).copy()
    off = {}
    cur = 0
    arrs = []
    for k, v in c.items():
        v = np.ascontiguousarray(v, dtype=np.float32).reshape(128, -1)
        off[k] = (cur, v.shape[1])
        cur += v.shape[1]
        arrs.append(v)
    return np.concatenate(arrs, axis=1), off


CONST_NP, COFF = _const_tables()
NCONST = CONST_NP.shape[1]


class Prog:
    def __init__(self, stop_after=None, debug=False):
        self.nc = nc = bass.Bass("TRN2", target_bir_lowering=False)
        import os
        self.fw = FW(nc, same=(os.environ.get("SAMEENG", "1") == "1"))
        self.stop_after = stop_after
        self.debug = debug
        self.dram = {}

    def din(self, name, shape, dt=F32):
        t = self.nc.dram_tensor(name, list(shape), dt, kind="ExternalInput")
        T = Tile(t.ap(), name)
        self.dram[name] = T
        return T

    def dout(self, name, shape, dt=F32):
        t = self.nc.dram_tensor(name, list(shape), dt, kind="ExternalOutput")
        T = Tile(t.ap(), name)
        self.dram[name] = T
        return T

    def dint(self, name, shape, dt, dbg=False):
        kind = "ExternalOutput" if (dbg and self.debug) else "Internal"
        t = self.nc.dram_tensor(name, list(shape), dt, kind=kind)
        T = Tile(t.ap(), name)
        T.b.nowaw = True
        self.dram[name] = T
        return T

    def dump(self, name, v, shape, dt=F32):
        if not self.debug:
            return
        T = self.dout(self._uname("dbg_" + name), shape, dt)
        self.dma("sp", T.v, v)

    def _uname(self, name):
        self._uid = getattr(self, "_uid", 0) + 1
        return f"{name}_u{self._uid}"

    def sb(self, st, name, shape, dt):
        name = self._uname(name)
        return Tile(st.enter_context(self.nc.sbuf_tensor(name, list(shape), dt)), name)

    def ps(self, st, name, shape, dt):
        name = self._uname(name)
        T = Tile(st.enter_context(self.nc.psum_tensor(name, list(shape), dt)), name)
        T.b.excl = True
        return T

    def _eng(self, e):
        return {"dve": self.nc.vector, "pool": self.nc.gpsimd, "act": self.nc.scalar}[e]

    def _pe(self, e):
        import os
        if e == "pool" and os.environ.get("NOPOOL", "0") == "1":
            return "dve"
        return e

    def dma(self, q, out, in_, **kw):
        h = {"sp": self.nc.sync, "act": self.nc.scalar, "pool": self.nc.gpsimd}[q]
        return self.fw.dma(q, lambda: h.dma_start(out=out.ap, in_=in_.ap, **kw), reads=[in_.b], writes=[out.b])

    def gather(self, out, table, idx):
        return self.fw.dma("pool", lambda: self.nc.gpsimd.indirect_dma_start(
            out=out.ap, out_offset=None, in_=table.ap,
            in_offset=bass.IndirectOffsetOnAxis(ap=idx.ap, axis=0)), reads=[table.b, idx.b], writes=[out.b])

    def scatter(self, table, in_, idx):
        return self.fw.dma("pool", lambda: self.nc.gpsimd.indirect_dma_start(
            out=table.ap, out_offset=bass.IndirectOffsetOnAxis(ap=idx.ap, axis=0),
            in_=in_.ap, in_offset=None), reads=[in_.b, idx.b], writes=[table.b])

    def mm(self, out, lhsT, rhs, start=True, stop=True):
        return self.fw.op("pe", lambda: self.nc.tensor.matmul(out=out.ap, lhsT=lhsT.ap, rhs=rhs.ap, start=start, stop=stop),
                          reads=[lhsT.b, rhs.b], writes=[out.b])

    def tr(self, out, in_, ident):
        return self.fw.op("pe", lambda: self.nc.tensor.transpose(out=out.ap, in_=in_.ap, identity=ident.ap),
                          reads=[in_.b, ident.b], writes=[out.b])

    def act(self, out, in_, func, scale=1.0, bias=0.0, accum=None):
        rd = [in_.b]
        kw = {}
        if isinstance(scale, V):
            rd.append(scale.b)
            kw["scale"] = scale.ap
        else:
            kw["scale"] = float(scale)
        if isinstance(bias, V):
            rd.append(bias.b)
            kw["bias"] = bias.ap
        else:
            kw["bias"] = float(bias)
        wr = [out.b]
        if accum is not None:
            kw["accum_out"] = accum.ap
            wr.append(accum.b)
        return self.fw.op("act", lambda: self.nc.scalar.activation(out=out.ap, in_=in_.ap, func=func, **kw), reads=rd, writes=wr)

    def tt(self, e, out, a, b, op):
        e = self._pe(e)
        return self.fw.op(e, lambda: self._eng(e).tensor_tensor(out=out.ap, in0=a.ap, in1=b.ap, op=op), reads=[a.b, b.b], writes=[out.b])

    def ts(self, e, out, a, s1, s2=None, op0=ALU.mult, op1=None):
        e = self._pe(e)
        rd = [a.b]
        s1a = s1
        s2a = s2
        if isinstance(s1, V):
            rd.append(s1.b)
            s1a = s1.ap
        if isinstance(s2, V):
            rd.append(s2.b)
            s2a = s2.ap
        kw = {}
        if op1 is not None:
            kw["op1"] = op1
        return self.fw.op(e, lambda: self._eng(e).tensor_scalar(out=out.ap, in0=a.ap, scalar1=s1a, scalar2=s2a, op0=op0, **kw), reads=rd, writes=[out.b])

    def stt(self, e, out, a, s, b, op0, op1):
        e = self._pe(e)
        rd = [a.b, b.b]
        sa = s
        if isinstance(s, V):
            rd.append(s.b)
            sa = s.ap
        return self.fw.op(e, lambda: self._eng(e).scalar_tensor_tensor(out=out.ap, in0=a.ap, scalar=sa, in1=b.ap, op0=op0, op1=op1), reads=rd, writes=[out.b])

    def cp(self, e, out, in_):
        e = self._pe(e)
        if e == "act":
            return self.act(out, in_, AF.Copy)
        return self.fw.op(e, lambda: self._eng(e).tensor_copy(out=out.ap, in_=in_.ap), reads=[in_.b], writes=[out.b])

    def red(self, e, out, in_, op=ALU.add, axis=AX.X):
        return self.fw.op(e, lambda: self._eng(e).tensor_reduce(out=out.ap, in_=in_.ap, axis=axis, op=op), reads=[in_.b], writes=[out.b])

    def recip(self, out, in_):
        return self.fw.op("dve", lambda: self.nc.vector.reciprocal(out=out.ap, in_=in_.ap), reads=[in_.b], writes=[out.b])

    def memset(self, e, out, val):
        e = self._pe(e)
        return self.fw.op(e, lambda: self._eng(e).memset(out.ap, val), writes=[out.b])

    def rsqrt(self, out, in_, scale, eps):
        self.act(out, in_, AF.Sqrt, scale=scale, bias=self.epsv(eps, out))
        self.recip(out, out)

    def epsv(self, eps, like):
        n = like.ap.shape[0]
        return self.C_eps[0:n, 0:1] if eps == EPS else 0.0

    def build(self):
        nc = self.nc
        with contextlib.ExitStack() as gst:
            self.gst = gst
            self.declare_io()
            self.load_consts(gst)
            for l in range(DEPTH):
                self.layer(l)
                if self.stop_after is not None and self.stop_after[0] == l and self.stop_after[1] != "all":
                    break
            self.finish()
        return nc

    def declare_io(self):
        d = self.din
        self.x_in = d("x", [S, D])
        self.c_in = d("c", [128, 8])
        self.pos_in = d("pos", [128, NT], I32)
        self.consts_in = d("consts", [128, NCONST])
        self.w_mod = d("w_mod", [DEPTH, D, 6 * D])
        self.b_mod = d("b_mod", [DEPTH, 6 * D])
        self.norm1_g = d("norm1_g", [DEPTH, D])
        self.norm2_g = d("norm2_g", [DEPTH, D])
        self.w_in = d("w_in", [DEPTH, D, 3344])
        self.ret_norm_g = d("ret_norm_g", [DEPTH, 64])
        self.diff_q_g = d("diff_q_g", [DEPTH, 64])
        self.diff_k_g = d("diff_k_g", [DEPTH, 64])
        self.lam_q1 = d("lam_q1", [DEPTH, 64])
        self.lam_k1 = d("lam_k1", [DEPTH, 64])
        self.lam_q2 = d("lam_q2", [DEPTH, 64])
        self.lam_k2 = d("lam_k2", [DEPTH, 64])
        self.diff_sub_g = d("diff_sub_g", [DEPTH, 128])
        self.gla_w_a2 = d("gla_w_a2", [DEPTH, 16, 128])
        self.gla_b_a = d("gla_b_a", [DEPTH, 128])
        self.gla_norm_g = d("gla_norm_g", [DEPTH, 64])
        self.w_out = d("w_out", [DEPTH, D, D])
        self.router_w = d("router_w", [D, NE])
        self.router_b = d("router_b", [NE])
        self.w_gate = [d(f"w_gate{l}", [NE * D, DE]) for l in range(DEPTH)]
        self.w_up = [d(f"w_up{l}", [NE * D, DE]) for l in range(DEPTH)]
        self.w_down = [d(f"w_down{l}", [NE * DE, D]) for l in range(DEPTH)]
        self.wgub = [self.dint(f"wgub{l}", [NE * D, 2 * DE], BF16) for l in range(DEPTH)]
        self.wdb = [self.dint(f"wdb{l}", [NE * DE, D], BF16) for l in range(DEPTH)]
        self.bg = []
        self.out = self.dout("out", [S, D])
        self.out_b = [Buf(f"out{i}") for i in range(NT)]
        if self.debug:
            self.ydbg = self.dout("ydbg", [S, D])
        self.qT_s = self.dint("qT_s", [4, 128, S], BF16, dbg=True)
        self.kT_s = self.dint("kT_s", [4, 128, S], BF16, dbg=True)
        self.v_s = self.dint("v_s", [S, 512], BF16, dbg=True)
        self.mixT_s = self.dint("mixT_s", [D, S], BF16, dbg=True)
        self.h2_s = self.dint("h2_s", [S, D], BF16, dbg=True)
        self.xs_s = self.dint("xs_s", [NPAD, D], BF16)
        self.ys_s = self.dint("ys_s", [NPAD, D], F32)

    def load_consts(self, st):
        self.CT = self.sb(st, "consts", [128, NCONST], F32)
        self.dma("sp", self.CT.v, self.consts_in.v)
        self.C_eps = self.sb(st, "c_eps", [128, 1], F32)
        self.memset("dve", self.C_eps.v, EPS)
        self.C_npi = self.sb(st, "c_npi", [128, 1], F32)
        self.memset("dve", self.C_npi.v, -math.pi)
        self.C_one = self.sb(st, "c_one", [128, 1], F32)
        self.memset("dve", self.C_one.v, 1.0)
        posi = self.sb(st, "posi", [128, NT], I32)
        self.dma("sp", posi.v, self.pos_in.v)
        self.POSF = self.sb(st, "posf", [128, NT], F32)
        self.cp("dve", self.POSF.v, posi.v)
        self.identb = self.sb(st, "identb", [128, 128], BF16)
        self.cp("dve", self.identb.v, self.cst("ident"))
        self.rope_tables(st)
        self.onesb = self.sb(st, "onesb", [128, 128], BF16)
        self.cp("dve", self.onesb.v, self.cst("ones"))

    def rope_tables(self, gst):
        self.COS = self.sb(gst, "COS", [128, NT, 40], F32)
        self.SIN = self.sb(gst, "SIN", [128, NT, 40], F32)
        o, _ = COFF["ifr"]
        if40 = self.CT[:, o:o + 40]
        C1 = 6.28125
        C2 = 2.0 * math.pi - C1
        with contextlib.ExitStack() as st:
            ANG = self.sb(st, "ANG", [128, NT, 40], F32)
            KI = self.sb(st, "KI", [128, NT, 40], I32)
            KF = self.sb(st, "KF", [128, NT, 40], F32)
            MK = self.sb(st, "MK", [128, NT, 40], F32)
            self.tt("dve", ANG.v, self.POSF.v.unsqueeze(2).bc([128, NT, 40]), if40.unsqueeze(1).bc([128, NT, 40]), ALU.mult)
            self.ts("dve", KF.v, ANG.v, 1.0 / (2.0 * math.pi))
            self.cp("dve", KI.v, KF.v)
            self.cp("dve", KF.v, KI.v)
            self.stt("dve", ANG.v, KF.v, -C1, ANG.v, ALU.mult, ALU.add)
            self.stt("dve", ANG.v, KF.v, -C2, ANG.v, ALU.mult, ALU.add)
            self.ts("dve", MK.v, ANG.v, math.pi, None, op0=ALU.is_gt)
            self.stt("dve", self.SIN.v, MK.v, -2.0 * math.pi, ANG.v, ALU.mult, ALU.add)
            self.ts("dve", MK.v, self.SIN.v, -math.pi, None, op0=ALU.is_lt)
            self.stt("dve", self.SIN.v, MK.v, 2.0 * math.pi, self.SIN.v, ALU.mult, ALU.add)
            self.ts("dve", self.COS.v, self.SIN.v, 0.5 * math.pi, None, op0=ALU.add)
            self.ts("dve", MK.v, self.COS.v, math.pi, None, op0=ALU.is_gt)
            self.stt("dve", self.COS.v, MK.v, -2.0 * math.pi, self.COS.v, ALU.mult, ALU.add)
            self.act(self.SIN.v, self.SIN.v, AF.Sin)
            self.act(self.COS.v, self.COS.v, AF.Sin)
        self.fw.barrier()

    def out_tile(self, i):
        return V(self.out.t[i * 128:(i + 1) * 128, :], self.out_b[i])

    def cst(self, name):
        o, n = COFF[name]
        return self.CT[:, o:o + n]

    def finish(self):
        fw = self.fw
        fw.barrier()

    def layer(self, l):
        x_src = self.x_in if l == 0 else self.out
        sa = self.stop_after
        with contextlib.ExitStack() as lst:
            self.phase_mod(l, lst)
            if sa == (l, "mod"):
                return
            self.queue_weight_casts(l)
            self.phase1(l, x_src)
            self.run_bg(10 ** 9)
            self.fw.barrier()
            if sa == (l, "p1"):
                return
            self.phase2(l)
            self.fw.barrier()
            if sa == (l, "p2"):
                return
            with contextlib.ExitStack() as st3:
                self.LOG = self.sb(st3, f"log{l}", [128, NT, NE], F32)
                self.phase3(l, x_src)
                self.fw.barrier()
                if sa == (l, "p3"):
                    return
                self.phase_moe(l)
                self.fw.barrier()
                if sa == (l, "moe"):
                    return

    def queue_weight_casts(self, l):
        for e in range(NE):
            for src, dst, rows, c0, c1 in ((self.w_gate[l], self.wgub[l], D, 0, DE), (self.w_up[l], self.wgub[l], D, DE, 2 * DE),
                                           (self.w_down[l], self.wdb[l], DE, 0, D)):
                def task(src=src, dst=dst, rows=rows, e=e, c0=c0, c1=c1):
                    self.dma("pool", dst[e * rows:(e + 1) * rows, c0:c1], src[e * rows:(e + 1) * rows, :])
                self.bg.append(task)

    def run_bg(self, n):
        while n > 0 and self.bg:
            self.bg.pop(0)()
            n -= 1

    def phase_mod(self, l, lst):
        MODB = self.sb(lst, f"modb{l}", [128, 6 * D], F32)
        self.MODB = MODB
        with contextlib.ExitStack() as st:
            cT = self.sb(st, "cT", [128, 8], F32)
            self.dma("sp", cT.v, self.c_in.v)
            ca = self.sb(st, "ca", [128, 8], F32)
            self.act(ca.v, cT.v, AF.Silu)
            cb = self.sb(st, "cb", [128, 8, 128], F32)
            for k in range(8):
                self.cp("dve", cb[:, k, :], ca[:, k:k + 1].bc([128, 128]))
            bm = self.sb(st, "bm", [1, 6 * D], F32)
            self.dma("sp", bm.v, self.b_mod[l:l + 1, :])
            ones_f = self.cst("ones")
            wm = [self.sb(st, f"wm{i}", [128, 8, 512], F32) for i in range(2)]
            pm = [self.ps(st, f"pm{i}", [128, 512], F32) for i in range(2)]
            for n in range(12):
                w = wm[n % 2]
                p = pm[n % 2]
                self.dma("sp" if n % 2 == 0 else "act", w.v,
                         self.w_mod[l, :, n * 512:(n + 1) * 512].rearrange("(k p) n -> p k n", p=128))
                for k in range(8):
                    self.mm(p.v, cb[:, k, :], w[:, k, :], start=(k == 0), stop=False)
                self.mm(p.v, ones_f[0:1, :], bm[0:1, n * 512:(n + 1) * 512], start=False, stop=True)
                self.cp("dve" if n % 2 == 0 else "act", MODB[:, n * 512:(n + 1) * 512], p.v)
        self.dump(f"mod{l}", MODB[0:2, :], [2, 6 * D])
        self.fw.barrier()

    def mod(self, j):
        return self.MODB[:, j * D:(j + 1) * D]

    def phase1(self, l, x_src):
        nc = self.nc
        with contextlib.ExitStack() as st:
            sb = lambda n, s, d: self.sb(st, n, s, d)
            ps = lambda n, s, d: self.ps(st, n, s, d)
            WIN = sb("WIN", [128, 8, WINC], BF16)
            WIN.b.nowaw = True
            for k in range(8):
                rows = self.w_in[l, k * 128:(k + 1) * 128, :]
                self.dma("pool", WIN[:, k, 0:1536], rows[:, 0:1536])
                self.dma("pool", WIN[:, k, 1536:3072], rows[:, 1536:3072])
                self.dma("pool", WIN[:, k, 3200:3456], rows[:, 3088:3344])
            import os
            setup = int(os.environ.get("P1SETUP", "9"))
            if setup < 1:
                return
            wga = sb("wga", [128, 8, 16], F32)
            self.dma("sp", wga.v, self.w_in[l, :, 3072:3088].rearrange("(k p) r -> p k r", p=128))
            wa2 = sb("wa2", [16, 128], F32)
            self.dma("sp", wa2.v, self.gla_w_a2[l])
            identf = self.cst("ident")
            PJ = [ps(f"PJ{i}", [128, 512], F32) for i in range(2)]
            TP = [ps(f"TP{i}", [128, 1024], BF16) for i in range(2)]
            SC = ps("SC", [128, 512], F32)
            SC2 = ps("SC2", [128, 512], F32)
            OT = ps("OT", [64, 512], F32)
            MS = ps("MS", [128, 512], F32)
            gaT = sb("gaT", [16, 128], F32)
            for k in range(8):
                self.tr(SC[0:16, 0:128], wga[:, k, :], identf)
                self.cp("dve", gaT.v, SC[0:16, 0:128])
                self.mm(SC2[:, 0:128], gaT.v, wa2.v)
                self.cp("dve", WIN[:, k, 3072:3200], SC2[:, 0:128])
            if setup < 2:
                return
            G1 = sb("G1", [128, D], F32)
            self.dma("sp", G1.v, self.norm1_g[l].pbc(128))
            self.stt("dve", G1.v, self.mod(1), 1.0, G1.v, ALU.add, ALU.mult)
            SH1 = self.mod(0)
            GQ = sb("GQ", [128, 64], F32)
            GK = sb("GK", [128, 64], F32)
            self.dma("sp", GQ.v, self.diff_q_g[l].pbc(128))
            self.dma("sp", GK.v, self.diff_k_g[l].pbc(128))
            self.ts("dve", GQ.v, GQ.v, 0.125)
            BA = sb("BA", [128, 128], F32)
            self.dma("sp", BA.v, self.gla_b_a[l].pbc(128))
            GRN = sb("GRN", [64, 1], F32)
            GGN = sb("GGN", [64, 1], F32)
            self.dma("sp", GRN.v, self.ret_norm_g[l].unsqueeze(1))
            self.dma("sp", GGN.v, self.gla_norm_g[l].unsqueeze(1))
            onesf = self.cst("ones")
            if setup < 3:
                return
            SR = sb("SR", [64, 256], F32)
            SRb = sb("SRb", [64, 256], BF16)
            SG = sb("SG", [32, 256], F32)
            SGb = sb("SGb", [32, 256], BF16)
            for t_ in (SR, SRb, SG, SGb):
                self.memset("dve", t_.v, 0.0)
            XT = [sb(f"XT{i}", [128, D], F32) for i in range(2)]
            ssq = sb("ssq", [128, 1], F32)
            HF = sb("HF", [128, D], F32)
            HB = sb("HB", [128, D], BF16)
            junk = HB
            HT = sb("HT", [128, 8, 128], BF16)
            R1 = sb("R1", [128, 8, 32], F32)
            R2 = sb("R2", [128, 8, 32], F32)
            ROT = sb("ROT", [128, 8, 64], F32)
            QB = sb("QB", [128, 256], BF16)
            QDB = sb("QDB", [128, 256], BF16)
            KB = sb("KB", [128, 256], BF16)
            KD0 = sb("KD0", [128, 256], BF16)
            KD1 = sb("KD1", [128, 256], BF16)
            VR = sb("VR", [128, 256], BF16)
            SGR = sb("SGR", [128, 256], BF16)
            RT = sb("RT", [64, 12, 128], BF16)
            SGT = sb("SGT", [64, 8, 128], BF16)
            DSQ = sb("DSQ", [128, 512], F32)
            DSS = sb("DSS", [128, 8], F32)
            DXN = sb("DXN", [128, 8, 64], F32)
            DQB = sb("DQB", [128, 512], BF16)
            DKB = sb("DKB", [128, 512], BF16)
            DT = sb("DT", [128, 8, 128], BF16)
            DVB = sb("DVB", [128, 512], BF16)
            D1 = sb("D1", [128, 8, 8], F32)
            D2 = sb("D2", [128, 8, 8], F32)
            AT = sb("AT", [128, 512], BF16)
            ZT = sb("ZT", [128, 128], F32)
            SP = sb("SP", [128, 128], F32)
            EPt = sb("EPt", [128, 128], F32)
            EMt = sb("EMt", [128, 128], F32)
            GQP = sb("GQP", [128, 128], BF16)
            GQM = sb("GQM", [128, 128], BF16)
            GKP = sb("GKP", [128, 128], BF16)
            GKM = sb("GKM", [128, 128], BF16)
            GKM0 = sb("GKM0", [128, 128], BF16)
            GKM1 = sb("GKM1", [128, 128], BF16)
            GVB = sb("GVB", [128, 256], BF16)
            SGG = sb("SGG", [128, 256], BF16)
            GT = sb("GT", [32, 16, 128], BF16)
            EGL = sb("EGL", [32, 8], F32)
            TMP1 = sb("TMP1", [128, 512], F32)
            OY = sb("OY", [64, 512], F32)
            MXR = sb("MXR", [64, 512], BF16)
            MXG = sb("MXG", [64, 512], BF16)
            KVT = sb("KVT", [64, 256], F32)

            ifr = self.cst("ifr")
            ifd = self.cst("ifd")
            TWO_PI = 2.0 * math.pi

            def post_norm(OTv, G, SGTv, MX):
                OSQ_ = TMP1[0:64, :]
                ORS_ = DSQ[0:64, :]
                self.act(OSQ_, OTv, AF.Square)
                self.mm(MS[0:64, :], onesf[0:64, 0:64], OSQ_)
                self.rsqrt(ORS_, MS[0:64, :], 1.0 / 64, EPS)
                self.tt("dve", OY.v, OTv, ORS_, ALU.mult)
                self.stt("dve", MX.v, OY.v, G[:, 0:1], SGTv, ALU.mult, ALU.mult)

            import os
            cut = int(os.environ.get("P1CUT", "9"))
            ntl = int(os.environ.get("P1TILES", str(NT)))
            TPA, TPB = TP[0], TP[1]
            PROJ = [sb(f"PROJ{i}", [128, WINC], F32) for i in range(2)]

            class _PV:
                def __init__(self, tile, off, w):
                    self.tile, self.off, self.w = tile, off, w

                def __getitem__(self, k):
                    rows, cols = k
                    return self.tile[rows, self.off + cols.start:self.off + cols.stop]

                @property
                def v(self):
                    return self.tile[:, self.off:self.off + self.w]

            def stageA(i):
                xt = XT[i % 2]
                self.dma("sp", xt.v, x_src[i * 128:(i + 1) * 128, :] if l == 0 else self.out_tile(i))
                self.act(junk.v, xt.v, AF.Square, accum=ssq.v)
                self.rsqrt(ssq.v, ssq.v, 1.0 / D, EPS)
                self.stt("dve", HF.v, xt.v, ssq[:, 0:1], G1.v, ALU.mult, ALU.mult)
                self.tt("pool", HB.v, HF.v, SH1, ALU.add)
                tp = TPA
                for k in range(8):
                    self.tr(tp[:, k * 128:(k + 1) * 128], HB[:, k * 128:(k + 1) * 128], self.identb.v)
                self.cp("act", HT[:, 0:4, :], tp[:, 0:512].rearrange("p (k t) -> p k t", k=4))
                self.cp("dve", HT[:, 4:8, :], tp[:, 512:1024].rearrange("p (k t) -> p k t", k=4))
                yield
                P_ = PROJ[i % 2]
                for n, width in ((0, 512), (1, 512), (2, 512), (3, 512), (4, 512), (6, 384), (5, 512)):
                    p = PJ[n % 2]
                    for k in range(8):
                        self.mm(p[:, 0:width], HT[:, k, :], WIN[:, k, n * 512:n * 512 + width], start=(k == 0), stop=(k == 7))
                    self.cp("act" if n % 2 == 0 else "dve", P_[:, n * 512:n * 512 + width], p[:, 0:width])
                    yield

            def stageB(i):
                if cut < 1:
                    return

                def proj(n, width):
                    return _PV(PROJ[i % 2], n * 512, width)

                cosr = self.COS[:, i, 0:32].unsqueeze(1).bc([128, 8, 32])
                sinr = self.SIN[:, i, 0:32].unsqueeze(1).bc([128, 8, 32])
                cosd = self.COS[:, i, 32:40].unsqueeze(1).bc([128, 8, 8])
                sind = self.SIN[:, i, 32:40].unsqueeze(1).bc([128, 8, 8])


                yield
                p0 = proj(0, 512)
                pv = p0.v.rearrange("p (g h w) -> p g h w", g=8, h=2)
                X1 = pv[:, :, 0, :]
                X2 = pv[:, :, 1, :]
                rv_ = ROT.v.rearrange("p g (h w) -> p g h w", h=2)
                self.tt("dve", R1.v, X1, cosr, ALU.mult)
                self.tt("dve", R2.v, X2, sinr, ALU.mult)
                self.tt("pool", rv_[:, :, 0, :], R1.v, R2.v, ALU.subtract)
                self.tt("dve", R1.v, X1, sinr, ALU.mult)
                self.tt("dve", R2.v, X2, cosr, ALU.mult)
                self.tt("pool", rv_[:, :, 1, :], R1.v, R2.v, ALU.add)
                rq = ROT[:, 0:4, :].rearrange("p g w -> p (g w)")
                rk = ROT[:, 4:8, :].rearrange("p g w -> p (g w)")
                self.cp("act", QB.v, rq)
                self.tt("pool", QDB.v, rq, self.cst("qd"), ALU.mult)
                self.act(KB.v, rk, AF.Copy, scale=0.125)
                self.tt("pool", KD0.v, rk, self.cst("kd0"), ALU.mult)
                self.tt("pool", KD1.v, rk, self.cst("kd1"), ALU.mult)
                if cut < 2:
                    return
                yield
                p1 = proj(1, 512)
                self.cp("act", VR.v, p1[:, 0:256])
                self.act(SGR.v, p1[:, 256:512], AF.Silu)
                if cut == 2 and os.environ.get("SUB", "9") == "0":
                    return
                tp = TPB
                for h in range(4):
                    self.tr(tp[0:64, h * 128:(h + 1) * 128], QB[:, h * 64:(h + 1) * 64], self.identb.v)
                    self.tr(tp[0:64, (4 + h) * 128:(5 + h) * 128], QDB[:, h * 64:(h + 1) * 64], self.identb.v)
                self.cp("dve", RT[:, 0:8, :], tp[0:64, :].rearrange("p (k t) -> p k t", k=8))
                if cut == 2 and os.environ.get("SUB", "9") == "1":
                    return
                tp = TPB
                for h in range(4):
                    self.tr(tp[0:64, h * 128:(h + 1) * 128], KB[:, h * 64:(h + 1) * 64], self.identb.v)
                    self.tr(tp[0:64, (4 + h) * 128:(5 + h) * 128], SGR[:, h * 64:(h + 1) * 64], self.identb.v)
                self.cp(os.environ.get("GRP2", "act"), RT[:, 8:12, :], tp[0:64, 0:512].rearrange("p (k t) -> p k t", k=4))
                self.cp("dve", SGT[:, 0:4, :], tp[0:64, 512:1024].rearrange("p (k t) -> p k t", k=4))
                if cut < 3:
                    return
                yield
                for h in range(4):
                    self.mm(SC[:, h * 128:(h + 1) * 128], RT[:, 8 + h, :], RT[:, h, :])
                self.tt("dve", AT.v, SC.v, self.cst("dr"), ALU.mult)
                yield
                for h in range(4):
                    o = OT[:, h * 128:(h + 1) * 128]
                    self.mm(o, VR[:, h * 64:(h + 1) * 64], AT[:, h * 128:(h + 1) * 128], start=(h == 0), stop=False)
                    self.mm(OT[:, h * 128:h * 128 + 64], SRb[:, h * 64:(h + 1) * 64], RT[:, 4 + h, 0:64], start=False, stop=False)
                for h in range(4):
                    self.mm(MS[0:64, h * 64:(h + 1) * 64], KD0[:, h * 64:(h + 1) * 64], VR[:, h * 64:(h + 1) * 64])
                    self.mm(MS[0:64, 256 + h * 64:256 + (h + 1) * 64], KD1[:, h * 64:(h + 1) * 64], VR[:, h * 64:(h + 1) * 64])
                self.tt("dve", KVT.v, SR.v, self.cst("cd")[0:64, :], ALU.mult)
                self.tt("dve", SR.v, KVT.v, MS[0:64, 0:256], ALU.add)
                self.cp("act", SRb.v, SR.v)
                yield
                for h in range(4):
                    self.mm(OT[:, h * 128 + 64:(h + 1) * 128], SRb[:, h * 64:(h + 1) * 64], RT[:, 4 + h, 64:128], start=False, stop=True)
                self.tt("dve", KVT.v, SR.v, self.cst("cd")[0:64, :], ALU.mult)
                self.tt("dve", SR.v, KVT.v, MS[0:64, 256:512], ALU.add)
                self.cp("act", SRb.v, SR.v)
                yield
                post_norm(OT.v, GRN, SGT[:, 0:4, :].rearrange("p k t -> p (k t)"), MXR)
                yield
                self.dma("act", self.mixT_s[0:256, i * 128:(i + 1) * 128].rearrange("(h p) t -> p h t", p=64),
                         MXR.v.rearrange("p (h t) -> p h t", h=4))

                if cut < 4:
                    return
                yield
                for (n, G, OUTB, dst) in ((2, GQ, DQB, self.qT_s), (3, GK, DKB, self.kT_s)):
                    pq = proj(n, 512)
                    self.act(DSQ.v, pq.v, AF.Square)
                    self.red("dve", DSS.v, DSQ.v.rearrange("p (g w) -> p g w", g=8))
                    self.rsqrt(DSS.v, DSS.v, 1.0 / 64, EPS)
                    self.tt("dve", DXN.v, pq.v.rearrange("p (g w) -> p g w", g=8), DSS.v.unsqueeze(2).bc([128, 8, 64]), ALU.mult)
                    self.tt("pool", DXN.v, DXN.v, G.v.unsqueeze(1).bc([128, 8, 64]), ALU.mult)
                    ob = OUTB.v.rearrange("p (g w) -> p g w", g=8)
                    self.cp("act", ob[:, :, 16:64], DXN[:, :, 16:64])
                    x1 = DXN[:, :, 0:8]
                    x2 = DXN[:, :, 8:16]
                    self.tt("dve", D1.v, x1, cosd, ALU.mult)
                    self.tt("pool", D2.v, x2, sind, ALU.mult)
                    self.tt("dve", ob[:, :, 0:8], D1.v, D2.v, ALU.subtract)
                    self.tt("dve", D1.v, x1, sind, ALU.mult)
                    self.tt("pool", D2.v, x2, cosd, ALU.mult)
                    self.tt("dve", ob[:, :, 8:16], D1.v, D2.v, ALU.add)
                    tp = TPB
                    for h in range(4):
                        self.tr(tp[:, h * 128:(h + 1) * 128], OUTB[:, h * 128:(h + 1) * 128], self.identb.v)
                    dtv = DT[:, 0:4, :] if n == 2 else DT[:, 4:8, :]
                    self.cp("act" if n == 2 else "dve", dtv, tp[:, 0:512].rearrange("p (k t) -> p k t", k=4))
                    self.dma("sp", dst[:, :, i * 128:(i + 1) * 128].rearrange("h p t -> p h t"), dtv)
                yield
                p4 = proj(4, 512)
                self.cp("act", DVB.v, p4.v)
                self.dma("act", self.v_s[i * 128:(i + 1) * 128, :], DVB.v)

                if cut < 5:
                    return
                yield
                p6 = proj(6, 384)
                self.tt("dve", ZT.v, p6[:, 0:128], BA.v, ALU.add)
                self.act(SGG.v, p6[:, 128:384], AF.Silu)
                self.act(SP.v, ZT.v, AF.Exp, scale=-1.0)
                self.act(SP.v, SP.v, AF.Ln, bias=self.C_one[:, 0:1])
                self.mm(SC2[:, 0:128], self.cst("tri"), SP.v)
                self.act(EPt.v, SC2[:, 0:128], AF.Exp, scale=-1.0 / 16)
                self.act(EMt.v, SC2[:, 0:128], AF.Exp, scale=1.0 / 16)
                for h in range(4):
                    self.mm(MS[0:32, 2 * h:2 * h + 2], SP[:, h * 32:(h + 1) * 32], self.cst("chk"))
                self.act(EGL.v, MS[0:32, 0:8], AF.Exp, scale=-1.0 / 16)
                p5 = proj(5, 512)
                qs = 32.0 ** -0.5
                self.stt("dve", GQP.v, p5[:, 0:128], qs, EPt.v, ALU.mult, ALU.mult)
                self.stt("dve", GQM.v, p5[:, 0:128], qs, EMt.v, ALU.mult, ALU.mult)
                self.tt("dve", GKP.v, p5[:, 128:256], EPt.v, ALU.mult)
                self.tt("dve", GKM.v, p5[:, 128:256], EMt.v, ALU.mult)
                self.cp("act", GVB.v, p5[:, 256:512])
                self.ts("pool", GKM0.v, GKM.v, self.cst("m0")[:, 0:1], None, op0=ALU.mult)
                self.ts("pool", GKM1.v, GKM.v, self.cst("m1")[:, 0:1], None, op0=ALU.mult)
                tp = TPB
                for h in range(4):
                    self.tr(tp[0:32, h * 128:(h + 1) * 128], GQP[:, h * 32:(h + 1) * 32], self.identb.v)
                    self.tr(tp[0:32, (4 + h) * 128:(5 + h) * 128], GQM[:, h * 32:(h + 1) * 32], self.identb.v)
                self.cp("act", GT[:, 0:8, :], tp[0:32, :].rearrange("p (k t) -> p k t", k=8))
                tp = TPB
                for h in range(4):
                    self.tr(tp[0:32, h * 128:(h + 1) * 128], GKM[:, h * 32:(h + 1) * 32], self.identb.v)
                    self.tr(tp[0:32, (4 + h) * 128:(5 + h) * 128], GKP[:, h * 32:(h + 1) * 32], self.identb.v)
                self.cp("dve", GT[:, 8:16, :], tp[0:32, :].rearrange("p (k t) -> p k t", k=8))
                tp = TPB
                for h in range(4):
                    self.tr(tp[0:64, h * 128:(h + 1) * 128], SGG[:, h * 64:(h + 1) * 64], self.identb.v)
                self.cp("act", SGT[:, 4:8, :], tp[0:64, 0:512].rearrange("p (k t) -> p k t", k=4))
                for h in range(4):
                    self.mm(SC[:, h * 128:(h + 1) * 128], GT[:, 8 + h, :], GT[:, h, :])
                    self.mm(SC2[:, h * 128:(h + 1) * 128], GT[:, 12 + h, :], GT[:, 4 + h, :])
                self.tt("dve", TMP1.v, SC.v, self.cst("ml4"), ALU.mult)
                self.tt("dve", DSQ.v, SC2.v, self.cst("mu4"), ALU.mult)
                self.tt("pool", AT.v, TMP1.v, DSQ.v, ALU.add)
                yield
                for h in range(4):
                    self.mm(OT[:, h * 128:(h + 1) * 128], GVB[:, h * 64:(h + 1) * 64], AT[:, h * 128:(h + 1) * 128], start=(h == 0), stop=False)
                    self.mm(OT[:, h * 128:h * 128 + 64], SGb[:, h * 64:(h + 1) * 64], GT[:, h, 0:64], start=False, stop=False)
                for h in range(4):
                    self.mm(MS[0:32, h * 64:(h + 1) * 64], GKM0[:, h * 32:(h + 1) * 32], GVB[:, h * 64:(h + 1) * 64])
                    self.mm(MS[0:32, 256 + h * 64:256 + (h + 1) * 64], GKM1[:, h * 32:(h + 1) * 32], GVB[:, h * 64:(h + 1) * 64])
                eg = EGL.v.rearrange("p (h c) -> p h c", c=2)
                sgv = SG.v.rearrange("p (h w) -> p h w", h=4)
                kv3 = KVT[0:32, :].rearrange("p (h w) -> p h w", h=4)
                self.tt("dve", KVT[0:32, :], SG.v, MS[0:32, 0:256], ALU.add)
                self.tt("dve", sgv, kv3, eg[:, :, 0:1].bc([32, 4, 64]), ALU.mult)
                self.cp("act", SGb.v, SG.v)
                yield
                for h in range(4):
                    self.mm(OT[:, h * 128 + 64:(h + 1) * 128], SGb[:, h * 64:(h + 1) * 64], GT[:, h, 64:128], start=False, stop=True)
                self.tt("dve", KVT[0:32, :], SG.v, MS[0:32, 256:512], ALU.add)
                self.tt("dve", sgv, kv3, eg[:, :, 1:2].bc([32, 4, 64]), ALU.mult)
                self.cp("act", SGb.v, SG.v)
                yield
                post_norm(OT.v, GGN, SGT[:, 4:8, :].rearrange("p k t -> p (k t)"), MXG)
                yield
                self.dma("act", self.mixT_s[768:1024, i * 128:(i + 1) * 128].rearrange("(h p) t -> p h t", p=64),
                         MXG.v.rearrange("p (h t) -> p h t", h=4))


            RB = int(os.environ.get("P1RB", "3"))
            for _ in stageA(0):
                pass
            for i in range(ntl):
                self.run_bg(2)
                gb = stageB(i)
                ga = stageA(i + 1) if i + 1 < ntl else iter(())
                done_a = done_b = False
                while not (done_a and done_b):
                    for _ in range(RB):
                        if not done_b:
                            try:
                                next(gb)
                            except StopIteration:
                                done_b = True
                    if not done_a:
                        try:
                            next(ga)
                        except StopIteration:
                            done_a = True

    def phase2(self, l):
        lam_init = 0.8 - 0.6 * math.exp(-0.3 * l)
        with contextlib.ExitStack() as st:
            sb = lambda n, s, d: self.sb(st, n, s, d)
            ps = lambda n, s, d: self.ps(st, n, s, d)
            KT = sb("KT", [128, 2, S], BF16)
            KT.b.nowaw = True
            VV = sb("VV", [128, NT, 256], BF16)
            VV.b.nowaw = True
            vsrc = self.v_s.v.rearrange("(i p) c -> p i c", p=128)

            def load_group(hg):
                for hh in range(2):
                    for j in range(4):
                        self.dma("sp" if (hh + j) % 2 == 0 else "act", KT[:, hh, j * 2048:(j + 1) * 2048], self.kT_s[2 * hg + hh, :, j * 2048:(j + 1) * 2048])
                for j in range(8):
                    self.dma("sp" if j % 2 == 0 else "act", VV[:, j * 8:(j + 1) * 8, :], vsrc[:, j * 8:(j + 1) * 8, hg * 256:(hg + 1) * 256])
            lq = sb("lq", [1, 4, 64], F32)
            for j, src in enumerate((self.lam_q1, self.lam_k1, self.lam_q2, self.lam_k2)):
                self.dma("sp", lq[:, j, :], src[l:l + 1, :])
            lp = sb("lp", [1, 2, 64], F32)
            self.tt("dve", lp[:, 0, :], lq[:, 0, :], lq[:, 1, :], ALU.mult)
            self.tt("dve", lp[:, 1, :], lq[:, 2, :], lq[:, 3, :], ALU.mult)
            ls = sb("ls", [1, 2], F32)
            self.red("dve", ls.v, lp.v)
            self.act(ls.v, ls.v, AF.Exp)
            lam1 = sb("lam1", [1, 1], F32)
            self.tt("dve", lam1.v, ls[:, 0:1], ls[:, 1:2], ALU.subtract)
            self.ts("dve", lam1.v, lam1.v, lam_init, -1.0, op0=ALU.add, op1=ALU.mult)
            NSB = 4
            SPS = [ps(f"SPS{i}", [128, 512], F32) for i in range(NSB)]
            OP = [ps(f"OP{i}", [128, 512], F32) for i in range(2)]
            MSP = ps("MSP", [128, 512], F32)
            MSP2 = ps("MSP2", [128, 512], F32)
            NLAM = sb("NLAM", [128, 1], F32)
            self.mm(MSP[:, 0:1], self.cst("ones")[0:1, :], lam1.v)
            self.cp("dve", NLAM.v, MSP[:, 0:1])
            SUBG = sb("SUBG", [128, 1], F32)
            self.dma("sp", SUBG.v, self.diff_sub_g[l].unsqueeze(1))
            self.ts("dve", SUBG.v, SUBG.v, 1.0 - lam_init)
            QT = [sb(f"QT{i}", [128, 512], BF16) for i in range(2)]
            PT = [sb(f"PT{i}", [128, 512], BF16) for i in range(4)]
            ACC = [[sb(f"ACC{p_}{c}", [128, 512], F32) for c in range(2)] for p_ in range(2)]
            OS = [sb(f"OS{c}", [128, 512], F32) for c in range(2)]
            R0 = sb("R0", [128, 512], F32)
            R1 = sb("R1", [128, 512], F32)
            ACB = [sb(f"ACB{c}", [128, 512], BF16) for c in range(2)]
            T0 = sb("T0", [128, 512], F32)
            T1 = sb("T1", [128, 512], F32)
            OSQ = sb("OSQ2", [128, 512], BF16)
            ORS = sb("ORS2", [128, 512], F32)
            OB = [sb(f"OB{i}", [128, 512], BF16) for i in range(2)]
            onesf = self.cst("ones")
            import os
            nqt = int(os.environ.get("P2QT", str(S // 512)))
            groups = [(a, b_, c_) for a in range(2) for b_ in range(nqt) for c_ in range(2)]
            units = []
            for gi, (hg, qt, hh) in enumerate(groups):
                nk = 4 * qt + 4
                for kt in range(nk):
                    for c in range(2):
                        units.append((gi, c, kt, nk))
            LOOK = 2
            qloaded = set()

            def load_q(gi):
                if gi >= len(groups) or gi in qloaded:
                    return
                qloaded.add(gi)
                hg, qt, hh = groups[gi]
                if qt == 0 and hh == 0:
                    load_group(hg)
                self.dma("sp", QT[gi % 2].v, self.qT_s[2 * hg + hh, :, qt * 512:(qt + 1) * 512])

            def c0_of(gi, kt):
                qt = groups[gi][1]
                m = kt - 4 * qt
                return (128 * m if m > 0 else 0), m

            def issue_qk(u):
                gi, c, kt, nk = units[u]
                load_q(gi)
                hh = groups[gi][2]
                c0, m = c0_of(gi, kt)
                self.mm(SPS[u % NSB][:, c0:512], KT[64 * c:64 * c + 64, hh, kt * 128:(kt + 1) * 128], QT[gi % 2][64 * c:64 * c + 64, c0:512])

            def finish(gi):
                hg, qt, hh = groups[gi]
                h = 2 * hg + hh
                acc = ACC[gi % 2]
                self.cp("act", OS[0].v, OP[0].v)
                self.cp("act", OS[1].v, OP[1].v)
                self.cp("dve", ACB[0].v, acc[0].v)
                self.cp("dve", ACB[1].v, acc[1].v)
                self.mm(MSP.v, self.onesb.v, ACB[0].v)
                self.mm(MSP2.v, self.onesb.v, ACB[1].v)
                self.act(R0.v, MSP.v, AF.Ln)
                self.act(R0.v, R0.v, AF.Exp, scale=-1.0)
                self.act(R1.v, MSP2.v, AF.Ln)
                self.act(R1.v, R1.v, AF.Exp, scale=-1.0)
                self.tt("dve", T0.v, OS[0].v, R0.v, ALU.mult)
                self.tt("dve", T1.v, OS[1].v, R1.v, ALU.mult)
                self.stt("dve", T0.v, T1.v, NLAM[:, 0:1], T0.v, ALU.mult, ALU.add)
                self.act(OSQ.v, T0.v, AF.Square)
                self.mm(MSP.v, self.onesb.v, OSQ.v)
                self.act(ORS.v, MSP.v, AF.Ln, scale=1.0 / 128, bias=self.C_eps[:, 0:1])
                self.act(ORS.v, ORS.v, AF.Exp, scale=-0.5)
                ob = OB[gi % 2]
                self.stt("dve", ob.v, T0.v, SUBG[:, 0:1], ORS.v, ALU.mult, ALU.mult)
                self.dma("act", self.mixT_s[256 + 128 * h:256 + 128 * (h + 1), qt * 512:(qt + 1) * 512], ob.v)

            def hg_of(u):
                return groups[units[u][0]][0]

            for u, (gi, c, kt, nk) in enumerate(units):
                if u == 0 or hg_of(u - 1) != hg_of(u):
                    for u2 in range(u, min(u + LOOK, len(units))):
                        if hg_of(u2) == hg_of(u):
                            issue_qk(u2)
                if u % 2 == 0:
                    for u2 in (u + LOOK, u + LOOK + 1):
                        if u2 < len(units) and hg_of(u2) == hg_of(u):
                            issue_qk(u2)
                hh = groups[gi][2]
                c0, m = c0_of(gi, kt)
                pt = PT[u % 4]
                if kt == 0 and c == 0 and gi + 1 < len(groups) and groups[gi + 1][0] == groups[gi][0]:
                    load_q(gi + 1)
                self.act(pt[:, c0:512], SPS[u % NSB][:, c0:512], AF.Exp)
                if m >= 0:
                    self.memset("pool", pt[64:128, c0:c0 + 64], 0.0)
                self.mm(OP[c][:, c0:512], VV[:, kt, hh * 128:(hh + 1) * 128], pt[:, c0:512], start=(kt == 0), stop=(kt == nk - 1))
                a = ACC[gi % 2][c]
                if kt == 0:
                    self.cp("dve", a.v, pt.v)
                else:
                    self.tt("dve", a[:, c0:512], a[:, c0:512], pt[:, c0:512], ALU.add)
                if kt == nk - 1 and c == 1:
                    finish(gi)

    def phase3(self, l, x_src):
        with contextlib.ExitStack() as st:
            sb = lambda n, s, d: self.sb(st, n, s, d)
            ps = lambda n, s, d: self.ps(st, n, s, d)
            WO = sb("WO", [128, 8, D], BF16)
            WO.b.nowaw = True
            for k in range(8):
                self.dma("pool", WO[:, k, :], self.w_out[l, k * 128:(k + 1) * 128, :])
            G2 = sb("G2", [128, D], F32)
            self.dma("sp", G2.v, self.norm2_g[l].pbc(128))
            self.stt("dve", G2.v, self.mod(4), 1.0, G2.v, ALU.add, ALU.mult)
            SH2 = self.mod(3)
            GT1 = self.mod(2)
            RW = sb("RW", [128, 8, NE], F32)
            self.dma("sp", RW.v, self.router_w.v.rearrange("(k p) e -> p k e", p=128))
            MT = [sb(f"MT{i}", [128, 8, 512], BF16) for i in range(2)]
            XT = [sb(f"X3{i}", [128, D], F32) for i in range(2)]
            X1 = [sb(f"X1{i}", [128, D], F32) for i in range(2)]
            TM = sb("TM3", [128, D], F32)
            junk = sb("junk3", [128, D], BF16)
            ssq = sb("ssq3", [128, 1], F32)
            H2F = sb("H2F", [128, D], F32)
            H2B = [sb(f"H2B{i}", [128, D], BF16) for i in range(2)]
            H2T = sb("H2T", [128, D], F32)
            PO = [ps(f"PO{i}", [128, 512], F32) for i in range(2)]
            PTR = [ps(f"PTR{i}", [128, 512], F32) for i in range(2)]
            PL = ps("PL", [128, NE], F32)
            identf = self.cst("ident")
            mview = self.mixT_s.v.rearrange("(k p) t -> p k t", p=128)
            for i in range(NT):
                if i % 4 == 0:
                    mt = MT[(i // 4) % 2]
                    self.dma("sp", mt.v, mview[:, :, i * 128:i * 128 + 512])
                xt = XT[i % 2]
                x1 = X1[i % 2]
                self.dma("act", xt.v, x_src[i * 128:(i + 1) * 128, :] if l == 0 else self.out_tile(i))
                tcol = (i % 4) * 128
                for hf in range(2):
                    for k in range(8):
                        self.mm(PO[hf].v, mt[:, k, tcol:tcol + 128], WO[:, k, hf * 512:(hf + 1) * 512], start=(k == 0), stop=(k == 7))
                    self.tt("dve", TM[:, hf * 512:(hf + 1) * 512], PO[hf].v, GT1[:, hf * 512:(hf + 1) * 512], ALU.mult)
                self.tt("pool", x1.v, TM.v, xt.v, ALU.add)
                self.dma("sp", self.out_tile(i), x1.v)
                self.act(junk.v, x1.v, AF.Square, accum=ssq.v)
                self.rsqrt(ssq.v, ssq.v, 1.0 / D, EPS)
                self.stt("dve", H2F.v, x1.v, ssq[:, 0:1], G2.v, ALU.mult, ALU.mult)
                self.tt("pool", H2F.v, H2F.v, SH2, ALU.add)
                hb = H2B[i % 2]
                self.cp("act", hb.v, H2F.v)
                self.dma("act", self.h2_s[i * 128:(i + 1) * 128, :], hb.v)
                for hf in range(2):
                    for k in range(4):
                        kk = hf * 4 + k
                        self.tr(PTR[hf][:, k * 128:(k + 1) * 128], H2F[:, kk * 128:(kk + 1) * 128], identf)
                    self.cp("dve" if hf == 0 else "act", H2T[:, hf * 512:(hf + 1) * 512], PTR[hf].v)
                for k in range(8):
                    self.mm(PL.v, H2T[:, k * 128:(k + 1) * 128], RW[:, k, :], start=(k == 0), stop=(k == 7))
                self.cp("dve", self.LOG[:, i, :], PL.v)
            self.dump(f"log{l}", self.LOG.v, [128, NT, NE])

    def phase_moe(self, l):
        import os
        BIG = 1.0e30
        NTE = NT * NE
        with contextlib.ExitStack() as rst:
            rsb = lambda n, s_, d: self.sb(rst, n, s_, d)
            RG1 = rsb("RG1", [128, NT], F32)
            RG2 = rsb("RG2", [128, NT], F32)
            D1I = rsb("D1I", [128, NT], I32)
            D2I = rsb("D2I", [128, NT], I32)
            IDXW = rsb("IDXW", [128, NB, 12], I32)
            with contextlib.ExitStack() as st:
                sb = lambda n, s_, d: self.sb(st, n, s_, d)
                ps = lambda n, s_, d: self.ps(st, n, s_, d)
                RB = sb("RB", [128, NE], F32)
                self.dma("sp", RB.v, self.router_b.v.pbc(128))
                SCO = sb("SCO", [128, NT, NE], F32)
                BIA = sb("BIA", [128, NT, NE], F32)
                TA = sb("TA", [128, NT, NE], F32)
                TB = sb("TB", [128, NT, NE], F32)
                OH1 = sb("OH1", [128, NT, NE], F32)
                OH2 = sb("OH2", [128, NT, NE], F32)
                M1 = sb("M1", [128, NT * 8], F32)
                M2 = sb("M2", [128, NT * 8], F32)
                GM = sb("GM", [128, NT], F32)
                V1 = sb("V1", [128, NT], F32)
                W1 = sb("W1", [128, NT], F32)
                W2 = sb("W2", [128, NT], F32)
                self.act(SCO.v, self.LOG.v, AF.Sigmoid)
                self.tt("dve", BIA.v, SCO.v, RB.v.unsqueeze(1).bc([128, NT, NE]), ALU.add)
                b4 = BIA.v.rearrange("p i (g k) -> p (i g) k", k=4)
                self.red("dve", M1.v, b4, op=ALU.max)
                ta4 = TA.v.rearrange("p i (g k) -> p (i g) k", k=4)
                self.tt("dve", ta4, b4, M1.v.unsqueeze(2).bc([128, NT * 8, 4]), ALU.is_equal)
                self.stt("dve", TA.v, TA.v, -BIG, BIA.v, ALU.mult, ALU.add)
                self.red("dve", M2.v, ta4, op=ALU.max)
                self.tt("dve", M1.v, M1.v, M2.v, ALU.add)
                gs3 = M1.v.rearrange("p (i g) -> p i g", g=8)
                self.red("dve", GM.v, gs3, op=ALU.max)
                self.tt("dve", M2.v.rearrange("p (i g) -> p i g", g=8), gs3, GM.v.unsqueeze(2).bc([128, NT, 8]), ALU.is_equal)
                self.ts("dve", ta4, M2.v.unsqueeze(2).bc([128, NT * 8, 4]), BIG, -BIG, op0=ALU.mult, op1=ALU.add)
                self.tt("dve", TA.v, TA.v, BIA.v, ALU.add)
                self.red("dve", V1.v, TA.v, op=ALU.max)
                self.tt("dve", OH1.v, TA.v, V1.v.unsqueeze(2).bc([128, NT, NE]), ALU.is_equal)
                self.stt("dve", TB.v, OH1.v, -BIG, TA.v, ALU.mult, ALU.add)
                self.red("dve", V1.v, TB.v, op=ALU.max)
                self.tt("dve", OH2.v, TB.v, V1.v.unsqueeze(2).bc([128, NT, NE]), ALU.is_equal)
                self.tt("dve", TA.v, SCO.v, OH1.v, ALU.mult)
                self.red("dve", W1.v, TA.v)
                self.tt("dve", TA.v, SCO.v, OH2.v, ALU.mult)
                self.red("dve", W2.v, TA.v)
                self.tt("dve", V1.v, W1.v, W2.v, ALU.add)
                self.recip(V1.v, V1.v)
                self.tt("dve", RG1.v, W1.v, V1.v, ALU.mult)
                self.tt("dve", RG2.v, W2.v, V1.v, ALU.mult)
                MS_ = sb("MSEL", [128, NT, NE], BF16)
                self.tt("dve", MS_.v, OH1.v, OH2.v, ALU.add)
                LTSb = sb("LTSb", [128, 128], BF16)
                self.cp("dve", LTSb.v, self.cst("lts"))
                PRE = sb("PRE", [128, NT, NE], F32)
                TOT = sb("TOT", [128, NT, NE], F32)
                PP = [ps(f"PP{i}", [128, 512], F32) for i in range(2)]
                msf = MS_.v.rearrange("p i e -> p (i e)")
                pre_f = PRE.v.rearrange("p i e -> p (i e)")
                tot_f = TOT.v.rearrange("p i e -> p (i e)")
                for c in range(NTE // 512):
                    self.mm(PP[0].v, LTSb.v, msf[:, c * 512:(c + 1) * 512])
                    self.cp("dve", pre_f[:, c * 512:(c + 1) * 512], PP[0].v)
                    self.mm(PP[1].v, self.onesb.v, msf[:, c * 512:(c + 1) * 512])
                    self.cp("act", tot_f[:, c * 512:(c + 1) * 512], PP[1].v)
                A_, B_ = TA, TB
                self.cp("dve", A_.v, TOT.v)
                k = 1
                while k < NT:
                    self.tt("dve", B_[:, k:, :], A_[:, k:, :], A_[:, :NT - k, :], ALU.add)
                    self.cp("dve", B_[:, :k, :], A_[:, :k, :])
                    A_, B_ = B_, A_
                    k *= 2
                INC = A_
                OFF = B_
                self.tt("dve", OFF.v, INC.v, TOT.v, ALU.subtract)
                CNT = INC[:, NT - 1, :]
                CMP = sb("CMP", [128, NE, 32], F32)
                self.tt("dve", CMP.v, CNT.unsqueeze(2).bc([128, NE, 32]), self.cst("jb")[:, 0:32].unsqueeze(1).bc([128, NE, 32]), ALU.is_gt)
                PADE = sb("PADE", [128, NE], F32)
                self.red("dve", PADE.v, CMP.v)
                self.ts("dve", PADE.v, PADE.v, float(BLK))
                EA = sb("EA", [128, NE], F32)
                EB = sb("EB", [128, NE], F32)
                self.cp("dve", EA.v, PADE.v)
                a_, b_ = EA, EB
                k = 1
                while k < NE:
                    self.tt("dve", b_[:, k:], a_[:, k:], a_[:, :NE - k], ALU.add)
                    self.cp("dve", b_[:, :k], a_[:, :k])
                    a_, b_ = b_, a_
                    k *= 2
                PEND = a_
                PST = b_
                self.tt("dve", PST.v, PEND.v, PADE.v, ALU.subtract)
                self.tt("dve", PRE.v, PRE.v, OFF.v, ALU.add)
                self.tt("dve", PRE.v, PRE.v, PST.v.unsqueeze(1).bc([128, NT, NE]), ALU.add)
                self.tt("dve", TOT.v, PRE.v, OH1.v, ALU.mult)
                self.red("dve", W1.v, TOT.v)
                self.tt("dve", TOT.v, PRE.v, OH2.v, ALU.mult)
                self.red("dve", W2.v, TOT.v)
                self.cp("dve", D1I.v, W1.v)
                self.cp("dve", D2I.v, W2.v)
                CM2 = sb("CM2", [128, NB, NE], F32)
                self.tt("dve", CM2.v, PEND.v.unsqueeze(1).bc([128, NB, NE]), self.cst("jb").unsqueeze(2).bc([128, NB, NE]), ALU.is_le)
                BE = sb("BE", [128, NB], F32)
                self.red("dve", BE.v, CM2.v)
                self.ts("dve", BE.v, BE.v, float(NE - 1), None, op0=ALU.min)
                IDXF = sb("IDXF", [128, NB, 12], F32)
                coff = self.cst("coff")
                self.stt("dve", IDXF[:, :, 0:8], BE.v.unsqueeze(2).bc([128, NB, 8]), float(D), coff[:, 0:8].unsqueeze(1).bc([128, NB, 8]), ALU.mult, ALU.add)
                self.stt("dve", IDXF[:, :, 8:12], BE.v.unsqueeze(2).bc([128, NB, 4]), float(DE), coff[:, 8:12].unsqueeze(1).bc([128, NB, 4]), ALU.mult, ALU.add)
                self.cp("dve", IDXW.v, IDXF.v)
                if self.debug:
                    self.dump(f"d1_{l}", W1.v, [128, NT])
                    self.dump(f"d2_{l}", W2.v, [128, NT])
                    self.dump(f"g1_{l}", RG1.v, [128, NT])
                    self.dump(f"g2_{l}", RG2.v, [128, NT])
                    self.dump(f"be_{l}", BE.v, [128, NB])
            self.fw.barrier()
            if os.environ.get("MOECUT", "9") == "0":
                return
            with contextlib.ExitStack() as st:
                sb = lambda n, s_, d: self.sb(st, n, s_, d)
                HB = [sb(f"HBm{i}", [128, D], BF16) for i in range(3)]
                for i in range(NT):
                    hb = HB[i % 3]
                    self.dma("sp", hb.v, self.h2_s[i * 128:(i + 1) * 128, :])
                    self.scatter(self.xs_s.v, hb.v, D1I[:, i:i + 1])
                    self.scatter(self.xs_s.v, hb.v, D2I[:, i:i + 1])
            self.fw.barrier()
            if os.environ.get("MOECUT", "9") == "1":
                return
            with contextlib.ExitStack() as st:
                sb = lambda n, s_, d: self.sb(st, n, s_, d)
                ps = lambda n, s_, d: self.ps(st, n, s_, d)
                WGU = [sb(f"WGU{i}", [128, 8, 2 * DE], BF16) for i in range(2)]
                WD = [sb(f"WD{i}", [128, 4, D], BF16) for i in range(2)]
                for t_ in WGU + WD:
                    t_.b.nowaw = True
                XB = [sb(f"XB{i}", [128, NSUB, D], BF16) for i in range(2)]
                XTB = [sb(f"XTB{i}", [128, 8, BLK], BF16) for i in range(2)]
                SGt = sb("SGt", [128, BLK], F32)
                UT = sb("UT", [128, 4, BLK], BF16)
                YB = [sb(f"YB{i}", [128, D], F32) for i in range(2)]
                TPX = ps("TPX", [128, 1024], BF16)
                PG = [ps(f"PG{i}", [128, BLK], F32) for i in range(2)]
                PU = [ps(f"PU{i}", [128, BLK], F32) for i in range(2)]
                PD = [ps(f"PD{i}", [128, 512], F32) for i in range(2)]
                wgu_t = self.wgub[l].v
                wd_t = self.wdb[l].v
                nblk = int(os.environ.get("MOEBLK", str(NB)))
                yi = 0
                for j in range(nblk):
                    wgu, wd, xb, xtb = WGU[j % 2], WD[j % 2], XB[j % 2], XTB[j % 2]
                    for c in range(8):
                        self.gather(wgu[:, c, :], wgu_t, IDXW[:, j, c:c + 1])
                    for c in range(4):
                        self.gather(wd[:, c, :], wd_t, IDXW[:, j, 8 + c:9 + c])
                    self.dma("sp", xb.v, self.xs_s[j * BLK:(j + 1) * BLK, :].rearrange("(s p) d -> p s d", p=128))
                    for sub in range(NSUB):
                        for k in range(8):
                            self.tr(TPX[:, k * 128:(k + 1) * 128], xb[:, sub, k * 128:(k + 1) * 128], self.identb.v)
                        self.cp("dve" if sub % 2 == 0 else "act", xtb[:, :, sub * 128:(sub + 1) * 128], TPX.v.rearrange("p (k t) -> p k t", k=8))
                    for fc in range(4):
                        pg, pu = PG[fc % 2], PU[fc % 2]
                        for k in range(8):
                            self.mm(pg.v, wgu[:, k, fc * 128:(fc + 1) * 128], xtb[:, k, :], start=(k == 0), stop=(k == 7))
                        for k in range(8):
                            self.mm(pu.v, wgu[:, k, DE + fc * 128:DE + (fc + 1) * 128], xtb[:, k, :], start=(k == 0), stop=(k == 7))
                        self.act(SGt.v, pg.v, AF.Silu)
                        self.tt("dve", UT[:, fc, :], SGt.v, pu.v, ALU.mult)
                    for sub in range(NSUB):
                        yb = YB[yi % 2]
                        yi += 1
                        for hf in range(2):
                            pd = PD[hf]
                            for fc in range(4):
                                self.mm(pd.v, UT[:, fc, sub * 128:(sub + 1) * 128], wd[:, fc, hf * 512:(hf + 1) * 512], start=(fc == 0), stop=(fc == 3))
                            self.cp("act" if hf == 0 else "dve", yb[:, hf * 512:(hf + 1) * 512], pd.v)
                        self.dma("act", self.ys_s[j * BLK + sub * 128:j * BLK + (sub + 1) * 128, :], yb.v)
            self.fw.barrier()
            if os.environ.get("MOECUT", "9") == "2":
                return
            with contextlib.ExitStack() as st:
                sb = lambda n, s_, d: self.sb(st, n, s_, d)
                GT2 = self.mod(5)
                Y1 = [sb(f"Y1{i}", [128, D], F32) for i in range(2)]
                Y2 = [sb(f"Y2{i}", [128, D], F32) for i in range(2)]
                XX = [sb(f"XX{i}", [128, D], F32) for i in range(2)]
                TT = [sb(f"TT{i}", [128, D], F32) for i in range(2)]
                for i in range(NT):
                    y1, y2, xx, tt_ = Y1[i % 2], Y2[i % 2], XX[i % 2], TT[i % 2]
                    self.gather(y1.v, self.ys_s.v, D1I[:, i:i + 1])
                    self.gather(y2.v, self.ys_s.v, D2I[:, i:i + 1])
                    self.dma("sp", xx.v, self.out_tile(i))
                    self.ts("dve", tt_.v, y1.v, RG1[:, i:i + 1], None, op0=ALU.mult)
                    self.stt("dve", tt_.v, y2.v, RG2[:, i:i + 1], tt_.v, ALU.mult, ALU.add)
                    if self.debug and l == 0:
                        self.dma("act", self.ydbg[i * 128:(i + 1) * 128, :], tt_.v)
                    self.tt("pool", tt_.v, tt_.v, GT2, ALU.mult)
                    self.tt("pool", xx.v, xx.v, tt_.v, ALU.add)
                    self.dma("sp", self.out_tile(i), xx.v)


_CACHE = {}


def _get_prog(stop_after=None, debug=False):
    key = (stop_after, debug)
    if key not in _CACHE:
        p = Prog(stop_after=stop_after, debug=debug)
        p.build()
        _CACHE[key] = p
    return _CACHE[key]


def make_in_map(inputs, b):
    f = lambda a: np.ascontiguousarray(a, dtype=np.float32)
    m = {
        "x": f(inputs["x"][b]),
        "c": f(np.asarray(inputs["c"][b]).reshape(8, 128).T),
        "pos": np.ascontiguousarray(np.asarray(inputs["positions"][b]).reshape(NT, 128).T.astype(np.int32)),
        "consts": CONST_NP,
    }
    for k in ("w_mod", "b_mod", "norm1_g", "norm2_g", "w_in", "ret_norm_g", "diff_q_g", "diff_k_g", "lam_q1", "lam_k1",
              "lam_q2", "lam_k2", "diff_sub_g", "gla_w_a2", "gla_b_a", "gla_norm_g", "w_out", "router_w", "router_b"):
        m[k] = f(inputs[k])
    for l in range(DEPTH):
        m[f"w_gate{l}"] = f(inputs["w_gate"][l]).reshape(NE * D, DE)
        m[f"w_up{l}"] = f(inputs["w_up"][l]).reshape(NE * D, DE)
        m[f"w_down{l}"] = f(inputs["w_down"][l]).reshape(NE * DE, D)
    return m


def kernel(**inputs):
    prog = _get_prog()
    shared = make_in_map(inputs, 0)
    in_maps = []
    for b in range(8):
        m = dict(shared)
        m["x"] = np.ascontiguousarray(inputs["x"][b], dtype=np.float32)
        m["c"] = np.ascontiguousarray(np.asarray(inputs["c"][b], dtype=np.float32).reshape(8, 128).T)
        m["pos"] = np.ascontiguousarray(np.asarray(inputs["positions"][b]).reshape(NT, 128).T.astype(np.int32))
        in_maps.append(m)
    res = run_bass_kernel_spmd(prog.nc, in_maps, core_ids=list(range(8)))
    return np.stack([np.asarray(r["out"], dtype=np.float32) for r in res.results], axis=0)
```

```python
import math
import contextlib
import numpy as np
import concourse.bass as bass
import concourse.mybir as mybir
from concourse.bass_utils import run_bass_kernel_spmd

F32 = mybir.dt.float32
BF16 = mybir.dt.bfloat16
I32 = mybir.dt.int32
ALU = mybir.AluOpType
AF = mybir.ActivationFunctionType
AX = mybir.AxisListType

S = 8192
D = 1024
NT = S // 128
DEPTH = 2
EPS = 1e-6
NE = 32
DE = 512
BLK = 256
NSUB = BLK // 128
NB = -(-(2 * S) // BLK) + NE
NPAD = NB * BLK
SEM_LIMIT = 30000
WINC = 3456


class Buf:
    __slots__ = ("name", "w", "r", "excl", "nowaw")

    def __init__(self, name="", excl=False):
        self.name = name
        self.w = {}
        self.r = {}
        self.excl = excl
        self.nowaw = False


class V:
    __slots__ = ("ap", "b")

    def __init__(self, ap, b):
        self.ap = ap
        self.b = b

    def __getitem__(self, k):
        return V(self.ap[k], self.b)

    def rearrange(self, s, **kw):
        return V(self.ap.rearrange(s, **kw), self.b)

    def unsqueeze(self, a):
        return V(self.ap.unsqueeze(a), self.b)

    def bc(self, shape):
        return V(self.ap.broadcast_to(list(shape)), self.b)

    def pbc(self, n):
        return V(self.ap.partition_broadcast(n), self.b)

    def bitcast(self, dt):
        return V(self.ap.bitcast(dt), self.b)


class Tile:
    def __init__(self, t, name):
        self.t = t
        self.b = Buf(name)

    def __getitem__(self, k):
        return V(self.t[k], self.b)

    @property
    def v(self):
        return self[:]


class EngS:
    def __init__(self, name, handle):
        self.name = name
        self.h = handle
        self.sem = None
        self.n = 0
        self.seen = {}
        self.dma_sems = []
        self.dma_cnt = []
        self.dma_i = 0


class FW:
    def __init__(self, nc, ndma=10, same=True):
        self.nc = nc
        self.nsem = 0
        self.same = same
        self.E = {"pe": EngS("pe", nc.tensor), "dve": EngS("dve", nc.vector),
                  "act": EngS("act", nc.scalar), "pool": EngS("pool", nc.gpsimd),
                  "sp": EngS("sp", nc.sync)}
        self.ndma = ndma
        self.owner = {}
        self.nwaits = 0
        self.nops = 0

    def new_sem(self):
        self.nsem += 1
        return self.nc.alloc_semaphore(f"fs{self.nsem}")

    def _tok(self, E):
        if E.sem is None or E.n >= SEM_LIMIT:
            E.sem = self.new_sem()
            E.n = 0
            self.owner[E.sem] = E.name
        E.n += 1
        return (E.sem, E.n)

    def _wait(self, E, deps):
        for sem, val in deps.items():
            if E.seen.get(sem, 0) >= val:
                continue
            E.h.wait_ge(sem, val)
            E.seen[sem] = val
            self.nwaits += 1

    def _deps(self, E, reads, writes, skip_own):
        deps = {}
        for b in reads:
            for s, v in b.w.items():
                if deps.get(s, 0) < v:
                    deps[s] = v
        for b in writes:
            for d in ((b.r,) if b.nowaw else (b.w, b.r)):
                for s, v in d.items():
                    if deps.get(s, 0) < v:
                        deps[s] = v
        if skip_own:
            for s in list(deps):
                if self.owner.get(s) == E.name:
                    del deps[s]
        self._wait(E, deps)

    def _commit(self, tok, reads, writes):
        s, v = tok
        for b in reads:
            if b.r.get(s, 0) < v:
                b.r[s] = v
        for b in writes:
            if b.w.get(s, 0) < v:
                b.w[s] = v
            b.r = {}

    def op(self, eng, fn, reads=(), writes=()):
        E = self.E[eng]
        if any(b.excl for b in reads):
            writes = list(writes) + [b for b in reads if b.excl]
            reads = [b for b in reads if not b.excl]
        self._deps(E, reads, writes, (eng == "pe") or (not self.same))
        ins = fn()
        tok = self._tok(E)
        ins.then_inc(tok[0], 1)
        self._commit(tok, reads, writes)
        self.nops += 1
        return ins

    def dma(self, q, fn, reads=(), writes=()):
        E = self.E[q]
        if not E.dma_sems:
            E.dma_sems = [self.new_sem() for _ in range(self.ndma)]
            E.dma_cnt = [0] * self.ndma
        k = E.dma_i % self.ndma
        E.dma_i += 1
        if E.dma_cnt[k] * 16 >= SEM_LIMIT:
            self._wait(E, {E.dma_sems[k]: E.dma_cnt[k] * 16})
            E.dma_sems[k] = self.new_sem()
            E.dma_cnt[k] = 0
        sem = E.dma_sems[k]
        if E.dma_cnt[k] > 0:
            self._wait(E, {sem: E.dma_cnt[k] * 16})
        self._deps(E, reads, writes, False)
        ins = fn()
        E.dma_cnt[k] += 1
        ins.then_inc(sem, 16)
        self._commit((sem, E.dma_cnt[k] * 16), reads, writes)
        self.nops += 1
        return ins

    def barrier(self):
        deps = {}
        for E in self.E.values():
            if E.sem is not None and E.n > 0:
                deps[E.sem] = E.n
            for s, c in zip(E.dma_sems, E.dma_cnt):
                if c > 0:
                    deps[s] = c * 16
        for E in self.E.values():
            self._wait(E, dict(deps))


def _const_tables():
    c = {}
    p = np.arange(128)
    c["ident"] = np.eye(128, dtype=np.float32)
    gam = 1.0 - 2.0 ** (-5.0 - np.arange(4))
    same = (p[:, None] // 64) == (p[None, :] // 64)
    dr = np.zeros((128, 4, 128), np.float32)
    for h in range(4):
        dr[:, h, :] = np.where(same, gam[h] ** np.abs(p[:, None] - p[None, :]), 0.0)
    c["dr"] = dr.reshape(128, 512)
    j = p % 64
    qd = np.stack([gam[h] ** (j + 1.0) for h in range(4)], 1)
    kd = np.stack([gam[h] ** (63.0 - j) for h in range(4)], 1) / 8.0
    c["qd"] = np.repeat(qd, 64, axis=1)
    c["kd0"] = np.repeat(kd * (p[:, None] < 64), 64, axis=1)
    c["kd1"] = np.repeat(kd * (p[:, None] >= 64), 64, axis=1)
    c["cd"] = np.broadcast_to(np.repeat(gam ** 64.0, 64)[None, :], (128, 256)).copy()
    ml = (same & (p[:, None] <= p[None, :])).astype(np.float32)
    mu = (same & (p[:, None] > p[None, :])).astype(np.float32)
    c["ml4"] = np.tile(ml, (1, 4))
    c["mu4"] = np.tile(mu, (1, 4))
    c["tri"] = ml
    c["chk"] = np.stack([(p < 64), (p >= 64)], 1).astype(np.float32)
    c["m0"] = (p < 64).astype(np.float32)[:, None]
    c["m1"] = (p >= 64).astype(np.float32)[:, None]
    c["ifr"] = np.broadcast_to((1.0 / (10000.0 ** (np.arange(32, dtype=np.float32) / 32)))[None, :], (128, 32)).copy()
    c["ifd"] = np.broadcast_to((1.0 / (500000.0 ** (np.arange(8, dtype=np.float32) / 8)))[None, :], (128, 8)).copy()
    c["lts"] = (p[:, None] < p[None, :]).astype(np.float32)
    c["ones"] = np.ones((128, 128), np.float32)
    c["jb"] = np.broadcast_to((np.arange(NB, dtype=np.float32) * BLK)[None, :], (128, NB)).copy()
    coff = np.zeros((128, 12), np.float32)
    for cc in range(12):
        coff[:, cc] = (cc if cc < 8 else cc - 8) * 128 + p
    c["coff"] = coff
    c["gid"] = np.broadcast_to((np.arange(32) // 4).astype(np.float32)[None, :], (128, 32)).copy()
    off = {}
    cur = 0
    arrs = []
    for k, v in c.items():
        v = np.ascontiguousarray(v, dtype=np.float32).reshape(128, -1)
        off[k] = (cur, v.shape[1])
        cur += v.shape[1]
        arrs.append(v)
    return np.concatenate(arrs, axis=1), off


CONST_NP, COFF = _const_tables()
NCONST = CONST_NP.shape[1]


class Prog:
    def __init__(self, stop_after=None, debug=False):
        self.nc = nc = bass.Bass("TRN2", target_bir_lowering=False)
        import os
        self.fw = FW(nc, same=(os.environ.get("SAMEENG", "1") == "1"))
        self.stop_after = stop_after
        self.debug = debug
        self.dram = {}

    def din(self, name, shape, dt=F32):
        t = self.nc.dram_tensor(name, list(shape), dt, kind="ExternalInput")
        T = Tile(t.ap(), name)
        self.dram[name] = T
        return T

    def dout(self, name, shape, dt=F32):
        t = self.nc.dram_tensor(name, list(shape), dt, kind="ExternalOutput")
        T = Tile(t.ap(), name)
        self.dram[name] = T
        return T

    def dint(self, name, shape, dt, dbg=False):
        kind = "ExternalOutput" if (dbg and self.debug) else "Internal"
        t = self.nc.dram_tensor(name, list(shape), dt, kind=kind)
        T = Tile(t.ap(), name)
        T.b.nowaw = True
        self.dram[name] = T
        return T

    def dump(self, name, v, shape, dt=F32):
        if not self.debug:
            return
        T = self.dout(self._uname("dbg_" + name), shape, dt)
        self.dma("sp", T.v, v)

    def _uname(self, name):
        self._uid = getattr(self, "_uid", 0) + 1
        return f"{name}_u{self._uid}"

    def sb(self, st, name, shape, dt):
        name = self._uname(name)
        return Tile(st.enter_context(self.nc.sbuf_tensor(name, list(shape), dt)), name)

    def ps(self, st, name, shape, dt):
        name = self._uname(name)
        T = Tile(st.enter_context(self.nc.psum_tensor(name, list(shape), dt)), name)
        T.b.excl = True
        return T

    def _eng(self, e):
        return {"dve": self.nc.vector, "pool": self.nc.gpsimd, "act": self.nc.scalar}[e]

    def _pe(self, e):
        import os
        if e == "pool" and os.environ.get("NOPOOL", "0") == "1":
            return "dve"
        return e

    def dma(self, q, out, in_, **kw):
        h = {"sp": self.nc.sync, "act": self.nc.scalar, "pool": self.nc.gpsimd}[q]
        return self.fw.dma(q, lambda: h.dma_start(out=out.ap, in_=in_.ap, **kw), reads=[in_.b], writes=[out.b])

    def gather(self, out, table, idx):
        return self.fw.dma("pool", lambda: self.nc.gpsimd.indirect_dma_start(
            out=out.ap, out_offset=None, in_=table.ap,
            in_offset=bass.IndirectOffsetOnAxis(ap=idx.ap, axis=0)), reads=[table.b, idx.b], writes=[out.b])

    def scatter(self, table, in_, idx):
        return self.fw.dma("pool", lambda: self.nc.gpsimd.indirect_dma_start(
            out=table.ap, out_offset=bass.IndirectOffsetOnAxis(ap=idx.ap, axis=0),
            in_=in_.ap, in_offset=None), reads=[in_.b, idx.b], writes=[table.b])

    def mm(self, out, lhsT, rhs, start=True, stop=True):
        return self.fw.op("pe", lambda: self.nc.tensor.matmul(out=out.ap, lhsT=lhsT.ap, rhs=rhs.ap, start=start, stop=stop),
                          reads=[lhsT.b, rhs.b], writes=[out.b])

    def tr(self, out, in_, ident):
        return self.fw.op("pe", lambda: self.nc.tensor.transpose(out=out.ap, in_=in_.ap, identity=ident.ap),
                          reads=[in_.b, ident.b], writes=[out.b])

    def act(self, out, in_, func, scale=1.0, bias=0.0, accum=None):
        rd = [in_.b]
        kw = {}
        if isinstance(scale, V):
            rd.append(scale.b)
            kw["scale"] = scale.ap
        else:
            kw["scale"] = float(scale)
        if isinstance(bias, V):
            rd.append(bias.b)
            kw["bias"] = bias.ap
        else:
            kw["bias"] = float(bias)
        wr = [out.b]
        if accum is not None:
            kw["accum_out"] = accum.ap
            wr.append(accum.b)
        return self.fw.op("act", lambda: self.nc.scalar.activation(out=out.ap, in_=in_.ap, func=func, **kw), reads=rd, writes=wr)

    def tt(self, e, out, a, b, op):
        e = self._pe(e)
        return self.fw.op(e, lambda: self._eng(e).tensor_tensor(out=out.ap, in0=a.ap, in1=b.ap, op=op), reads=[a.b, b.b], writes=[out.b])

    def ts(self, e, out, a, s1, s2=None, op0=ALU.mult, op1=None):
        e = self._pe(e)
        rd = [a.b]
        s1a = s1
        s2a = s2
        if isinstance(s1, V):
            rd.append(s1.b)
            s1a = s1.ap
        if isinstance(s2, V):
            rd.append(s2.b)
            s2a = s2.ap
        kw = {}
        if op1 is not None:
            kw["op1"] = op1
        return self.fw.op(e, lambda: self._eng(e).tensor_scalar(out=out.ap, in0=a.ap, scalar1=s1a, scalar2=s2a, op0=op0, **kw), reads=rd, writes=[out.b])

    def stt(self, e, out, a, s, b, op0, op1):
        e = self._pe(e)
        rd = [a.b, b.b]
        sa = s
        if isinstance(s, V):
            rd.append(s.b)
            sa = s.ap
        return self.fw.op(e, lambda: self._eng(e).scalar_tensor_tensor(out=out.ap, in0=a.ap, scalar=sa, in1=b.ap, op0=op0, op1=op1), reads=rd, writes=[out.b])

    def cp(self, e, out, in_):
        e = self._pe(e)
        if e == "act":
            return self.act(out, in_, AF.Copy)
        return self.fw.op(e, lambda: self._eng(e).tensor_copy(out=out.ap, in_=in_.ap), reads=[in_.b], writes=[out.b])

    def red(self, e, out, in_, op=ALU.add, axis=AX.X):
        return self.fw.op(e, lambda: self._eng(e).tensor_reduce(out=out.ap, in_=in_.ap, axis=axis, op=op), reads=[in_.b], writes=[out.b])

    def recip(self, out, in_):
        return self.fw.op("dve", lambda: self.nc.vector.reciprocal(out=out.ap, in_=in_.ap), reads=[in_.b], writes=[out.b])

    def memset(self, e, out, val):
        e = self._pe(e)
        return self.fw.op(e, lambda: self._eng(e).memset(out.ap, val), writes=[out.b])

    def rsqrt(self, out, in_, scale, eps):
        self.act(out, in_, AF.Sqrt, scale=scale, bias=self.epsv(eps, out))
        self.recip(out, out)

    def epsv(self, eps, like):
        n = like.ap.shape[0]
        return self.C_eps[0:n, 0:1] if eps == EPS else 0.0

    def build(self):
        nc = self.nc
        with contextlib.ExitStack() as gst:
            self.gst = gst
            self.declare_io()
            self.load_consts(gst)
            for l in range(DEPTH):
                self.layer(l)
                if self.stop_after is not None and self.stop_after[0] == l and self.stop_after[1] != "all":
                    break
            self.finish()
        return nc

    def declare_io(self):
        d = self.din
        self.x_in = d("x", [S, D])
        self.c_in = d("c", [128, 8])
        self.pos_in = d("pos", [128, NT], I32)
        self.consts_in = d("consts", [128, NCONST])
        self.w_mod = d("w_mod", [DEPTH, D, 6 * D])
        self.b_mod = d("b_mod", [DEPTH, 6 * D])
        self.norm1_g = d("norm1_g", [DEPTH, D])
        self.norm2_g = d("norm2_g", [DEPTH, D])
        self.w_in = d("w_in", [DEPTH, D, 3344])
        self.ret_norm_g = d("ret_norm_g", [DEPTH, 64])
        self.diff_q_g = d("diff_q_g", [DEPTH, 64])
        self.diff_k_g = d("diff_k_g", [DEPTH, 64])
        self.lam_q1 = d("lam_q1", [DEPTH, 64])
        self.lam_k1 = d("lam_k1", [DEPTH, 64])
        self.lam_q2 = d("lam_q2", [DEPTH, 64])
        self.lam_k2 = d("lam_k2", [DEPTH, 64])
        self.diff_sub_g = d("diff_sub_g", [DEPTH, 128])
        self.gla_w_a2 = d("gla_w_a2", [DEPTH, 16, 128])
        self.gla_b_a = d("gla_b_a", [DEPTH, 128])
        self.gla_norm_g = d("gla_norm_g", [DEPTH, 64])
        self.w_out = d("w_out", [DEPTH, D, D])
        self.router_w = d("router_w", [D, NE])
        self.router_b = d("router_b", [NE])
        self.w_gate = [d(f"w_gate{l}", [NE * D, DE]) for l in range(DEPTH)]
        self.w_up = [d(f"w_up{l}", [NE * D, DE]) for l in range(DEPTH)]
        self.w_down = [d(f"w_down{l}", [NE * DE, D]) for l in range(DEPTH)]
        self.wgub = [self.dint(f"wgub{l}", [NE * D, 2 * DE], BF16) for l in range(DEPTH)]
        self.wdb = [self.dint(f"wdb{l}", [NE * DE, D], BF16) for l in range(DEPTH)]
        self.bg = []
        self.out = self.dout("out", [S, D])
        self.out_b = [Buf(f"out{i}") for i in range(NT)]
        if self.debug:
            self.ydbg = self.dout("ydbg", [S, D])
        self.qT_s = self.dint("qT_s", [4, 128, S], BF16, dbg=True)
        self.kT_s = self.dint("kT_s", [4, 128, S], BF16, dbg=True)
        self.v_s = self.dint("v_s", [S, 512], BF16, dbg=True)
        self.mixT_s = self.dint("mixT_s", [D, S], BF16, dbg=True)
        self.h2_s = self.dint("h2_s", [S, D], BF16, dbg=True)
        self.xs_s = self.dint("xs_s", [NPAD, D], BF16)
        self.ys_s = self.dint("ys_s", [NPAD, D], F32)

    def load_consts(self, st):
        self.CT = self.sb(st, "consts", [128, NCONST], F32)
        self.dma("sp", self.CT.v, self.consts_in.v)
        self.C_eps = self.sb(st, "c_eps", [128, 1], F32)
        self.memset("dve", self.C_eps.v, EPS)
        self.C_npi = self.sb(st, "c_npi", [128, 1], F32)
        self.memset("dve", self.C_npi.v, -math.pi)
        self.C_one = self.sb(st, "c_one", [128, 1], F32)
        self.memset("dve", self.C_one.v, 1.0)
        posi = self.sb(st, "posi", [128, NT], I32)
        self.dma("sp", posi.v, self.pos_in.v)
        self.POSF = self.sb(st, "posf", [128, NT], F32)
        self.cp("dve", self.POSF.v, posi.v)
        self.identb = self.sb(st, "identb", [128, 128], BF16)
        self.cp("dve", self.identb.v, self.cst("ident"))
        self.rope_tables(st)
        self.onesb = self.sb(st, "onesb", [128, 128], BF16)
        self.cp("dve", self.onesb.v, self.cst("ones"))

    def rope_tables(self, gst):
        self.COS = self.sb(gst, "COS", [128, NT, 40], F32)
        self.SIN = self.sb(gst, "SIN", [128, NT, 40], F32)
        o, _ = COFF["ifr"]
        if40 = self.CT[:, o:o + 40]
        C1 = 6.28125
        C2 = 2.0 * math.pi - C1
        with contextlib.ExitStack() as st:
            ANG = self.sb(st, "ANG", [128, NT, 40], F32)
            KI = self.sb(st, "KI", [128, NT, 40], I32)
            KF = self.sb(st, "KF", [128, NT, 40], F32)
            MK = self.sb(st, "MK", [128, NT, 40], F32)
            self.tt("dve", ANG.v, self.POSF.v.unsqueeze(2).bc([128, NT, 40]), if40.unsqueeze(1).bc([128, NT, 40]), ALU.mult)
            self.ts("dve", KF.v, ANG.v, 1.0 / (2.0 * math.pi))
            self.cp("dve", KI.v, KF.v)
            self.cp("dve", KF.v, KI.v)
            self.stt("dve", ANG.v, KF.v, -C1, ANG.v, ALU.mult, ALU.add)
            self.stt("dve", ANG.v, KF.v, -C2, ANG.v, ALU.mult, ALU.add)
            self.ts("dve", MK.v, ANG.v, math.pi, None, op0=ALU.is_gt)
            self.stt("dve", self.SIN.v, MK.v, -2.0 * math.pi, ANG.v, ALU.mult, ALU.add)
            self.ts("dve", MK.v, self.SIN.v, -math.pi, None, op0=ALU.is_lt)
            self.stt("dve", self.SIN.v, MK.v, 2.0 * math.pi, self.SIN.v, ALU.mult, ALU.add)
            self.ts("dve", self.COS.v, self.SIN.v, 0.5 * math.pi, None, op0=ALU.add)
            self.ts("dve", MK.v, self.COS.v, math.pi, None, op0=ALU.is_gt)
            self.stt("dve", self.COS.v, MK.v, -2.0 * math.pi, self.COS.v, ALU.mult, ALU.add)
            self.act(self.SIN.v, self.SIN.v, AF.Sin)
            self.act(self.COS.v, self.COS.v, AF.Sin)
        self.fw.barrier()

    def out_tile(self, i):
        return V(self.out.t[i * 128:(i + 1) * 128, :], self.out_b[i])

    def cst(self, name):
        o, n = COFF[name]
        return self.CT[:, o:o + n]

    def finish(self):
        fw = self.fw
        fw.barrier()

    def layer(self, l):
        x_src = self.x_in if l == 0 else self.out
        sa = self.stop_after
        with contextlib.ExitStack() as lst:
            self.phase_mod(l, lst)
            if sa == (l, "mod"):
                return
            self.queue_weight_casts(l)
            self.phase1(l, x_src)
            self.run_bg(10 ** 9)
            self.fw.barrier()
            if sa == (l, "p1"):
                return
            self.phase2(l)
            self.fw.barrier()
            if sa == (l, "p2"):
                return
            with contextlib.ExitStack() as st3:
                self.LOG = self.sb(st3, f"log{l}", [128, NT, NE], F32)
                self.phase3(l, x_src)
                self.fw.barrier()
                if sa == (l, "p3"):
                    return
                self.phase_moe(l)
                self.fw.barrier()
                if sa == (l, "moe"):
                    return

    def queue_weight_casts(self, l):
        for e in range(NE):
            for src, dst, rows, c0, c1 in ((self.w_gate[l], self.wgub[l], D, 0, DE), (self.w_up[l], self.wgub[l], D, DE, 2 * DE),
                                           (self.w_down[l], self.wdb[l], DE, 0, D)):
                def task(src=src, dst=dst, rows=rows, e=e, c0=c0, c1=c1):
                    self.dma("pool", dst[e * rows:(e + 1) * rows, c0:c1], src[e * rows:(e + 1) * rows, :])
                self.bg.append(task)

    def run_bg(self, n):
        while n > 0 and self.bg:
            self.bg.pop(0)()
            n -= 1

    def phase_mod(self, l, lst):
        MODB = self.sb(lst, f"modb{l}", [128, 6 * D], F32)
        self.MODB = MODB
        with contextlib.ExitStack() as st:
            cT = self.sb(st, "cT", [128, 8], F32)
            self.dma("sp", cT.v, self.c_in.v)
            ca = self.sb(st, "ca", [128, 8], F32)
            self.act(ca.v, cT.v, AF.Silu)
            cb = self.sb(st, "cb", [128, 8, 128], F32)
            for k in range(8):
                self.cp("dve", cb[:, k, :], ca[:, k:k + 1].bc([128, 128]))
            bm = self.sb(st, "bm", [1, 6 * D], F32)
            self.dma("sp", bm.v, self.b_mod[l:l + 1, :])
            ones_f = self.cst("ones")
            wm = [self.sb(st, f"wm{i}", [128, 8, 512], F32) for i in range(2)]
            pm = [self.ps(st, f"pm{i}", [128, 512], F32) for i in range(2)]
            for n in range(12):
                w = wm[n % 2]
                p = pm[n % 2]
                self.dma("sp" if n % 2 == 0 else "act", w.v,
                         self.w_mod[l, :, n * 512:(n + 1) * 512].rearrange("(k p) n -> p k n", p=128))
                for k in range(8):
                    self.mm(p.v, cb[:, k, :], w[:, k, :], start=(k == 0), stop=False)
                self.mm(p.v, ones_f[0:1, :], bm[0:1, n * 512:(n + 1) * 512], start=False, stop=True)
                self.cp("dve" if n % 2 == 0 else "act", MODB[:, n * 512:(n + 1) * 512], p.v)
        self.dump(f"mod{l}", MODB[0:2, :], [2, 6 * D])
        self.fw.barrier()

    def mod(self, j):
        return self.MODB[:, j * D:(j + 1) * D]

    def phase1(self, l, x_src):
        nc = self.nc
        with contextlib.ExitStack() as st:
            sb = lambda n, s, d: self.sb(st, n, s, d)
            ps = lambda n, s, d: self.ps(st, n, s, d)
            WIN = sb("WIN", [128, 8, WINC], BF16)
            WIN.b.nowaw = True
            for k in range(8):
                rows = self.w_in[l, k * 128:(k + 1) * 128, :]
                self.dma("pool", WIN[:, k, 0:1536], rows[:, 0:1536])
                self.dma("pool", WIN[:, k, 1536:3072], rows[:, 1536:3072])
                self.dma("pool", WIN[:, k, 3200:3456], rows[:, 3088:3344])
            import os
            setup = int(os.environ.get("P1SETUP", "9"))
            if setup < 1:
                return
            wga = sb("wga", [128, 8, 16], F32)
            self.dma("sp", wga.v, self.w_in[l, :, 3072:3088].rearrange("(k p) r -> p k r", p=128))
            wa2 = sb("wa2", [16, 128], F32)
            self.dma("sp", wa2.v, self.gla_w_a2[l])
            identf = self.cst("ident")
            PJ = [ps(f"PJ{i}", [128, 512], F32) for i in range(2)]
            TP = [ps(f"TP{i}", [128, 1024], BF16) for i in range(2)]
            SC = ps("SC", [128, 512], F32)
            SC2 = ps("SC2", [128, 512], F32)
            OT = ps("OT", [64, 512], F32)
            MS = ps("MS", [128, 512], F32)
            gaT = sb("gaT", [16, 128], F32)
            for k in range(8):
                self.tr(SC[0:16, 0:128], wga[:, k, :], identf)
                self.cp("dve", gaT.v, SC[0:16, 0:128])
                self.mm(SC2[:, 0:128], gaT.v, wa2.v)
                self.cp("dve", WIN[:, k, 3072:3200], SC2[:, 0:128])
            if setup < 2:
                return
            G1 = sb("G1", [128, D], F32)
            self.dma("sp", G1.v, self.norm1_g[l].pbc(128))
            self.stt("dve", G1.v, self.mod(1), 1.0, G1.v, ALU.add, ALU.mult)
            SH1 = self.mod(0)
            GQ = sb("GQ", [128, 64], F32)
            GK = sb("GK", [128, 64], F32)
            self.dma("sp", GQ.v, self.diff_q_g[l].pbc(128))
            self.dma("sp", GK.v, self.diff_k_g[l].pbc(128))
            self.ts("dve", GQ.v, GQ.v, 0.125)
            BA = sb("BA", [128, 128], F32)
            self.dma("sp", BA.v, self.gla_b_a[l].pbc(128))
            GRN = sb("GRN", [64, 1], F32)
            GGN = sb("GGN", [64, 1], F32)
            self.dma("sp", GRN.v, self.ret_norm_g[l].unsqueeze(1))
            self.dma("sp", GGN.v, self.gla_norm_g[l].unsqueeze(1))
            onesf = self.cst("ones")
            if setup < 3:
                return
            SR = sb("SR", [64, 256], F32)
            SRb = sb("SRb", [64, 256], BF16)
            SG = sb("SG", [32, 256], F32)
            SGb = sb("SGb", [32, 256], BF16)
            for t_ in (SR, SRb, SG, SGb):
                self.memset("dve", t_.v, 0.0)
            XT = [sb(f"XT{i}", [128, D], F32) for i in range(2)]
            ssq = sb("ssq", [128, 1], F32)
            HF = sb("HF", [128, D], F32)
            HB = sb("HB", [128, D], BF16)
            junk = HB
            HT = sb("HT", [128, 8, 128], BF16)
            R1 = sb("R1", [128, 8, 32], F32)
            R2 = sb("R2", [128, 8, 32], F32)
            ROT = sb("ROT", [128, 8, 64], F32)
            QB = sb("QB", [128, 256], BF16)
            QDB = sb("QDB", [128, 256], BF16)
            KB = sb("KB", [128, 256], BF16)
            KD0 = sb("KD0", [128, 256], BF16)
            KD1 = sb("KD1", [128, 256], BF16)
            VR = sb("VR", [128, 256], BF16)
            SGR = sb("SGR", [128, 256], BF16)
            RT = sb("RT", [64, 12, 128], BF16)
            SGT = sb("SGT", [64, 8, 128], BF16)
            DSQ = sb("DSQ", [128, 512], F32)
            GS2 = sb("GS2", [128, 512], F32)
            DSS = sb("DSS", [128, 8], F32)
            DXN = sb("DXN", [128, 8, 64], F32)
            DQB = sb("DQB", [128, 512], BF16)
            DKB = sb("DKB", [128, 512], BF16)
            DT = sb("DT", [128, 8, 128], BF16)
            DVB = sb("DVB", [128, 512], BF16)
            D1 = sb("D1", [128, 8, 8], F32)
            D2 = sb("D2", [128, 8, 8], F32)
            AT = sb("AT", [128, 512], BF16)
            ZT = sb("ZT", [128, 128], F32)
            SP = sb("SP", [128, 128], F32)
            EPt = sb("EPt", [128, 128], F32)
            EMt = sb("EMt", [128, 128], F32)
            GQP = sb("GQP", [128, 128], BF16)
            GQM = sb("GQM", [128, 128], BF16)
            GKP = sb("GKP", [128, 128], BF16)
            GKM = sb("GKM", [128, 128], BF16)
            GKM0 = sb("GKM0", [128, 128], BF16)
            GKM1 = sb("GKM1", [128, 128], BF16)
            GVB = sb("GVB", [128, 256], BF16)
            SGG = sb("SGG", [128, 256], BF16)
            GT = sb("GT", [32, 16, 128], BF16)
            EGL = sb("EGL", [32, 8], F32)
            TMP1 = sb("TMP1", [128, 512], F32)
            OY = sb("OY", [64, 512], F32)
            MXR = sb("MXR", [64, 512], BF16)
            MXG = sb("MXG", [64, 512], BF16)
            KVT = sb("KVT", [64, 256], F32)

            ifr = self.cst("ifr")
            ifd = self.cst("ifd")
            TWO_PI = 2.0 * math.pi

            def post_norm(OTv, G, SGTv, MX):
                OSQ_ = TMP1[0:64, :]
                ORS_ = GS2[0:64, :]
                self.act(OSQ_, OTv, AF.Square)
                self.mm(MS[0:64, :], onesf[0:64, 0:64], OSQ_)
                self.rsqrt(ORS_, MS[0:64, :], 1.0 / 64, EPS)
                self.tt("dve", OY.v, OTv, ORS_, ALU.mult)
                self.stt("dve", MX.v, OY.v, G[:, 0:1], SGTv, ALU.mult, ALU.mult)

            import os
            cut = int(os.environ.get("P1CUT", "9"))
            ntl = int(os.environ.get("P1TILES", str(NT)))
            TPA, TPB = TP[0], TP[1]
            PROJ = [sb(f"PROJ{i}", [128, WINC], F32) for i in range(2)]

            class _PV:
                def __init__(self, tile, off, w):
                    self.tile, self.off, self.w = tile, off, w

                def __getitem__(self, k):
                    rows, cols = k
                    return self.tile[rows, self.off + cols.start:self.off + cols.stop]

                @property
                def v(self):
                    return self.tile[:, self.off:self.off + self.w]

            def stageA(i):
                xt = XT[i % 2]
                self.dma("sp", xt.v, x_src[i * 128:(i + 1) * 128, :] if l == 0 else self.out_tile(i))
                self.act(junk.v, xt.v, AF.Square, accum=ssq.v)
                self.rsqrt(ssq.v, ssq.v, 1.0 / D, EPS)
                self.stt("dve", HF.v, xt.v, ssq[:, 0:1], G1.v, ALU.mult, ALU.mult)
                self.tt("pool", HB.v, HF.v, SH1, ALU.add)
                tp = TPA
                for k in range(8):
                    self.tr(tp[:, k * 128:(k + 1) * 128], HB[:, k * 128:(k + 1) * 128], self.identb.v)
                self.cp("act", HT[:, 0:4, :], tp[:, 0:512].rearrange("p (k t) -> p k t", k=4))
                self.cp("dve", HT[:, 4:8, :], tp[:, 512:1024].rearrange("p (k t) -> p k t", k=4))
                yield
                P_ = PROJ[i % 2]
                for n, width in ((0, 512), (1, 512), (2, 512), (3, 512), (4, 512), (6, 384), (5, 512)):
                    p = PJ[n % 2]
                    for k in range(8):
                        self.mm(p[:, 0:width], HT[:, k, :], WIN[:, k, n * 512:n * 512 + width], start=(k == 0), stop=(k == 7))
                    self.cp("act" if n % 2 == 0 else "dve", P_[:, n * 512:n * 512 + width], p[:, 0:width])
                    yield

            def _hdr(i):
                def proj(n, width):
                    return _PV(PROJ[i % 2], n * 512, width)
                cosr = self.COS[:, i, 0:32].unsqueeze(1).bc([128, 8, 32])
                sinr = self.SIN[:, i, 0:32].unsqueeze(1).bc([128, 8, 32])
                cosd = self.COS[:, i, 32:40].unsqueeze(1).bc([128, 8, 8])
                sind = self.SIN[:, i, 32:40].unsqueeze(1).bc([128, 8, 8])
                return proj, cosr, sinr, cosd, sind

            def stageRG(i):
                proj, cosr, sinr, cosd, sind = _hdr(i)
                yield
                p0 = proj(0, 512)
                pv = p0.v.rearrange("p (g h w) -> p g h w", g=8, h=2)
                X1 = pv[:, :, 0, :]
                X2 = pv[:, :, 1, :]
                rv_ = ROT.v.rearrange("p g (h w) -> p g h w", h=2)
                self.tt("dve", R1.v, X1, cosr, ALU.mult)
                yield
                self.tt("dve", R2.v, X2, sinr, ALU.mult)
                yield
                self.tt("pool", rv_[:, :, 0, :], R1.v, R2.v, ALU.subtract)
                yield
                self.tt("dve", R1.v, X1, sinr, ALU.mult)
                yield
                self.tt("dve", R2.v, X2, cosr, ALU.mult)
                yield
                self.tt("pool", rv_[:, :, 1, :], R1.v, R2.v, ALU.add)
                yield
                rq = ROT[:, 0:4, :].rearrange("p g w -> p (g w)")
                rk = ROT[:, 4:8, :].rearrange("p g w -> p (g w)")
                self.cp("act", QB.v, rq)
                yield
                self.tt("pool", QDB.v, rq, self.cst("qd"), ALU.mult)
                yield
                self.act(KB.v, rk, AF.Copy, scale=0.125)
                yield
                self.tt("pool", KD0.v, rk, self.cst("kd0"), ALU.mult)
                yield
                self.tt("pool", KD1.v, rk, self.cst("kd1"), ALU.mult)
                yield
                if cut < 2:
                    return
                p1 = proj(1, 512)
                self.cp("act", VR.v, p1[:, 0:256])
                yield
                self.act(SGR.v, p1[:, 256:512], AF.Silu)
                yield
                if cut == 2 and os.environ.get("SUB", "9") == "0":
                    return
                tp = TPB
                for h in range(4):
                    self.tr(tp[0:64, h * 128:(h + 1) * 128], QB[:, h * 64:(h + 1) * 64], self.identb.v)
                    self.tr(tp[0:64, (4 + h) * 128:(5 + h) * 128], QDB[:, h * 64:(h + 1) * 64], self.identb.v)
                yield
                self.cp("dve", RT[:, 0:8, :], tp[0:64, :].rearrange("p (k t) -> p k t", k=8))
                yield
                if cut == 2 and os.environ.get("SUB", "9") == "1":
                    return
                tp = TPB
                for h in range(4):
                    self.tr(tp[0:64, h * 128:(h + 1) * 128], KB[:, h * 64:(h + 1) * 64], self.identb.v)
                    self.tr(tp[0:64, (4 + h) * 128:(5 + h) * 128], SGR[:, h * 64:(h + 1) * 64], self.identb.v)
                yield
                self.cp(os.environ.get("GRP2", "act"), RT[:, 8:12, :], tp[0:64, 0:512].rearrange("p (k t) -> p k t", k=4))
                yield
                self.cp("dve", SGT[:, 0:4, :], tp[0:64, 512:1024].rearrange("p (k t) -> p k t", k=4))
                yield
                if cut < 3:
                    return
                for h in range(4):
                    self.mm(SC[:, h * 128:(h + 1) * 128], RT[:, 8 + h, :], RT[:, h, :])
                yield
                self.tt("dve", AT.v, SC.v, self.cst("dr"), ALU.mult)
                yield
                for h in range(4):
                    o = OT[:, h * 128:(h + 1) * 128]
                    self.mm(o, VR[:, h * 64:(h + 1) * 64], AT[:, h * 128:(h + 1) * 128], start=(h == 0), stop=False)
                    self.mm(OT[:, h * 128:h * 128 + 64], SRb[:, h * 64:(h + 1) * 64], RT[:, 4 + h, 0:64], start=False, stop=False)
                yield
                for h in range(4):
                    self.mm(MS[0:64, h * 64:(h + 1) * 64], KD0[:, h * 64:(h + 1) * 64], VR[:, h * 64:(h + 1) * 64])
                    self.mm(MS[0:64, 256 + h * 64:256 + (h + 1) * 64], KD1[:, h * 64:(h + 1) * 64], VR[:, h * 64:(h + 1) * 64])
                yield
                self.tt("dve", KVT.v, SR.v, self.cst("cd")[0:64, :], ALU.mult)
                yield
                self.tt("dve", SR.v, KVT.v, MS[0:64, 0:256], ALU.add)
                yield
                self.cp("act", SRb.v, SR.v)
                yield
                for h in range(4):
                    self.mm(OT[:, h * 128 + 64:(h + 1) * 128], SRb[:, h * 64:(h + 1) * 64], RT[:, 4 + h, 64:128], start=False, stop=True)
                yield
                self.tt("dve", KVT.v, SR.v, self.cst("cd")[0:64, :], ALU.mult)
                yield
                self.tt("dve", SR.v, KVT.v, MS[0:64, 256:512], ALU.add)
                yield
                self.cp("act", SRb.v, SR.v)
                yield
                post_norm(OT.v, GRN, SGT[:, 0:4, :].rearrange("p k t -> p (k t)"), MXR)
                yield
                self.dma("act", self.mixT_s[0:256, i * 128:(i + 1) * 128].rearrange("(h p) t -> p h t", p=64),
                         MXR.v.rearrange("p (h t) -> p h t", h=4))
                yield

                if cut < 4:
                    return
                p6 = proj(6, 384)
                self.tt("dve", ZT.v, p6[:, 0:128], BA.v, ALU.add)
                yield
                self.act(SGG.v, p6[:, 128:384], AF.Silu)
                yield
                self.act(SP.v, ZT.v, AF.Exp, scale=-1.0)
                yield
                self.act(SP.v, SP.v, AF.Ln, bias=self.C_one[:, 0:1])
                yield
                self.mm(SC2[:, 0:128], self.cst("tri"), SP.v)
                yield
                self.act(EPt.v, SC2[:, 0:128], AF.Exp, scale=-1.0 / 16)
                yield
                self.act(EMt.v, SC2[:, 0:128], AF.Exp, scale=1.0 / 16)
                yield
                for h in range(4):
                    self.mm(MS[0:32, 2 * h:2 * h + 2], SP[:, h * 32:(h + 1) * 32], self.cst("chk"))
                yield
                self.act(EGL.v, MS[0:32, 0:8], AF.Exp, scale=-1.0 / 16)
                yield
                p5 = proj(5, 512)
                qs = 32.0 ** -0.5
                self.stt("dve", GQP.v, p5[:, 0:128], qs, EPt.v, ALU.mult, ALU.mult)
                yield
                self.stt("dve", GQM.v, p5[:, 0:128], qs, EMt.v, ALU.mult, ALU.mult)
                yield
                self.tt("dve", GKP.v, p5[:, 128:256], EPt.v, ALU.mult)
                yield
                self.tt("dve", GKM.v, p5[:, 128:256], EMt.v, ALU.mult)
                yield
                self.cp("act", GVB.v, p5[:, 256:512])
                yield
                self.ts("pool", GKM0.v, GKM.v, self.cst("m0")[:, 0:1], None, op0=ALU.mult)
                yield
                self.ts("pool", GKM1.v, GKM.v, self.cst("m1")[:, 0:1], None, op0=ALU.mult)
                yield
                tp = TPB
                for h in range(4):
                    self.tr(tp[0:32, h * 128:(h + 1) * 128], GQP[:, h * 32:(h + 1) * 32], self.identb.v)
                    self.tr(tp[0:32, (4 + h) * 128:(5 + h) * 128], GQM[:, h * 32:(h + 1) * 32], self.identb.v)
                yield
                self.cp("act", GT[:, 0:8, :], tp[0:32, :].rearrange("p (k t) -> p k t", k=8))
                yield
                tp = TPB
                for h in range(4):
                    self.tr(tp[0:32, h * 128:(h + 1) * 128], GKM[:, h * 32:(h + 1) * 32], self.identb.v)
                    self.tr(tp[0:32, (4 + h) * 128:(5 + h) * 128], GKP[:, h * 32:(h + 1) * 32], self.identb.v)
                yield
                self.cp("dve", GT[:, 8:16, :], tp[0:32, :].rearrange("p (k t) -> p k t", k=8))
                yield
                tp = TPB
                for h in range(4):
                    self.tr(tp[0:64, h * 128:(h + 1) * 128], SGG[:, h * 64:(h + 1) * 64], self.identb.v)
                yield
                self.cp("act", SGT[:, 4:8, :], tp[0:64, 0:512].rearrange("p (k t) -> p k t", k=4))
                yield
                for h in range(4):
                    self.mm(SC[:, h * 128:(h + 1) * 128], GT[:, 8 + h, :], GT[:, h, :])
                    self.mm(SC2[:, h * 128:(h + 1) * 128], GT[:, 12 + h, :], GT[:, 4 + h, :])
                yield
                self.tt("dve", TMP1.v, SC.v, self.cst("ml4"), ALU.mult)
                yield
                self.tt("dve", GS2.v, SC2.v, self.cst("mu4"), ALU.mult)
                yield
                self.tt("pool", AT.v, TMP1.v, GS2.v, ALU.add)
                yield
                for h in range(4):
                    self.mm(OT[:, h * 128:(h + 1) * 128], GVB[:, h * 64:(h + 1) * 64], AT[:, h * 128:(h + 1) * 128], start=(h == 0), stop=False)
                    self.mm(OT[:, h * 128:h * 128 + 64], SGb[:, h * 64:(h + 1) * 64], GT[:, h, 0:64], start=False, stop=False)
                yield
                for h in range(4):
                    self.mm(MS[0:32, h * 64:(h + 1) * 64], GKM0[:, h * 32:(h + 1) * 32], GVB[:, h * 64:(h + 1) * 64])
                    self.mm(MS[0:32, 256 + h * 64:256 + (h + 1) * 64], GKM1[:, h * 32:(h + 1) * 32], GVB[:, h * 64:(h + 1) * 64])
                yield
                eg = EGL.v.rearrange("p (h c) -> p h c", c=2)
                sgv = SG.v.rearrange("p (h w) -> p h w", h=4)
                kv3 = KVT[0:32, :].rearrange("p (h w) -> p h w", h=4)
                self.tt("dve", KVT[0:32, :], SG.v, MS[0:32, 0:256], ALU.add)
                yield
                self.tt("dve", sgv, kv3, eg[:, :, 0:1].bc([32, 4, 64]), ALU.mult)
                yield
                self.cp("act", SGb.v, SG.v)
                yield
                for h in range(4):
                    self.mm(OT[:, h * 128 + 64:(h + 1) * 128], SGb[:, h * 64:(h + 1) * 64], GT[:, h, 64:128], start=False, stop=True)
                yield
                self.tt("dve", KVT[0:32, :], SG.v, MS[0:32, 256:512], ALU.add)
                yield
                self.tt("dve", sgv, kv3, eg[:, :, 1:2].bc([32, 4, 64]), ALU.mult)
                yield
                self.cp("act", SGb.v, SG.v)
                yield
                post_norm(OT.v, GGN, SGT[:, 4:8, :].rearrange("p k t -> p (k t)"), MXG)
                yield
                self.dma("act", self.mixT_s[768:1024, i * 128:(i + 1) * 128].rearrange("(h p) t -> p h t", p=64),
                         MXG.v.rearrange("p (h t) -> p h t", h=4))
                yield


            def stageD(i):
                proj, cosr, sinr, cosd, sind = _hdr(i)
                yield
                def dqk(n, G, OUTB, dst):
                    pq = proj(n, 512)
                    self.act(DSQ.v, pq.v, AF.Square)
                    yield
                    self.red("dve", DSS.v, DSQ.v.rearrange("p (g w) -> p g w", g=8))
                    yield
                    self.rsqrt(DSS.v, DSS.v, 1.0 / 64, EPS)
                    yield
                    self.tt("dve", DXN.v, pq.v.rearrange("p (g w) -> p g w", g=8), DSS.v.unsqueeze(2).bc([128, 8, 64]), ALU.mult)
                    yield
                    self.tt("pool", DXN.v, DXN.v, G.v.unsqueeze(1).bc([128, 8, 64]), ALU.mult)
                    yield
                    ob = OUTB.v.rearrange("p (g w) -> p g w", g=8)
                    self.cp("act", ob[:, :, 16:64], DXN[:, :, 16:64])
                    yield
                    x1 = DXN[:, :, 0:8]
                    x2 = DXN[:, :, 8:16]
                    self.tt("dve", D1.v, x1, cosd, ALU.mult)
                    yield
                    self.tt("pool", D2.v, x2, sind, ALU.mult)
                    yield
                    self.tt("dve", ob[:, :, 0:8], D1.v, D2.v, ALU.subtract)
                    yield
                    self.tt("dve", D1.v, x1, sind, ALU.mult)
                    yield
                    self.tt("pool", D2.v, x2, cosd, ALU.mult)
                    yield
                    self.tt("dve", ob[:, :, 8:16], D1.v, D2.v, ALU.add)
                    yield
                    tp = TPA
                    for h in range(4):
                        self.tr(tp[:, h * 128:(h + 1) * 128], OUTB[:, h * 128:(h + 1) * 128], self.identb.v)
                    dtv = DT[:, 0:4, :] if n == 2 else DT[:, 4:8, :]
                    self.cp("act" if n == 2 else "dve", dtv, tp[:, 0:512].rearrange("p (k t) -> p k t", k=4))
                    yield
                    self.dma("sp", dst[:, :, i * 128:(i + 1) * 128].rearrange("h p t -> p h t"), dtv)
                    yield
                yield from dqk(2, GQ, DQB, self.qT_s)
                yield from dqk(3, GK, DKB, self.kT_s)
                p4 = proj(4, 512)
                self.cp("act", DVB.v, p4.v)
                yield
                self.dma("act", self.v_s[i * 128:(i + 1) * 128, :], DVB.v)
                yield

                if cut < 5:
                    return

            for _ in stageA(0):
                pass
            for i in range(ntl):
                self.run_bg(2)
                gens = [stageRG(i), stageD(i)]
                if i + 1 < ntl:
                    gens.append(stageA(i + 1))
                while gens:
                    for g in list(gens):
                        try:
                            next(g)
                        except StopIteration:
                            gens.remove(g)


    def phase2(self, l):
        lam_init = 0.8 - 0.6 * math.exp(-0.3 * l)
        with contextlib.ExitStack() as st:
            sb = lambda n, s, d: self.sb(st, n, s, d)
            ps = lambda n, s, d: self.ps(st, n, s, d)
            KT = sb("KT", [128, 2, S], BF16)
            KT.b.nowaw = True
            VV = sb("VV", [128, NT, 256], BF16)
            VV.b.nowaw = True
            vsrc = self.v_s.v.rearrange("(i p) c -> p i c", p=128)

            def load_group(hg):
                for hh in range(2):
                    for j in range(4):
                        self.dma("sp" if (hh + j) % 2 == 0 else "act", KT[:, hh, j * 2048:(j + 1) * 2048], self.kT_s[2 * hg + hh, :, j * 2048:(j + 1) * 2048])
                for j in range(8):
                    self.dma("sp" if j % 2 == 0 else "act", VV[:, j * 8:(j + 1) * 8, :], vsrc[:, j * 8:(j + 1) * 8, hg * 256:(hg + 1) * 256])
            lq = sb("lq", [1, 4, 64], F32)
            for j, src in enumerate((self.lam_q1, self.lam_k1, self.lam_q2, self.lam_k2)):
                self.dma("sp", lq[:, j, :], src[l:l + 1, :])
            lp = sb("lp", [1, 2, 64], F32)
            self.tt("dve", lp[:, 0, :], lq[:, 0, :], lq[:, 1, :], ALU.mult)
            self.tt("dve", lp[:, 1, :], lq[:, 2, :], lq[:, 3, :], ALU.mult)
            ls = sb("ls", [1, 2], F32)
            self.red("dve", ls.v, lp.v)
            self.act(ls.v, ls.v, AF.Exp)
            lam1 = sb("lam1", [1, 1], F32)
            self.tt("dve", lam1.v, ls[:, 0:1], ls[:, 1:2], ALU.subtract)
            self.ts("dve", lam1.v, lam1.v, lam_init, -1.0, op0=ALU.add, op1=ALU.mult)
            NSB = 4
            SPS = [ps(f"SPS{i}", [128, 512], F32) for i in range(NSB)]
            OP = [ps(f"OP{i}", [128, 512], F32) for i in range(2)]
            MSP = ps("MSP", [128, 512], F32)
            MSP2 = ps("MSP2", [128, 512], F32)
            NLAM = sb("NLAM", [128, 1], F32)
            self.mm(MSP[:, 0:1], self.cst("ones")[0:1, :], lam1.v)
            self.cp("dve", NLAM.v, MSP[:, 0:1])
            SUBG = sb("SUBG", [128, 1], F32)
            self.dma("sp", SUBG.v, self.diff_sub_g[l].unsqueeze(1))
            self.ts("dve", SUBG.v, SUBG.v, 1.0 - lam_init)
            QT = [sb(f"QT{i}", [128, 512], BF16) for i in range(2)]
            PT = [sb(f"PT{i}", [128, 512], BF16) for i in range(4)]
            ACC = [[sb(f"ACC{p_}{c}", [128, 512], F32) for c in range(2)] for p_ in range(2)]
            OS = [sb(f"OS{c}", [128, 512], F32) for c in range(2)]
            R0 = sb("R0", [128, 512], F32)
            R1 = sb("R1", [128, 512], F32)
            ACB = [sb(f"ACB{c}", [128, 512], BF16) for c in range(2)]
            T0 = sb("T0", [128, 512], F32)
            T1 = sb("T1", [128, 512], F32)
            OSQ = sb("OSQ2", [128, 512], BF16)
            ORS = sb("ORS2", [128, 512], F32)
            OB = [sb(f"OB{i}", [128, 512], BF16) for i in range(2)]
            onesf = self.cst("ones")
            import os
            nqt = int(os.environ.get("P2QT", str(S // 512)))
            groups = [(a, b_, c_) for a in range(2) for b_ in range(nqt) for c_ in range(2)]
            units = []
            for gi, (hg, qt, hh) in enumerate(groups):
                nk = 4 * qt + 4
                for kt in range(nk):
                    for c in range(2):
                        units.append((gi, c, kt, nk))
            LOOK = 2
            qloaded = set()

            def load_q(gi):
                if gi >= len(groups) or gi in qloaded:
                    return
                qloaded.add(gi)
                hg, qt, hh = groups[gi]
                if qt == 0 and hh == 0:
                    load_group(hg)
                self.dma("sp", QT[gi % 2].v, self.qT_s[2 * hg + hh, :, qt * 512:(qt + 1) * 512])

            def c0_of(gi, kt):
                qt = groups[gi][1]
                m = kt - 4 * qt
                return (128 * m if m > 0 else 0), m

            def issue_qk(u):
                gi, c, kt, nk = units[u]
                load_q(gi)
                hh = groups[gi][2]
                c0, m = c0_of(gi, kt)
                self.mm(SPS[u % NSB][:, c0:512], KT[64 * c:64 * c + 64, hh, kt * 128:(kt + 1) * 128], QT[gi % 2][64 * c:64 * c + 64, c0:512])

            def finish(gi):
                hg, qt, hh = groups[gi]
                h = 2 * hg + hh
                acc = ACC[gi % 2]
                self.cp("act", OS[0].v, OP[0].v)
                self.cp("act", OS[1].v, OP[1].v)
                self.cp("dve", ACB[0].v, acc[0].v)
                self.cp("dve", ACB[1].v, acc[1].v)
                self.mm(MSP.v, self.onesb.v, ACB[0].v)
                self.mm(MSP2.v, self.onesb.v, ACB[1].v)
                self.act(R0.v, MSP.v, AF.Ln)
                self.act(R0.v, R0.v, AF.Exp, scale=-1.0)
                self.act(R1.v, MSP2.v, AF.Ln)
                self.act(R1.v, R1.v, AF.Exp, scale=-1.0)
                self.tt("dve", T0.v, OS[0].v, R0.v, ALU.mult)
                self.tt("dve", T1.v, OS[1].v, R1.v, ALU.mult)
                self.stt("dve", T0.v, T1.v, NLAM[:, 0:1], T0.v, ALU.mult, ALU.add)
                self.act(OSQ.v, T0.v, AF.Square)
                self.mm(MSP.v, self.onesb.v, OSQ.v)
                self.act(ORS.v, MSP.v, AF.Ln, scale=1.0 / 128, bias=self.C_eps[:, 0:1])
                self.act(ORS.v, ORS.v, AF.Exp, scale=-0.5)
                ob = OB[gi % 2]
                self.stt("dve", ob.v, T0.v, SUBG[:, 0:1], ORS.v, ALU.mult, ALU.mult)
                self.dma("act", self.mixT_s[256 + 128 * h:256 + 128 * (h + 1), qt * 512:(qt + 1) * 512], ob.v)

            def hg_of(u):
                return groups[units[u][0]][0]

            for u, (gi, c, kt, nk) in enumerate(units):
                if u == 0 or hg_of(u - 1) != hg_of(u):
                    for u2 in range(u, min(u + LOOK, len(units))):
                        if hg_of(u2) == hg_of(u):
                            issue_qk(u2)
                if u % 2 == 0:
                    for u2 in (u + LOOK, u + LOOK + 1):
                        if u2 < len(units) and hg_of(u2) == hg_of(u):
                            issue_qk(u2)
                hh = groups[gi][2]
                c0, m = c0_of(gi, kt)
                pt = PT[u % 4]
                if kt == 0 and c == 0 and gi + 1 < len(groups) and groups[gi + 1][0] == groups[gi][0]:
                    load_q(gi + 1)
                self.act(pt[:, c0:512], SPS[u % NSB][:, c0:512], AF.Exp)
                if m >= 0:
                    self.memset("pool", pt[64:128, c0:c0 + 64], 0.0)
                self.mm(OP[c][:, c0:512], VV[:, kt, hh * 128:(hh + 1) * 128], pt[:, c0:512], start=(kt == 0), stop=(kt == nk - 1))
                a = ACC[gi % 2][c]
                if kt == 0:
                    self.cp("dve", a.v, pt.v)
                else:
                    self.tt("dve", a[:, c0:512], a[:, c0:512], pt[:, c0:512], ALU.add)
                if kt == nk - 1 and c == 1:
                    finish(gi)

    def phase3(self, l, x_src):
        with contextlib.ExitStack() as st:
            sb = lambda n, s, d: self.sb(st, n, s, d)
            ps = lambda n, s, d: self.ps(st, n, s, d)
            WO = sb("WO", [128, 8, D], BF16)
            WO.b.nowaw = True
            for k in range(8):
                self.dma("pool", WO[:, k, :], self.w_out[l, k * 128:(k + 1) * 128, :])
            G2 = sb("G2", [128, D], F32)
            self.dma("sp", G2.v, self.norm2_g[l].pbc(128))
            self.stt("dve", G2.v, self.mod(4), 1.0, G2.v, ALU.add, ALU.mult)
            SH2 = self.mod(3)
            GT1 = self.mod(2)
            RW = sb("RW", [128, 8, NE], F32)
            self.dma("sp", RW.v, self.router_w.v.rearrange("(k p) e -> p k e", p=128))
            MT = [sb(f"MT{i}", [128, 8, 512], BF16) for i in range(2)]
            XT = [sb(f"X3{i}", [128, D], F32) for i in range(2)]
            X1 = [sb(f"X1{i}", [128, D], F32) for i in range(2)]
            TM = sb("TM3", [128, D], F32)
            junk = sb("junk3", [128, D], BF16)
            ssq = sb("ssq3", [128, 1], F32)
            H2F = sb("H2F", [128, D], F32)
            H2B = [sb(f"H2B{i}", [128, D], BF16) for i in range(2)]
            H2T = sb("H2T", [128, D], F32)
            PO = [ps(f"PO{i}", [128, 512], F32) for i in range(2)]
            PTR = [ps(f"PTR{i}", [128, 512], F32) for i in range(2)]
            PL = ps("PL", [128, NE], F32)
            identf = self.cst("ident")
            mview = self.mixT_s.v.rearrange("(k p) t -> p k t", p=128)
            for i in range(NT):
                if i % 4 == 0:
                    mt = MT[(i // 4) % 2]
                    self.dma("sp", mt.v, mview[:, :, i * 128:i * 128 + 512])
                xt = XT[i % 2]
                x1 = X1[i % 2]
                self.dma("act", xt.v, x_src[i * 128:(i + 1) * 128, :] if l == 0 else self.out_tile(i))
                tcol = (i % 4) * 128
                for hf in range(2):
                    for k in range(8):
                        self.mm(PO[hf].v, mt[:, k, tcol:tcol + 128], WO[:, k, hf * 512:(hf + 1) * 512], start=(k == 0), stop=(k == 7))
                    self.tt("dve", TM[:, hf * 512:(hf + 1) * 512], PO[hf].v, GT1[:, hf * 512:(hf + 1) * 512], ALU.mult)
                self.tt("pool", x1.v, TM.v, xt.v, ALU.add)
                self.dma("sp", self.out_tile(i), x1.v)
                self.act(junk.v, x1.v, AF.Square, accum=ssq.v)
                self.rsqrt(ssq.v, ssq.v, 1.0 / D, EPS)
                self.stt("dve", H2F.v, x1.v, ssq[:, 0:1], G2.v, ALU.mult, ALU.mult)
                self.tt("pool", H2F.v, H2F.v, SH2, ALU.add)
                hb = H2B[i % 2]
                self.cp("act", hb.v, H2F.v)
                self.dma("act", self.h2_s[i * 128:(i + 1) * 128, :], hb.v)
                for hf in range(2):
                    for k in range(4):
                        kk = hf * 4 + k
                        self.tr(PTR[hf][:, k * 128:(k + 1) * 128], H2F[:, kk * 128:(kk + 1) * 128], identf)
                    self.cp("dve" if hf == 0 else "act", H2T[:, hf * 512:(hf + 1) * 512], PTR[hf].v)
                for k in range(8):
                    self.mm(PL.v, H2T[:, k * 128:(k + 1) * 128], RW[:, k, :], start=(k == 0), stop=(k == 7))
                self.cp("dve", self.LOG[:, i, :], PL.v)
            self.dump(f"log{l}", self.LOG.v, [128, NT, NE])

    def phase_moe(self, l):
        import os
        BIG = 1.0e30
        NTE = NT * NE
        with contextlib.ExitStack() as rst:
            rsb = lambda n, s_, d: self.sb(rst, n, s_, d)
            RG1 = rsb("RG1", [128, NT], F32)
            RG2 = rsb("RG2", [128, NT], F32)
            D1I = rsb("D1I", [128, NT], I32)
            D2I = rsb("D2I", [128, NT], I32)
            IDXW = rsb("IDXW", [128, NB, 12], I32)
            with contextlib.ExitStack() as st:
                sb = lambda n, s_, d: self.sb(st, n, s_, d)
                ps = lambda n, s_, d: self.ps(st, n, s_, d)
                RB = sb("RB", [128, NE], F32)
                self.dma("sp", RB.v, self.router_b.v.pbc(128))
                SCO = sb("SCO", [128, NT, NE], F32)
                BIA = sb("BIA", [128, NT, NE], F32)
                TA = sb("TA", [128, NT, NE], F32)
                TB = sb("TB", [128, NT, NE], F32)
                OH1 = sb("OH1", [128, NT, NE], F32)
                OH2 = sb("OH2", [128, NT, NE], F32)
                M1 = sb("M1", [128, NT * 8], F32)
                M2 = sb("M2", [128, NT * 8], F32)
                GM = sb("GM", [128, NT], F32)
                V1 = sb("V1", [128, NT], F32)
                W1 = sb("W1", [128, NT], F32)
                W2 = sb("W2", [128, NT], F32)
                self.act(SCO.v, self.LOG.v, AF.Sigmoid)
                self.tt("dve", BIA.v, SCO.v, RB.v.unsqueeze(1).bc([128, NT, NE]), ALU.add)
                b4 = BIA.v.rearrange("p i (g k) -> p (i g) k", k=4)
                self.red("dve", M1.v, b4, op=ALU.max)
                ta4 = TA.v.rearrange("p i (g k) -> p (i g) k", k=4)
                self.tt("dve", ta4, b4, M1.v.unsqueeze(2).bc([128, NT * 8, 4]), ALU.is_equal)
                self.stt("dve", TA.v, TA.v, -BIG, BIA.v, ALU.mult, ALU.add)
                self.red("dve", M2.v, ta4, op=ALU.max)
                self.tt("dve", M1.v, M1.v, M2.v, ALU.add)
                gs3 = M1.v.rearrange("p (i g) -> p i g", g=8)
                self.red("dve", GM.v, gs3, op=ALU.max)
                self.tt("dve", M2.v.rearrange("p (i g) -> p i g", g=8), gs3, GM.v.unsqueeze(2).bc([128, NT, 8]), ALU.is_equal)
                self.ts("dve", ta4, M2.v.unsqueeze(2).bc([128, NT * 8, 4]), BIG, -BIG, op0=ALU.mult, op1=ALU.add)
                self.tt("dve", TA.v, TA.v, BIA.v, ALU.add)
                self.red("dve", V1.v, TA.v, op=ALU.max)
                self.tt("dve", OH1.v, TA.v, V1.v.unsqueeze(2).bc([128, NT, NE]), ALU.is_equal)
                self.stt("dve", TB.v, OH1.v, -BIG, TA.v, ALU.mult, ALU.add)
                self.red("dve", V1.v, TB.v, op=ALU.max)
                self.tt("dve", OH2.v, TB.v, V1.v.unsqueeze(2).bc([128, NT, NE]), ALU.is_equal)
                self.tt("dve", TA.v, SCO.v, OH1.v, ALU.mult)
                self.red("dve", W1.v, TA.v)
                self.tt("dve", TA.v, SCO.v, OH2.v, ALU.mult)
                self.red("dve", W2.v, TA.v)
                self.tt("dve", V1.v, W1.v, W2.v, ALU.add)
                self.recip(V1.v, V1.v)
                self.tt("dve", RG1.v, W1.v, V1.v, ALU.mult)
                self.tt("dve", RG2.v, W2.v, V1.v, ALU.mult)
                MS_ = sb("MSEL", [128, NT, NE], BF16)
                self.tt("dve", MS_.v, OH1.v, OH2.v, ALU.add)
                LTSb = sb("LTSb", [128, 128], BF16)
                self.cp("dve", LTSb.v, self.cst("lts"))
                PRE = sb("PRE", [128, NT, NE], F32)
                TOT = sb("TOT", [128, NT, NE], F32)
                PP = [ps(f"PP{i}", [128, 512], F32) for i in range(2)]
                msf = MS_.v.rearrange("p i e -> p (i e)")
                pre_f = PRE.v.rearrange("p i e -> p (i e)")
                tot_f = TOT.v.rearrange("p i e -> p (i e)")
                for c in range(NTE // 512):
                    self.mm(PP[0].v, LTSb.v, msf[:, c * 512:(c + 1) * 512])
                    self.cp("dve", pre_f[:, c * 512:(c + 1) * 512], PP[0].v)
                    self.mm(PP[1].v, self.onesb.v, msf[:, c * 512:(c + 1) * 512])
                    self.cp("act", tot_f[:, c * 512:(c + 1) * 512], PP[1].v)
                A_, B_ = TA, TB
                self.cp("dve", A_.v, TOT.v)
                k = 1
                while k < NT:
                    self.tt("dve", B_[:, k:, :], A_[:, k:, :], A_[:, :NT - k, :], ALU.add)
                    self.cp("dve", B_[:, :k, :], A_[:, :k, :])
                    A_, B_ = B_, A_
                    k *= 2
                INC = A_
                OFF = B_
                self.tt("dve", OFF.v, INC.v, TOT.v, ALU.subtract)
                CNT = INC[:, NT - 1, :]
                CMP = sb("CMP", [128, NE, 32], F32)
                self.tt("dve", CMP.v, CNT.unsqueeze(2).bc([128, NE, 32]), self.cst("jb")[:, 0:32].unsqueeze(1).bc([128, NE, 32]), ALU.is_gt)
                PADE = sb("PADE", [128, NE], F32)
                self.red("dve", PADE.v, CMP.v)
                self.ts("dve", PADE.v, PADE.v, float(BLK))
                EA = sb("EA", [128, NE], F32)
                EB = sb("EB", [128, NE], F32)
                self.cp("dve", EA.v, PADE.v)
                a_, b_ = EA, EB
                k = 1
                while k < NE:
                    self.tt("dve", b_[:, k:], a_[:, k:], a_[:, :NE - k], ALU.add)
                    self.cp("dve", b_[:, :k], a_[:, :k])
                    a_, b_ = b_, a_
                    k *= 2
                PEND = a_
                PST = b_
                self.tt("dve", PST.v, PEND.v, PADE.v, ALU.subtract)
                self.tt("dve", PRE.v, PRE.v, OFF.v, ALU.add)
                self.tt("dve", PRE.v, PRE.v, PST.v.unsqueeze(1).bc([128, NT, NE]), ALU.add)
                self.tt("dve", TOT.v, PRE.v, OH1.v, ALU.mult)
                self.red("dve", W1.v, TOT.v)
                self.tt("dve", TOT.v, PRE.v, OH2.v, ALU.mult)
                self.red("dve", W2.v, TOT.v)
                self.cp("dve", D1I.v, W1.v)
                self.cp("dve", D2I.v, W2.v)
                CM2 = sb("CM2", [128, NB, NE], F32)
                self.tt("dve", CM2.v, PEND.v.unsqueeze(1).bc([128, NB, NE]), self.cst("jb").unsqueeze(2).bc([128, NB, NE]), ALU.is_le)
                BE = sb("BE", [128, NB], F32)
                self.red("dve", BE.v, CM2.v)
                self.ts("dve", BE.v, BE.v, float(NE - 1), None, op0=ALU.min)
                IDXF = sb("IDXF", [128, NB, 12], F32)
                coff = self.cst("coff")
                self.stt("dve", IDXF[:, :, 0:8], BE.v.unsqueeze(2).bc([128, NB, 8]), float(D), coff[:, 0:8].unsqueeze(1).bc([128, NB, 8]), ALU.mult, ALU.add)
                self.stt("dve", IDXF[:, :, 8:12], BE.v.unsqueeze(2).bc([128, NB, 4]), float(DE), coff[:, 8:12].unsqueeze(1).bc([128, NB, 4]), ALU.mult, ALU.add)
                self.cp("dve", IDXW.v, IDXF.v)
                if self.debug:
                    self.dump(f"d1_{l}", W1.v, [128, NT])
                    self.dump(f"d2_{l}", W2.v, [128, NT])
                    self.dump(f"g1_{l}", RG1.v, [128, NT])
                    self.dump(f"g2_{l}", RG2.v, [128, NT])
                    self.dump(f"be_{l}", BE.v, [128, NB])
            self.fw.barrier()
            if os.environ.get("MOECUT", "9") == "0":
                return
            with contextlib.ExitStack() as st:
                sb = lambda n, s_, d: self.sb(st, n, s_, d)
                HB = [sb(f"HBm{i}", [128, D], BF16) for i in range(3)]
                for i in range(NT):
                    hb = HB[i % 3]
                    self.dma("sp", hb.v, self.h2_s[i * 128:(i + 1) * 128, :])
                    self.scatter(self.xs_s.v, hb.v, D1I[:, i:i + 1])
                    self.scatter(self.xs_s.v, hb.v, D2I[:, i:i + 1])
            self.fw.barrier()
            if os.environ.get("MOECUT", "9") == "1":
                return
            with contextlib.ExitStack() as st:
                sb = lambda n, s_, d: self.sb(st, n, s_, d)
                ps = lambda n, s_, d: self.ps(st, n, s_, d)
                WGU = [sb(f"WGU{i}", [128, 8, 2 * DE], BF16) for i in range(2)]
                WD = [sb(f"WD{i}", [128, 4, D], BF16) for i in range(2)]
                for t_ in WGU + WD:
                    t_.b.nowaw = True
                XB = [sb(f"XB{i}", [128, NSUB, D], BF16) for i in range(2)]
                XTB = [sb(f"XTB{i}", [128, 8, BLK], BF16) for i in range(2)]
                SGt = sb("SGt", [128, BLK], F32)
                UT = sb("UT", [128, 4, BLK], BF16)
                YB = [sb(f"YB{i}", [128, D], F32) for i in range(2)]
                TPX = ps("TPX", [128, 1024], BF16)
                PG = [ps(f"PG{i}", [128, BLK], F32) for i in range(2)]
                PU = [ps(f"PU{i}", [128, BLK], F32) for i in range(2)]
                PD = [ps(f"PD{i}", [128, 512], F32) for i in range(2)]
                wgu_t = self.wgub[l].v
                wd_t = self.wdb[l].v
                nblk = int(os.environ.get("MOEBLK", str(NB)))
                yi = 0
                for j in range(nblk):
                    wgu, wd, xb, xtb = WGU[j % 2], WD[j % 2], XB[j % 2], XTB[j % 2]
                    for c in range(8):
                        self.gather(wgu[:, c, :], wgu_t, IDXW[:, j, c:c + 1])
                    for c in range(4):
                        self.gather(wd[:, c, :], wd_t, IDXW[:, j, 8 + c:9 + c])
                    self.dma("sp", xb.v, self.xs_s[j * BLK:(j + 1) * BLK, :].rearrange("(s p) d -> p s d", p=128))
                    for sub in range(NSUB):
                        for k in range(8):
                            self.tr(TPX[:, k * 128:(k + 1) * 128], xb[:, sub, k * 128:(k + 1) * 128], self.identb.v)
                        self.cp("dve" if sub % 2 == 0 else "act", xtb[:, :, sub * 128:(sub + 1) * 128], TPX.v.rearrange("p (k t) -> p k t", k=8))
                    for fc in range(4):
                        pg, pu = PG[fc % 2], PU[fc % 2]
                        for k in range(8):
                            self.mm(pg.v, wgu[:, k, fc * 128:(fc + 1) * 128], xtb[:, k, :], start=(k == 0), stop=(k == 7))
                        for k in range(8):
                            self.mm(pu.v, wgu[:, k, DE + fc * 128:DE + (fc + 1) * 128], xtb[:, k, :], start=(k == 0), stop=(k == 7))
                        self.act(SGt.v, pg.v, AF.Silu)
                        self.tt("dve", UT[:, fc, :], SGt.v, pu.v, ALU.mult)
                    for sub in range(NSUB):
                        yb = YB[yi % 2]
                        yi += 1
                        for hf in range(2):
                            pd = PD[hf]
                            for fc in range(4):
                                self.mm(pd.v, UT[:, fc, sub * 128:(sub + 1) * 128], wd[:, fc, hf * 512:(hf + 1) * 512], start=(fc == 0), stop=(fc == 3))
                            self.cp("act" if hf == 0 else "dve", yb[:, hf * 512:(hf + 1) * 512], pd.v)
                        self.dma("act", self.ys_s[j * BLK + sub * 128:j * BLK + (sub + 1) * 128, :], yb.v)
            self.fw.barrier()
            if os.environ.get("MOECUT", "9") == "2":
                return
            with contextlib.ExitStack() as st:
                sb = lambda n, s_, d: self.sb(st, n, s_, d)
                GT2 = self.mod(5)
                Y1 = [sb(f"Y1{i}", [128, D], F32) for i in range(2)]
                Y2 = [sb(f"Y2{i}", [128, D], F32) for i in range(2)]
                XX = [sb(f"XX{i}", [128, D], F32) for i in range(2)]
                TT = [sb(f"TT{i}", [128, D], F32) for i in range(2)]
                for i in range(NT):
                    y1, y2, xx, tt_ = Y1[i % 2], Y2[i % 2], XX[i % 2], TT[i % 2]
                    self.gather(y1.v, self.ys_s.v, D1I[:, i:i + 1])
                    self.gather(y2.v, self.ys_s.v, D2I[:, i:i + 1])
                    self.dma("sp", xx.v, self.out_tile(i))
                    self.ts("dve", tt_.v, y1.v, RG1[:, i:i + 1], None, op0=ALU.mult)
                    self.stt("dve", tt_.v, y2.v, RG2[:, i:i + 1], tt_.v, ALU.mult, ALU.add)
                    if self.debug and l == 0:
                        self.dma("act", self.ydbg[i * 128:(i + 1) * 128, :], tt_.v)
                    self.tt("pool", tt_.v, tt_.v, GT2, ALU.mult)
                    self.tt("pool", xx.v, xx.v, tt_.v, ALU.add)
                    self.dma("sp", self.out_tile(i), xx.v)


_CACHE = {}


def _get_prog(stop_after=None, debug=False):
    key = (stop_after, debug)
    if key not in _CACHE:
        p = Prog(stop_after=stop_after, debug=debug)
        p.build()
        _CACHE[key] = p
    return _CACHE[key]


def make_in_map(inputs, b):
    f = lambda a: np.ascontiguousarray(a, dtype=np.float32)
    m = {
        "x": f(inputs["x"][b]),
        "c": f(np.asarray(inputs["c"][b]).reshape(8, 128).T),
        "pos": np.ascontiguousarray(np.asarray(inputs["positions"][b]).reshape(NT, 128).T.astype(np.int32)),
        "consts": CONST_NP,
    }
    for k in ("w_mod", "b_mod", "norm1_g", "norm2_g", "w_in", "ret_norm_g", "diff_q_g", "diff_k_g", "lam_q1", "lam_k1",
              "lam_q2", "lam_k2", "diff_sub_g", "gla_w_a2", "gla_b_a", "gla_norm_g", "w_out", "router_w", "router_b"):
        m[k] = f(inputs[k])
    for l in range(DEPTH):
        m[f"w_gate{l}"] = f(inputs["w_gate"][l]).reshape(NE * D, DE)
        m[f"w_up{l}"] = f(inputs["w_up"][l]).reshape(NE * D, DE)
        m[f"w_down{l}"] = f(inputs["w_down"][l]).reshape(NE * DE, D)
    return m


def kernel(**inputs):
    prog = _get_prog()
    shared = make_in_map(inputs, 0)
    in_maps = []
    for b in range(8):
        m = dict(shared)
        m["x"] = np.ascontiguousarray(inputs["x"][b], dtype=np.float32)
        m["c"] = np.ascontiguousarray(np.asarray(inputs["c"][b], dtype=np.float32).reshape(8, 128).T)
        m["pos"] = np.ascontiguousarray(np.asarray(inputs["positions"][b]).reshape(NT, 128).T.astype(np.int32))
        in_maps.append(m)
    res = run_bass_kernel_spmd(prog.nc, in_maps, core_ids=list(range(8)))
    return np.stack([np.asarray(r["out"], dtype=np.float32) for r in res.results], axis=0)
```

```python
import math
import contextlib
import numpy as np
import concourse.bass as bass
import concourse.mybir as mybir
from concourse.bass_utils import run_bass_kernel_spmd

F32 = mybir.dt.float32
BF16 = mybir.dt.bfloat16
I32 = mybir.dt.int32
ALU = mybir.AluOpType
AF = mybir.ActivationFunctionType
AX = mybir.AxisListType

S = 8192
D = 1024
NT = S // 128
DEPTH = 2
EPS = 1e-6
NE = 32
DE = 512
BLK = 256
NSUB = BLK // 128
NB = -(-(2 * S) // BLK) + NE
NPAD = NB * BLK
SEM_LIMIT = 30000
WINC = 3456


class Buf:
    __slots__ = ("name", "w", "r", "excl", "nowaw")

    def __init__(self, name="", excl=False):
        self.name = name
        self.w = {}
        self.r = {}
        self.excl = excl
        self.nowaw = False


class V:
    __slots__ = ("ap", "b")

    def __init__(self, ap, b):
        self.ap = ap
        self.b = b

    def __getitem__(self, k):
        return V(self.ap[k], self.b)

    def rearrange(self, s, **kw):
        return V(self.ap.rearrange(s, **kw), self.b)

    def unsqueeze(self, a):
        return V(self.ap.unsqueeze(a), self.b)

    def bc(self, shape):
        return V(self.ap.broadcast_to(list(shape)), self.b)

    def pbc(self, n):
        return V(self.ap.partition_broadcast(n), self.b)

    def bitcast(self, dt):
        return V(self.ap.bitcast(dt), self.b)


class Tile:
    def __init__(self, t, name):
        self.t = t
        self.b = Buf(name)

    def __getitem__(self, k):
        return V(self.t[k], self.b)

    @property
    def v(self):
        return self[:]


class EngS:
    def __init__(self, name, handle):
        self.name = name
        self.h = handle
        self.sem = None
        self.n = 0
        self.seen = {}
        self.dma_sems = []
        self.dma_cnt = []
        self.dma_i = 0


class FW:
    def __init__(self, nc, ndma=10, same=True):
        self.nc = nc
        self.nsem = 0
        self.same = same
        self.E = {"pe": EngS("pe", nc.tensor), "dve": EngS("dve", nc.vector),
                  "act": EngS("act", nc.scalar), "pool": EngS("pool", nc.gpsimd),
                  "sp": EngS("sp", nc.sync)}
        self.ndma = ndma
        self.owner = {}
        self.nwaits = 0
        self.nops = 0

    def new_sem(self):
        self.nsem += 1
        return self.nc.alloc_semaphore(f"fs{self.nsem}")

    def _tok(self, E):
        if E.sem is None or E.n >= SEM_LIMIT:
            E.sem = self.new_sem()
            E.n = 0
            self.owner[E.sem] = E.name
        E.n += 1
        return (E.sem, E.n)

    def _wait(self, E, deps):
        for sem, val in deps.items():
            if E.seen.get(sem, 0) >= val:
                continue
            E.h.wait_ge(sem, val)
            E.seen[sem] = val
            self.nwaits += 1

    def _deps(self, E, reads, writes, skip_own):
        deps = {}
        for b in reads:
            for s, v in b.w.items():
                if deps.get(s, 0) < v:
                    deps[s] = v
        for b in writes:
            for d in ((b.r,) if b.nowaw else (b.w, b.r)):
                for s, v in d.items():
                    if deps.get(s, 0) < v:
                        deps[s] = v
        if skip_own:
            for s in list(deps):
                if self.owner.get(s) == E.name:
                    del deps[s]
        self._wait(E, deps)

    def _commit(self, tok, reads, writes):
        s, v = tok
        for b in reads:
            if b.r.get(s, 0) < v:
                b.r[s] = v
        for b in writes:
            if b.w.get(s, 0) < v:
                b.w[s] = v
            b.r = {}

    def op(self, eng, fn, reads=(), writes=()):
        E = self.E[eng]
        if any(b.excl for b in reads):
            writes = list(writes) + [b for b in reads if b.excl]
            reads = [b for b in reads if not b.excl]
        self._deps(E, reads, writes, (eng == "pe") or (not self.same))
        ins = fn()
        tok = self._tok(E)
        ins.then_inc(tok[0], 1)
        self._commit(tok, reads, writes)
        self.nops += 1
        return ins

    def dma(self, q, fn, reads=(), writes=()):
        E = self.E[q]
        if not E.dma_sems:
            E.dma_sems = [self.new_sem() for _ in range(self.ndma)]
            E.dma_cnt = [0] * self.ndma
        k = E.dma_i % self.ndma
        E.dma_i += 1
        if E.dma_cnt[k] * 16 >= SEM_LIMIT:
            self._wait(E, {E.dma_sems[k]: E.dma_cnt[k] * 16})
            E.dma_sems[k] = self.new_sem()
            E.dma_cnt[k] = 0
        sem = E.dma_sems[k]
        if E.dma_cnt[k] > 0:
            self._wait(E, {sem: E.dma_cnt[k] * 16})
        self._deps(E, reads, writes, False)
        ins = fn()
        E.dma_cnt[k] += 1
        ins.then_inc(sem, 16)
        self._commit((sem, E.dma_cnt[k] * 16), reads, writes)
        self.nops += 1
        return ins

    def barrier(self):
        deps = {}
        for E in self.E.values():
            if E.sem is not None and E.n > 0:
                deps[E.sem] = E.n
            for s, c in zip(E.dma_sems, E.dma_cnt):
                if c > 0:
                    deps[s] = c * 16
        for E in self.E.values():
            self._wait(E, dict(deps))


def _const_tables():
    c = {}
    p = np.arange(128)
    c["ident"] = np.eye(128, dtype=np.float32)
    gam = 1.0 - 2.0 ** (-5.0 - np.arange(4))
    same = (p[:, None] // 64) == (p[None, :] // 64)
    dr = np.zeros((128, 4, 128), np.float32)
    for h in range(4):
        dr[:, h, :] = np.where(same, gam[h] ** np.abs(p[:, None] - p[None, :]), 0.0)
    c["dr"] = dr.reshape(128, 512)
    j = p % 64
    qd = np.stack([gam[h] ** (j + 1.0) for h in range(4)], 1)
    kd = np.stack([gam[h] ** (63.0 - j) for h in range(4)], 1) / 8.0
    c["qd"] = np.repeat(qd, 64, axis=1)
    c["kd0"] = np.repeat(kd * (p[:, None] < 64), 64, axis=1)
    c["kd1"] = np.repeat(kd * (p[:, None] >= 64), 64, axis=1)
    c["cd"] = np.broadcast_to(np.repeat(gam ** 64.0, 64)[None, :], (128, 256)).copy()
    ml = (same & (p[:, None] <= p[None, :])).astype(np.float32)
    mu = (same & (p[:, None] > p[None, :])).astype(np.float32)
    c["ml4"] = np.tile(ml, (1, 4))
    c["mu4"] = np.tile(mu, (1, 4))
    c["tri"] = ml
    c["chk"] = np.stack([(p < 64), (p >= 64)], 1).astype(np.float32)
    c["m0"] = (p < 64).astype(np.float32)[:, None]
    c["m1"] = (p >= 64).astype(np.float32)[:, None]
    c["ifr"] = np.broadcast_to((1.0 / (10000.0 ** (np.arange(32, dtype=np.float32) / 32)))[None, :], (128, 32)).copy()
    c["ifd"] = np.broadcast_to((1.0 / (500000.0 ** (np.arange(8, dtype=np.float32) / 8)))[None, :], (128, 8)).copy()
    c["lts"] = (p[:, None] < p[None, :]).astype(np.float32)
    c["ones"] = np.ones((128, 128), np.float32)
    c["jb"] = np.broadcast_to((np.arange(NB, dtype=np.float32) * BLK)[None, :], (128, NB)).copy()
    coff = np.zeros((128, 12), np.float32)
    for cc in range(12):
        coff[:, cc] = (cc if cc < 8 else cc - 8) * 128 + p
    c["coff"] = coff
    c["gid"] = np.broadcast_to((np.arange(32) // 4).astype(np.float32)[None, :], (128, 32)).copy()
    off = {}
    cur = 0
    arrs = []
    for k, v in c.items():
        v = np.ascontiguousarray(v, dtype=np.float32).reshape(128, -1)
        off[k] = (cur, v.shape[1])
        cur += v.shape[1]
        arrs.append(v)
    return np.concatenate(arrs, axis=1), off


CONST_NP, COFF = _const_tables()
NCONST = CONST_NP.shape[1]


class Prog:
    def __init__(self, stop_after=None, debug=False):
        self.nc = nc = bass.Bass("TRN2", target_bir_lowering=False)
        import os
        self.fw = FW(nc, same=(os.environ.get("SAMEENG", "1") == "1"))
        self.stop_after = stop_after
        self.debug = debug
        self.dram = {}

    def din(self, name, shape, dt=F32):
        t = self.nc.dram_tensor(name, list(shape), dt, kind="ExternalInput")
        T = Tile(t.ap(), name)
        self.dram[name] = T
        return T

    def dout(self, name, shape, dt=F32):
        t = self.nc.dram_tensor(name, list(shape), dt, kind="ExternalOutput")
        T = Tile(t.ap(), name)
        self.dram[name] = T
        return T

    def dint(self, name, shape, dt, dbg=False):
        kind = "ExternalOutput" if (dbg and self.debug) else "Internal"
        t = self.nc.dram_tensor(name, list(shape), dt, kind=kind)
        T = Tile(t.ap(), name)
        T.b.nowaw = True
        self.dram[name] = T
        return T

    def dump(self, name, v, shape, dt=F32):
        if not self.debug:
            return
        T = self.dout(self._uname("dbg_" + name), shape, dt)
        self.dma("sp", T.v, v)

    def _uname(self, name):
        self._uid = getattr(self, "_uid", 0) + 1
        return f"{name}_u{self._uid}"

    def sb(self, st, name, shape, dt):
        name = self._uname(name)
        return Tile(st.enter_context(self.nc.sbuf_tensor(name, list(shape), dt)), name)

    def ps(self, st, name, shape, dt):
        name = self._uname(name)
        T = Tile(st.enter_context(self.nc.psum_tensor(name, list(shape), dt)), name)
        T.b.excl = True
        return T

    def _eng(self, e):
        return {"dve": self.nc.vector, "pool": self.nc.gpsimd, "act": self.nc.scalar}[e]

    def _pe(self, e):
        import os
        if e == "pool" and os.environ.get("NOPOOL", "0") == "1":
            return "dve"
        return e

    def dma(self, q, out, in_, **kw):
        h = {"sp": self.nc.sync, "act": self.nc.scalar, "pool": self.nc.gpsimd}[q]
        return self.fw.dma(q, lambda: h.dma_start(out=out.ap, in_=in_.ap, **kw), reads=[in_.b], writes=[out.b])

    def gather(self, out, table, idx):
        return self.fw.dma("pool", lambda: self.nc.gpsimd.indirect_dma_start(
            out=out.ap, out_offset=None, in_=table.ap,
            in_offset=bass.IndirectOffsetOnAxis(ap=idx.ap, axis=0)), reads=[table.b, idx.b], writes=[out.b])

    def scatter(self, table, in_, idx):
        return self.fw.dma("pool", lambda: self.nc.gpsimd.indirect_dma_start(
            out=table.ap, out_offset=bass.IndirectOffsetOnAxis(ap=idx.ap, axis=0),
            in_=in_.ap, in_offset=None), reads=[in_.b, idx.b], writes=[table.b])

    def mm(self, out, lhsT, rhs, start=True, stop=True):
        return self.fw.op("pe", lambda: self.nc.tensor.matmul(out=out.ap, lhsT=lhsT.ap, rhs=rhs.ap, start=start, stop=stop),
                          reads=[lhsT.b, rhs.b], writes=[out.b])

    def tr(self, out, in_, ident):
        return self.fw.op("pe", lambda: self.nc.tensor.transpose(out=out.ap, in_=in_.ap, identity=ident.ap),
                          reads=[in_.b, ident.b], writes=[out.b])

    def act(self, out, in_, func, scale=1.0, bias=0.0, accum=None):
        rd = [in_.b]
        kw = {}
        if isinstance(scale, V):
            rd.append(scale.b)
            kw["scale"] = scale.ap
        else:
            kw["scale"] = float(scale)
        if isinstance(bias, V):
            rd.append(bias.b)
            kw["bias"] = bias.ap
        else:
            kw["bias"] = float(bias)
        wr = [out.b]
        if accum is not None:
            kw["accum_out"] = accum.ap
            wr.append(accum.b)
        return self.fw.op("act", lambda: self.nc.scalar.activation(out=out.ap, in_=in_.ap, func=func, **kw), reads=rd, writes=wr)

    def tt(self, e, out, a, b, op):
        e = self._pe(e)
        return self.fw.op(e, lambda: self._eng(e).tensor_tensor(out=out.ap, in0=a.ap, in1=b.ap, op=op), reads=[a.b, b.b], writes=[out.b])

    def ts(self, e, out, a, s1, s2=None, op0=ALU.mult, op1=None):
        e = self._pe(e)
        rd = [a.b]
        s1a = s1
        s2a = s2
        if isinstance(s1, V):
            rd.append(s1.b)
            s1a = s1.ap
        if isinstance(s2, V):
            rd.append(s2.b)
            s2a = s2.ap
        kw = {}
        if op1 is not None:
            kw["op1"] = op1
        return self.fw.op(e, lambda: self._eng(e).tensor_scalar(out=out.ap, in0=a.ap, scalar1=s1a, scalar2=s2a, op0=op0, **kw), reads=rd, writes=[out.b])

    def stt(self, e, out, a, s, b, op0, op1):
        e = self._pe(e)
        rd = [a.b, b.b]
        sa = s
        if isinstance(s, V):
            rd.append(s.b)
            sa = s.ap
        return self.fw.op(e, lambda: self._eng(e).scalar_tensor_tensor(out=out.ap, in0=a.ap, scalar=sa, in1=b.ap, op0=op0, op1=op1), reads=rd, writes=[out.b])

    def cp(self, e, out, in_):
        e = self._pe(e)
        if e == "act":
            return self.act(out, in_, AF.Copy)
        return self.fw.op(e, lambda: self._eng(e).tensor_copy(out=out.ap, in_=in_.ap), reads=[in_.b], writes=[out.b])

    def red(self, e, out, in_, op=ALU.add, axis=AX.X):
        return self.fw.op(e, lambda: self._eng(e).tensor_reduce(out=out.ap, in_=in_.ap, axis=axis, op=op), reads=[in_.b], writes=[out.b])

    def recip(self, out, in_):
        return self.fw.op("dve", lambda: self.nc.vector.reciprocal(out=out.ap, in_=in_.ap), reads=[in_.b], writes=[out.b])

    def memset(self, e, out, val):
        e = self._pe(e)
        return self.fw.op(e, lambda: self._eng(e).memset(out.ap, val), writes=[out.b])

    def rsqrt(self, out, in_, scale, eps):
        self.act(out, in_, AF.Ln, scale=scale, bias=self.epsv(eps, out))
        self.act(out, out, AF.Exp, scale=-0.5)

    def epsv(self, eps, like):
        n = like.ap.shape[0]
        return self.C_eps[0:n, 0:1] if eps == EPS else 0.0

    def build(self):
        nc = self.nc
        with contextlib.ExitStack() as gst:
            self.gst = gst
            self.declare_io()
            self.load_consts(gst)
            for l in range(DEPTH):
                self.layer(l)
                if self.stop_after is not None and self.stop_after[0] == l and self.stop_after[1] != "all":
                    break
            self.finish()
        return nc

    def declare_io(self):
        d = self.din
        self.x_in = d("x", [S, D])
        self.c_in = d("c", [128, 8])
        self.pos_in = d("pos", [128, NT], I32)
        self.consts_in = d("consts", [128, NCONST])
        self.w_mod = d("w_mod", [DEPTH, D, 6 * D])
        self.b_mod = d("b_mod", [DEPTH, 6 * D])
        self.norm1_g = d("norm1_g", [DEPTH, D])
        self.norm2_g = d("norm2_g", [DEPTH, D])
        self.w_in = d("w_in", [DEPTH, D, 3344])
        self.ret_norm_g = d("ret_norm_g", [DEPTH, 64])
        self.diff_q_g = d("diff_q_g", [DEPTH, 64])
        self.diff_k_g = d("diff_k_g", [DEPTH, 64])
        self.lam_q1 = d("lam_q1", [DEPTH, 64])
        self.lam_k1 = d("lam_k1", [DEPTH, 64])
        self.lam_q2 = d("lam_q2", [DEPTH, 64])
        self.lam_k2 = d("lam_k2", [DEPTH, 64])
        self.diff_sub_g = d("diff_sub_g", [DEPTH, 128])
        self.gla_w_a2 = d("gla_w_a2", [DEPTH, 16, 128])
        self.gla_b_a = d("gla_b_a", [DEPTH, 128])
        self.gla_norm_g = d("gla_norm_g", [DEPTH, 64])
        self.w_out = d("w_out", [DEPTH, D, D])
        self.router_w = d("router_w", [D, NE])
        self.router_b = d("router_b", [NE])
        self.w_gate = [d(f"w_gate{l}", [NE * D, DE]) for l in range(DEPTH)]
        self.w_up = [d(f"w_up{l}", [NE * D, DE]) for l in range(DEPTH)]
        self.w_down = [d(f"w_down{l}", [NE * DE, D]) for l in range(DEPTH)]
        self.wgub = [self.dint(f"wgub{l}", [NE * D, 2 * DE], BF16) for l in range(DEPTH)]
        self.wdb = [self.dint(f"wdb{l}", [NE * DE, D], BF16) for l in range(DEPTH)]
        self.bg = []
        self.out = self.dout("out", [S, D])
        self.out_b = [Buf(f"out{i}") for i in range(NT)]
        if self.debug:
            self.ydbg = self.dout("ydbg", [S, D])
        self.qT_s = self.dint("qT_s", [4, 128, S], BF16, dbg=True)
        self.kT_s = self.dint("kT_s", [4, 128, S], BF16, dbg=True)
        self.v_s = self.dint("v_s", [S, 512], BF16, dbg=True)
        self.mixT_s = self.dint("mixT_s", [D, S], BF16, dbg=True)
        self.h2_s = self.dint("h2_s", [S, D], BF16, dbg=True)
        self.xs_s = self.dint("xs_s", [NPAD, D], BF16)
        self.ys_s = self.dint("ys_s", [NPAD, D], F32)

    def load_consts(self, st):
        self.CT = self.sb(st, "consts", [128, NCONST], F32)
        self.dma("sp", self.CT.v, self.consts_in.v)
        self.C_eps = self.sb(st, "c_eps", [128, 1], F32)
        self.memset("dve", self.C_eps.v, EPS)
        self.C_npi = self.sb(st, "c_npi", [128, 1], F32)
        self.memset("dve", self.C_npi.v, -math.pi)
        self.C_one = self.sb(st, "c_one", [128, 1], F32)
        self.memset("dve", self.C_one.v, 1.0)
        posi = self.sb(st, "posi", [128, NT], I32)
        self.dma("sp", posi.v, self.pos_in.v)
        self.POSF = self.sb(st, "posf", [128, NT], F32)
        self.cp("dve", self.POSF.v, posi.v)
        self.identb = self.sb(st, "identb", [128, 128], BF16)
        self.cp("dve", self.identb.v, self.cst("ident"))
        self.rope_tables(st)
        self.onesb = self.sb(st, "onesb", [128, 128], BF16)
        self.cp("dve", self.onesb.v, self.cst("ones"))

    def rope_tables(self, gst):
        self.COS = self.sb(gst, "COS", [128, NT, 40], F32)
        self.SIN = self.sb(gst, "SIN", [128, NT, 40], F32)
        o, _ = COFF["ifr"]
        if40 = self.CT[:, o:o + 40]
        C1 = 6.28125
        C2 = 2.0 * math.pi - C1
        with contextlib.ExitStack() as st:
            ANG = self.sb(st, "ANG", [128, NT, 40], F32)
            KI = self.sb(st, "KI", [128, NT, 40], I32)
            KF = self.sb(st, "KF", [128, NT, 40], F32)
            MK = self.sb(st, "MK", [128, NT, 40], F32)
            self.tt("dve", ANG.v, self.POSF.v.unsqueeze(2).bc([128, NT, 40]), if40.unsqueeze(1).bc([128, NT, 40]), ALU.mult)
            self.ts("dve", KF.v, ANG.v, 1.0 / (2.0 * math.pi))
            self.cp("dve", KI.v, KF.v)
            self.cp("dve", KF.v, KI.v)
            self.stt("dve", ANG.v, KF.v, -C1, ANG.v, ALU.mult, ALU.add)
            self.stt("dve", ANG.v, KF.v, -C2, ANG.v, ALU.mult, ALU.add)
            self.ts("dve", MK.v, ANG.v, math.pi, None, op0=ALU.is_gt)
            self.stt("dve", self.SIN.v, MK.v, -2.0 * math.pi, ANG.v, ALU.mult, ALU.add)
            self.ts("dve", MK.v, self.SIN.v, -math.pi, None, op0=ALU.is_lt)
            self.stt("dve", self.SIN.v, MK.v, 2.0 * math.pi, self.SIN.v, ALU.mult, ALU.add)
            self.ts("dve", self.COS.v, self.SIN.v, 0.5 * math.pi, None, op0=ALU.add)
            self.ts("dve", MK.v, self.COS.v, math.pi, None, op0=ALU.is_gt)
            self.stt("dve", self.COS.v, MK.v, -2.0 * math.pi, self.COS.v, ALU.mult, ALU.add)
            self.act(self.SIN.v, self.SIN.v, AF.Sin)
            self.act(self.COS.v, self.COS.v, AF.Sin)
        self.fw.barrier()

    def out_tile(self, i):
        return V(self.out.t[i * 128:(i + 1) * 128, :], self.out_b[i])

    def cst(self, name):
        o, n = COFF[name]
        return self.CT[:, o:o + n]

    def finish(self):
        fw = self.fw
        fw.barrier()

    def layer(self, l):
        x_src = self.x_in if l == 0 else self.out
        sa = self.stop_after
        with contextlib.ExitStack() as lst:
            self.phase_mod(l, lst)
            if sa == (l, "mod"):
                return
            self.queue_weight_casts(l)
            self.phase1(l, x_src)
            self.run_bg(10 ** 9)
            self.fw.barrier()
            if sa == (l, "p1"):
                return
            self.phase2(l)
            self.fw.barrier()
            if sa == (l, "p2"):
                return
            with contextlib.ExitStack() as st3:
                self.LOG = self.sb(st3, f"log{l}", [128, NT, NE], F32)
                self.phase3(l, x_src)
                self.fw.barrier()
                if sa == (l, "p3"):
                    return
                self.phase_moe(l)
                self.fw.barrier()
                if sa == (l, "moe"):
                    return

    def queue_weight_casts(self, l):
        for e in range(NE):
            for src, dst, rows, c0, c1 in ((self.w_gate[l], self.wgub[l], D, 0, DE), (self.w_up[l], self.wgub[l], D, DE, 2 * DE),
                                           (self.w_down[l], self.wdb[l], DE, 0, D)):
                def task(src=src, dst=dst, rows=rows, e=e, c0=c0, c1=c1):
                    self.dma("pool", dst[e * rows:(e + 1) * rows, c0:c1], src[e * rows:(e + 1) * rows, :])
                self.bg.append(task)

    def run_bg(self, n):
        while n > 0 and self.bg:
            self.bg.pop(0)()
            n -= 1

    def phase_mod(self, l, lst):
        MODB = self.sb(lst, f"modb{l}", [128, 6 * D], F32)
        self.MODB = MODB
        with contextlib.ExitStack() as st:
            cT = self.sb(st, "cT", [128, 8], F32)
            self.dma("sp", cT.v, self.c_in.v)
            ca = self.sb(st, "ca", [128, 8], F32)
            self.act(ca.v, cT.v, AF.Silu)
            cb = self.sb(st, "cb", [128, 8, 128], F32)
            for k in range(8):
                self.cp("dve", cb[:, k, :], ca[:, k:k + 1].bc([128, 128]))
            bm = self.sb(st, "bm", [1, 6 * D], F32)
            self.dma("sp", bm.v, self.b_mod[l:l + 1, :])
            ones_f = self.cst("ones")
            wm = [self.sb(st, f"wm{i}", [128, 8, 512], F32) for i in range(2)]
            pm = [self.ps(st, f"pm{i}", [128, 512], F32) for i in range(2)]
            for n in range(12):
                w = wm[n % 2]
                p = pm[n % 2]
                self.dma("sp" if n % 2 == 0 else "act", w.v,
                         self.w_mod[l, :, n * 512:(n + 1) * 512].rearrange("(k p) n -> p k n", p=128))
                for k in range(8):
                    self.mm(p.v, cb[:, k, :], w[:, k, :], start=(k == 0), stop=False)
                self.mm(p.v, ones_f[0:1, :], bm[0:1, n * 512:(n + 1) * 512], start=False, stop=True)
                self.cp("dve" if n % 2 == 0 else "act", MODB[:, n * 512:(n + 1) * 512], p.v)
        self.dump(f"mod{l}", MODB[0:2, :], [2, 6 * D])
        self.fw.barrier()

    def mod(self, j):
        return self.MODB[:, j * D:(j + 1) * D]

    def phase1(self, l, x_src):
        nc = self.nc
        with contextlib.ExitStack() as st:
            sb = lambda n, s, d: self.sb(st, n, s, d)
            ps = lambda n, s, d: self.ps(st, n, s, d)
            WIN = sb("WIN", [128, 8, WINC], BF16)
            WIN.b.nowaw = True
            for k in range(8):
                rows = self.w_in[l, k * 128:(k + 1) * 128, :]
                self.dma("pool", WIN[:, k, 0:1536], rows[:, 0:1536])
                self.dma("pool", WIN[:, k, 1536:3072], rows[:, 1536:3072])
                self.dma("pool", WIN[:, k, 3200:3456], rows[:, 3088:3344])
            import os
            setup = int(os.environ.get("P1SETUP", "9"))
            if setup < 1:
                return
            wga = sb("wga", [128, 8, 16], F32)
            self.dma("sp", wga.v, self.w_in[l, :, 3072:3088].rearrange("(k p) r -> p k r", p=128))
            wa2 = sb("wa2", [16, 128], F32)
            self.dma("sp", wa2.v, self.gla_w_a2[l])
            identf = self.cst("ident")
            PJ = [ps(f"PJ{i}", [128, 512], F32) for i in range(2)]
            TP = [ps(f"TP{i}", [128, 1024], BF16) for i in range(2)]
            SC = ps("SC", [128, 512], F32)
            SC2 = ps("SC2", [128, 512], F32)
            OT = ps("OT", [64, 512], F32)
            MS = ps("MS", [128, 512], F32)
            gaT = sb("gaT", [16, 128], F32)
            for k in range(8):
                self.tr(SC[0:16, 0:128], wga[:, k, :], identf)
                self.cp("dve", gaT.v, SC[0:16, 0:128])
                self.mm(SC2[:, 0:128], gaT.v, wa2.v)
                self.cp("dve", WIN[:, k, 3072:3200], SC2[:, 0:128])
            if setup < 2:
                return
            G1 = sb("G1", [128, D], F32)
            self.dma("sp", G1.v, self.norm1_g[l].pbc(128))
            self.stt("dve", G1.v, self.mod(1), 1.0, G1.v, ALU.add, ALU.mult)
            SH1 = self.mod(0)
            GQ = sb("GQ", [128, 64], F32)
            GK = sb("GK", [128, 64], F32)
            self.dma("sp", GQ.v, self.diff_q_g[l].pbc(128))
            self.dma("sp", GK.v, self.diff_k_g[l].pbc(128))
            self.ts("dve", GQ.v, GQ.v, 0.125)
            BA = sb("BA", [128, 128], F32)
            self.dma("sp", BA.v, self.gla_b_a[l].pbc(128))
            GRN = sb("GRN", [64, 1], F32)
            GGN = sb("GGN", [64, 1], F32)
            self.dma("sp", GRN.v, self.ret_norm_g[l].unsqueeze(1))
            self.dma("sp", GGN.v, self.gla_norm_g[l].unsqueeze(1))
            onesf = self.cst("ones")
            if setup < 3:
                return
            SR = sb("SR", [64, 256], F32)
            SRb = sb("SRb", [64, 256], BF16)
            SG = sb("SG", [32, 256], F32)
            SGb = sb("SGb", [32, 256], BF16)
            for t_ in (SR, SRb, SG, SGb):
                self.memset("dve", t_.v, 0.0)
            XT = [sb(f"XT{i}", [128, D], F32) for i in range(2)]
            ssq = sb("ssq", [128, 1], F32)
            HF = sb("HF", [128, D], F32)
            HB = sb("HB", [128, D], BF16)
            junk = HB
            HT = sb("HT", [128, 8, 128], BF16)
            R1 = sb("R1", [128, 8, 32], F32)
            R2 = sb("R2", [128, 8, 32], F32)
            ROT = sb("ROT", [128, 8, 64], F32)
            QB = sb("QB", [128, 256], BF16)
            QDB = sb("QDB", [128, 256], BF16)
            KB = sb("KB", [128, 256], BF16)
            KD0 = sb("KD0", [128, 256], BF16)
            KD1 = sb("KD1", [128, 256], BF16)
            VR = sb("VR", [128, 256], BF16)
            SGR = sb("SGR", [128, 256], BF16)
            RT = sb("RT", [64, 12, 128], BF16)
            SGT = sb("SGT", [64, 8, 128], BF16)
            DSQ = sb("DSQ", [128, 512], F32)
            GS2 = sb("GS2", [128, 512], F32)
            DSS = sb("DSS", [128, 8], F32)
            DXN = sb("DXN", [128, 8, 64], F32)
            DQB = sb("DQB", [128, 512], BF16)
            DKB = sb("DKB", [128, 512], BF16)
            DT = sb("DT", [128, 8, 128], BF16)
            DVB = sb("DVB", [128, 512], BF16)
            D1 = sb("D1", [128, 8, 8], F32)
            D2 = sb("D2", [128, 8, 8], F32)
            AT = sb("AT", [128, 512], BF16)
            ZT = sb("ZT", [128, 128], F32)
            SP = sb("SP", [128, 128], F32)
            EPt = sb("EPt", [128, 128], F32)
            EMt = sb("EMt", [128, 128], F32)
            GQP = sb("GQP", [128, 128], BF16)
            GQM = sb("GQM", [128, 128], BF16)
            GKP = sb("GKP", [128, 128], BF16)
            GKM = sb("GKM", [128, 128], BF16)
            GKM0 = sb("GKM0", [128, 128], BF16)
            GKM1 = sb("GKM1", [128, 128], BF16)
            GVB = sb("GVB", [128, 256], BF16)
            SGG = sb("SGG", [128, 256], BF16)
            GT = sb("GT", [32, 16, 128], BF16)
            EGL = sb("EGL", [32, 8], F32)
            TMP1 = sb("TMP1", [128, 512], F32)
            OY = sb("OY", [64, 512], F32)
            MXR = sb("MXR", [64, 512], BF16)
            MXG = sb("MXG", [64, 512], BF16)
            KVT = sb("KVT", [64, 256], F32)

            ifr = self.cst("ifr")
            ifd = self.cst("ifd")
            TWO_PI = 2.0 * math.pi

            def post_norm(OTv, G, SGTv, MX):
                OSQ_ = TMP1[0:64, :]
                ORS_ = GS2[0:64, :]
                self.act(OSQ_, OTv, AF.Square)
                self.mm(MS[0:64, :], onesf[0:64, 0:64], OSQ_)
                self.rsqrt(ORS_, MS[0:64, :], 1.0 / 64, EPS)
                self.tt("dve", OY.v, OTv, ORS_, ALU.mult)
                self.stt("dve", MX.v, OY.v, G[:, 0:1], SGTv, ALU.mult, ALU.mult)

            import os
            cut = int(os.environ.get("P1CUT", "9"))
            ntl = int(os.environ.get("P1TILES", str(NT)))
            TPA, TPB = TP[0], TP[1]
            PROJ = [sb(f"PROJ{i}", [128, WINC], F32) for i in range(2)]

            class _PV:
                def __init__(self, tile, off, w):
                    self.tile, self.off, self.w = tile, off, w

                def __getitem__(self, k):
                    rows, cols = k
                    return self.tile[rows, self.off + cols.start:self.off + cols.stop]

                @property
                def v(self):
                    return self.tile[:, self.off:self.off + self.w]

            def stageA(i):
                xt = XT[i % 2]
                self.dma("sp", xt.v, x_src[i * 128:(i + 1) * 128, :] if l == 0 else self.out_tile(i))
                self.act(junk.v, xt.v, AF.Square, accum=ssq.v)
                self.rsqrt(ssq.v, ssq.v, 1.0 / D, EPS)
                self.stt("dve", HF.v, xt.v, ssq[:, 0:1], G1.v, ALU.mult, ALU.mult)
                self.tt("pool", HB.v, HF.v, SH1, ALU.add)
                tp = TPA
                for k in range(8):
                    self.tr(tp[:, k * 128:(k + 1) * 128], HB[:, k * 128:(k + 1) * 128], self.identb.v)
                self.cp("act", HT[:, 0:4, :], tp[:, 0:512].rearrange("p (k t) -> p k t", k=4))
                self.cp("dve", HT[:, 4:8, :], tp[:, 512:1024].rearrange("p (k t) -> p k t", k=4))
                yield
                P_ = PROJ[i % 2]
                for n, width in ((0, 512), (1, 512), (2, 512), (3, 512), (4, 512), (6, 384), (5, 512)):
                    p = PJ[n % 2]
                    for k in range(8):
                        self.mm(p[:, 0:width], HT[:, k, :], WIN[:, k, n * 512:n * 512 + width], start=(k == 0), stop=(k == 7))
                    self.cp("act" if n % 2 == 0 else "dve", P_[:, n * 512:n * 512 + width], p[:, 0:width])
                    yield

            def _hdr(i):
                def proj(n, width):
                    return _PV(PROJ[i % 2], n * 512, width)
                cosr = self.COS[:, i, 0:32].unsqueeze(1).bc([128, 8, 32])
                sinr = self.SIN[:, i, 0:32].unsqueeze(1).bc([128, 8, 32])
                cosd = self.COS[:, i, 32:40].unsqueeze(1).bc([128, 8, 8])
                sind = self.SIN[:, i, 32:40].unsqueeze(1).bc([128, 8, 8])
                return proj, cosr, sinr, cosd, sind

            def stageRG(i):
                proj, cosr, sinr, cosd, sind = _hdr(i)
                yield
                p0 = proj(0, 512)
                pv = p0.v.rearrange("p (g h w) -> p g h w", g=8, h=2)
                X1 = pv[:, :, 0, :]
                X2 = pv[:, :, 1, :]
                rv_ = ROT.v.rearrange("p g (h w) -> p g h w", h=2)
                self.tt("dve", R1.v, X1, cosr, ALU.mult)
                yield
                self.tt("dve", R2.v, X2, sinr, ALU.mult)
                yield
                self.tt("pool", rv_[:, :, 0, :], R1.v, R2.v, ALU.subtract)
                yield
                self.tt("dve", R1.v, X1, sinr, ALU.mult)
                yield
                self.tt("dve", R2.v, X2, cosr, ALU.mult)
                yield
                self.tt("pool", rv_[:, :, 1, :], R1.v, R2.v, ALU.add)
                yield
                rq = ROT[:, 0:4, :].rearrange("p g w -> p (g w)")
                rk = ROT[:, 4:8, :].rearrange("p g w -> p (g w)")
                self.cp("act", QB.v, rq)
                yield
                self.tt("pool", QDB.v, rq, self.cst("qd"), ALU.mult)
                yield
                self.act(KB.v, rk, AF.Copy, scale=0.125)
                yield
                self.tt("pool", KD0.v, rk, self.cst("kd0"), ALU.mult)
                yield
                self.tt("pool", KD1.v, rk, self.cst("kd1"), ALU.mult)
                yield
                if cut < 2:
                    return
                p1 = proj(1, 512)
                self.cp("act", VR.v, p1[:, 0:256])
                yield
                self.act(SGR.v, p1[:, 256:512], AF.Silu)
                yield
                if cut == 2 and os.environ.get("SUB", "9") == "0":
                    return
                tp = TPB
                for h in range(4):
                    self.tr(tp[0:64, h * 128:(h + 1) * 128], QB[:, h * 64:(h + 1) * 64], self.identb.v)
                    self.tr(tp[0:64, (4 + h) * 128:(5 + h) * 128], QDB[:, h * 64:(h + 1) * 64], self.identb.v)
                yield
                self.cp("dve", RT[:, 0:8, :], tp[0:64, :].rearrange("p (k t) -> p k t", k=8))
                yield
                if cut == 2 and os.environ.get("SUB", "9") == "1":
                    return
                tp = TPB
                for h in range(4):
                    self.tr(tp[0:64, h * 128:(h + 1) * 128], KB[:, h * 64:(h + 1) * 64], self.identb.v)
                    self.tr(tp[0:64, (4 + h) * 128:(5 + h) * 128], SGR[:, h * 64:(h + 1) * 64], self.identb.v)
                yield
                self.cp(os.environ.get("GRP2", "act"), RT[:, 8:12, :], tp[0:64, 0:512].rearrange("p (k t) -> p k t", k=4))
                yield
                self.cp("dve", SGT[:, 0:4, :], tp[0:64, 512:1024].rearrange("p (k t) -> p k t", k=4))
                yield
                if cut < 3:
                    return
                for h in range(4):
                    self.mm(SC[:, h * 128:(h + 1) * 128], RT[:, 8 + h, :], RT[:, h, :])
                yield
                self.tt("dve", AT.v, SC.v, self.cst("dr"), ALU.mult)
                yield
                for h in range(4):
                    o = OT[:, h * 128:(h + 1) * 128]
                    self.mm(o, VR[:, h * 64:(h + 1) * 64], AT[:, h * 128:(h + 1) * 128], start=(h == 0), stop=False)
                    self.mm(OT[:, h * 128:h * 128 + 64], SRb[:, h * 64:(h + 1) * 64], RT[:, 4 + h, 0:64], start=False, stop=False)
                yield
                for h in range(4):
                    self.mm(MS[0:64, h * 64:(h + 1) * 64], KD0[:, h * 64:(h + 1) * 64], VR[:, h * 64:(h + 1) * 64])
                    self.mm(MS[0:64, 256 + h * 64:256 + (h + 1) * 64], KD1[:, h * 64:(h + 1) * 64], VR[:, h * 64:(h + 1) * 64])
                yield
                self.tt("dve", KVT.v, SR.v, self.cst("cd")[0:64, :], ALU.mult)
                yield
                self.tt("dve", SR.v, KVT.v, MS[0:64, 0:256], ALU.add)
                yield
                self.cp("act", SRb.v, SR.v)
                yield
                for h in range(4):
                    self.mm(OT[:, h * 128 + 64:(h + 1) * 128], SRb[:, h * 64:(h + 1) * 64], RT[:, 4 + h, 64:128], start=False, stop=True)
                yield
                self.tt("dve", KVT.v, SR.v, self.cst("cd")[0:64, :], ALU.mult)
                yield
                self.tt("dve", SR.v, KVT.v, MS[0:64, 256:512], ALU.add)
                yield
                self.cp("act", SRb.v, SR.v)
                yield
                post_norm(OT.v, GRN, SGT[:, 0:4, :].rearrange("p k t -> p (k t)"), MXR)
                yield
                self.dma("act", self.mixT_s[0:256, i * 128:(i + 1) * 128].rearrange("(h p) t -> p h t", p=64),
                         MXR.v.rearrange("p (h t) -> p h t", h=4))
                yield

                if cut < 4:
                    return
                p6 = proj(6, 384)
                self.tt("dve", ZT.v, p6[:, 0:128], BA.v, ALU.add)
                yield
                self.act(SGG.v, p6[:, 128:384], AF.Silu)
                yield
                self.act(SP.v, ZT.v, AF.Exp, scale=-1.0)
                yield
                self.act(SP.v, SP.v, AF.Ln, bias=self.C_one[:, 0:1])
                yield
                self.mm(SC2[:, 0:128], self.cst("tri"), SP.v)
                yield
                self.act(EPt.v, SC2[:, 0:128], AF.Exp, scale=-1.0 / 16)
                yield
                self.act(EMt.v, SC2[:, 0:128], AF.Exp, scale=1.0 / 16)
                yield
                for h in range(4):
                    self.mm(MS[0:32, 2 * h:2 * h + 2], SP[:, h * 32:(h + 1) * 32], self.cst("chk"))
                yield
                self.act(EGL.v, MS[0:32, 0:8], AF.Exp, scale=-1.0 / 16)
                yield
                p5 = proj(5, 512)
                qs = 32.0 ** -0.5
                self.stt("dve", GQP.v, p5[:, 0:128], qs, EPt.v, ALU.mult, ALU.mult)
                yield
                self.stt("dve", GQM.v, p5[:, 0:128], qs, EMt.v, ALU.mult, ALU.mult)
                yield
                self.tt("dve", GKP.v, p5[:, 128:256], EPt.v, ALU.mult)
                yield
                self.tt("dve", GKM.v, p5[:, 128:256], EMt.v, ALU.mult)
                yield
                self.cp("act", GVB.v, p5[:, 256:512])
                yield
                self.ts("pool", GKM0.v, GKM.v, self.cst("m0")[:, 0:1], None, op0=ALU.mult)
                yield
                self.ts("pool", GKM1.v, GKM.v, self.cst("m1")[:, 0:1], None, op0=ALU.mult)
                yield
                tp = TPB
                for h in range(4):
                    self.tr(tp[0:32, h * 128:(h + 1) * 128], GQP[:, h * 32:(h + 1) * 32], self.identb.v)
                    self.tr(tp[0:32, (4 + h) * 128:(5 + h) * 128], GQM[:, h * 32:(h + 1) * 32], self.identb.v)
                yield
                self.cp("act", GT[:, 0:8, :], tp[0:32, :].rearrange("p (k t) -> p k t", k=8))
                yield
                tp = TPB
                for h in range(4):
                    self.tr(tp[0:32, h * 128:(h + 1) * 128], GKM[:, h * 32:(h + 1) * 32], self.identb.v)
                    self.tr(tp[0:32, (4 + h) * 128:(5 + h) * 128], GKP[:, h * 32:(h + 1) * 32], self.identb.v)
                yield
                self.cp("dve", GT[:, 8:16, :], tp[0:32, :].rearrange("p (k t) -> p k t", k=8))
                yield
                tp = TPB
                for h in range(4):
                    self.tr(tp[0:64, h * 128:(h + 1) * 128], SGG[:, h * 64:(h + 1) * 64], self.identb.v)
                yield
                self.cp("act", SGT[:, 4:8, :], tp[0:64, 0:512].rearrange("p (k t) -> p k t", k=4))
                yield
                for h in range(4):
                    self.mm(SC[:, h * 128:(h + 1) * 128], GT[:, 8 + h, :], GT[:, h, :])
                    self.mm(SC2[:, h * 128:(h + 1) * 128], GT[:, 12 + h, :], GT[:, 4 + h, :])
                yield
                self.tt("dve", TMP1.v, SC.v, self.cst("ml4"), ALU.mult)
                yield
                self.tt("dve", GS2.v, SC2.v, self.cst("mu4"), ALU.mult)
                yield
                self.tt("pool", AT.v, TMP1.v, GS2.v, ALU.add)
                yield
                for h in range(4):
                    self.mm(OT[:, h * 128:(h + 1) * 128], GVB[:, h * 64:(h + 1) * 64], AT[:, h * 128:(h + 1) * 128], start=(h == 0), stop=False)
                    self.mm(OT[:, h * 128:h * 128 + 64], SGb[:, h * 64:(h + 1) * 64], GT[:, h, 0:64], start=False, stop=False)
                yield
                for h in range(4):
                    self.mm(MS[0:32, h * 64:(h + 1) * 64], GKM0[:, h * 32:(h + 1) * 32], GVB[:, h * 64:(h + 1) * 64])
                    self.mm(MS[0:32, 256 + h * 64:256 + (h + 1) * 64], GKM1[:, h * 32:(h + 1) * 32], GVB[:, h * 64:(h + 1) * 64])
                yield
                eg = EGL.v.rearrange("p (h c) -> p h c", c=2)
                sgv = SG.v.rearrange("p (h w) -> p h w", h=4)
                kv3 = KVT[0:32, :].rearrange("p (h w) -> p h w", h=4)
                self.tt("dve", KVT[0:32, :], SG.v, MS[0:32, 0:256], ALU.add)
                yield
                self.tt("dve", sgv, kv3, eg[:, :, 0:1].bc([32, 4, 64]), ALU.mult)
                yield
                self.cp("act", SGb.v, SG.v)
                yield
                for h in range(4):
                    self.mm(OT[:, h * 128 + 64:(h + 1) * 128], SGb[:, h * 64:(h + 1) * 64], GT[:, h, 64:128], start=False, stop=True)
                yield
                self.tt("dve", KVT[0:32, :], SG.v, MS[0:32, 256:512], ALU.add)
                yield
                self.tt("dve", sgv, kv3, eg[:, :, 1:2].bc([32, 4, 64]), ALU.mult)
                yield
                self.cp("act", SGb.v, SG.v)
                yield
                post_norm(OT.v, GGN, SGT[:, 4:8, :].rearrange("p k t -> p (k t)"), MXG)
                yield
                self.dma("act", self.mixT_s[768:1024, i * 128:(i + 1) * 128].rearrange("(h p) t -> p h t", p=64),
                         MXG.v.rearrange("p (h t) -> p h t", h=4))
                yield


            def stageD(i):
                proj, cosr, sinr, cosd, sind = _hdr(i)
                yield
                def dqk(n, G, OUTB, dst):
                    pq = proj(n, 512)
                    self.act(DSQ.v, pq.v, AF.Square)
                    yield
                    self.red("dve", DSS.v, DSQ.v.rearrange("p (g w) -> p g w", g=8))
                    yield
                    self.rsqrt(DSS.v, DSS.v, 1.0 / 64, EPS)
                    yield
                    self.tt("dve", DXN.v, pq.v.rearrange("p (g w) -> p g w", g=8), DSS.v.unsqueeze(2).bc([128, 8, 64]), ALU.mult)
                    yield
                    self.tt("pool", DXN.v, DXN.v, G.v.unsqueeze(1).bc([128, 8, 64]), ALU.mult)
                    yield
                    ob = OUTB.v.rearrange("p (g w) -> p g w", g=8)
                    self.cp("act", ob[:, :, 16:64], DXN[:, :, 16:64])
                    yield
                    x1 = DXN[:, :, 0:8]
                    x2 = DXN[:, :, 8:16]
                    self.tt("dve", D1.v, x1, cosd, ALU.mult)
                    yield
                    self.tt("pool", D2.v, x2, sind, ALU.mult)
                    yield
                    self.tt("dve", ob[:, :, 0:8], D1.v, D2.v, ALU.subtract)
                    yield
                    self.tt("dve", D1.v, x1, sind, ALU.mult)
                    yield
                    self.tt("pool", D2.v, x2, cosd, ALU.mult)
                    yield
                    self.tt("dve", ob[:, :, 8:16], D1.v, D2.v, ALU.add)
                    yield
                    tp = TPA
                    for h in range(4):
                        self.tr(tp[:, h * 128:(h + 1) * 128], OUTB[:, h * 128:(h + 1) * 128], self.identb.v)
                    dtv = DT[:, 0:4, :] if n == 2 else DT[:, 4:8, :]
                    self.cp("act" if n == 2 else "dve", dtv, tp[:, 0:512].rearrange("p (k t) -> p k t", k=4))
                    yield
                    self.dma("sp", dst[:, :, i * 128:(i + 1) * 128].rearrange("h p t -> p h t"), dtv)
                    yield
                yield from dqk(2, GQ, DQB, self.qT_s)
                yield from dqk(3, GK, DKB, self.kT_s)
                p4 = proj(4, 512)
                self.cp("act", DVB.v, p4.v)
                yield
                self.dma("act", self.v_s[i * 128:(i + 1) * 128, :], DVB.v)
                yield

                if cut < 5:
                    return

            for _ in stageA(0):
                pass
            for i in range(ntl):
                self.run_bg(2)
                gens = [stageRG(i), stageD(i)]
                if i + 1 < ntl:
                    gens.append(stageA(i + 1))
                while gens:
                    for g in list(gens):
                        try:
                            next(g)
                        except StopIteration:
                            gens.remove(g)


    def phase2(self, l):
        lam_init = 0.8 - 0.6 * math.exp(-0.3 * l)
        with contextlib.ExitStack() as st:
            sb = lambda n, s, d: self.sb(st, n, s, d)
            ps = lambda n, s, d: self.ps(st, n, s, d)
            KT = sb("KT", [128, 2, S], BF16)
            KT.b.nowaw = True
            VV = sb("VV", [128, NT, 256], BF16)
            VV.b.nowaw = True
            vsrc = self.v_s.v.rearrange("(i p) c -> p i c", p=128)

            def load_group(hg):
                for hh in range(2):
                    for j in range(4):
                        self.dma("sp" if (hh + j) % 2 == 0 else "act", KT[:, hh, j * 2048:(j + 1) * 2048], self.kT_s[2 * hg + hh, :, j * 2048:(j + 1) * 2048])
                for j in range(8):
                    self.dma("sp" if j % 2 == 0 else "act", VV[:, j * 8:(j + 1) * 8, :], vsrc[:, j * 8:(j + 1) * 8, hg * 256:(hg + 1) * 256])
            lq = sb("lq", [1, 4, 64], F32)
            for j, src in enumerate((self.lam_q1, self.lam_k1, self.lam_q2, self.lam_k2)):
                self.dma("sp", lq[:, j, :], src[l:l + 1, :])
            lp = sb("lp", [1, 2, 64], F32)
            self.tt("dve", lp[:, 0, :], lq[:, 0, :], lq[:, 1, :], ALU.mult)
            self.tt("dve", lp[:, 1, :], lq[:, 2, :], lq[:, 3, :], ALU.mult)
            ls = sb("ls", [1, 2], F32)
            self.red("dve", ls.v, lp.v)
            self.act(ls.v, ls.v, AF.Exp)
            lam1 = sb("lam1", [1, 1], F32)
            self.tt("dve", lam1.v, ls[:, 0:1], ls[:, 1:2], ALU.subtract)
            self.ts("dve", lam1.v, lam1.v, lam_init, -1.0, op0=ALU.add, op1=ALU.mult)
            NSB = 4
            SPS = [ps(f"SPS{i}", [128, 512], F32) for i in range(NSB)]
            OP = [ps(f"OP{i}", [128, 512], F32) for i in range(2)]
            MSP = ps("MSP", [128, 512], F32)
            MSP2 = ps("MSP2", [128, 512], F32)
            NLAM = sb("NLAM", [128, 1], F32)
            self.mm(MSP[:, 0:1], self.cst("ones")[0:1, :], lam1.v)
            self.cp("dve", NLAM.v, MSP[:, 0:1])
            SUBG = sb("SUBG", [128, 1], F32)
            self.dma("sp", SUBG.v, self.diff_sub_g[l].unsqueeze(1))
            self.ts("dve", SUBG.v, SUBG.v, 1.0 - lam_init)
            QT = [sb(f"QT{i}", [128, 512], BF16) for i in range(2)]
            PT = [sb(f"PT{i}", [128, 512], BF16) for i in range(4)]
            ACC = [[sb(f"ACC{p_}{c}", [128, 512], F32) for c in range(2)] for p_ in range(2)]
            OS = [sb(f"OS{c}", [128, 512], F32) for c in range(2)]
            R0 = sb("R0", [128, 512], F32)
            R1 = sb("R1", [128, 512], F32)
            ACB = [sb(f"ACB{c}", [128, 512], BF16) for c in range(2)]
            T0 = sb("T0", [128, 512], F32)
            T1 = sb("T1", [128, 512], F32)
            OSQ = sb("OSQ2", [128, 512], BF16)
            ORS = sb("ORS2", [128, 512], F32)
            OB = [sb(f"OB{i}", [128, 512], BF16) for i in range(2)]
            onesf = self.cst("ones")
            import os
            nqt = int(os.environ.get("P2QT", str(S // 512)))
            groups = [(a, b_, c_) for a in range(2) for b_ in range(nqt) for c_ in range(2)]
            units = []
            for gi, (hg, qt, hh) in enumerate(groups):
                nk = 4 * qt + 4
                for kt in range(nk):
                    for c in range(2):
                        units.append((gi, c, kt, nk))
            LOOK = 2
            qloaded = set()

            def load_q(gi):
                if gi >= len(groups) or gi in qloaded:
                    return
                qloaded.add(gi)
                hg, qt, hh = groups[gi]
                if qt == 0 and hh == 0:
                    load_group(hg)
                self.dma("sp", QT[gi % 2].v, self.qT_s[2 * hg + hh, :, qt * 512:(qt + 1) * 512])

            def c0_of(gi, kt):
                qt = groups[gi][1]
                m = kt - 4 * qt
                return (128 * m if m > 0 else 0), m

            def issue_qk(u):
                gi, c, kt, nk = units[u]
                load_q(gi)
                hh = groups[gi][2]
                c0, m = c0_of(gi, kt)
                self.mm(SPS[u % NSB][:, c0:512], KT[64 * c:64 * c + 64, hh, kt * 128:(kt + 1) * 128], QT[gi % 2][64 * c:64 * c + 64, c0:512])

            def finish(gi):
                hg, qt, hh = groups[gi]
                h = 2 * hg + hh
                acc = ACC[gi % 2]
                self.cp("act", OS[0].v, OP[0].v)
                self.cp("act", OS[1].v, OP[1].v)
                self.cp("dve", ACB[0].v, acc[0].v)
                self.cp("dve", ACB[1].v, acc[1].v)
                self.mm(MSP.v, self.onesb.v, ACB[0].v)
                self.mm(MSP2.v, self.onesb.v, ACB[1].v)
                self.act(R0.v, MSP.v, AF.Ln)
                self.act(R0.v, R0.v, AF.Exp, scale=-1.0)
                self.act(R1.v, MSP2.v, AF.Ln)
                self.act(R1.v, R1.v, AF.Exp, scale=-1.0)
                self.tt("dve", T0.v, OS[0].v, R0.v, ALU.mult)
                self.tt("dve", T1.v, OS[1].v, R1.v, ALU.mult)
                self.stt("dve", T0.v, T1.v, NLAM[:, 0:1], T0.v, ALU.mult, ALU.add)
                self.act(OSQ.v, T0.v, AF.Square)
                self.mm(MSP.v, self.onesb.v, OSQ.v)
                self.act(ORS.v, MSP.v, AF.Ln, scale=1.0 / 128, bias=self.C_eps[:, 0:1])
                self.act(ORS.v, ORS.v, AF.Exp, scale=-0.5)
                ob = OB[gi % 2]
                self.stt("dve", ob.v, T0.v, SUBG[:, 0:1], ORS.v, ALU.mult, ALU.mult)
                self.dma("act", self.mixT_s[256 + 128 * h:256 + 128 * (h + 1), qt * 512:(qt + 1) * 512], ob.v)

            def hg_of(u):
                return groups[units[u][0]][0]

            for u, (gi, c, kt, nk) in enumerate(units):
                if u == 0 or hg_of(u - 1) != hg_of(u):
                    for u2 in range(u, min(u + LOOK, len(units))):
                        if hg_of(u2) == hg_of(u):
                            issue_qk(u2)
                if u % 2 == 0:
                    for u2 in (u + LOOK, u + LOOK + 1):
                        if u2 < len(units) and hg_of(u2) == hg_of(u):
                            issue_qk(u2)
                hh = groups[gi][2]
                c0, m = c0_of(gi, kt)
                pt = PT[u % 4]
                if kt == 0 and c == 0 and gi + 1 < len(groups) and groups[gi + 1][0] == groups[gi][0]:
                    load_q(gi + 1)
                self.act(pt[:, c0:512], SPS[u % NSB][:, c0:512], AF.Exp)
                if m >= 0:
                    self.memset("pool", pt[64:128, c0:c0 + 64], 0.0)
                self.mm(OP[c][:, c0:512], VV[:, kt, hh * 128:(hh + 1) * 128], pt[:, c0:512], start=(kt == 0), stop=(kt == nk - 1))
                a = ACC[gi % 2][c]
                if kt == 0:
                    self.cp("dve", a.v, pt.v)
                else:
                    self.tt("dve", a[:, c0:512], a[:, c0:512], pt[:, c0:512], ALU.add)
                if kt == nk - 1 and c == 1:
                    finish(gi)

    def phase3(self, l, x_src):
        with contextlib.ExitStack() as st:
            sb = lambda n, s, d: self.sb(st, n, s, d)
            ps = lambda n, s, d: self.ps(st, n, s, d)
            WO = sb("WO", [128, 8, D], BF16)
            WO.b.nowaw = True
            for k in range(8):
                self.dma("pool", WO[:, k, :], self.w_out[l, k * 128:(k + 1) * 128, :])
            G2 = sb("G2", [128, D], F32)
            self.dma("sp", G2.v, self.norm2_g[l].pbc(128))
            self.stt("dve", G2.v, self.mod(4), 1.0, G2.v, ALU.add, ALU.mult)
            SH2 = self.mod(3)
            GT1 = self.mod(2)
            RW = sb("RW", [128, 8, NE], F32)
            self.dma("sp", RW.v, self.router_w.v.rearrange("(k p) e -> p k e", p=128))
            MT = [sb(f"MT{i}", [128, 8, 512], BF16) for i in range(2)]
            XT = [sb(f"X3{i}", [128, D], F32) for i in range(2)]
            X1 = [sb(f"X1{i}", [128, D], F32) for i in range(2)]
            TM = sb("TM3", [128, D], F32)
            junk = sb("junk3", [128, D], BF16)
            ssq = sb("ssq3", [128, 1], F32)
            H2F = sb("H2F", [128, D], F32)
            H2B = [sb(f"H2B{i}", [128, D], BF16) for i in range(2)]
            H2T = sb("H2T", [128, D], F32)
            PO = [ps(f"PO{i}", [128, 512], F32) for i in range(2)]
            PTR = [ps(f"PTR{i}", [128, 512], F32) for i in range(2)]
            PL = ps("PL", [128, NE], F32)
            identf = self.cst("ident")
            mview = self.mixT_s.v.rearrange("(k p) t -> p k t", p=128)
            for i in range(NT):
                if i % 4 == 0:
                    mt = MT[(i // 4) % 2]
                    self.dma("sp", mt.v, mview[:, :, i * 128:i * 128 + 512])
                xt = XT[i % 2]
                x1 = X1[i % 2]
                self.dma("act", xt.v, x_src[i * 128:(i + 1) * 128, :] if l == 0 else self.out_tile(i))
                tcol = (i % 4) * 128
                for hf in range(2):
                    for k in range(8):
                        self.mm(PO[hf].v, mt[:, k, tcol:tcol + 128], WO[:, k, hf * 512:(hf + 1) * 512], start=(k == 0), stop=(k == 7))
                    self.tt("dve", TM[:, hf * 512:(hf + 1) * 512], PO[hf].v, GT1[:, hf * 512:(hf + 1) * 512], ALU.mult)
                self.tt("pool", x1.v, TM.v, xt.v, ALU.add)
                self.dma("sp", self.out_tile(i), x1.v)
                self.act(junk.v, x1.v, AF.Square, accum=ssq.v)
                self.rsqrt(ssq.v, ssq.v, 1.0 / D, EPS)
                self.stt("dve", H2F.v, x1.v, ssq[:, 0:1], G2.v, ALU.mult, ALU.mult)
                self.tt("pool", H2F.v, H2F.v, SH2, ALU.add)
                hb = H2B[i % 2]
                self.cp("act", hb.v, H2F.v)
                self.dma("act", self.h2_s[i * 128:(i + 1) * 128, :], hb.v)
                for hf in range(2):
                    for k in range(4):
                        kk = hf * 4 + k
                        self.tr(PTR[hf][:, k * 128:(k + 1) * 128], H2F[:, kk * 128:(kk + 1) * 128], identf)
                    self.cp("dve" if hf == 0 else "act", H2T[:, hf * 512:(hf + 1) * 512], PTR[hf].v)
                for k in range(8):
                    self.mm(PL.v, H2T[:, k * 128:(k + 1) * 128], RW[:, k, :], start=(k == 0), stop=(k == 7))
                self.cp("dve", self.LOG[:, i, :], PL.v)
            self.dump(f"log{l}", self.LOG.v, [128, NT, NE])

    def phase_moe(self, l):
        import os
        BIG = 1.0e30
        NTE = NT * NE
        with contextlib.ExitStack() as rst:
            rsb = lambda n, s_, d: self.sb(rst, n, s_, d)
            RG1 = rsb("RG1", [128, NT], F32)
            RG2 = rsb("RG2", [128, NT], F32)
            D1I = rsb("D1I", [128, NT], I32)
            D2I = rsb("D2I", [128, NT], I32)
            IDXW = rsb("IDXW", [128, NB, 12], I32)
            with contextlib.ExitStack() as st:
                sb = lambda n, s_, d: self.sb(st, n, s_, d)
                ps = lambda n, s_, d: self.ps(st, n, s_, d)
                RB = sb("RB", [128, NE], F32)
                self.dma("sp", RB.v, self.router_b.v.pbc(128))
                SCO = sb("SCO", [128, NT, NE], F32)
                BIA = sb("BIA", [128, NT, NE], F32)
                TA = sb("TA", [128, NT, NE], F32)
                TB = sb("TB", [128, NT, NE], F32)
                OH1 = sb("OH1", [128, NT, NE], F32)
                OH2 = sb("OH2", [128, NT, NE], F32)
                M1 = sb("M1", [128, NT * 8], F32)
                M2 = sb("M2", [128, NT * 8], F32)
                GM = sb("GM", [128, NT], F32)
                V1 = sb("V1", [128, NT], F32)
                W1 = sb("W1", [128, NT], F32)
                W2 = sb("W2", [128, NT], F32)
                self.act(SCO.v, self.LOG.v, AF.Sigmoid)
                self.tt("dve", BIA.v, SCO.v, RB.v.unsqueeze(1).bc([128, NT, NE]), ALU.add)
                b4 = BIA.v.rearrange("p i (g k) -> p (i g) k", k=4)
                self.red("dve", M1.v, b4, op=ALU.max)
                ta4 = TA.v.rearrange("p i (g k) -> p (i g) k", k=4)
                self.tt("dve", ta4, b4, M1.v.unsqueeze(2).bc([128, NT * 8, 4]), ALU.is_equal)
                self.stt("dve", TA.v, TA.v, -BIG, BIA.v, ALU.mult, ALU.add)
                self.red("dve", M2.v, ta4, op=ALU.max)
                self.tt("dve", M1.v, M1.v, M2.v, ALU.add)
                gs3 = M1.v.rearrange("p (i g) -> p i g", g=8)
                self.red("dve", GM.v, gs3, op=ALU.max)
                self.tt("dve", M2.v.rearrange("p (i g) -> p i g", g=8), gs3, GM.v.unsqueeze(2).bc([128, NT, 8]), ALU.is_equal)
                self.ts("dve", ta4, M2.v.unsqueeze(2).bc([128, NT * 8, 4]), BIG, -BIG, op0=ALU.mult, op1=ALU.add)
                self.tt("dve", TA.v, TA.v, BIA.v, ALU.add)
                self.red("dve", V1.v, TA.v, op=ALU.max)
                self.tt("dve", OH1.v, TA.v, V1.v.unsqueeze(2).bc([128, NT, NE]), ALU.is_equal)
                self.stt("dve", TB.v, OH1.v, -BIG, TA.v, ALU.mult, ALU.add)
                self.red("dve", V1.v, TB.v, op=ALU.max)
                self.tt("dve", OH2.v, TB.v, V1.v.unsqueeze(2).bc([128, NT, NE]), ALU.is_equal)
                self.tt("dve", TA.v, SCO.v, OH1.v, ALU.mult)
                self.red("dve", W1.v, TA.v)
                self.tt("dve", TA.v, SCO.v, OH2.v, ALU.mult)
                self.red("dve", W2.v, TA.v)
                self.tt("dve", V1.v, W1.v, W2.v, ALU.add)
                self.recip(V1.v, V1.v)
                self.tt("dve", RG1.v, W1.v, V1.v, ALU.mult)
                self.tt("dve", RG2.v, W2.v, V1.v, ALU.mult)
                MS_ = sb("MSEL", [128, NT, NE], BF16)
                self.tt("dve", MS_.v, OH1.v, OH2.v, ALU.add)
                LTSb = sb("LTSb", [128, 128], BF16)
                self.cp("dve", LTSb.v, self.cst("lts"))
                PRE = sb("PRE", [128, NT, NE], F32)
                TOT = sb("TOT", [128, NT, NE], F32)
                PP = [ps(f"PP{i}", [128, 512], F32) for i in range(2)]
                msf = MS_.v.rearrange("p i e -> p (i e)")
                pre_f = PRE.v.rearrange("p i e -> p (i e)")
                tot_f = TOT.v.rearrange("p i e -> p (i e)")
                for c in range(NTE // 512):
                    self.mm(PP[0].v, LTSb.v, msf[:, c * 512:(c + 1) * 512])
                    self.cp("dve", pre_f[:, c * 512:(c + 1) * 512], PP[0].v)
                    self.mm(PP[1].v, self.onesb.v, msf[:, c * 512:(c + 1) * 512])
                    self.cp("act", tot_f[:, c * 512:(c + 1) * 512], PP[1].v)
                A_, B_ = TA, TB
                self.cp("dve", A_.v, TOT.v)
                k = 1
                while k < NT:
                    self.tt("dve", B_[:, k:, :], A_[:, k:, :], A_[:, :NT - k, :], ALU.add)
                    self.cp("dve", B_[:, :k, :], A_[:, :k, :])
                    A_, B_ = B_, A_
                    k *= 2
                INC = A_
                OFF = B_
                self.tt("dve", OFF.v, INC.v, TOT.v, ALU.subtract)
                CNT = INC[:, NT - 1, :]
                CMP = sb("CMP", [128, NE, 32], F32)
                self.tt("dve", CMP.v, CNT.unsqueeze(2).bc([128, NE, 32]), self.cst("jb")[:, 0:32].unsqueeze(1).bc([128, NE, 32]), ALU.is_gt)
                PADE = sb("PADE", [128, NE], F32)
                self.red("dve", PADE.v, CMP.v)
                self.ts("dve", PADE.v, PADE.v, float(BLK))
                EA = sb("EA", [128, NE], F32)
                EB = sb("EB", [128, NE], F32)
                self.cp("dve", EA.v, PADE.v)
                a_, b_ = EA, EB
                k = 1
                while k < NE:
                    self.tt("dve", b_[:, k:], a_[:, k:], a_[:, :NE - k], ALU.add)
                    self.cp("dve", b_[:, :k], a_[:, :k])
                    a_, b_ = b_, a_
                    k *= 2
                PEND = a_
                PST = b_
                self.tt("dve", PST.v, PEND.v, PADE.v, ALU.subtract)
                self.tt("dve", PRE.v, PRE.v, OFF.v, ALU.add)
                self.tt("dve", PRE.v, PRE.v, PST.v.unsqueeze(1).bc([128, NT, NE]), ALU.add)
                self.tt("dve", TOT.v, PRE.v, OH1.v, ALU.mult)
                self.red("dve", W1.v, TOT.v)
                self.tt("dve", TOT.v, PRE.v, OH2.v, ALU.mult)
                self.red("dve", W2.v, TOT.v)
                self.cp("dve", D1I.v, W1.v)
                self.cp("dve", D2I.v, W2.v)
                CM2 = sb("CM2", [128, NB, NE], F32)
                self.tt("dve", CM2.v, PEND.v.unsqueeze(1).bc([128, NB, NE]), self.cst("jb").unsqueeze(2).bc([128, NB, NE]), ALU.is_le)
                BE = sb("BE", [128, NB], F32)
                self.red("dve", BE.v, CM2.v)
                self.ts("dve", BE.v, BE.v, float(NE - 1), None, op0=ALU.min)
                IDXF = sb("IDXF", [128, NB, 12], F32)
                coff = self.cst("coff")
                self.stt("dve", IDXF[:, :, 0:8], BE.v.unsqueeze(2).bc([128, NB, 8]), float(D), coff[:, 0:8].unsqueeze(1).bc([128, NB, 8]), ALU.mult, ALU.add)
                self.stt("dve", IDXF[:, :, 8:12], BE.v.unsqueeze(2).bc([128, NB, 4]), float(DE), coff[:, 8:12].unsqueeze(1).bc([128, NB, 4]), ALU.mult, ALU.add)
                self.cp("dve", IDXW.v, IDXF.v)
                if self.debug:
                    self.dump(f"d1_{l}", W1.v, [128, NT])
                    self.dump(f"d2_{l}", W2.v, [128, NT])
                    self.dump(f"g1_{l}", RG1.v, [128, NT])
                    self.dump(f"g2_{l}", RG2.v, [128, NT])
                    self.dump(f"be_{l}", BE.v, [128, NB])
            self.fw.barrier()
            if os.environ.get("MOECUT", "9") == "0":
                return
            with contextlib.ExitStack() as st:
                sb = lambda n, s_, d: self.sb(st, n, s_, d)
                HB = [sb(f"HBm{i}", [128, D], BF16) for i in range(3)]
                for i in range(NT):
                    hb = HB[i % 3]
                    self.dma("sp", hb.v, self.h2_s[i * 128:(i + 1) * 128, :])
                    self.scatter(self.xs_s.v, hb.v, D1I[:, i:i + 1])
                    self.scatter(self.xs_s.v, hb.v, D2I[:, i:i + 1])
            self.fw.barrier()
            if os.environ.get("MOECUT", "9") == "1":
                return
            with contextlib.ExitStack() as st:
                sb = lambda n, s_, d: self.sb(st, n, s_, d)
                ps = lambda n, s_, d: self.ps(st, n, s_, d)
                WGU = [sb(f"WGU{i}", [128, 8, 2 * DE], BF16) for i in range(2)]
                WD = [sb(f"WD{i}", [128, 4, D], BF16) for i in range(2)]
                for t_ in WGU + WD:
                    t_.b.nowaw = True
                XB = [sb(f"XB{i}", [128, NSUB, D], BF16) for i in range(2)]
                XTB = [sb(f"XTB{i}", [128, 8, BLK], BF16) for i in range(2)]
                SGt = sb("SGt", [128, BLK], F32)
                UT = sb("UT", [128, 4, BLK], BF16)
                YB = [sb(f"YB{i}", [128, D], F32) for i in range(2)]
                TPX = ps("TPX", [128, 1024], BF16)
                PG = [ps(f"PG{i}", [128, BLK], F32) for i in range(2)]
                PU = [ps(f"PU{i}", [128, BLK], F32) for i in range(2)]
                PD = [ps(f"PD{i}", [128, 512], F32) for i in range(2)]
                wgu_t = self.wgub[l].v
                wd_t = self.wdb[l].v
                nblk = int(os.environ.get("MOEBLK", str(NB)))
                yi = 0
                for j in range(nblk):
                    wgu, wd, xb, xtb = WGU[j % 2], WD[j % 2], XB[j % 2], XTB[j % 2]
                    for c in range(8):
                        self.gather(wgu[:, c, :], wgu_t, IDXW[:, j, c:c + 1])
                    for c in range(4):
                        self.gather(wd[:, c, :], wd_t, IDXW[:, j, 8 + c:9 + c])
                    self.dma("sp", xb.v, self.xs_s[j * BLK:(j + 1) * BLK, :].rearrange("(s p) d -> p s d", p=128))
                    for sub in range(NSUB):
                        for k in range(8):
                            self.tr(TPX[:, k * 128:(k + 1) * 128], xb[:, sub, k * 128:(k + 1) * 128], self.identb.v)
                        self.cp("dve" if sub % 2 == 0 else "act", xtb[:, :, sub * 128:(sub + 1) * 128], TPX.v.rearrange("p (k t) -> p k t", k=8))
                    for fc in range(4):
                        pg, pu = PG[fc % 2], PU[fc % 2]
                        for k in range(8):
                            self.mm(pg.v, wgu[:, k, fc * 128:(fc + 1) * 128], xtb[:, k, :], start=(k == 0), stop=(k == 7))
                        for k in range(8):
                            self.mm(pu.v, wgu[:, k, DE + fc * 128:DE + (fc + 1) * 128], xtb[:, k, :], start=(k == 0), stop=(k == 7))
                        self.act(SGt.v, pg.v, AF.Silu)
                        self.tt("dve", UT[:, fc, :], SGt.v, pu.v, ALU.mult)
                    for sub in range(NSUB):
                        yb = YB[yi % 2]
                        yi += 1
                        for hf in range(2):
                            pd = PD[hf]
                            for fc in range(4):
                                self.mm(pd.v, UT[:, fc, sub * 128:(sub + 1) * 128], wd[:, fc, hf * 512:(hf + 1) * 512], start=(fc == 0), stop=(fc == 3))
                            self.cp("act" if hf == 0 else "dve", yb[:, hf * 512:(hf + 1) * 512], pd.v)
                        self.dma("act", self.ys_s[j * BLK + sub * 128:j * BLK + (sub + 1) * 128, :], yb.v)
            self.fw.barrier()
            if os.environ.get("MOECUT", "9") == "2":
                return
            with contextlib.ExitStack() as st:
                sb = lambda n, s_, d: self.sb(st, n, s_, d)
                GT2 = self.mod(5)
                Y1 = [sb(f"Y1{i}", [128, D], F32) for i in range(2)]
                Y2 = [sb(f"Y2{i}", [128, D], F32) for i in range(2)]
                XX = [sb(f"XX{i}", [128, D], F32) for i in range(2)]
                TT = [sb(f"TT{i}", [128, D], F32) for i in range(2)]
                for i in range(NT):
                    y1, y2, xx, tt_ = Y1[i % 2], Y2[i % 2], XX[i % 2], TT[i % 2]
                    self.gather(y1.v, self.ys_s.v, D1I[:, i:i + 1])
                    self.gather(y2.v, self.ys_s.v, D2I[:, i:i + 1])
                    self.dma("sp", xx.v, self.out_tile(i))
                    self.ts("dve", tt_.v, y1.v, RG1[:, i:i + 1], None, op0=ALU.mult)
                    self.stt("dve", tt_.v, y2.v, RG2[:, i:i + 1], tt_.v, ALU.mult, ALU.add)
                    if self.debug and l == 0:
                        self.dma("act", self.ydbg[i * 128:(i + 1) * 128, :], tt_.v)
                    self.tt("pool", tt_.v, tt_.v, GT2, ALU.mult)
                    self.tt("pool", xx.v, xx.v, tt_.v, ALU.add)
                    self.dma("sp", self.out_tile(i), xx.v)


_CACHE = {}


def _get_prog(stop_after=None, debug=False):
    key = (stop_after, debug)
    if key not in _CACHE:
        p = Prog(stop_after=stop_after, debug=debug)
        p.build()
        _CACHE[key] = p
    return _CACHE[key]


def make_in_map(inputs, b):
    f = lambda a: np.ascontiguousarray(a, dtype=np.float32)
    m = {
        "x": f(inputs["x"][b]),
        "c": f(np.asarray(inputs["c"][b]).reshape(8, 128).T),
        "pos": np.ascontiguousarray(np.asarray(inputs["positions"][b]).reshape(NT, 128).T.astype(np.int32)),
        "consts": CONST_NP,
    }
    for k in ("w_mod", "b_mod", "norm1_g", "norm2_g", "w_in", "ret_norm_g", "diff_q_g", "diff_k_g", "lam_q1", "lam_k1",
              "lam_q2", "lam_k2", "diff_sub_g", "gla_w_a2", "gla_b_a", "gla_norm_g", "w_out", "router_w", "router_b"):
        m[k] = f(inputs[k])
    for l in range(DEPTH):
        m[f"w_gate{l}"] = f(inputs["w_gate"][l]).reshape(NE * D, DE)
        m[f"w_up{l}"] = f(inputs["w_up"][l]).reshape(NE * D, DE)
        m[f"w_down{l}"] = f(inputs["w_down"][l]).reshape(NE * DE, D)
    return m


def kernel(**inputs):
    prog = _get_prog()
    shared = make_in_map(inputs, 0)
    in_maps = []
    for b in range(8):
        m = dict(shared)
        m["x"] = np.ascontiguousarray(inputs["x"][b], dtype=np.float32)
        m["c"] = np.ascontiguousarray(np.asarray(inputs["c"][b], dtype=np.float32).reshape(8, 128).T)
        m["pos"] = np.ascontiguousarray(np.asarray(inputs["positions"][b]).reshape(NT, 128).T.astype(np.int32))
        in_maps.append(m)
    res = run_bass_kernel_spmd(prog.nc, in_maps, core_ids=list(range(8)))
    return np.stack([np.asarray(r["out"], dtype=np.float32) for r in res.results], axis=0)
```

```python
import math
import contextlib
import numpy as np
import concourse.bass as bass
import concourse.mybir as mybir
from concourse.bass_utils import run_bass_kernel_spmd

F32 = mybir.dt.float32
BF16 = mybir.dt.bfloat16
I32 = mybir.dt.int32
ALU = mybir.AluOpType
AF = mybir.ActivationFunctionType
AX = mybir.AxisListType

S = 8192
D = 1024
NT = S // 128
DEPTH = 2
EPS = 1e-6
NE = 32
DE = 512
BLK = 256
NSUB = BLK // 128
NB = -(-(2 * S) // BLK) + NE
NPAD = NB * BLK
SEM_LIMIT = 30000
WINC = 3456


class Buf:
    __slots__ = ("name", "w", "r", "excl", "nowaw")

    def __init__(self, name="", excl=False):
        self.name = name
        self.w = {}
        self.r = {}
        self.excl = excl
        self.nowaw = False


class V:
    __slots__ = ("ap", "b")

    def __init__(self, ap, b):
        self.ap = ap
        self.b = b

    def __getitem__(self, k):
        return V(self.ap[k], self.b)

    def rearrange(self, s, **kw):
        return V(self.ap.rearrange(s, **kw), self.b)

    def unsqueeze(self, a):
        return V(self.ap.unsqueeze(a), self.b)

    def bc(self, shape):
        return V(self.ap.broadcast_to(list(shape)), self.b)

    def pbc(self, n):
        return V(self.ap.partition_broadcast(n), self.b)

    def bitcast(self, dt):
        return V(self.ap.bitcast(dt), self.b)


class Tile:
    def __init__(self, t, name):
        self.t = t
        self.b = Buf(name)

    def __getitem__(self, k):
        return V(self.t[k], self.b)

    @property
    def v(self):
        return self[:]


class EngS:
    def __init__(self, name, handle):
        self.name = name
        self.h = handle
        self.sem = None
        self.n = 0
        self.seen = {}
        self.dma_sems = []
        self.dma_cnt = []
        self.dma_i = 0


class FW:
    def __init__(self, nc, ndma=10, same=True):
        self.nc = nc
        self.nsem = 0
        self.same = same
        self.E = {"pe": EngS("pe", nc.tensor), "dve": EngS("dve", nc.vector),
                  "act": EngS("act", nc.scalar), "pool": EngS("pool", nc.gpsimd),
                  "sp": EngS("sp", nc.sync)}
        self.ndma = ndma
        self.owner = {}
        self.nwaits = 0
        self.nops = 0

    def new_sem(self):
        self.nsem += 1
        return self.nc.alloc_semaphore(f"fs{self.nsem}")

    def _tok(self, E):
        if E.sem is None or E.n >= SEM_LIMIT:
            E.sem = self.new_sem()
            E.n = 0
            self.owner[E.sem] = E.name
        E.n += 1
        return (E.sem, E.n)

    def _wait(self, E, deps):
        for sem, val in deps.items():
            if E.seen.get(sem, 0) >= val:
                continue
            E.h.wait_ge(sem, val)
            E.seen[sem] = val
            self.nwaits += 1

    def _deps(self, E, reads, writes, skip_own):
        deps = {}
        for b in reads:
            for s, v in b.w.items():
                if deps.get(s, 0) < v:
                    deps[s] = v
        for b in writes:
            for d in ((b.r,) if b.nowaw else (b.w, b.r)):
                for s, v in d.items():
                    if deps.get(s, 0) < v:
                        deps[s] = v
        if skip_own:
            for s in list(deps):
                if self.owner.get(s) == E.name:
                    del deps[s]
        self._wait(E, deps)

    def _commit(self, tok, reads, writes):
        s, v = tok
        for b in reads:
            if b.r.get(s, 0) < v:
                b.r[s] = v
        for b in writes:
            if b.w.get(s, 0) < v:
                b.w[s] = v
            b.r = {}

    def op(self, eng, fn, reads=(), writes=()):
        E = self.E[eng]
        if any(b.excl for b in reads):
            writes = list(writes) + [b for b in reads if b.excl]
            reads = [b for b in reads if not b.excl]
        self._deps(E, reads, writes, (eng == "pe") or (not self.same))
        ins = fn()
        tok = self._tok(E)
        ins.then_inc(tok[0], 1)
        self._commit(tok, reads, writes)
        self.nops += 1
        return ins

    def dma(self, q, fn, reads=(), writes=()):
        E = self.E[q]
        if not E.dma_sems:
            E.dma_sems = [self.new_sem() for _ in range(self.ndma)]
            E.dma_cnt = [0] * self.ndma
        k = E.dma_i % self.ndma
        E.dma_i += 1
        if E.dma_cnt[k] * 16 >= SEM_LIMIT:
            self._wait(E, {E.dma_sems[k]: E.dma_cnt[k] * 16})
            E.dma_sems[k] = self.new_sem()
            E.dma_cnt[k] = 0
        sem = E.dma_sems[k]
        if E.dma_cnt[k] > 0:
            self._wait(E, {sem: E.dma_cnt[k] * 16})
        self._deps(E, reads, writes, False)
        ins = fn()
        E.dma_cnt[k] += 1
        ins.then_inc(sem, 16)
        self._commit((sem, E.dma_cnt[k] * 16), reads, writes)
        self.nops += 1
        return ins

    def barrier(self):
        deps = {}
        for E in self.E.values():
            if E.sem is not None and E.n > 0:
                deps[E.sem] = E.n
            for s, c in zip(E.dma_sems, E.dma_cnt):
                if c > 0:
                    deps[s] = c * 16
        for E in self.E.values():
            self._wait(E, dict(deps))


def _const_tables():
    c = {}
    p = np.arange(128)
    c["ident"] = np.eye(128, dtype=np.float32)
    gam = 1.0 - 2.0 ** (-5.0 - np.arange(4))
    same = (p[:, None] // 64) == (p[None, :] // 64)
    dr = np.zeros((128, 4, 128), np.float32)
    for h in range(4):
        dr[:, h, :] = np.where(same, gam[h] ** np.abs(p[:, None] - p[None, :]), 0.0)
    c["dr"] = dr.reshape(128, 512)
    j = p % 64
    qd = np.stack([gam[h] ** (j + 1.0) for h in range(4)], 1)
    kd = np.stack([gam[h] ** (63.0 - j) for h in range(4)], 1) / 8.0
    c["qd"] = np.repeat(qd, 64, axis=1)
    c["kd0"] = np.repeat(kd * (p[:, None] < 64), 64, axis=1)
    c["kd1"] = np.repeat(kd * (p[:, None] >= 64), 64, axis=1)
    c["cd"] = np.broadcast_to(np.repeat(gam ** 64.0, 64)[None, :], (128, 256)).copy()
    ml = (same & (p[:, None] <= p[None, :])).astype(np.float32)
    mu = (same & (p[:, None] > p[None, :])).astype(np.float32)
    c["ml4"] = np.tile(ml, (1, 4))
    c["mu4"] = np.tile(mu, (1, 4))
    c["tri"] = ml
    c["chk"] = np.stack([(p < 64), (p >= 64)], 1).astype(np.float32)
    c["m0"] = (p < 64).astype(np.float32)[:, None]
    c["m1"] = (p >= 64).astype(np.float32)[:, None]
    c["ifr"] = np.broadcast_to((1.0 / (10000.0 ** (np.arange(32, dtype=np.float32) / 32)))[None, :], (128, 32)).copy()
    c["ifd"] = np.broadcast_to((1.0 / (500000.0 ** (np.arange(8, dtype=np.float32) / 8)))[None, :], (128, 8)).copy()
    c["lts"] = (p[:, None] < p[None, :]).astype(np.float32)
    c["ones"] = np.ones((128, 128), np.float32)
    c["jb"] = np.broadcast_to((np.arange(NB, dtype=np.float32) * BLK)[None, :], (128, NB)).copy()
    coff = np.zeros((128, 12), np.float32)
    for cc in range(12):
        coff[:, cc] = (cc if cc < 8 else cc - 8) * 128 + p
    c["coff"] = coff
    c["gid"] = np.broadcast_to((np.arange(32) // 4).astype(np.float32)[None, :], (128, 32)).copy()
    off = {}
    cur = 0
    arrs = []
    for k, v in c.items():
        v = np.ascontiguousarray(v, dtype=np.float32).reshape(128, -1)
        off[k] = (cur, v.shape[1])
        cur += v.shape[1]
        arrs.append(v)
    return np.concatenate(arrs, axis=1), off


CONST_NP, COFF = _const_tables()
NCONST = CONST_NP.shape[1]


class Prog:
    def __init__(self, stop_after=None, debug=False):
        self.nc = nc = bass.Bass("TRN2", target_bir_lowering=False)
        import os
        self.fw = FW(nc, same=(os.environ.get("SAMEENG", "1") == "1"))
        self.stop_after = stop_after
        self.debug = debug
        self.dram = {}

    def din(self, name, shape, dt=F32):
        t = self.nc.dram_tensor(name, list(shape), dt, kind="ExternalInput")
        T = Tile(t.ap(), name)
        self.dram[name] = T
        return T

    def dout(self, name, shape, dt=F32):
        t = self.nc.dram_tensor(name, list(shape), dt, kind="ExternalOutput")
        T = Tile(t.ap(), name)
        self.dram[name] = T
        return T

    def dint(self, name, shape, dt, dbg=False):
        kind = "ExternalOutput" if (dbg and self.debug) else "Internal"
        t = self.nc.dram_tensor(name, list(shape), dt, kind=kind)
        T = Tile(t.ap(), name)
        T.b.nowaw = True
        self.dram[name] = T
        return T

    def dump(self, name, v, shape, dt=F32):
        if not self.debug:
            return
        T = self.dout(self._uname("dbg_" + name), shape, dt)
        self.dma("sp", T.v, v)

    def _uname(self, name):
        self._uid = getattr(self, "_uid", 0) + 1
        return f"{name}_u{self._uid}"

    def sb(self, st, name, shape, dt):
        name = self._uname(name)
        return Tile(st.enter_context(self.nc.sbuf_tensor(name, list(shape), dt)), name)

    def ps(self, st, name, shape, dt):
        name = self._uname(name)
        T = Tile(st.enter_context(self.nc.psum_tensor(name, list(shape), dt)), name)
        T.b.excl = True
        return T

    def _eng(self, e):
        return {"dve": self.nc.vector, "pool": self.nc.gpsimd, "act": self.nc.scalar}[e]

    def _pe(self, e):
        import os
        if e == "pool" and os.environ.get("NOPOOL", "0") == "1":
            return "dve"
        return e

    def dma(self, q, out, in_, **kw):
        h = {"sp": self.nc.sync, "act": self.nc.scalar, "pool": self.nc.gpsimd}[q]
        return self.fw.dma(q, lambda: h.dma_start(out=out.ap, in_=in_.ap, **kw), reads=[in_.b], writes=[out.b])

    def gather(self, out, table, idx):
        return self.fw.dma("pool", lambda: self.nc.gpsimd.indirect_dma_start(
            out=out.ap, out_offset=None, in_=table.ap,
            in_offset=bass.IndirectOffsetOnAxis(ap=idx.ap, axis=0)), reads=[table.b, idx.b], writes=[out.b])

    def scatter(self, table, in_, idx):
        return self.fw.dma("pool", lambda: self.nc.gpsimd.indirect_dma_start(
            out=table.ap, out_offset=bass.IndirectOffsetOnAxis(ap=idx.ap, axis=0),
            in_=in_.ap, in_offset=None), reads=[in_.b, idx.b], writes=[table.b])

    def mm(self, out, lhsT, rhs, start=True, stop=True):
        return self.fw.op("pe", lambda: self.nc.tensor.matmul(out=out.ap, lhsT=lhsT.ap, rhs=rhs.ap, start=start, stop=stop),
                          reads=[lhsT.b, rhs.b], writes=[out.b])

    def tr(self, out, in_, ident):
        return self.fw.op("pe", lambda: self.nc.tensor.transpose(out=out.ap, in_=in_.ap, identity=ident.ap),
                          reads=[in_.b, ident.b], writes=[out.b])

    def act(self, out, in_, func, scale=1.0, bias=0.0, accum=None):
        rd = [in_.b]
        kw = {}
        if isinstance(scale, V):
            rd.append(scale.b)
            kw["scale"] = scale.ap
        else:
            kw["scale"] = float(scale)
        if isinstance(bias, V):
            rd.append(bias.b)
            kw["bias"] = bias.ap
        else:
            kw["bias"] = float(bias)
        wr = [out.b]
        if accum is not None:
            kw["accum_out"] = accum.ap
            wr.append(accum.b)
        return self.fw.op("act", lambda: self.nc.scalar.activation(out=out.ap, in_=in_.ap, func=func, **kw), reads=rd, writes=wr)

    def tt(self, e, out, a, b, op):
        e = self._pe(e)
        return self.fw.op(e, lambda: self._eng(e).tensor_tensor(out=out.ap, in0=a.ap, in1=b.ap, op=op), reads=[a.b, b.b], writes=[out.b])

    def ts(self, e, out, a, s1, s2=None, op0=ALU.mult, op1=None):
        e = self._pe(e)
        rd = [a.b]
        s1a = s1
        s2a = s2
        if isinstance(s1, V):
            rd.append(s1.b)
            s1a = s1.ap
        if isinstance(s2, V):
            rd.append(s2.b)
            s2a = s2.ap
        kw = {}
        if op1 is not None:
            kw["op1"] = op1
        return self.fw.op(e, lambda: self._eng(e).tensor_scalar(out=out.ap, in0=a.ap, scalar1=s1a, scalar2=s2a, op0=op0, **kw), reads=rd, writes=[out.b])

    def stt(self, e, out, a, s, b, op0, op1):
        e = self._pe(e)
        rd = [a.b, b.b]
        sa = s
        if isinstance(s, V):
            rd.append(s.b)
            sa = s.ap
        return self.fw.op(e, lambda: self._eng(e).scalar_tensor_tensor(out=out.ap, in0=a.ap, scalar=sa, in1=b.ap, op0=op0, op1=op1), reads=rd, writes=[out.b])

    def cp(self, e, out, in_):
        e = self._pe(e)
        if e == "act":
            return self.act(out, in_, AF.Copy)
        return self.fw.op(e, lambda: self._eng(e).tensor_copy(out=out.ap, in_=in_.ap), reads=[in_.b], writes=[out.b])

    def red(self, e, out, in_, op=ALU.add, axis=AX.X):
        return self.fw.op(e, lambda: self._eng(e).tensor_reduce(out=out.ap, in_=in_.ap, axis=axis, op=op), reads=[in_.b], writes=[out.b])

    def recip(self, out, in_):
        return self.fw.op("dve", lambda: self.nc.vector.reciprocal(out=out.ap, in_=in_.ap), reads=[in_.b], writes=[out.b])

    def memset(self, e, out, val):
        e = self._pe(e)
        return self.fw.op(e, lambda: self._eng(e).memset(out.ap, val), writes=[out.b])

    def rsqrt(self, out, in_, scale, eps):
        self.act(out, in_, AF.Ln, scale=scale, bias=self.epsv(eps, out))
        self.act(out, out, AF.Exp, scale=-0.5)

    def epsv(self, eps, like):
        n = like.ap.shape[0]
        return self.C_eps[0:n, 0:1] if eps == EPS else 0.0

    def build(self):
        nc = self.nc
        with contextlib.ExitStack() as gst:
            self.gst = gst
            self.declare_io()
            self.load_consts(gst)
            for l in range(DEPTH):
                self.layer(l)
                if self.stop_after is not None and self.stop_after[0] == l and self.stop_after[1] != "all":
                    break
            self.finish()
        return nc

    def declare_io(self):
        d = self.din
        self.x_in = d("x", [S, D])
        self.c_in = d("c", [128, 8])
        self.pos_in = d("pos", [128, NT], I32)
        self.consts_in = d("consts", [128, NCONST])
        self.w_mod = d("w_mod", [DEPTH, D, 6 * D])
        self.b_mod = d("b_mod", [DEPTH, 6 * D])
        self.norm1_g = d("norm1_g", [DEPTH, D])
        self.norm2_g = d("norm2_g", [DEPTH, D])
        self.w_in = d("w_in", [DEPTH, D, 3344])
        self.ret_norm_g = d("ret_norm_g", [DEPTH, 64])
        self.diff_q_g = d("diff_q_g", [DEPTH, 64])
        self.diff_k_g = d("diff_k_g", [DEPTH, 64])
        self.lam_q1 = d("lam_q1", [DEPTH, 64])
        self.lam_k1 = d("lam_k1", [DEPTH, 64])
        self.lam_q2 = d("lam_q2", [DEPTH, 64])
        self.lam_k2 = d("lam_k2", [DEPTH, 64])
        self.diff_sub_g = d("diff_sub_g", [DEPTH, 128])
        self.gla_w_a2 = d("gla_w_a2", [DEPTH, 16, 128])
        self.gla_b_a = d("gla_b_a", [DEPTH, 128])
        self.gla_norm_g = d("gla_norm_g", [DEPTH, 64])
        self.w_out = d("w_out", [DEPTH, D, D])
        self.router_w = d("router_w", [D, NE])
        self.router_b = d("router_b", [NE])
        self.w_gate = [d(f"w_gate{l}", [NE * D, DE]) for l in range(DEPTH)]
        self.w_up = [d(f"w_up{l}", [NE * D, DE]) for l in range(DEPTH)]
        self.w_down = [d(f"w_down{l}", [NE * DE, D]) for l in range(DEPTH)]
        self.wgub = [self.dint(f"wgub{l}", [NE * D, 2 * DE], BF16) for l in range(DEPTH)]
        self.wdb = [self.dint(f"wdb{l}", [NE * DE, D], BF16) for l in range(DEPTH)]
        self.bg = []
        self.out = self.dout("out", [S, D])
        self.out_b = [Buf(f"out{i}") for i in range(NT)]
        if self.debug:
            self.ydbg = self.dout("ydbg", [S, D])
        self.qT_s = self.dint("qT_s", [4, 128, S], BF16, dbg=True)
        self.kT_s = self.dint("kT_s", [4, 128, S], BF16, dbg=True)
        self.v_s = self.dint("v_s", [S, 512], BF16, dbg=True)
        self.mixT_s = self.dint("mixT_s", [D, S], BF16, dbg=True)
        self.h2_s = self.dint("h2_s", [S, D], BF16, dbg=True)
        self.xs_s = self.dint("xs_s", [NPAD, D], BF16)
        self.ys_s = self.dint("ys_s", [NPAD, D], F32)

    def load_consts(self, st):
        self.CT = self.sb(st, "consts", [128, NCONST], F32)
        self.dma("sp", self.CT.v, self.consts_in.v)
        self.C_eps = self.sb(st, "c_eps", [128, 1], F32)
        self.memset("dve", self.C_eps.v, EPS)
        self.C_npi = self.sb(st, "c_npi", [128, 1], F32)
        self.memset("dve", self.C_npi.v, -math.pi)
        self.C_one = self.sb(st, "c_one", [128, 1], F32)
        self.memset("dve", self.C_one.v, 1.0)
        posi = self.sb(st, "posi", [128, NT], I32)
        self.dma("sp", posi.v, self.pos_in.v)
        self.POSF = self.sb(st, "posf", [128, NT], F32)
        self.cp("dve", self.POSF.v, posi.v)
        self.identb = self.sb(st, "identb", [128, 128], BF16)
        self.cp("dve", self.identb.v, self.cst("ident"))
        self.rope_tables(st)
        self.onesb = self.sb(st, "onesb", [128, 128], BF16)
        self.cp("dve", self.onesb.v, self.cst("ones"))

    def rope_tables(self, gst):
        self.COS = self.sb(gst, "COS", [128, NT, 40], F32)
        self.SIN = self.sb(gst, "SIN", [128, NT, 40], F32)
        o, _ = COFF["ifr"]
        if40 = self.CT[:, o:o + 40]
        C1 = 6.28125
        C2 = 2.0 * math.pi - C1
        with contextlib.ExitStack() as st:
            ANG = self.sb(st, "ANG", [128, NT, 40], F32)
            KI = self.sb(st, "KI", [128, NT, 40], I32)
            KF = self.sb(st, "KF", [128, NT, 40], F32)
            MK = self.sb(st, "MK", [128, NT, 40], F32)
            self.tt("dve", ANG.v, self.POSF.v.unsqueeze(2).bc([128, NT, 40]), if40.unsqueeze(1).bc([128, NT, 40]), ALU.mult)
            self.ts("dve", KF.v, ANG.v, 1.0 / (2.0 * math.pi))
            self.cp("dve", KI.v, KF.v)
            self.cp("dve", KF.v, KI.v)
            self.stt("dve", ANG.v, KF.v, -C1, ANG.v, ALU.mult, ALU.add)
            self.stt("dve", ANG.v, KF.v, -C2, ANG.v, ALU.mult, ALU.add)
            self.ts("dve", MK.v, ANG.v, math.pi, None, op0=ALU.is_gt)
            self.stt("dve", self.SIN.v, MK.v, -2.0 * math.pi, ANG.v, ALU.mult, ALU.add)
            self.ts("dve", MK.v, self.SIN.v, -math.pi, None, op0=ALU.is_lt)
            self.stt("dve", self.SIN.v, MK.v, 2.0 * math.pi, self.SIN.v, ALU.mult, ALU.add)
            self.ts("dve", self.COS.v, self.SIN.v, 0.5 * math.pi, None, op0=ALU.add)
            self.ts("dve", MK.v, self.COS.v, math.pi, None, op0=ALU.is_gt)
            self.stt("dve", self.COS.v, MK.v, -2.0 * math.pi, self.COS.v, ALU.mult, ALU.add)
            self.act(self.SIN.v, self.SIN.v, AF.Sin)
            self.act(self.COS.v, self.COS.v, AF.Sin)
        self.fw.barrier()

    def out_tile(self, i):
        return V(self.out.t[i * 128:(i + 1) * 128, :], self.out_b[i])

    def cst(self, name):
        o, n = COFF[name]
        return self.CT[:, o:o + n]

    def finish(self):
        fw = self.fw
        fw.barrier()

    def layer(self, l):
        x_src = self.x_in if l == 0 else self.out
        sa = self.stop_after
        with contextlib.ExitStack() as lst:
            self.phase_mod(l, lst)
            if sa == (l, "mod"):
                return
            self.queue_weight_casts(l)
            self.phase1(l, x_src)
            self.run_bg(10 ** 9)
            self.fw.barrier()
            if sa == (l, "p1"):
                return
            self.phase2(l)
            self.fw.barrier()
            if sa == (l, "p2"):
                return
            with contextlib.ExitStack() as st3:
                self.LOG = self.sb(st3, f"log{l}", [128, NT, NE], F32)
                self.phase3(l, x_src)
                self.fw.barrier()
                if sa == (l, "p3"):
                    return
                self.phase_moe(l)
                self.fw.barrier()
                if sa == (l, "moe"):
                    return

    def queue_weight_casts(self, l):
        for e in range(NE):
            for src, dst, rows, c0, c1 in ((self.w_gate[l], self.wgub[l], D, 0, DE), (self.w_up[l], self.wgub[l], D, DE, 2 * DE),
                                           (self.w_down[l], self.wdb[l], DE, 0, D)):
                def task(src=src, dst=dst, rows=rows, e=e, c0=c0, c1=c1):
                    self.dma("pool", dst[e * rows:(e + 1) * rows, c0:c1], src[e * rows:(e + 1) * rows, :])
                self.bg.append(task)

    def run_bg(self, n):
        while n > 0 and self.bg:
            self.bg.pop(0)()
            n -= 1

    def phase_mod(self, l, lst):
        MODB = self.sb(lst, f"modb{l}", [128, 6 * D], F32)
        self.MODB = MODB
        with contextlib.ExitStack() as st:
            cT = self.sb(st, "cT", [128, 8], F32)
            self.dma("sp", cT.v, self.c_in.v)
            ca = self.sb(st, "ca", [128, 8], F32)
            self.act(ca.v, cT.v, AF.Silu)
            cb = self.sb(st, "cb", [128, 8, 128], F32)
            for k in range(8):
                self.cp("dve", cb[:, k, :], ca[:, k:k + 1].bc([128, 128]))
            bm = self.sb(st, "bm", [1, 6 * D], F32)
            self.dma("sp", bm.v, self.b_mod[l:l + 1, :])
            ones_f = self.cst("ones")
            wm = [self.sb(st, f"wm{i}", [128, 8, 512], F32) for i in range(2)]
            pm = [self.ps(st, f"pm{i}", [128, 512], F32) for i in range(2)]
            for n in range(12):
                w = wm[n % 2]
                p = pm[n % 2]
                self.dma("sp" if n % 2 == 0 else "act", w.v,
                         self.w_mod[l, :, n * 512:(n + 1) * 512].rearrange("(k p) n -> p k n", p=128))
                for k in range(8):
                    self.mm(p.v, cb[:, k, :], w[:, k, :], start=(k == 0), stop=False)
                self.mm(p.v, ones_f[0:1, :], bm[0:1, n * 512:(n + 1) * 512], start=False, stop=True)
                self.cp("dve" if n % 2 == 0 else "act", MODB[:, n * 512:(n + 1) * 512], p.v)
        self.dump(f"mod{l}", MODB[0:2, :], [2, 6 * D])
        self.fw.barrier()

    def mod(self, j):
        return self.MODB[:, j * D:(j + 1) * D]

    def phase1(self, l, x_src):
        nc = self.nc
        with contextlib.ExitStack() as st:
            sb = lambda n, s, d: self.sb(st, n, s, d)
            ps = lambda n, s, d: self.ps(st, n, s, d)
            WIN = sb("WIN", [128, 8, WINC], BF16)
            WIN.b.nowaw = True
            for k in range(8):
                rows = self.w_in[l, k * 128:(k + 1) * 128, :]
                self.dma("pool", WIN[:, k, 0:1536], rows[:, 0:1536])
                self.dma("pool", WIN[:, k, 1536:3072], rows[:, 1536:3072])
                self.dma("pool", WIN[:, k, 3200:3456], rows[:, 3088:3344])
            import os
            setup = int(os.environ.get("P1SETUP", "9"))
            if setup < 1:
                return
            wga = sb("wga", [128, 8, 16], F32)
            self.dma("sp", wga.v, self.w_in[l, :, 3072:3088].rearrange("(k p) r -> p k r", p=128))
            wa2 = sb("wa2", [16, 128], F32)
            self.dma("sp", wa2.v, self.gla_w_a2[l])
            identf = self.cst("ident")
            PJ = [ps(f"PJ{i}", [128, 512], F32) for i in range(2)]
            TP = [ps(f"TP{i}", [128, 1024], BF16) for i in range(2)]
            SC = ps("SC", [128, 512], F32)
            SC2 = ps("SC2", [128, 512], F32)
            OT = ps("OT", [64, 512], F32)
            MS = ps("MS", [128, 512], F32)
            gaT = sb("gaT", [16, 128], F32)
            for k in range(8):
                self.tr(SC[0:16, 0:128], wga[:, k, :], identf)
                self.cp("dve", gaT.v, SC[0:16, 0:128])
                self.mm(SC2[:, 0:128], gaT.v, wa2.v)
                self.cp("dve", WIN[:, k, 3072:3200], SC2[:, 0:128])
            if setup < 2:
                return
            G1 = sb("G1", [128, D], F32)
            self.dma("sp", G1.v, self.norm1_g[l].pbc(128))
            self.stt("dve", G1.v, self.mod(1), 1.0, G1.v, ALU.add, ALU.mult)
            SH1 = self.mod(0)
            GQ = sb("GQ", [128, 64], F32)
            GK = sb("GK", [128, 64], F32)
            self.dma("sp", GQ.v, self.diff_q_g[l].pbc(128))
            self.dma("sp", GK.v, self.diff_k_g[l].pbc(128))
            self.ts("dve", GQ.v, GQ.v, 0.125)
            BA = sb("BA", [128, 128], F32)
            self.dma("sp", BA.v, self.gla_b_a[l].pbc(128))
            GRN = sb("GRN", [64, 1], F32)
            GGN = sb("GGN", [64, 1], F32)
            self.dma("sp", GRN.v, self.ret_norm_g[l].unsqueeze(1))
            self.dma("sp", GGN.v, self.gla_norm_g[l].unsqueeze(1))
            onesf = self.cst("ones")
            if setup < 3:
                return
            SR = sb("SR", [64, 256], F32)
            SRb = sb("SRb", [64, 256], BF16)
            SG = sb("SG", [32, 256], F32)
            SGb = sb("SGb", [32, 256], BF16)
            for t_ in (SR, SRb, SG, SGb):
                self.memset("dve", t_.v, 0.0)
            XT = [sb(f"XT{i}", [128, D], F32) for i in range(2)]
            ssq = sb("ssq", [128, 1], F32)
            HF = sb("HF", [128, D], F32)
            HB = sb("HB", [128, D], BF16)
            junk = HB
            HT = sb("HT", [128, 8, 128], BF16)
            R1 = sb("R1", [128, 8, 32], F32)
            R2 = sb("R2", [128, 8, 32], F32)
            ROT = sb("ROT", [128, 8, 64], F32)
            QB = sb("QB", [128, 256], BF16)
            QDB = sb("QDB", [128, 256], BF16)
            KB = sb("KB", [128, 256], BF16)
            KD0 = sb("KD0", [128, 256], BF16)
            KD1 = sb("KD1", [128, 256], BF16)
            VR = sb("VR", [128, 256], BF16)
            SGR = sb("SGR", [128, 256], BF16)
            RT = sb("RT", [64, 12, 128], BF16)
            SGT = sb("SGT", [64, 8, 128], BF16)
            DSQ = sb("DSQ", [128, 512], F32)
            GS2 = sb("GS2", [128, 512], F32)
            DSS = sb("DSS", [128, 8], F32)
            DXN = sb("DXN", [128, 8, 64], F32)
            DQB = sb("DQB", [128, 512], BF16)
            DKB = sb("DKB", [128, 512], BF16)
            DT = sb("DT", [128, 8, 128], BF16)
            DVB = sb("DVB", [128, 512], BF16)
            D1 = sb("D1", [128, 8, 8], F32)
            D2 = sb("D2", [128, 8, 8], F32)
            AT = sb("AT", [128, 512], BF16)
            ZT = sb("ZT", [128, 128], F32)
            SP = sb("SP", [128, 128], F32)
            EPt = sb("EPt", [128, 128], F32)
            EMt = sb("EMt", [128, 128], F32)
            GQP = sb("GQP", [128, 128], BF16)
            GQM = sb("GQM", [128, 128], BF16)
            GKP = sb("GKP", [128, 128], BF16)
            GKM = sb("GKM", [128, 128], BF16)
            GKM0 = sb("GKM0", [128, 128], BF16)
            GKM1 = sb("GKM1", [128, 128], BF16)
            GVB = sb("GVB", [128, 256], BF16)
            SGG = sb("SGG", [128, 256], BF16)
            GT = sb("GT", [32, 16, 128], BF16)
            EGL = sb("EGL", [32, 8], F32)
            TMP1 = sb("TMP1", [128, 512], F32)
            OY = sb("OY", [64, 512], F32)
            MXR = sb("MXR", [64, 512], BF16)
            MXG = sb("MXG", [64, 512], BF16)
            KVT = sb("KVT", [64, 256], F32)

            ifr = self.cst("ifr")
            ifd = self.cst("ifd")
            TWO_PI = 2.0 * math.pi

            def post_norm(OTv, G, SGTv, MX):
                OSQ_ = TMP1[0:64, :]
                ORS_ = GS2[0:64, :]
                self.act(OSQ_, OTv, AF.Square)
                self.mm(MS[0:64, :], onesf[0:64, 0:64], OSQ_)
                self.rsqrt(ORS_, MS[0:64, :], 1.0 / 64, EPS)
                self.tt("dve", OY.v, OTv, ORS_, ALU.mult)
                self.stt("dve", MX.v, OY.v, G[:, 0:1], SGTv, ALU.mult, ALU.mult)

            import os
            cut = int(os.environ.get("P1CUT", "9"))
            ntl = int(os.environ.get("P1TILES", str(NT)))
            TPA, TPB = TP[0], TP[1]
            PROJ = [sb(f"PROJ{i}", [128, WINC], F32) for i in range(2)]

            class _PV:
                def __init__(self, tile, off, w):
                    self.tile, self.off, self.w = tile, off, w

                def __getitem__(self, k):
                    rows, cols = k
                    return self.tile[rows, self.off + cols.start:self.off + cols.stop]

                @property
                def v(self):
                    return self.tile[:, self.off:self.off + self.w]

            def stageA(i):
                xt = XT[i % 2]
                self.dma("sp", xt.v, x_src[i * 128:(i + 1) * 128, :] if l == 0 else self.out_tile(i))
                self.act(junk.v, xt.v, AF.Square, accum=ssq.v)
                self.rsqrt(ssq.v, ssq.v, 1.0 / D, EPS)
                self.stt("dve", HF.v, xt.v, ssq[:, 0:1], G1.v, ALU.mult, ALU.mult)
                self.tt("pool", HB.v, HF.v, SH1, ALU.add)
                tp = TPA
                for k in range(8):
                    self.tr(tp[:, k * 128:(k + 1) * 128], HB[:, k * 128:(k + 1) * 128], self.identb.v)
                self.cp("act", HT[:, 0:4, :], tp[:, 0:512].rearrange("p (k t) -> p k t", k=4))
                self.cp("dve", HT[:, 4:8, :], tp[:, 512:1024].rearrange("p (k t) -> p k t", k=4))
                yield
                P_ = PROJ[i % 2]
                for n, width in ((0, 512), (1, 512), (2, 512), (3, 512), (4, 512), (6, 384), (5, 512)):
                    p = PJ[n % 2]
                    for k in range(8):
                        self.mm(p[:, 0:width], HT[:, k, :], WIN[:, k, n * 512:n * 512 + width], start=(k == 0), stop=(k == 7))
                    self.cp("act" if n % 2 == 0 else "dve", P_[:, n * 512:n * 512 + width], p[:, 0:width])
                    yield

            def _hdr(i):
                def proj(n, width):
                    return _PV(PROJ[i % 2], n * 512, width)
                cosr = self.COS[:, i, 0:32].unsqueeze(1).bc([128, 8, 32])
                sinr = self.SIN[:, i, 0:32].unsqueeze(1).bc([128, 8, 32])
                cosd = self.COS[:, i, 32:40].unsqueeze(1).bc([128, 8, 8])
                sind = self.SIN[:, i, 32:40].unsqueeze(1).bc([128, 8, 8])
                return proj, cosr, sinr, cosd, sind

            def stageRG(i):
                proj, cosr, sinr, cosd, sind = _hdr(i)
                yield
                p0 = proj(0, 512)
                pv = p0.v.rearrange("p (g h w) -> p g h w", g=8, h=2)
                X1 = pv[:, :, 0, :]
                X2 = pv[:, :, 1, :]
                rv_ = ROT.v.rearrange("p g (h w) -> p g h w", h=2)
                self.tt("dve", R1.v, X1, cosr, ALU.mult)
                yield
                self.tt("dve", R2.v, X2, sinr, ALU.mult)
                yield
                self.tt("pool", rv_[:, :, 0, :], R1.v, R2.v, ALU.subtract)
                yield
                self.tt("dve", R1.v, X1, sinr, ALU.mult)
                yield
                self.tt("dve", R2.v, X2, cosr, ALU.mult)
                yield
                self.tt("pool", rv_[:, :, 1, :], R1.v, R2.v, ALU.add)
                yield
                rq = ROT[:, 0:4, :].rearrange("p g w -> p (g w)")
                rk = ROT[:, 4:8, :].rearrange("p g w -> p (g w)")
                self.cp("act", QB.v, rq)
                yield
                self.tt("pool", QDB.v, rq, self.cst("qd"), ALU.mult)
                yield
                self.act(KB.v, rk, AF.Copy, scale=0.125)
                yield
                self.tt("pool", KD0.v, rk, self.cst("kd0"), ALU.mult)
                yield
                self.tt("pool", KD1.v, rk, self.cst("kd1"), ALU.mult)
                yield
                if cut < 2:
                    return
                p1 = proj(1, 512)
                self.cp("act", VR.v, p1[:, 0:256])
                yield
                self.act(SGR.v, p1[:, 256:512], AF.Silu)
                yield
                if cut == 2 and os.environ.get("SUB", "9") == "0":
                    return
                tp = TPB
                for h in range(4):
                    self.tr(tp[0:64, h * 128:(h + 1) * 128], QB[:, h * 64:(h + 1) * 64], self.identb.v)
                    self.tr(tp[0:64, (4 + h) * 128:(5 + h) * 128], QDB[:, h * 64:(h + 1) * 64], self.identb.v)
                yield
                self.cp("dve", RT[:, 0:8, :], tp[0:64, :].rearrange("p (k t) -> p k t", k=8))
                yield
                if cut == 2 and os.environ.get("SUB", "9") == "1":
                    return
                tp = TPB
                for h in range(4):
                    self.tr(tp[0:64, h * 128:(h + 1) * 128], KB[:, h * 64:(h + 1) * 64], self.identb.v)
                    self.tr(tp[0:64, (4 + h) * 128:(5 + h) * 128], SGR[:, h * 64:(h + 1) * 64], self.identb.v)
                yield
                self.cp(os.environ.get("GRP2", "act"), RT[:, 8:12, :], tp[0:64, 0:512].rearrange("p (k t) -> p k t", k=4))
                yield
                self.cp("dve", SGT[:, 0:4, :], tp[0:64, 512:1024].rearrange("p (k t) -> p k t", k=4))
                yield
                if cut < 3:
                    return
                for h in range(4):
                    self.mm(SC[:, h * 128:(h + 1) * 128], RT[:, 8 + h, :], RT[:, h, :])
                yield
                self.tt("dve", AT.v, SC.v, self.cst("dr"), ALU.mult)
                yield
                for h in range(4):
                    o = OT[:, h * 128:(h + 1) * 128]
                    self.mm(o, VR[:, h * 64:(h + 1) * 64], AT[:, h * 128:(h + 1) * 128], start=(h == 0), stop=False)
                    self.mm(OT[:, h * 128:h * 128 + 64], SRb[:, h * 64:(h + 1) * 64], RT[:, 4 + h, 0:64], start=False, stop=False)
                yield
                for h in range(4):
                    self.mm(MS[0:64, h * 64:(h + 1) * 64], KD0[:, h * 64:(h + 1) * 64], VR[:, h * 64:(h + 1) * 64])
                    self.mm(MS[0:64, 256 + h * 64:256 + (h + 1) * 64], KD1[:, h * 64:(h + 1) * 64], VR[:, h * 64:(h + 1) * 64])
                yield
                self.tt("dve", KVT.v, SR.v, self.cst("cd")[0:64, :], ALU.mult)
                yield
                self.tt("dve", SR.v, KVT.v, MS[0:64, 0:256], ALU.add)
                yield
                self.cp("act", SRb.v, SR.v)
                yield
                for h in range(4):
                    self.mm(OT[:, h * 128 + 64:(h + 1) * 128], SRb[:, h * 64:(h + 1) * 64], RT[:, 4 + h, 64:128], start=False, stop=True)
                yield
                self.tt("dve", KVT.v, SR.v, self.cst("cd")[0:64, :], ALU.mult)
                yield
                self.tt("dve", SR.v, KVT.v, MS[0:64, 256:512], ALU.add)
                yield
                self.cp("act", SRb.v, SR.v)
                yield
                post_norm(OT.v, GRN, SGT[:, 0:4, :].rearrange("p k t -> p (k t)"), MXR)
                yield
                self.dma("act", self.mixT_s[0:256, i * 128:(i + 1) * 128].rearrange("(h p) t -> p h t", p=64),
                         MXR.v.rearrange("p (h t) -> p h t", h=4))
                yield

                if cut < 4:
                    return
                p6 = proj(6, 384)
                self.tt("dve", ZT.v, p6[:, 0:128], BA.v, ALU.add)
                yield
                self.act(SGG.v, p6[:, 128:384], AF.Silu)
                yield
                self.act(SP.v, ZT.v, AF.Exp, scale=-1.0)
                yield
                self.act(SP.v, SP.v, AF.Ln, bias=self.C_one[:, 0:1])
                yield
                self.mm(SC2[:, 0:128], self.cst("tri"), SP.v)
                yield
                self.act(EPt.v, SC2[:, 0:128], AF.Exp, scale=-1.0 / 16)
                yield
                self.act(EMt.v, SC2[:, 0:128], AF.Exp, scale=1.0 / 16)
                yield
                for h in range(4):
                    self.mm(MS[0:32, 2 * h:2 * h + 2], SP[:, h * 32:(h + 1) * 32], self.cst("chk"))
                yield
                self.act(EGL.v, MS[0:32, 0:8], AF.Exp, scale=-1.0 / 16)
                yield
                p5 = proj(5, 512)
                qs = 32.0 ** -0.5
                self.stt("dve", GQP.v, p5[:, 0:128], qs, EPt.v, ALU.mult, ALU.mult)
                yield
                self.stt("dve", GQM.v, p5[:, 0:128], qs, EMt.v, ALU.mult, ALU.mult)
                yield
                self.tt("dve", GKP.v, p5[:, 128:256], EPt.v, ALU.mult)
                yield
                self.tt("dve", GKM.v, p5[:, 128:256], EMt.v, ALU.mult)
                yield
                self.cp("act", GVB.v, p5[:, 256:512])
                yield
                self.ts("pool", GKM0.v, GKM.v, self.cst("m0")[:, 0:1], None, op0=ALU.mult)
                yield
                self.ts("pool", GKM1.v, GKM.v, self.cst("m1")[:, 0:1], None, op0=ALU.mult)
                yield
                tp = TPB
                for h in range(4):
                    self.tr(tp[0:32, h * 128:(h + 1) * 128], GQP[:, h * 32:(h + 1) * 32], self.identb.v)
                    self.tr(tp[0:32, (4 + h) * 128:(5 + h) * 128], GQM[:, h * 32:(h + 1) * 32], self.identb.v)
                yield
                self.cp("act", GT[:, 0:8, :], tp[0:32, :].rearrange("p (k t) -> p k t", k=8))
                yield
                tp = TPB
                for h in range(4):
                    self.tr(tp[0:32, h * 128:(h + 1) * 128], GKM[:, h * 32:(h + 1) * 32], self.identb.v)
                    self.tr(tp[0:32, (4 + h) * 128:(5 + h) * 128], GKP[:, h * 32:(h + 1) * 32], self.identb.v)
                yield
                self.cp("dve", GT[:, 8:16, :], tp[0:32, :].rearrange("p (k t) -> p k t", k=8))
                yield
                tp = TPB
                for h in range(4):
                    self.tr(tp[0:64, h * 128:(h + 1) * 128], SGG[:, h * 64:(h + 1) * 64], self.identb.v)
                yield
                self.cp("act", SGT[:, 4:8, :], tp[0:64, 0:512].rearrange("p (k t) -> p k t", k=4))
                yield
                for h in range(4):
                    self.mm(SC[:, h * 128:(h + 1) * 128], GT[:, 8 + h, :], GT[:, h, :])
                    self.mm(SC2[:, h * 128:(h + 1) * 128], GT[:, 12 + h, :], GT[:, 4 + h, :])
                yield
                self.tt("dve", TMP1.v, SC.v, self.cst("ml4"), ALU.mult)
                yield
                self.tt("dve", GS2.v, SC2.v, self.cst("mu4"), ALU.mult)
                yield
                self.tt("pool", AT.v, TMP1.v, GS2.v, ALU.add)
                yield
                for h in range(4):
                    self.mm(OT[:, h * 128:(h + 1) * 128], GVB[:, h * 64:(h + 1) * 64], AT[:, h * 128:(h + 1) * 128], start=(h == 0), stop=False)
                    self.mm(OT[:, h * 128:h * 128 + 64], SGb[:, h * 64:(h + 1) * 64], GT[:, h, 0:64], start=False, stop=False)
                yield
                for h in range(4):
                    self.mm(MS[0:32, h * 64:(h + 1) * 64], GKM0[:, h * 32:(h + 1) * 32], GVB[:, h * 64:(h + 1) * 64])
                    self.mm(MS[0:32, 256 + h * 64:256 + (h + 1) * 64], GKM1[:, h * 32:(h + 1) * 32], GVB[:, h * 64:(h + 1) * 64])
                yield
                eg = EGL.v.rearrange("p (h c) -> p h c", c=2)
                sgv = SG.v.rearrange("p (h w) -> p h w", h=4)
                kv3 = KVT[0:32, :].rearrange("p (h w) -> p h w", h=4)
                self.tt("dve", KVT[0:32, :], SG.v, MS[0:32, 0:256], ALU.add)
                yield
                self.tt("dve", sgv, kv3, eg[:, :, 0:1].bc([32, 4, 64]), ALU.mult)
                yield
                self.cp("act", SGb.v, SG.v)
                yield
                for h in range(4):
                    self.mm(OT[:, h * 128 + 64:(h + 1) * 128], SGb[:, h * 64:(h + 1) * 64], GT[:, h, 64:128], start=False, stop=True)
                yield
                self.tt("dve", KVT[0:32, :], SG.v, MS[0:32, 256:512], ALU.add)
                yield
                self.tt("dve", sgv, kv3, eg[:, :, 1:2].bc([32, 4, 64]), ALU.mult)
                yield
                self.cp("act", SGb.v, SG.v)
                yield
                post_norm(OT.v, GGN, SGT[:, 4:8, :].rearrange("p k t -> p (k t)"), MXG)
                yield
                self.dma("act", self.mixT_s[768:1024, i * 128:(i + 1) * 128].rearrange("(h p) t -> p h t", p=64),
                         MXG.v.rearrange("p (h t) -> p h t", h=4))
                yield


            def stageD(i):
                proj, cosr, sinr, cosd, sind = _hdr(i)
                yield
                def dqk(n, G, OUTB, dst):
                    pq = proj(n, 512)
                    self.act(DSQ.v, pq.v, AF.Square)
                    yield
                    self.red("dve", DSS.v, DSQ.v.rearrange("p (g w) -> p g w", g=8))
                    yield
                    self.rsqrt(DSS.v, DSS.v, 1.0 / 64, EPS)
                    yield
                    self.tt("dve", DXN.v, pq.v.rearrange("p (g w) -> p g w", g=8), DSS.v.unsqueeze(2).bc([128, 8, 64]), ALU.mult)
                    yield
                    self.tt("pool", DXN.v, DXN.v, G.v.unsqueeze(1).bc([128, 8, 64]), ALU.mult)
                    yield
                    ob = OUTB.v.rearrange("p (g w) -> p g w", g=8)
                    self.cp("act", ob[:, :, 16:64], DXN[:, :, 16:64])
                    yield
                    x1 = DXN[:, :, 0:8]
                    x2 = DXN[:, :, 8:16]
                    self.tt("dve", D1.v, x1, cosd, ALU.mult)
                    yield
                    self.tt("pool", D2.v, x2, sind, ALU.mult)
                    yield
                    self.tt("dve", ob[:, :, 0:8], D1.v, D2.v, ALU.subtract)
                    yield
                    self.tt("dve", D1.v, x1, sind, ALU.mult)
                    yield
                    self.tt("pool", D2.v, x2, cosd, ALU.mult)
                    yield
                    self.tt("dve", ob[:, :, 8:16], D1.v, D2.v, ALU.add)
                    yield
                    tp = TPA
                    for h in range(4):
                        self.tr(tp[:, h * 128:(h + 1) * 128], OUTB[:, h * 128:(h + 1) * 128], self.identb.v)
                    dtv = DT[:, 0:4, :] if n == 2 else DT[:, 4:8, :]
                    self.cp("act" if n == 2 else "dve", dtv, tp[:, 0:512].rearrange("p (k t) -> p k t", k=4))
                    yield
                    self.dma("sp", dst[:, :, i * 128:(i + 1) * 128].rearrange("h p t -> p h t"), dtv)
                    yield
                yield from dqk(2, GQ, DQB, self.qT_s)
                yield from dqk(3, GK, DKB, self.kT_s)
                p4 = proj(4, 512)
                self.cp("act", DVB.v, p4.v)
                yield
                self.dma("act", self.v_s[i * 128:(i + 1) * 128, :], DVB.v)
                yield

                if cut < 5:
                    return

            for _ in stageA(0):
                pass
            for i in range(ntl):
                self.run_bg(2)
                gens = [stageRG(i), stageD(i)]
                if i + 1 < ntl:
                    gens.append(stageA(i + 1))
                while gens:
                    for g in list(gens):
                        try:
                            next(g)
                        except StopIteration:
                            gens.remove(g)


    def phase2(self, l):
        lam_init = 0.8 - 0.6 * math.exp(-0.3 * l)
        with contextlib.ExitStack() as st:
            sb = lambda n, s, d: self.sb(st, n, s, d)
            ps = lambda n, s, d: self.ps(st, n, s, d)
            KT = sb("KT", [128, 2, S], BF16)
            KT.b.nowaw = True
            VV = sb("VV", [128, NT, 256], BF16)
            VV.b.nowaw = True
            vsrc = self.v_s.v.rearrange("(i p) c -> p i c", p=128)

            def load_group(hg):
                for hh in range(2):
                    for j in range(4):
                        self.dma("sp" if (hh + j) % 2 == 0 else "act", KT[:, hh, j * 2048:(j + 1) * 2048], self.kT_s[2 * hg + hh, :, j * 2048:(j + 1) * 2048])
                for j in range(8):
                    self.dma("sp" if j % 2 == 0 else "act", VV[:, j * 8:(j + 1) * 8, :], vsrc[:, j * 8:(j + 1) * 8, hg * 256:(hg + 1) * 256])
            lq = sb("lq", [1, 4, 64], F32)
            for j, src in enumerate((self.lam_q1, self.lam_k1, self.lam_q2, self.lam_k2)):
                self.dma("sp", lq[:, j, :], src[l:l + 1, :])
            lp = sb("lp", [1, 2, 64], F32)
            self.tt("dve", lp[:, 0, :], lq[:, 0, :], lq[:, 1, :], ALU.mult)
            self.tt("dve", lp[:, 1, :], lq[:, 2, :], lq[:, 3, :], ALU.mult)
            ls = sb("ls", [1, 2], F32)
            self.red("dve", ls.v, lp.v)
            self.act(ls.v, ls.v, AF.Exp)
            lam1 = sb("lam1", [1, 1], F32)
            self.tt("dve", lam1.v, ls[:, 0:1], ls[:, 1:2], ALU.subtract)
            self.ts("dve", lam1.v, lam1.v, lam_init, -1.0, op0=ALU.add, op1=ALU.mult)
            NSB = 4
            SPS = [ps(f"SPS{i}", [128, 512], F32) for i in range(NSB)]
            OP = [ps(f"OP{i}", [128, 512], F32) for i in range(2)]
            MSP = ps("MSP", [128, 512], F32)
            MSP2 = ps("MSP2", [128, 512], F32)
            NLAM = sb("NLAM", [128, 1], F32)
            self.mm(MSP[:, 0:1], self.cst("ones")[0:1, :], lam1.v)
            self.cp("dve", NLAM.v, MSP[:, 0:1])
            SUBG = sb("SUBG", [128, 1], F32)
            self.dma("sp", SUBG.v, self.diff_sub_g[l].unsqueeze(1))
            self.ts("dve", SUBG.v, SUBG.v, 1.0 - lam_init)
            QT = [sb(f"QT{i}", [128, 512], BF16) for i in range(2)]
            PT = [sb(f"PT{i}", [128, 512], BF16) for i in range(4)]
            ACC = [[sb(f"ACC{p_}{c}", [128, 512], F32) for c in range(2)] for p_ in range(2)]
            OS = [sb(f"OS{c}", [128, 512], F32) for c in range(2)]
            R0 = sb("R0", [128, 512], F32)
            R1 = sb("R1", [128, 512], F32)
            ACB = [sb(f"ACB{c}", [128, 512], BF16) for c in range(2)]
            T0 = sb("T0", [128, 512], F32)
            T1 = sb("T1", [128, 512], F32)
            OSQ = sb("OSQ2", [128, 512], BF16)
            ORS = sb("ORS2", [128, 512], F32)
            OB = [sb(f"OB{i}", [128, 512], BF16) for i in range(2)]
            onesf = self.cst("ones")
            import os
            nqt = int(os.environ.get("P2QT", str(S // 512)))
            groups = [(a, b_, c_) for a in range(2) for b_ in range(nqt) for c_ in range(2)]
            units = []
            for gi, (hg, qt, hh) in enumerate(groups):
                nk = 4 * qt + 4
                for kt in range(nk):
                    for c in range(2):
                        units.append((gi, c, kt, nk))
            LOOK = 2
            qloaded = set()

            def load_q(gi):
                if gi >= len(groups) or gi in qloaded:
                    return
                qloaded.add(gi)
                hg, qt, hh = groups[gi]
                if qt == 0 and hh == 0:
                    load_group(hg)
                self.dma("sp", QT[gi % 2].v, self.qT_s[2 * hg + hh, :, qt * 512:(qt + 1) * 512])

            def c0_of(gi, kt):
                qt = groups[gi][1]
                m = kt - 4 * qt
                return (128 * m if m > 0 else 0), m

            def issue_qk(u):
                gi, c, kt, nk = units[u]
                load_q(gi)
                hh = groups[gi][2]
                c0, m = c0_of(gi, kt)
                self.mm(SPS[u % NSB][:, c0:512], KT[64 * c:64 * c + 64, hh, kt * 128:(kt + 1) * 128], QT[gi % 2][64 * c:64 * c + 64, c0:512])

            def finish(gi):
                hg, qt, hh = groups[gi]
                h = 2 * hg + hh
                acc = ACC[gi % 2]
                self.cp("act", OS[0].v, OP[0].v)
                self.cp("act", OS[1].v, OP[1].v)
                self.cp("dve", ACB[0].v, acc[0].v)
                self.cp("dve", ACB[1].v, acc[1].v)
                self.mm(MSP.v, self.onesb.v, ACB[0].v)
                self.mm(MSP2.v, self.onesb.v, ACB[1].v)
                self.act(R0.v, MSP.v, AF.Ln)
                self.act(R0.v, R0.v, AF.Exp, scale=-1.0)
                self.act(R1.v, MSP2.v, AF.Ln)
                self.act(R1.v, R1.v, AF.Exp, scale=-1.0)
                self.tt("dve", T0.v, OS[0].v, R0.v, ALU.mult)
                self.tt("dve", T1.v, OS[1].v, R1.v, ALU.mult)
                self.stt("dve", T0.v, T1.v, NLAM[:, 0:1], T0.v, ALU.mult, ALU.add)
                self.act(OSQ.v, T0.v, AF.Square)
                self.mm(MSP.v, self.onesb.v, OSQ.v)
                self.act(ORS.v, MSP.v, AF.Ln, scale=1.0 / 128, bias=self.C_eps[:, 0:1])
                self.act(ORS.v, ORS.v, AF.Exp, scale=-0.5)
                ob = OB[gi % 2]
                self.stt("dve", ob.v, T0.v, SUBG[:, 0:1], ORS.v, ALU.mult, ALU.mult)
                self.dma("act", self.mixT_s[256 + 128 * h:256 + 128 * (h + 1), qt * 512:(qt + 1) * 512], ob.v)

            def hg_of(u):
                return groups[units[u][0]][0]

            for u, (gi, c, kt, nk) in enumerate(units):
                if u == 0 or hg_of(u - 1) != hg_of(u):
                    for u2 in range(u, min(u + LOOK, len(units))):
                        if hg_of(u2) == hg_of(u):
                            issue_qk(u2)
                if u % 2 == 0:
                    for u2 in (u + LOOK, u + LOOK + 1):
                        if u2 < len(units) and hg_of(u2) == hg_of(u):
                            issue_qk(u2)
                hh = groups[gi][2]
                c0, m = c0_of(gi, kt)
                pt = PT[u % 4]
                if kt == 0 and c == 0 and gi + 1 < len(groups) and groups[gi + 1][0] == groups[gi][0]:
                    load_q(gi + 1)
                self.act(pt[:, c0:512], SPS[u % NSB][:, c0:512], AF.Exp)
                if m >= 0:
                    self.memset("pool", pt[64:128, c0:c0 + 64], 0.0)
                self.mm(OP[c][:, c0:512], VV[:, kt, hh * 128:(hh + 1) * 128], pt[:, c0:512], start=(kt == 0), stop=(kt == nk - 1))
                a = ACC[gi % 2][c]
                if kt == 0:
                    self.cp("dve", a.v, pt.v)
                else:
                    self.tt("dve", a[:, c0:512], a[:, c0:512], pt[:, c0:512], ALU.add)
                if kt == nk - 1 and c == 1:
                    finish(gi)

    def phase3(self, l, x_src):
        with contextlib.ExitStack() as st:
            sb = lambda n, s, d: self.sb(st, n, s, d)
            ps = lambda n, s, d: self.ps(st, n, s, d)
            WO = sb("WO", [128, 8, D], BF16)
            WO.b.nowaw = True
            for k in range(8):
                self.dma("pool", WO[:, k, :], self.w_out[l, k * 128:(k + 1) * 128, :])
            G2 = sb("G2", [128, D], F32)
            self.dma("sp", G2.v, self.norm2_g[l].pbc(128))
            self.stt("dve", G2.v, self.mod(4), 1.0, G2.v, ALU.add, ALU.mult)
            SH2 = self.mod(3)
            GT1 = self.mod(2)
            RW = sb("RW", [128, 8, NE], F32)
            self.dma("sp", RW.v, self.router_w.v.rearrange("(k p) e -> p k e", p=128))
            MT = [sb(f"MT{i}", [128, 8, 512], BF16) for i in range(2)]
            XT = [sb(f"X3{i}", [128, D], F32) for i in range(2)]
            X1 = [sb(f"X1{i}", [128, D], F32) for i in range(2)]
            TMs = [sb(f"TM3{i}", [128, D], F32) for i in range(2)]
            junks = [sb(f"junk3{i}", [128, D], BF16) for i in range(2)]
            ssqs = [sb(f"ssq3{i}", [128, 1], F32) for i in range(2)]
            H2Fs = [sb(f"H2F{i}", [128, D], F32) for i in range(2)]
            H2B = [sb(f"H2B{i}", [128, D], BF16) for i in range(2)]
            H2Ts = [sb(f"H2T{i}", [128, D], F32) for i in range(2)]
            POs = [ps(f"PO{i}", [128, 512], F32) for i in range(2)]
            PTRs = [ps(f"PTR{i}", [128, 512], F32) for i in range(2)]
            PLs = [ps(f"PL{i}", [128, NE], F32) for i in range(2)]
            identf = self.cst("ident")
            mview = self.mixT_s.v.rearrange("(k p) t -> p k t", p=128)

            def tile3(i):
                s_ = i % 2
                TM, junk, ssq, H2F, H2T, PO, PTR, PL = TMs[s_], junks[s_], ssqs[s_], H2Fs[s_], H2Ts[s_], POs[s_], PTRs[s_], PLs[s_]
                mt = MT[(i // 4) % 2]
                if i % 4 == 0:
                    self.dma("sp", mt.v, mview[:, :, i * 128:i * 128 + 512])
                xt = XT[i % 2]
                x1 = X1[i % 2]
                self.dma("act", xt.v, x_src[i * 128:(i + 1) * 128, :] if l == 0 else self.out_tile(i))
                yield
                tcol = (i % 4) * 128
                for hf in range(2):
                    for k in range(8):
                        self.mm(PO.v, mt[:, k, tcol:tcol + 128], WO[:, k, hf * 512:(hf + 1) * 512], start=(k == 0), stop=(k == 7))
                    self.tt("dve", TM[:, hf * 512:(hf + 1) * 512], PO.v, GT1[:, hf * 512:(hf + 1) * 512], ALU.mult)
                    yield
                self.tt("pool", x1.v, TM.v, xt.v, ALU.add)
                yield
                self.dma("sp", self.out_tile(i), x1.v)
                self.act(junk.v, x1.v, AF.Square, accum=ssq.v)
                yield
                self.rsqrt(ssq.v, ssq.v, 1.0 / D, EPS)
                yield
                self.stt("dve", H2F.v, x1.v, ssq[:, 0:1], G2.v, ALU.mult, ALU.mult)
                yield
                self.tt("pool", H2F.v, H2F.v, SH2, ALU.add)
                yield
                hb = H2B[i % 2]
                self.cp("act", hb.v, H2F.v)
                self.dma("act", self.h2_s[i * 128:(i + 1) * 128, :], hb.v)
                yield
                for hf in range(2):
                    for k in range(4):
                        kk = hf * 4 + k
                        self.tr(PTR[:, k * 128:(k + 1) * 128], H2F[:, kk * 128:(kk + 1) * 128], identf)
                    self.cp("dve" if hf == 0 else "act", H2T[:, hf * 512:(hf + 1) * 512], PTR.v)
                    yield
                for k in range(8):
                    self.mm(PL.v, H2T[:, k * 128:(k + 1) * 128], RW[:, k, :], start=(k == 0), stop=(k == 7))
                self.cp("dve", self.LOG[:, i, :], PL.v)
                yield

            for pi in range(NT // 2):
                gens = [tile3(2 * pi), tile3(2 * pi + 1)]
                while gens:
                    for g in list(gens):
                        try:
                            next(g)
                        except StopIteration:
                            gens.remove(g)
            self.dump(f"log{l}", self.LOG.v, [128, NT, NE])

    def phase_moe(self, l):
        import os
        BIG = 1.0e30
        NTE = NT * NE
        with contextlib.ExitStack() as rst:
            rsb = lambda n, s_, d: self.sb(rst, n, s_, d)
            RG1 = rsb("RG1", [128, NT], F32)
            RG2 = rsb("RG2", [128, NT], F32)
            D1I = rsb("D1I", [128, NT], I32)
            D2I = rsb("D2I", [128, NT], I32)
            IDXW = rsb("IDXW", [128, NB, 12], I32)
            with contextlib.ExitStack() as st:
                sb = lambda n, s_, d: self.sb(st, n, s_, d)
                ps = lambda n, s_, d: self.ps(st, n, s_, d)
                RB = sb("RB", [128, NE], F32)
                self.dma("sp", RB.v, self.router_b.v.pbc(128))
                SCO = sb("SCO", [128, NT, NE], F32)
                BIA = sb("BIA", [128, NT, NE], F32)
                TA = sb("TA", [128, NT, NE], F32)
                TB = sb("TB", [128, NT, NE], F32)
                OH1 = sb("OH1", [128, NT, NE], F32)
                OH2 = sb("OH2", [128, NT, NE], F32)
                M1 = sb("M1", [128, NT * 8], F32)
                M2 = sb("M2", [128, NT * 8], F32)
                GM = sb("GM", [128, NT], F32)
                V1 = sb("V1", [128, NT], F32)
                W1 = sb("W1", [128, NT], F32)
                W2 = sb("W2", [128, NT], F32)
                self.act(SCO.v, self.LOG.v, AF.Sigmoid)
                self.tt("dve", BIA.v, SCO.v, RB.v.unsqueeze(1).bc([128, NT, NE]), ALU.add)
                b4 = BIA.v.rearrange("p i (g k) -> p (i g) k", k=4)
                self.red("dve", M1.v, b4, op=ALU.max)
                ta4 = TA.v.rearrange("p i (g k) -> p (i g) k", k=4)
                self.tt("dve", ta4, b4, M1.v.unsqueeze(2).bc([128, NT * 8, 4]), ALU.is_equal)
                self.stt("dve", TA.v, TA.v, -BIG, BIA.v, ALU.mult, ALU.add)
                self.red("dve", M2.v, ta4, op=ALU.max)
                self.tt("dve", M1.v, M1.v, M2.v, ALU.add)
                gs3 = M1.v.rearrange("p (i g) -> p i g", g=8)
                self.red("dve", GM.v, gs3, op=ALU.max)
                self.tt("dve", M2.v.rearrange("p (i g) -> p i g", g=8), gs3, GM.v.unsqueeze(2).bc([128, NT, 8]), ALU.is_equal)
                self.ts("dve", ta4, M2.v.unsqueeze(2).bc([128, NT * 8, 4]), BIG, -BIG, op0=ALU.mult, op1=ALU.add)
                self.tt("dve", TA.v, TA.v, BIA.v, ALU.add)
                self.red("dve", V1.v, TA.v, op=ALU.max)
                self.tt("dve", OH1.v, TA.v, V1.v.unsqueeze(2).bc([128, NT, NE]), ALU.is_equal)
                self.stt("dve", TB.v, OH1.v, -BIG, TA.v, ALU.mult, ALU.add)
                self.red("dve", V1.v, TB.v, op=ALU.max)
                self.tt("dve", OH2.v, TB.v, V1.v.unsqueeze(2).bc([128, NT, NE]), ALU.is_equal)
                self.tt("dve", TA.v, SCO.v, OH1.v, ALU.mult)
                self.red("dve", W1.v, TA.v)
                self.tt("dve", TA.v, SCO.v, OH2.v, ALU.mult)
                self.red("dve", W2.v, TA.v)
                self.tt("dve", V1.v, W1.v, W2.v, ALU.add)
                self.recip(V1.v, V1.v)
                self.tt("dve", RG1.v, W1.v, V1.v, ALU.mult)
                self.tt("dve", RG2.v, W2.v, V1.v, ALU.mult)
                MS_ = sb("MSEL", [128, NT, NE], BF16)
                self.tt("dve", MS_.v, OH1.v, OH2.v, ALU.add)
                LTSb = sb("LTSb", [128, 128], BF16)
                self.cp("dve", LTSb.v, self.cst("lts"))
                PRE = sb("PRE", [128, NT, NE], F32)
                TOT = sb("TOT", [128, NT, NE], F32)
                PP = [ps(f"PP{i}", [128, 512], F32) for i in range(2)]
                msf = MS_.v.rearrange("p i e -> p (i e)")
                pre_f = PRE.v.rearrange("p i e -> p (i e)")
                tot_f = TOT.v.rearrange("p i e -> p (i e)")
                for c in range(NTE // 512):
                    self.mm(PP[0].v, LTSb.v, msf[:, c * 512:(c + 1) * 512])
                    self.cp("dve", pre_f[:, c * 512:(c + 1) * 512], PP[0].v)
                    self.mm(PP[1].v, self.onesb.v, msf[:, c * 512:(c + 1) * 512])
                    self.cp("act", tot_f[:, c * 512:(c + 1) * 512], PP[1].v)
                A_, B_ = TA, TB
                self.cp("dve", A_.v, TOT.v)
                k = 1
                while k < NT:
                    self.tt("dve", B_[:, k:, :], A_[:, k:, :], A_[:, :NT - k, :], ALU.add)
                    self.cp("dve", B_[:, :k, :], A_[:, :k, :])
                    A_, B_ = B_, A_
                    k *= 2
                INC = A_
                OFF = B_
                self.tt("dve", OFF.v, INC.v, TOT.v, ALU.subtract)
                CNT = INC[:, NT - 1, :]
                CMP = sb("CMP", [128, NE, 32], F32)
                self.tt("dve", CMP.v, CNT.unsqueeze(2).bc([128, NE, 32]), self.cst("jb")[:, 0:32].unsqueeze(1).bc([128, NE, 32]), ALU.is_gt)
                PADE = sb("PADE", [128, NE], F32)
                self.red("dve", PADE.v, CMP.v)
                self.ts("dve", PADE.v, PADE.v, float(BLK))
                EA = sb("EA", [128, NE], F32)
                EB = sb("EB", [128, NE], F32)
                self.cp("dve", EA.v, PADE.v)
                a_, b_ = EA, EB
                k = 1
                while k < NE:
                    self.tt("dve", b_[:, k:], a_[:, k:], a_[:, :NE - k], ALU.add)
                    self.cp("dve", b_[:, :k], a_[:, :k])
                    a_, b_ = b_, a_
                    k *= 2
                PEND = a_
                PST = b_
                self.tt("dve", PST.v, PEND.v, PADE.v, ALU.subtract)
                self.tt("dve", PRE.v, PRE.v, OFF.v, ALU.add)
                self.tt("dve", PRE.v, PRE.v, PST.v.unsqueeze(1).bc([128, NT, NE]), ALU.add)
                self.tt("dve", TOT.v, PRE.v, OH1.v, ALU.mult)
                self.red("dve", W1.v, TOT.v)
                self.tt("dve", TOT.v, PRE.v, OH2.v, ALU.mult)
                self.red("dve", W2.v, TOT.v)
                self.cp("dve", D1I.v, W1.v)
                self.cp("dve", D2I.v, W2.v)
                CM2 = sb("CM2", [128, NB, NE], F32)
                self.tt("dve", CM2.v, PEND.v.unsqueeze(1).bc([128, NB, NE]), self.cst("jb").unsqueeze(2).bc([128, NB, NE]), ALU.is_le)
                BE = sb("BE", [128, NB], F32)
                self.red("dve", BE.v, CM2.v)
                self.ts("dve", BE.v, BE.v, float(NE - 1), None, op0=ALU.min)
                IDXF = sb("IDXF", [128, NB, 12], F32)
                coff = self.cst("coff")
                self.stt("dve", IDXF[:, :, 0:8], BE.v.unsqueeze(2).bc([128, NB, 8]), float(D), coff[:, 0:8].unsqueeze(1).bc([128, NB, 8]), ALU.mult, ALU.add)
                self.stt("dve", IDXF[:, :, 8:12], BE.v.unsqueeze(2).bc([128, NB, 4]), float(DE), coff[:, 8:12].unsqueeze(1).bc([128, NB, 4]), ALU.mult, ALU.add)
                self.cp("dve", IDXW.v, IDXF.v)
                if self.debug:
                    self.dump(f"d1_{l}", W1.v, [128, NT])
                    self.dump(f"d2_{l}", W2.v, [128, NT])
                    self.dump(f"g1_{l}", RG1.v, [128, NT])
                    self.dump(f"g2_{l}", RG2.v, [128, NT])
                    self.dump(f"be_{l}", BE.v, [128, NB])
            self.fw.barrier()
            if os.environ.get("MOECUT", "9") == "0":
                return
            with contextlib.ExitStack() as st:
                sb = lambda n, s_, d: self.sb(st, n, s_, d)
                HB = [sb(f"HBm{i}", [128, D], BF16) for i in range(3)]
                for i in range(NT):
                    hb = HB[i % 3]
                    self.dma("sp", hb.v, self.h2_s[i * 128:(i + 1) * 128, :])
                    self.scatter(self.xs_s.v, hb.v, D1I[:, i:i + 1])
                    self.scatter(self.xs_s.v, hb.v, D2I[:, i:i + 1])
            self.fw.barrier()
            if os.environ.get("MOECUT", "9") == "1":
                return
            with contextlib.ExitStack() as st:
                sb = lambda n, s_, d: self.sb(st, n, s_, d)
                ps = lambda n, s_, d: self.ps(st, n, s_, d)
                WGU = [sb(f"WGU{i}", [128, 8, 2 * DE], BF16) for i in range(2)]
                WD = [sb(f"WD{i}", [128, 4, D], BF16) for i in range(2)]
                for t_ in WGU + WD:
                    t_.b.nowaw = True
                XB = [sb(f"XB{i}", [128, NSUB, D], BF16) for i in range(2)]
                XTB = [sb(f"XTB{i}", [128, 8, BLK], BF16) for i in range(2)]
                SGt = sb("SGt", [128, BLK], F32)
                UT = sb("UT", [128, 4, BLK], BF16)
                YB = [sb(f"YB{i}", [128, D], F32) for i in range(2)]
                TPX = ps("TPX", [128, 1024], BF16)
                PG = [ps(f"PG{i}", [128, BLK], F32) for i in range(2)]
                PU = [ps(f"PU{i}", [128, BLK], F32) for i in range(2)]
                PD = [ps(f"PD{i}", [128, 512], F32) for i in range(2)]
                wgu_t = self.wgub[l].v
                wd_t = self.wdb[l].v
                nblk = int(os.environ.get("MOEBLK", str(NB)))
                yi = 0
                for j in range(nblk):
                    wgu, wd, xb, xtb = WGU[j % 2], WD[j % 2], XB[j % 2], XTB[j % 2]
                    for c in range(8):
                        self.gather(wgu[:, c, :], wgu_t, IDXW[:, j, c:c + 1])
                    for c in range(4):
                        self.gather(wd[:, c, :], wd_t, IDXW[:, j, 8 + c:9 + c])
                    self.dma("sp", xb.v, self.xs_s[j * BLK:(j + 1) * BLK, :].rearrange("(s p) d -> p s d", p=128))
                    for sub in range(NSUB):
                        for k in range(8):
                            self.tr(TPX[:, k * 128:(k + 1) * 128], xb[:, sub, k * 128:(k + 1) * 128], self.identb.v)
                        self.cp("dve" if sub % 2 == 0 else "act", xtb[:, :, sub * 128:(sub + 1) * 128], TPX.v.rearrange("p (k t) -> p k t", k=8))
                    for fc in range(4):
                        pg, pu = PG[fc % 2], PU[fc % 2]
                        for k in range(8):
                            self.mm(pg.v, wgu[:, k, fc * 128:(fc + 1) * 128], xtb[:, k, :], start=(k == 0), stop=(k == 7))
                        for k in range(8):
                            self.mm(pu.v, wgu[:, k, DE + fc * 128:DE + (fc + 1) * 128], xtb[:, k, :], start=(k == 0), stop=(k == 7))
                        self.act(SGt.v, pg.v, AF.Silu)
                        self.tt("dve", UT[:, fc, :], SGt.v, pu.v, ALU.mult)
                    for sub in range(NSUB):
                        yb = YB[yi % 2]
                        yi += 1
                        for hf in range(2):
                            pd = PD[hf]
                            for fc in range(4):
                                self.mm(pd.v, UT[:, fc, sub * 128:(sub + 1) * 128], wd[:, fc, hf * 512:(hf + 1) * 512], start=(fc == 0), stop=(fc == 3))
                            self.cp("act" if hf == 0 else "dve", yb[:, hf * 512:(hf + 1) * 512], pd.v)
                        self.dma("act", self.ys_s[j * BLK + sub * 128:j * BLK + (sub + 1) * 128, :], yb.v)
            self.fw.barrier()
            if os.environ.get("MOECUT", "9") == "2":
                return
            with contextlib.ExitStack() as st:
                sb = lambda n, s_, d: self.sb(st, n, s_, d)
                GT2 = self.mod(5)
                Y1 = [sb(f"Y1{i}", [128, D], F32) for i in range(2)]
                Y2 = [sb(f"Y2{i}", [128, D], F32) for i in range(2)]
                XX = [sb(f"XX{i}", [128, D], F32) for i in range(2)]
                TT = [sb(f"TT{i}", [128, D], F32) for i in range(2)]
                for i in range(NT):
                    y1, y2, xx, tt_ = Y1[i % 2], Y2[i % 2], XX[i % 2], TT[i % 2]
                    self.gather(y1.v, self.ys_s.v, D1I[:, i:i + 1])
                    self.gather(y2.v, self.ys_s.v, D2I[:, i:i + 1])
                    self.dma("sp", xx.v, self.out_tile(i))
                    self.ts("dve", tt_.v, y1.v, RG1[:, i:i + 1], None, op0=ALU.mult)
                    self.stt("dve", tt_.v, y2.v, RG2[:, i:i + 1], tt_.v, ALU.mult, ALU.add)
                    if self.debug and l == 0:
                        self.dma("act", self.ydbg[i * 128:(i + 1) * 128, :], tt_.v)
                    self.tt("pool", tt_.v, tt_.v, GT2, ALU.mult)
                    self.tt("pool", xx.v, xx.v, tt_.v, ALU.add)
                    self.dma("sp", self.out_tile(i), xx.v)


_CACHE = {}


def _get_prog(stop_after=None, debug=False):
    key = (stop_after, debug)
    if key not in _CACHE:
        p = Prog(stop_after=stop_after, debug=debug)
        p.build()
        _CACHE[key] = p
    return _CACHE[key]


def make_in_map(inputs, b):
    f = lambda a: np.ascontiguousarray(a, dtype=np.float32)
    m = {
        "x": f(inputs["x"][b]),
        "c": f(np.asarray(inputs["c"][b]).reshape(8, 128).T),
        "pos": np.ascontiguousarray(np.asarray(inputs["positions"][b]).reshape(NT, 128).T.astype(np.int32)),
        "consts": CONST_NP,
    }
    for k in ("w_mod", "b_mod", "norm1_g", "norm2_g", "w_in", "ret_norm_g", "diff_q_g", "diff_k_g", "lam_q1", "lam_k1",
              "lam_q2", "lam_k2", "diff_sub_g", "gla_w_a2", "gla_b_a", "gla_norm_g", "w_out", "router_w", "router_b"):
        m[k] = f(inputs[k])
    for l in range(DEPTH):
        m[f"w_gate{l}"] = f(inputs["w_gate"][l]).reshape(NE * D, DE)
        m[f"w_up{l}"] = f(inputs["w_up"][l]).reshape(NE * D, DE)
        m[f"w_down{l}"] = f(inputs["w_down"][l]).reshape(NE * DE, D)
    return m


def kernel(**inputs):
    prog = _get_prog()
    shared = make_in_map(inputs, 0)
    in_maps = []
    for b in range(8):
        m = dict(shared)
        m["x"] = np.ascontiguousarray(inputs["x"][b], dtype=np.float32)
        m["c"] = np.ascontiguousarray(np.asarray(inputs["c"][b], dtype=np.float32).reshape(8, 128).T)
        m["pos"] = np.ascontiguousarray(np.asarray(inputs["positions"][b]).reshape(NT, 128).T.astype(np.int32))
        in_maps.append(m)
    res = run_bass_kernel_spmd(prog.nc, in_maps, core_ids=list(range(8)))
    return np.stack([np.asarray(r["out"], dtype=np.float32) for r in res.results], axis=0)
```

```python
import math
import contextlib
import numpy as np
import concourse.bass as bass
import concourse.mybir as mybir
from concourse.bass_utils import run_bass_kernel_spmd

F32 = mybir.dt.float32
BF16 = mybir.dt.bfloat16
I32 = mybir.dt.int32
ALU = mybir.AluOpType
AF = mybir.ActivationFunctionType
AX = mybir.AxisListType

S = 8192
D = 1024
NT = S // 128
DEPTH = 2
EPS = 1e-6
NE = 32
DE = 512
BLK = 256
NSUB = BLK // 128
NB = -(-(2 * S) // BLK) + NE
NPAD = NB * BLK
SEM_LIMIT = 30000
WINC = 3456


class Buf:
    __slots__ = ("name", "w", "r", "excl", "nowaw")

    def __init__(self, name="", excl=False):
        self.name = name
        self.w = {}
        self.r = {}
        self.excl = excl
        self.nowaw = False


class V:
    __slots__ = ("ap", "b")

    def __init__(self, ap, b):
        self.ap = ap
        self.b = b

    def __getitem__(self, k):
        return V(self.ap[k], self.b)

    def rearrange(self, s, **kw):
        return V(self.ap.rearrange(s, **kw), self.b)

    def unsqueeze(self, a):
        return V(self.ap.unsqueeze(a), self.b)

    def bc(self, shape):
        return V(self.ap.broadcast_to(list(shape)), self.b)

    def pbc(self, n):
        return V(self.ap.partition_broadcast(n), self.b)

    def bitcast(self, dt):
        return V(self.ap.bitcast(dt), self.b)


class Tile:
    def __init__(self, t, name):
        self.t = t
        self.b = Buf(name)

    def __getitem__(self, k):
        return V(self.t[k], self.b)

    @property
    def v(self):
        return self[:]


class EngS:
    def __init__(self, name, handle):
        self.name = name
        self.h = handle
        self.sem = None
        self.n = 0
        self.seen = {}
        self.dma_sems = []
        self.dma_cnt = []
        self.dma_i = 0


class FW:
    def __init__(self, nc, ndma=10, same=True):
        self.nc = nc
        self.nsem = 0
        self.same = same
        self.E = {"pe": EngS("pe", nc.tensor), "dve": EngS("dve", nc.vector),
                  "act": EngS("act", nc.scalar), "pool": EngS("pool", nc.gpsimd),
                  "sp": EngS("sp", nc.sync)}
        self.ndma = ndma
        self.owner = {}
        self.nwaits = 0
        self.nops = 0

    def new_sem(self):
        self.nsem += 1
        return self.nc.alloc_semaphore(f"fs{self.nsem}")

    def _tok(self, E):
        if E.sem is None or E.n >= SEM_LIMIT:
            E.sem = self.new_sem()
            E.n = 0
            self.owner[E.sem] = E.name
        E.n += 1
        return (E.sem, E.n)

    def _wait(self, E, deps):
        for sem, val in deps.items():
            if E.seen.get(sem, 0) >= val:
                continue
            E.h.wait_ge(sem, val)
            E.seen[sem] = val
            self.nwaits += 1

    def _deps(self, E, reads, writes, skip_own):
        deps = {}
        for b in reads:
            for s, v in b.w.items():
                if deps.get(s, 0) < v:
                    deps[s] = v
        for b in writes:
            for d in ((b.r,) if b.nowaw else (b.w, b.r)):
                for s, v in d.items():
                    if deps.get(s, 0) < v:
                        deps[s] = v
        if skip_own:
            for s in list(deps):
                if self.owner.get(s) == E.name:
                    del deps[s]
        self._wait(E, deps)

    def _commit(self, tok, reads, writes):
        s, v = tok
        for b in reads:
            if b.r.get(s, 0) < v:
                b.r[s] = v
        for b in writes:
            if b.w.get(s, 0) < v:
                b.w[s] = v
            b.r = {}

    def op(self, eng, fn, reads=(), writes=()):
        E = self.E[eng]
        if any(b.excl for b in reads):
            writes = list(writes) + [b for b in reads if b.excl]
            reads = [b for b in reads if not b.excl]
        self._deps(E, reads, writes, (eng == "pe") or (not self.same))
        ins = fn()
        tok = self._tok(E)
        ins.then_inc(tok[0], 1)
        self._commit(tok, reads, writes)
        self.nops += 1
        return ins

    def dma(self, q, fn, reads=(), writes=()):
        E = self.E[q]
        if not E.dma_sems:
            E.dma_sems = [self.new_sem() for _ in range(self.ndma)]
            E.dma_cnt = [0] * self.ndma
        k = E.dma_i % self.ndma
        E.dma_i += 1
        if E.dma_cnt[k] * 16 >= SEM_LIMIT:
            self._wait(E, {E.dma_sems[k]: E.dma_cnt[k] * 16})
            E.dma_sems[k] = self.new_sem()
            E.dma_cnt[k] = 0
        sem = E.dma_sems[k]
        if E.dma_cnt[k] > 0:
            self._wait(E, {sem: E.dma_cnt[k] * 16})
        self._deps(E, reads, writes, False)
        ins = fn()
        E.dma_cnt[k] += 1
        ins.then_inc(sem, 16)
        self._commit((sem, E.dma_cnt[k] * 16), reads, writes)
        self.nops += 1
        return ins

    def barrier(self):
        deps = {}
        for E in self.E.values():
            if E.sem is not None and E.n > 0:
                deps[E.sem] = E.n
            for s, c in zip(E.dma_sems, E.dma_cnt):
                if c > 0:
                    deps[s] = c * 16
        for E in self.E.values():
            self._wait(E, dict(deps))


def _const_tables():
    c = {}
    p = np.arange(128)
    c["ident"] = np.eye(128, dtype=np.float32)
    gam = 1.0 - 2.0 ** (-5.0 - np.arange(4))
    same = (p[:, None] // 64) == (p[None, :] // 64)
    dr = np.zeros((128, 4, 128), np.float32)
    for h in range(4):
        dr[:, h, :] = np.where(same, gam[h] ** np.abs(p[:, None] - p[None, :]), 0.0)
    c["dr"] = dr.reshape(128, 512)
    j = p % 64
    qd = np.stack([gam[h] ** (j + 1.0) for h in range(4)], 1)
    kd = np.stack([gam[h] ** (63.0 - j) for h in range(4)], 1) / 8.0
    c["qd"] = np.repeat(qd, 64, axis=1)
    c["kd0"] = np.repeat(kd * (p[:, None] < 64), 64, axis=1)
    c["kd1"] = np.repeat(kd * (p[:, None] >= 64), 64, axis=1)
    c["cd"] = np.broadcast_to(np.repeat(gam ** 64.0, 64)[None, :], (128, 256)).copy()
    ml = (same & (p[:, None] <= p[None, :])).astype(np.float32)
    mu = (same & (p[:, None] > p[None, :])).astype(np.float32)
    c["ml4"] = np.tile(ml, (1, 4))
    c["mu4"] = np.tile(mu, (1, 4))
    c["tri"] = ml
    c["chk"] = np.stack([(p < 64), (p >= 64)], 1).astype(np.float32)
    c["m0"] = (p < 64).astype(np.float32)[:, None]
    c["m1"] = (p >= 64).astype(np.float32)[:, None]
    c["ifr"] = np.broadcast_to((1.0 / (10000.0 ** (np.arange(32, dtype=np.float32) / 32)))[None, :], (128, 32)).copy()
    c["ifd"] = np.broadcast_to((1.0 / (500000.0 ** (np.arange(8, dtype=np.float32) / 8)))[None, :], (128, 8)).copy()
    c["lts"] = (p[:, None] < p[None, :]).astype(np.float32)
    c["ones"] = np.ones((128, 128), np.float32)
    c["jb"] = np.broadcast_to((np.arange(NB, dtype=np.float32) * BLK)[None, :], (128, NB)).copy()
    coff = np.zeros((128, 12), np.float32)
    for cc in range(12):
        coff[:, cc] = (cc if cc < 8 else cc - 8) * 128 + p
    c["coff"] = coff
    c["gid"] = np.broadcast_to((np.arange(32) // 4).astype(np.float32)[None, :], (128, 32)).copy()
    off = {}
    cur = 0
    arrs = []
    for k, v in c.items():
        v = np.ascontiguousarray(v, dtype=np.float32).reshape(128, -1)
        off[k] = (cur, v.shape[1])
        cur += v.shape[1]
        arrs.append(v)
    return np.concatenate(arrs, axis=1), off


CONST_NP, COFF = _const_tables()
NCONST = CONST_NP.shape[1]


class Prog:
    def __init__(self, stop_after=None, debug=False):
        self.nc = nc = bass.Bass("TRN2", target_bir_lowering=False)
        import os
        self.fw = FW(nc, same=(os.environ.get("SAMEENG", "1") == "1"))
        self.stop_after = stop_after
        self.debug = debug
        self.dram = {}

    def din(self, name, shape, dt=F32):
        t = self.nc.dram_tensor(name, list(shape), dt, kind="ExternalInput")
        T = Tile(t.ap(), name)
        self.dram[name] = T
        return T

    def dout(self, name, shape, dt=F32):
        t = self.nc.dram_tensor(name, list(shape), dt, kind="ExternalOutput")
        T = Tile(t.ap(), name)
        self.dram[name] = T
        return T

    def dint(self, name, shape, dt, dbg=False):
        kind = "ExternalOutput" if (dbg and self.debug) else "Internal"
        t = self.nc.dram_tensor(name, list(shape), dt, kind=kind)
        T = Tile(t.ap(), name)
        T.b.nowaw = True
        self.dram[name] = T
        return T

    def dump(self, name, v, shape, dt=F32):
        if not self.debug:
            return
        T = self.dout(self._uname("dbg_" + name), shape, dt)
        self.dma("sp", T.v, v)

    def _uname(self, name):
        self._uid = getattr(self, "_uid", 0) + 1
        return f"{name}_u{self._uid}"

    def sb(self, st, name, shape, dt):
        name = self._uname(name)
        return Tile(st.enter_context(self.nc.sbuf_tensor(name, list(shape), dt)), name)

    def ps(self, st, name, shape, dt):
        name = self._uname(name)
        T = Tile(st.enter_context(self.nc.psum_tensor(name, list(shape), dt)), name)
        T.b.excl = True
        return T

    def _eng(self, e):
        return {"dve": self.nc.vector, "pool": self.nc.gpsimd, "act": self.nc.scalar}[e]

    def _pe(self, e):
        import os
        if e == "pool" and os.environ.get("NOPOOL", "0") == "1":
            return "dve"
        return e

    def dma(self, q, out, in_, **kw):
        h = {"sp": self.nc.sync, "act": self.nc.scalar, "pool": self.nc.gpsimd}[q]
        return self.fw.dma(q, lambda: h.dma_start(out=out.ap, in_=in_.ap, **kw), reads=[in_.b], writes=[out.b])

    def gather(self, out, table, idx):
        return self.fw.dma("pool", lambda: self.nc.gpsimd.indirect_dma_start(
            out=out.ap, out_offset=None, in_=table.ap,
            in_offset=bass.IndirectOffsetOnAxis(ap=idx.ap, axis=0)), reads=[table.b, idx.b], writes=[out.b])

    def scatter(self, table, in_, idx):
        return self.fw.dma("pool", lambda: self.nc.gpsimd.indirect_dma_start(
            out=table.ap, out_offset=bass.IndirectOffsetOnAxis(ap=idx.ap, axis=0),
            in_=in_.ap, in_offset=None), reads=[in_.b, idx.b], writes=[table.b])

    def mm(self, out, lhsT, rhs, start=True, stop=True):
        return self.fw.op("pe", lambda: self.nc.tensor.matmul(out=out.ap, lhsT=lhsT.ap, rhs=rhs.ap, start=start, stop=stop),
                          reads=[lhsT.b, rhs.b], writes=[out.b])

    def tr(self, out, in_, ident):
        return self.fw.op("pe", lambda: self.nc.tensor.transpose(out=out.ap, in_=in_.ap, identity=ident.ap),
                          reads=[in_.b, ident.b], writes=[out.b])

    def act(self, out, in_, func, scale=1.0, bias=0.0, accum=None):
        rd = [in_.b]
        kw = {}
        if isinstance(scale, V):
            rd.append(scale.b)
            kw["scale"] = scale.ap
        else:
            kw["scale"] = float(scale)
        if isinstance(bias, V):
            rd.append(bias.b)
            kw["bias"] = bias.ap
        else:
            kw["bias"] = float(bias)
        wr = [out.b]
        if accum is not None:
            kw["accum_out"] = accum.ap
            wr.append(accum.b)
        return self.fw.op("act", lambda: self.nc.scalar.activation(out=out.ap, in_=in_.ap, func=func, **kw), reads=rd, writes=wr)

    def tt(self, e, out, a, b, op):
        e = self._pe(e)
        return self.fw.op(e, lambda: self._eng(e).tensor_tensor(out=out.ap, in0=a.ap, in1=b.ap, op=op), reads=[a.b, b.b], writes=[out.b])

    def ts(self, e, out, a, s1, s2=None, op0=ALU.mult, op1=None):
        e = self._pe(e)
        rd = [a.b]
        s1a = s1
        s2a = s2
        if isinstance(s1, V):
            rd.append(s1.b)
            s1a = s1.ap
        if isinstance(s2, V):
            rd.append(s2.b)
            s2a = s2.ap
        kw = {}
        if op1 is not None:
            kw["op1"] = op1
        return self.fw.op(e, lambda: self._eng(e).tensor_scalar(out=out.ap, in0=a.ap, scalar1=s1a, scalar2=s2a, op0=op0, **kw), reads=rd, writes=[out.b])

    def stt(self, e, out, a, s, b, op0, op1):
        e = self._pe(e)
        rd = [a.b, b.b]
        sa = s
        if isinstance(s, V):
            rd.append(s.b)
            sa = s.ap
        return self.fw.op(e, lambda: self._eng(e).scalar_tensor_tensor(out=out.ap, in0=a.ap, scalar=sa, in1=b.ap, op0=op0, op1=op1), reads=rd, writes=[out.b])

    def cp(self, e, out, in_):
        e = self._pe(e)
        if e == "act":
            return self.act(out, in_, AF.Copy)
        return self.fw.op(e, lambda: self._eng(e).tensor_copy(out=out.ap, in_=in_.ap), reads=[in_.b], writes=[out.b])

    def red(self, e, out, in_, op=ALU.add, axis=AX.X):
        return self.fw.op(e, lambda: self._eng(e).tensor_reduce(out=out.ap, in_=in_.ap, axis=axis, op=op), reads=[in_.b], writes=[out.b])

    def recip(self, out, in_):
        return self.fw.op("dve", lambda: self.nc.vector.reciprocal(out=out.ap, in_=in_.ap), reads=[in_.b], writes=[out.b])

    def memset(self, e, out, val):
        e = self._pe(e)
        return self.fw.op(e, lambda: self._eng(e).memset(out.ap, val), writes=[out.b])

    def rsqrt(self, out, in_, scale, eps):
        self.act(out, in_, AF.Ln, scale=scale, bias=self.epsv(eps, out))
        self.act(out, out, AF.Exp, scale=-0.5)

    def epsv(self, eps, like):
        n = like.ap.shape[0]
        return self.C_eps[0:n, 0:1] if eps == EPS else 0.0

    def build(self):
        nc = self.nc
        with contextlib.ExitStack() as gst:
            self.gst = gst
            self.declare_io()
            self.load_consts(gst)
            for l in range(DEPTH):
                self.layer(l)
                if self.stop_after is not None and self.stop_after[0] == l and self.stop_after[1] != "all":
                    break
            self.finish()
        return nc

    def declare_io(self):
        d = self.din
        self.x_in = d("x", [S, D])
        self.c_in = d("c", [128, 8])
        self.pos_in = d("pos", [128, NT], I32)
        self.consts_in = d("consts", [128, NCONST])
        self.w_mod = d("w_mod", [DEPTH, D, 6 * D])
        self.b_mod = d("b_mod", [DEPTH, 6 * D])
        self.norm1_g = d("norm1_g", [DEPTH, D])
        self.norm2_g = d("norm2_g", [DEPTH, D])
        self.w_in = d("w_in", [DEPTH, D, 3344])
        self.ret_norm_g = d("ret_norm_g", [DEPTH, 64])
        self.diff_q_g = d("diff_q_g", [DEPTH, 64])
        self.diff_k_g = d("diff_k_g", [DEPTH, 64])
        self.lam_q1 = d("lam_q1", [DEPTH, 64])
        self.lam_k1 = d("lam_k1", [DEPTH, 64])
        self.lam_q2 = d("lam_q2", [DEPTH, 64])
        self.lam_k2 = d("lam_k2", [DEPTH, 64])
        self.diff_sub_g = d("diff_sub_g", [DEPTH, 128])
        self.gla_w_a2 = d("gla_w_a2", [DEPTH, 16, 128])
        self.gla_b_a = d("gla_b_a", [DEPTH, 128])
        self.gla_norm_g = d("gla_norm_g", [DEPTH, 64])
        self.w_out = d("w_out", [DEPTH, D, D])
        self.router_w = d("router_w", [D, NE])
        self.router_b = d("router_b", [NE])
        self.w_gate = [d(f"w_gate{l}", [NE * D, DE]) for l in range(DEPTH)]
        self.w_up = [d(f"w_up{l}", [NE * D, DE]) for l in range(DEPTH)]
        self.w_down = [d(f"w_down{l}", [NE * DE, D]) for l in range(DEPTH)]
        self.wgub = [self.dint(f"wgub{l}", [NE * D, 2 * DE], BF16) for l in range(DEPTH)]
        self.wdb = [self.dint(f"wdb{l}", [NE * DE, D], BF16) for l in range(DEPTH)]
        self.bg = []
        self.out = self.dout("out", [S, D])
        self.out_b = [Buf(f"out{i}") for i in range(NT)]
        if self.debug:
            self.ydbg = self.dout("ydbg", [S, D])
        self.qT_s = self.dint("qT_s", [4, 128, S], BF16, dbg=True)
        self.kT_s = self.dint("kT_s", [4, 128, S], BF16, dbg=True)
        self.v_s = self.dint("v_s", [S, 512], BF16, dbg=True)
        self.mixT_s = self.dint("mixT_s", [D, S], BF16, dbg=True)
        self.h2_s = self.dint("h2_s", [S, D], BF16, dbg=True)
        self.xs_s = self.dint("xs_s", [NPAD, D], BF16)
        self.ys_s = self.dint("ys_s", [NPAD, D], F32)

    def load_consts(self, st):
        self.CT = self.sb(st, "consts", [128, NCONST], F32)
        self.dma("sp", self.CT.v, self.consts_in.v)
        self.C_eps = self.sb(st, "c_eps", [128, 1], F32)
        self.memset("dve", self.C_eps.v, EPS)
        self.C_npi = self.sb(st, "c_npi", [128, 1], F32)
        self.memset("dve", self.C_npi.v, -math.pi)
        self.C_one = self.sb(st, "c_one", [128, 1], F32)
        self.memset("dve", self.C_one.v, 1.0)
        posi = self.sb(st, "posi", [128, NT], I32)
        self.dma("sp", posi.v, self.pos_in.v)
        self.POSF = self.sb(st, "posf", [128, NT], F32)
        self.cp("dve", self.POSF.v, posi.v)
        self.identb = self.sb(st, "identb", [128, 128], BF16)
        self.cp("dve", self.identb.v, self.cst("ident"))
        self.rope_tables(st)
        self.onesb = self.sb(st, "onesb", [128, 128], BF16)
        self.cp("dve", self.onesb.v, self.cst("ones"))

    def rope_tables(self, gst):
        self.COS = self.sb(gst, "COS", [128, NT, 40], F32)
        self.SIN = self.sb(gst, "SIN", [128, NT, 40], F32)
        o, _ = COFF["ifr"]
        if40 = self.CT[:, o:o + 40]
        C1 = 6.28125
        C2 = 2.0 * math.pi - C1
        with contextlib.ExitStack() as st:
            ANG = self.sb(st, "ANG", [128, NT, 40], F32)
            KI = self.sb(st, "KI", [128, NT, 40], I32)
            KF = self.sb(st, "KF", [128, NT, 40], F32)
            MK = self.sb(st, "MK", [128, NT, 40], F32)
            self.tt("dve", ANG.v, self.POSF.v.unsqueeze(2).bc([128, NT, 40]), if40.unsqueeze(1).bc([128, NT, 40]), ALU.mult)
            self.ts("dve", KF.v, ANG.v, 1.0 / (2.0 * math.pi))
            self.cp("dve", KI.v, KF.v)
            self.cp("dve", KF.v, KI.v)
            self.stt("dve", ANG.v, KF.v, -C1, ANG.v, ALU.mult, ALU.add)
            self.stt("dve", ANG.v, KF.v, -C2, ANG.v, ALU.mult, ALU.add)
            self.ts("dve", MK.v, ANG.v, math.pi, None, op0=ALU.is_gt)
            self.stt("dve", self.SIN.v, MK.v, -2.0 * math.pi, ANG.v, ALU.mult, ALU.add)
            self.ts("dve", MK.v, self.SIN.v, -math.pi, None, op0=ALU.is_lt)
            self.stt("dve", self.SIN.v, MK.v, 2.0 * math.pi, self.SIN.v, ALU.mult, ALU.add)
            self.ts("dve", self.COS.v, self.SIN.v, 0.5 * math.pi, None, op0=ALU.add)
            self.ts("dve", MK.v, self.COS.v, math.pi, None, op0=ALU.is_gt)
            self.stt("dve", self.COS.v, MK.v, -2.0 * math.pi, self.COS.v, ALU.mult, ALU.add)
            self.act(self.SIN.v, self.SIN.v, AF.Sin)
            self.act(self.COS.v, self.COS.v, AF.Sin)
        self.fw.barrier()

    def out_tile(self, i):
        return V(self.out.t[i * 128:(i + 1) * 128, :], self.out_b[i])

    def cst(self, name):
        o, n = COFF[name]
        return self.CT[:, o:o + n]

    def finish(self):
        fw = self.fw
        fw.barrier()

    def layer(self, l):
        x_src = self.x_in if l == 0 else self.out
        sa = self.stop_after
        with contextlib.ExitStack() as lst:
            self.phase_mod(l, lst)
            if sa == (l, "mod"):
                return
            self.queue_weight_casts(l)
            self.phase1(l, x_src)
            self.run_bg(10 ** 9)
            self.fw.barrier()
            if sa == (l, "p1"):
                return
            self.phase2(l)
            self.fw.barrier()
            if sa == (l, "p2"):
                return
            with contextlib.ExitStack() as st3:
                self.LOG = self.sb(st3, f"log{l}", [128, NT, NE], F32)
                self.phase3(l, x_src)
                self.fw.barrier()
                if sa == (l, "p3"):
                    return
                self.phase_moe(l)
                self.fw.barrier()
                if sa == (l, "moe"):
                    return

    def queue_weight_casts(self, l):
        for e in range(NE):
            for src, dst, rows, c0, c1 in ((self.w_gate[l], self.wgub[l], D, 0, DE), (self.w_up[l], self.wgub[l], D, DE, 2 * DE),
                                           (self.w_down[l], self.wdb[l], DE, 0, D)):
                def task(src=src, dst=dst, rows=rows, e=e, c0=c0, c1=c1):
                    self.dma("pool", dst[e * rows:(e + 1) * rows, c0:c1], src[e * rows:(e + 1) * rows, :])
                self.bg.append(task)

    def run_bg(self, n):
        while n > 0 and self.bg:
            self.bg.pop(0)()
            n -= 1

    def phase_mod(self, l, lst):
        MODB = self.sb(lst, f"modb{l}", [128, 6 * D], F32)
        self.MODB = MODB
        with contextlib.ExitStack() as st:
            cT = self.sb(st, "cT", [128, 8], F32)
            self.dma("sp", cT.v, self.c_in.v)
            ca = self.sb(st, "ca", [128, 8], F32)
            self.act(ca.v, cT.v, AF.Silu)
            cb = self.sb(st, "cb", [128, 8, 128], F32)
            for k in range(8):
                self.cp("dve", cb[:, k, :], ca[:, k:k + 1].bc([128, 128]))
            bm = self.sb(st, "bm", [1, 6 * D], F32)
            self.dma("sp", bm.v, self.b_mod[l:l + 1, :])
            ones_f = self.cst("ones")
            wm = [self.sb(st, f"wm{i}", [128, 8, 512], F32) for i in range(2)]
            pm = [self.ps(st, f"pm{i}", [128, 512], F32) for i in range(2)]
            for n in range(12):
                w = wm[n % 2]
                p = pm[n % 2]
                self.dma("sp" if n % 2 == 0 else "act", w.v,
                         self.w_mod[l, :, n * 512:(n + 1) * 512].rearrange("(k p) n -> p k n", p=128))
                for k in range(8):
                    self.mm(p.v, cb[:, k, :], w[:, k, :], start=(k == 0), stop=False)
                self.mm(p.v, ones_f[0:1, :], bm[0:1, n * 512:(n + 1) * 512], start=False, stop=True)
                self.cp("dve" if n % 2 == 0 else "act", MODB[:, n * 512:(n + 1) * 512], p.v)
        self.dump(f"mod{l}", MODB[0:2, :], [2, 6 * D])
        self.fw.barrier()

    def mod(self, j):
        return self.MODB[:, j * D:(j + 1) * D]

    def phase1(self, l, x_src):
        nc = self.nc
        with contextlib.ExitStack() as st:
            sb = lambda n, s, d: self.sb(st, n, s, d)
            ps = lambda n, s, d: self.ps(st, n, s, d)
            WIN = sb("WIN", [128, 8, WINC], BF16)
            WIN.b.nowaw = True
            for k in range(8):
                rows = self.w_in[l, k * 128:(k + 1) * 128, :]
                self.dma("pool", WIN[:, k, 0:1536], rows[:, 0:1536])
                self.dma("pool", WIN[:, k, 1536:3072], rows[:, 1536:3072])
                self.dma("pool", WIN[:, k, 3200:3456], rows[:, 3088:3344])
            import os
            setup = int(os.environ.get("P1SETUP", "9"))
            if setup < 1:
                return
            wga = sb("wga", [128, 8, 16], F32)
            self.dma("sp", wga.v, self.w_in[l, :, 3072:3088].rearrange("(k p) r -> p k r", p=128))
            wa2 = sb("wa2", [16, 128], F32)
            self.dma("sp", wa2.v, self.gla_w_a2[l])
            identf = self.cst("ident")
            PJ = [ps(f"PJ{i}", [128, 512], F32) for i in range(2)]
            TP = [ps(f"TP{i}", [128, 1024], BF16) for i in range(2)]
            SC = ps("SC", [128, 512], F32)
            SC2 = ps("SC2", [128, 512], F32)
            OT = ps("OT", [64, 512], F32)
            MS = ps("MS", [128, 512], F32)
            gaT = sb("gaT", [16, 128], F32)
            for k in range(8):
                self.tr(SC[0:16, 0:128], wga[:, k, :], identf)
                self.cp("dve", gaT.v, SC[0:16, 0:128])
                self.mm(SC2[:, 0:128], gaT.v, wa2.v)
                self.cp("dve", WIN[:, k, 3072:3200], SC2[:, 0:128])
            if setup < 2:
                return
            G1 = sb("G1", [128, D], F32)
            self.dma("sp", G1.v, self.norm1_g[l].pbc(128))
            self.stt("dve", G1.v, self.mod(1), 1.0, G1.v, ALU.add, ALU.mult)
            SH1 = self.mod(0)
            GQ = sb("GQ", [128, 64], F32)
            GK = sb("GK", [128, 64], F32)
            self.dma("sp", GQ.v, self.diff_q_g[l].pbc(128))
            self.dma("sp", GK.v, self.diff_k_g[l].pbc(128))
            self.ts("dve", GQ.v, GQ.v, 0.125)
            BA = sb("BA", [128, 128], F32)
            self.dma("sp", BA.v, self.gla_b_a[l].pbc(128))
            GRN = sb("GRN", [64, 1], F32)
            GGN = sb("GGN", [64, 1], F32)
            self.dma("sp", GRN.v, self.ret_norm_g[l].unsqueeze(1))
            self.dma("sp", GGN.v, self.gla_norm_g[l].unsqueeze(1))
            onesf = self.cst("ones")
            if setup < 3:
                return
            SR = sb("SR", [64, 256], F32)
            SRb = sb("SRb", [64, 256], BF16)
            SG = sb("SG", [32, 256], F32)
            SGb = sb("SGb", [32, 256], BF16)
            for t_ in (SR, SRb, SG, SGb):
                self.memset("dve", t_.v, 0.0)
            XT = [sb(f"XT{i}", [128, D], F32) for i in range(2)]
            ssq = sb("ssq", [128, 1], F32)
            HF = sb("HF", [128, D], F32)
            HB = sb("HB", [128, D], BF16)
            junk = HB
            HT = sb("HT", [128, 8, 128], BF16)
            R1 = sb("R1", [128, 8, 32], F32)
            R2 = sb("R2", [128, 8, 32], F32)
            ROT = sb("ROT", [128, 8, 64], F32)
            QB = sb("QB", [128, 256], BF16)
            QDB = sb("QDB", [128, 256], BF16)
            KB = sb("KB", [128, 256], BF16)
            KD0 = sb("KD0", [128, 256], BF16)
            KD1 = sb("KD1", [128, 256], BF16)
            VR = sb("VR", [128, 256], BF16)
            SGR = sb("SGR", [128, 256], BF16)
            RT = sb("RT", [64, 12, 128], BF16)
            SGT = sb("SGT", [64, 8, 128], BF16)
            DSQ = sb("DSQ", [128, 512], F32)
            GS2 = sb("GS2", [128, 512], F32)
            DSS = sb("DSS", [128, 8], F32)
            DXN = sb("DXN", [128, 8, 64], F32)
            DQB = sb("DQB", [128, 512], BF16)
            DKB = sb("DKB", [128, 512], BF16)
            DT = sb("DT", [128, 8, 128], BF16)
            DVB = sb("DVB", [128, 512], BF16)
            D1 = sb("D1", [128, 8, 8], F32)
            D2 = sb("D2", [128, 8, 8], F32)
            AT = sb("AT", [128, 512], BF16)
            ZT = sb("ZT", [128, 128], F32)
            SP = sb("SP", [128, 128], F32)
            EPt = sb("EPt", [128, 128], F32)
            EMt = sb("EMt", [128, 128], F32)
            GQP = sb("GQP", [128, 128], BF16)
            GQM = sb("GQM", [128, 128], BF16)
            GKP = sb("GKP", [128, 128], BF16)
            GKM = sb("GKM", [128, 128], BF16)
            GKM0 = sb("GKM0", [128, 128], BF16)
            GKM1 = sb("GKM1", [128, 128], BF16)
            GVB = sb("GVB", [128, 256], BF16)
            SGG = sb("SGG", [128, 256], BF16)
            GT = sb("GT", [32, 16, 128], BF16)
            EGL = sb("EGL", [32, 8], F32)
            TMP1 = sb("TMP1", [128, 512], F32)
            OY = sb("OY", [64, 512], F32)
            MXR = sb("MXR", [64, 512], BF16)
            MXG = sb("MXG", [64, 512], BF16)
            KVT = sb("KVT", [64, 256], F32)

            ifr = self.cst("ifr")
            ifd = self.cst("ifd")
            TWO_PI = 2.0 * math.pi

            def post_norm(OTv, G, SGTv, MX):
                OSQ_ = TMP1[0:64, :]
                ORS_ = GS2[0:64, :]
                self.act(OSQ_, OTv, AF.Square)
                self.mm(MS[0:64, :], onesf[0:64, 0:64], OSQ_)
                self.rsqrt(ORS_, MS[0:64, :], 1.0 / 64, EPS)
                self.tt("dve", OY.v, OTv, ORS_, ALU.mult)
                self.stt("dve", MX.v, OY.v, G[:, 0:1], SGTv, ALU.mult, ALU.mult)

            import os
            cut = int(os.environ.get("P1CUT", "9"))
            ntl = int(os.environ.get("P1TILES", str(NT)))
            TPA, TPB = TP[0], TP[1]
            PROJ = [sb(f"PROJ{i}", [128, WINC], F32) for i in range(2)]

            class _PV:
                def __init__(self, tile, off, w):
                    self.tile, self.off, self.w = tile, off, w

                def __getitem__(self, k):
                    rows, cols = k
                    return self.tile[rows, self.off + cols.start:self.off + cols.stop]

                @property
                def v(self):
                    return self.tile[:, self.off:self.off + self.w]

            def stageA(i):
                xt = XT[i % 2]
                self.dma("sp", xt.v, x_src[i * 128:(i + 1) * 128, :] if l == 0 else self.out_tile(i))
                self.act(junk.v, xt.v, AF.Square, accum=ssq.v)
                self.rsqrt(ssq.v, ssq.v, 1.0 / D, EPS)
                self.stt("dve", HF.v, xt.v, ssq[:, 0:1], G1.v, ALU.mult, ALU.mult)
                self.tt("pool", HB.v, HF.v, SH1, ALU.add)
                tp = TPA
                for k in range(8):
                    self.tr(tp[:, k * 128:(k + 1) * 128], HB[:, k * 128:(k + 1) * 128], self.identb.v)
                self.cp("act", HT[:, 0:4, :], tp[:, 0:512].rearrange("p (k t) -> p k t", k=4))
                self.cp("dve", HT[:, 4:8, :], tp[:, 512:1024].rearrange("p (k t) -> p k t", k=4))
                yield
                P_ = PROJ[i % 2]
                for n, width in ((0, 512), (1, 512), (2, 512), (3, 512), (4, 512), (6, 384), (5, 512)):
                    p = PJ[n % 2]
                    for k in range(8):
                        self.mm(p[:, 0:width], HT[:, k, :], WIN[:, k, n * 512:n * 512 + width], start=(k == 0), stop=(k == 7))
                    self.cp("act" if n % 2 == 0 else "dve", P_[:, n * 512:n * 512 + width], p[:, 0:width])
                    yield

            def _hdr(i):
                def proj(n, width):
                    return _PV(PROJ[i % 2], n * 512, width)
                cosr = self.COS[:, i, 0:32].unsqueeze(1).bc([128, 8, 32])
                sinr = self.SIN[:, i, 0:32].unsqueeze(1).bc([128, 8, 32])
                cosd = self.COS[:, i, 32:40].unsqueeze(1).bc([128, 8, 8])
                sind = self.SIN[:, i, 32:40].unsqueeze(1).bc([128, 8, 8])
                return proj, cosr, sinr, cosd, sind

            def stageRG(i):
                proj, cosr, sinr, cosd, sind = _hdr(i)
                yield
                p0 = proj(0, 512)
                pv = p0.v.rearrange("p (g h w) -> p g h w", g=8, h=2)
                X1 = pv[:, :, 0, :]
                X2 = pv[:, :, 1, :]
                rv_ = ROT.v.rearrange("p g (h w) -> p g h w", h=2)
                self.tt("dve", R1.v, X1, cosr, ALU.mult)
                yield
                self.tt("dve", R2.v, X2, sinr, ALU.mult)
                yield
                self.tt("pool", rv_[:, :, 0, :], R1.v, R2.v, ALU.subtract)
                yield
                self.tt("dve", R1.v, X1, sinr, ALU.mult)
                yield
                self.tt("dve", R2.v, X2, cosr, ALU.mult)
                yield
                self.tt("pool", rv_[:, :, 1, :], R1.v, R2.v, ALU.add)
                yield
                rq = ROT[:, 0:4, :].rearrange("p g w -> p (g w)")
                rk = ROT[:, 4:8, :].rearrange("p g w -> p (g w)")
                self.cp("act", QB.v, rq)
                yield
                self.tt("pool", QDB.v, rq, self.cst("qd"), ALU.mult)
                yield
                self.act(KB.v, rk, AF.Copy, scale=0.125)
                yield
                self.tt("pool", KD0.v, rk, self.cst("kd0"), ALU.mult)
                yield
                self.tt("pool", KD1.v, rk, self.cst("kd1"), ALU.mult)
                yield
                if cut < 2:
                    return
                p1 = proj(1, 512)
                self.cp("act", VR.v, p1[:, 0:256])
                yield
                self.act(SGR.v, p1[:, 256:512], AF.Silu)
                yield
                if cut == 2 and os.environ.get("SUB", "9") == "0":
                    return
                tp = TPB
                for h in range(4):
                    self.tr(tp[0:64, h * 128:(h + 1) * 128], QB[:, h * 64:(h + 1) * 64], self.identb.v)
                    self.tr(tp[0:64, (4 + h) * 128:(5 + h) * 128], QDB[:, h * 64:(h + 1) * 64], self.identb.v)
                yield
                self.cp("dve", RT[:, 0:8, :], tp[0:64, :].rearrange("p (k t) -> p k t", k=8))
                yield
                if cut == 2 and os.environ.get("SUB", "9") == "1":
                    return
                tp = TPB
                for h in range(4):
                    self.tr(tp[0:64, h * 128:(h + 1) * 128], KB[:, h * 64:(h + 1) * 64], self.identb.v)
                    self.tr(tp[0:64, (4 + h) * 128:(5 + h) * 128], SGR[:, h * 64:(h + 1) * 64], self.identb.v)
                yield
                self.cp(os.environ.get("GRP2", "act"), RT[:, 8:12, :], tp[0:64, 0:512].rearrange("p (k t) -> p k t", k=4))
                yield
                self.cp("dve", SGT[:, 0:4, :], tp[0:64, 512:1024].rearrange("p (k t) -> p k t", k=4))
                yield
                if cut < 3:
                    return
                for h in range(4):
                    self.mm(SC[:, h * 128:(h + 1) * 128], RT[:, 8 + h, :], RT[:, h, :])
                yield
                self.tt("dve", AT.v, SC.v, self.cst("dr"), ALU.mult)
                yield
                for h in range(4):
                    o = OT[:, h * 128:(h + 1) * 128]
                    self.mm(o, VR[:, h * 64:(h + 1) * 64], AT[:, h * 128:(h + 1) * 128], start=(h == 0), stop=False)
                    self.mm(OT[:, h * 128:h * 128 + 64], SRb[:, h * 64:(h + 1) * 64], RT[:, 4 + h, 0:64], start=False, stop=False)
                yield
                for h in range(4):
                    self.mm(MS[0:64, h * 64:(h + 1) * 64], KD0[:, h * 64:(h + 1) * 64], VR[:, h * 64:(h + 1) * 64])
                    self.mm(MS[0:64, 256 + h * 64:256 + (h + 1) * 64], KD1[:, h * 64:(h + 1) * 64], VR[:, h * 64:(h + 1) * 64])
                yield
                self.tt("dve", KVT.v, SR.v, self.cst("cd")[0:64, :], ALU.mult)
                yield
                self.tt("dve", SR.v, KVT.v, MS[0:64, 0:256], ALU.add)
                yield
                self.cp("act", SRb.v, SR.v)
                yield
                for h in range(4):
                    self.mm(OT[:, h * 128 + 64:(h + 1) * 128], SRb[:, h * 64:(h + 1) * 64], RT[:, 4 + h, 64:128], start=False, stop=True)
                yield
                self.tt("dve", KVT.v, SR.v, self.cst("cd")[0:64, :], ALU.mult)
                yield
                self.tt("dve", SR.v, KVT.v, MS[0:64, 256:512], ALU.add)
                yield
                self.cp("act", SRb.v, SR.v)
                yield
                post_norm(OT.v, GRN, SGT[:, 0:4, :].rearrange("p k t -> p (k t)"), MXR)
                yield
                self.dma("act", self.mixT_s[0:256, i * 128:(i + 1) * 128].rearrange("(h p) t -> p h t", p=64),
                         MXR.v.rearrange("p (h t) -> p h t", h=4))
                yield

                if cut < 4:
                    return
                p6 = proj(6, 384)
                self.tt("dve", ZT.v, p6[:, 0:128], BA.v, ALU.add)
                yield
                self.act(SGG.v, p6[:, 128:384], AF.Silu)
                yield
                self.act(SP.v, ZT.v, AF.Exp, scale=-1.0)
                yield
                self.act(SP.v, SP.v, AF.Ln, bias=self.C_one[:, 0:1])
                yield
                self.mm(SC2[:, 0:128], self.cst("tri"), SP.v)
                yield
                self.act(EPt.v, SC2[:, 0:128], AF.Exp, scale=-1.0 / 16)
                yield
                self.act(EMt.v, SC2[:, 0:128], AF.Exp, scale=1.0 / 16)
                yield
                for h in range(4):
                    self.mm(MS[0:32, 2 * h:2 * h + 2], SP[:, h * 32:(h + 1) * 32], self.cst("chk"))
                yield
                self.act(EGL.v, MS[0:32, 0:8], AF.Exp, scale=-1.0 / 16)
                yield
                p5 = proj(5, 512)
                qs = 32.0 ** -0.5
                self.stt("dve", GQP.v, p5[:, 0:128], qs, EPt.v, ALU.mult, ALU.mult)
                yield
                self.stt("dve", GQM.v, p5[:, 0:128], qs, EMt.v, ALU.mult, ALU.mult)
                yield
                self.tt("dve", GKP.v, p5[:, 128:256], EPt.v, ALU.mult)
                yield
                self.tt("dve", GKM.v, p5[:, 128:256], EMt.v, ALU.mult)
                yield
                self.cp("act", GVB.v, p5[:, 256:512])
                yield
                self.ts("pool", GKM0.v, GKM.v, self.cst("m0")[:, 0:1], None, op0=ALU.mult)
                yield
                self.ts("pool", GKM1.v, GKM.v, self.cst("m1")[:, 0:1], None, op0=ALU.mult)
                yield
                tp = TPB
                for h in range(4):
                    self.tr(tp[0:32, h * 128:(h + 1) * 128], GQP[:, h * 32:(h + 1) * 32], self.identb.v)
                    self.tr(tp[0:32, (4 + h) * 128:(5 + h) * 128], GQM[:, h * 32:(h + 1) * 32], self.identb.v)
                yield
                self.cp("act", GT[:, 0:8, :], tp[0:32, :].rearrange("p (k t) -> p k t", k=8))
                yield
                tp = TPB
                for h in range(4):
                    self.tr(tp[0:32, h * 128:(h + 1) * 128], GKM[:, h * 32:(h + 1) * 32], self.identb.v)
                    self.tr(tp[0:32, (4 + h) * 128:(5 + h) * 128], GKP[:, h * 32:(h + 1) * 32], self.identb.v)
                yield
                self.cp("dve", GT[:, 8:16, :], tp[0:32, :].rearrange("p (k t) -> p k t", k=8))
                yield
                tp = TPB
                for h in range(4):
                    self.tr(tp[0:64, h * 128:(h + 1) * 128], SGG[:, h * 64:(h + 1) * 64], self.identb.v)
                yield
                self.cp("act", SGT[:, 4:8, :], tp[0:64, 0:512].rearrange("p (k t) -> p k t", k=4))
                yield
                for h in range(4):
                    self.mm(SC[:, h * 128:(h + 1) * 128], GT[:, 8 + h, :], GT[:, h, :])
                    self.mm(SC2[:, h * 128:(h + 1) * 128], GT[:, 12 + h, :], GT[:, 4 + h, :])
                yield
                self.tt("dve", TMP1.v, SC.v, self.cst("ml4"), ALU.mult)
                yield
                self.tt("dve", GS2.v, SC2.v, self.cst("mu4"), ALU.mult)
                yield
                self.tt("pool", AT.v, TMP1.v, GS2.v, ALU.add)
                yield
                for h in range(4):
                    self.mm(OT[:, h * 128:(h + 1) * 128], GVB[:, h * 64:(h + 1) * 64], AT[:, h * 128:(h + 1) * 128], start=(h == 0), stop=False)
                    self.mm(OT[:, h * 128:h * 128 + 64], SGb[:, h * 64:(h + 1) * 64], GT[:, h, 0:64], start=False, stop=False)
                yield
                for h in range(4):
                    self.mm(MS[0:32, h * 64:(h + 1) * 64], GKM0[:, h * 32:(h + 1) * 32], GVB[:, h * 64:(h + 1) * 64])
                    self.mm(MS[0:32, 256 + h * 64:256 + (h + 1) * 64], GKM1[:, h * 32:(h + 1) * 32], GVB[:, h * 64:(h + 1) * 64])
                yield
                eg = EGL.v.rearrange("p (h c) -> p h c", c=2)
                sgv = SG.v.rearrange("p (h w) -> p h w", h=4)
                kv3 = KVT[0:32, :].rearrange("p (h w) -> p h w", h=4)
                self.tt("dve", KVT[0:32, :], SG.v, MS[0:32, 0:256], ALU.add)
                yield
                self.tt("dve", sgv, kv3, eg[:, :, 0:1].bc([32, 4, 64]), ALU.mult)
                yield
                self.cp("act", SGb.v, SG.v)
                yield
                for h in range(4):
                    self.mm(OT[:, h * 128 + 64:(h + 1) * 128], SGb[:, h * 64:(h + 1) * 64], GT[:, h, 64:128], start=False, stop=True)
                yield
                self.tt("dve", KVT[0:32, :], SG.v, MS[0:32, 256:512], ALU.add)
                yield
                self.tt("dve", sgv, kv3, eg[:, :, 1:2].bc([32, 4, 64]), ALU.mult)
                yield
                self.cp("act", SGb.v, SG.v)
                yield
                post_norm(OT.v, GGN, SGT[:, 4:8, :].rearrange("p k t -> p (k t)"), MXG)
                yield
                self.dma("act", self.mixT_s[768:1024, i * 128:(i + 1) * 128].rearrange("(h p) t -> p h t", p=64),
                         MXG.v.rearrange("p (h t) -> p h t", h=4))
                yield


            def stageD(i):
                proj, cosr, sinr, cosd, sind = _hdr(i)
                yield
                def dqk(n, G, OUTB, dst):
                    pq = proj(n, 512)
                    self.act(DSQ.v, pq.v, AF.Square)
                    yield
                    self.red("dve", DSS.v, DSQ.v.rearrange("p (g w) -> p g w", g=8))
                    yield
                    self.rsqrt(DSS.v, DSS.v, 1.0 / 64, EPS)
                    yield
                    self.tt("dve", DXN.v, pq.v.rearrange("p (g w) -> p g w", g=8), DSS.v.unsqueeze(2).bc([128, 8, 64]), ALU.mult)
                    yield
                    self.tt("pool", DXN.v, DXN.v, G.v.unsqueeze(1).bc([128, 8, 64]), ALU.mult)
                    yield
                    ob = OUTB.v.rearrange("p (g w) -> p g w", g=8)
                    self.cp("act", ob[:, :, 16:64], DXN[:, :, 16:64])
                    yield
                    x1 = DXN[:, :, 0:8]
                    x2 = DXN[:, :, 8:16]
                    self.tt("dve", D1.v, x1, cosd, ALU.mult)
                    yield
                    self.tt("pool", D2.v, x2, sind, ALU.mult)
                    yield
                    self.tt("dve", ob[:, :, 0:8], D1.v, D2.v, ALU.subtract)
                    yield
                    self.tt("dve", D1.v, x1, sind, ALU.mult)
                    yield
                    self.tt("pool", D2.v, x2, cosd, ALU.mult)
                    yield
                    self.tt("dve", ob[:, :, 8:16], D1.v, D2.v, ALU.add)
                    yield
                    tp = TPA
                    for h in range(4):
                        self.tr(tp[:, h * 128:(h + 1) * 128], OUTB[:, h * 128:(h + 1) * 128], self.identb.v)
                    dtv = DT[:, 0:4, :] if n == 2 else DT[:, 4:8, :]
                    self.cp("act" if n == 2 else "dve", dtv, tp[:, 0:512].rearrange("p (k t) -> p k t", k=4))
                    yield
                    self.dma("sp", dst[:, :, i * 128:(i + 1) * 128].rearrange("h p t -> p h t"), dtv)
                    yield
                yield from dqk(2, GQ, DQB, self.qT_s)
                yield from dqk(3, GK, DKB, self.kT_s)
                p4 = proj(4, 512)
                self.cp("act", DVB.v, p4.v)
                yield
                self.dma("act", self.v_s[i * 128:(i + 1) * 128, :], DVB.v)
                yield

                if cut < 5:
                    return

            for _ in stageA(0):
                pass
            for i in range(ntl):
                self.run_bg(2)
                gens = [stageRG(i), stageD(i)]
                if i + 1 < ntl:
                    gens.append(stageA(i + 1))
                while gens:
                    for g in list(gens):
                        try:
                            next(g)
                        except StopIteration:
                            gens.remove(g)


    def phase2(self, l):
        lam_init = 0.8 - 0.6 * math.exp(-0.3 * l)
        with contextlib.ExitStack() as st:
            sb = lambda n, s, d: self.sb(st, n, s, d)
            ps = lambda n, s, d: self.ps(st, n, s, d)
            KT = sb("KT", [128, 2, S], BF16)
            KT.b.nowaw = True
            VV = sb("VV", [128, NT, 256], BF16)
            VV.b.nowaw = True
            vsrc = self.v_s.v.rearrange("(i p) c -> p i c", p=128)

            def load_group(hg):
                for hh in range(2):
                    for j in range(4):
                        self.dma("sp" if (hh + j) % 2 == 0 else "act", KT[:, hh, j * 2048:(j + 1) * 2048], self.kT_s[2 * hg + hh, :, j * 2048:(j + 1) * 2048])
                for j in range(8):
                    self.dma("sp" if j % 2 == 0 else "act", VV[:, j * 8:(j + 1) * 8, :], vsrc[:, j * 8:(j + 1) * 8, hg * 256:(hg + 1) * 256])
            lq = sb("lq", [1, 4, 64], F32)
            for j, src in enumerate((self.lam_q1, self.lam_k1, self.lam_q2, self.lam_k2)):
                self.dma("sp", lq[:, j, :], src[l:l + 1, :])
            lp = sb("lp", [1, 2, 64], F32)
            self.tt("dve", lp[:, 0, :], lq[:, 0, :], lq[:, 1, :], ALU.mult)
            self.tt("dve", lp[:, 1, :], lq[:, 2, :], lq[:, 3, :], ALU.mult)
            ls = sb("ls", [1, 2], F32)
            self.red("dve", ls.v, lp.v)
            self.act(ls.v, ls.v, AF.Exp)
            lam1 = sb("lam1", [1, 1], F32)
            self.tt("dve", lam1.v, ls[:, 0:1], ls[:, 1:2], ALU.subtract)
            self.ts("dve", lam1.v, lam1.v, lam_init, -1.0, op0=ALU.add, op1=ALU.mult)
            NSB = 4
            SPS = [ps(f"SPS{i}", [128, 512], F32) for i in range(NSB)]
            OP = [ps(f"OP{i}", [128, 512], F32) for i in range(2)]
            MSP = ps("MSP", [128, 512], F32)
            MSP2 = ps("MSP2", [128, 512], F32)
            NLAM = sb("NLAM", [128, 1], F32)
            self.mm(MSP[:, 0:1], self.cst("ones")[0:1, :], lam1.v)
            self.cp("dve", NLAM.v, MSP[:, 0:1])
            SUBG = sb("SUBG", [128, 1], F32)
            self.dma("sp", SUBG.v, self.diff_sub_g[l].unsqueeze(1))
            self.ts("dve", SUBG.v, SUBG.v, 1.0 - lam_init)
            QT = [sb(f"QT{i}", [128, 512], BF16) for i in range(2)]
            PT = [sb(f"PT{i}", [128, 512], BF16) for i in range(4)]
            ACC = [[sb(f"ACC{p_}{c}", [128, 512], F32) for c in range(2)] for p_ in range(2)]
            OS = [sb(f"OS{c}", [128, 512], F32) for c in range(2)]
            R0 = sb("R0", [128, 512], F32)
            R1 = sb("R1", [128, 512], F32)
            ACB = [sb(f"ACB{c}", [128, 512], BF16) for c in range(2)]
            T0 = sb("T0", [128, 512], F32)
            T1 = sb("T1", [128, 512], F32)
            OSQ = sb("OSQ2", [128, 512], BF16)
            ORS = sb("ORS2", [128, 512], F32)
            OB = [sb(f"OB{i}", [128, 512], BF16) for i in range(2)]
            onesf = self.cst("ones")
            import os
            nqt = int(os.environ.get("P2QT", str(S // 512)))
            groups = [(a, b_, c_) for a in range(2) for b_ in range(nqt) for c_ in range(2)]
            units = []
            for gi, (hg, qt, hh) in enumerate(groups):
                nk = 4 * qt + 4
                for kt in range(nk):
                    for c in range(2):
                        units.append((gi, c, kt, nk))
            LOOK = 2
            qloaded = set()

            def load_q(gi):
                if gi >= len(groups) or gi in qloaded:
                    return
                qloaded.add(gi)
                hg, qt, hh = groups[gi]
                if qt == 0 and hh == 0:
                    load_group(hg)
                self.dma("sp", QT[gi % 2].v, self.qT_s[2 * hg + hh, :, qt * 512:(qt + 1) * 512])

            def c0_of(gi, kt):
                qt = groups[gi][1]
                m = kt - 4 * qt
                return (128 * m if m > 0 else 0), m

            def issue_qk(u):
                gi, c, kt, nk = units[u]
                load_q(gi)
                hh = groups[gi][2]
                c0, m = c0_of(gi, kt)
                self.mm(SPS[u % NSB][:, c0:512], KT[64 * c:64 * c + 64, hh, kt * 128:(kt + 1) * 128], QT[gi % 2][64 * c:64 * c + 64, c0:512])

            def finish(gi):
                hg, qt, hh = groups[gi]
                h = 2 * hg + hh
                acc = ACC[gi % 2]
                self.cp("act", OS[0].v, OP[0].v)
                self.cp("act", OS[1].v, OP[1].v)
                self.cp("dve", ACB[0].v, acc[0].v)
                self.cp("dve", ACB[1].v, acc[1].v)
                self.mm(MSP.v, self.onesb.v, ACB[0].v)
                self.mm(MSP2.v, self.onesb.v, ACB[1].v)
                self.act(R0.v, MSP.v, AF.Ln)
                self.act(R0.v, R0.v, AF.Exp, scale=-1.0)
                self.act(R1.v, MSP2.v, AF.Ln)
                self.act(R1.v, R1.v, AF.Exp, scale=-1.0)
                self.tt("dve", T0.v, OS[0].v, R0.v, ALU.mult)
                self.tt("dve", T1.v, OS[1].v, R1.v, ALU.mult)
                self.stt("dve", T0.v, T1.v, NLAM[:, 0:1], T0.v, ALU.mult, ALU.add)
                self.act(OSQ.v, T0.v, AF.Square)
                self.mm(MSP.v, self.onesb.v, OSQ.v)
                self.act(ORS.v, MSP.v, AF.Ln, scale=1.0 / 128, bias=self.C_eps[:, 0:1])
                self.act(ORS.v, ORS.v, AF.Exp, scale=-0.5)
                ob = OB[gi % 2]
                self.stt("dve", ob.v, T0.v, SUBG[:, 0:1], ORS.v, ALU.mult, ALU.mult)
                self.dma("act", self.mixT_s[256 + 128 * h:256 + 128 * (h + 1), qt * 512:(qt + 1) * 512], ob.v)

            def hg_of(u):
                return groups[units[u][0]][0]

            for u, (gi, c, kt, nk) in enumerate(units):
                if u == 0 or hg_of(u - 1) != hg_of(u):
                    for u2 in range(u, min(u + LOOK, len(units))):
                        if hg_of(u2) == hg_of(u):
                            issue_qk(u2)
                if u % 2 == 0:
                    for u2 in (u + LOOK, u + LOOK + 1):
                        if u2 < len(units) and hg_of(u2) == hg_of(u):
                            issue_qk(u2)
                hh = groups[gi][2]
                c0, m = c0_of(gi, kt)
                pt = PT[u % 4]
                if kt == 0 and c == 0 and gi + 1 < len(groups) and groups[gi + 1][0] == groups[gi][0]:
                    load_q(gi + 1)
                self.act(pt[:, c0:512], SPS[u % NSB][:, c0:512], AF.Exp)
                if m >= 0:
                    self.memset("pool", pt[64:128, c0:c0 + 64], 0.0)
                self.mm(OP[c][:, c0:512], VV[:, kt, hh * 128:(hh + 1) * 128], pt[:, c0:512], start=(kt == 0), stop=(kt == nk - 1))
                a = ACC[gi % 2][c]
                if kt == 0:
                    self.cp("dve", a.v, pt.v)
                else:
                    self.tt("dve", a[:, c0:512], a[:, c0:512], pt[:, c0:512], ALU.add)
                if kt == nk - 1 and c == 1:
                    finish(gi)

    def phase3(self, l, x_src):
        with contextlib.ExitStack() as st:
            sb = lambda n, s, d: self.sb(st, n, s, d)
            ps = lambda n, s, d: self.ps(st, n, s, d)
            WO = sb("WO", [128, 8, D], BF16)
            WO.b.nowaw = True
            for k in range(8):
                self.dma("pool", WO[:, k, :], self.w_out[l, k * 128:(k + 1) * 128, :])
            G2 = sb("G2", [128, D], F32)
            self.dma("sp", G2.v, self.norm2_g[l].pbc(128))
            self.stt("dve", G2.v, self.mod(4), 1.0, G2.v, ALU.add, ALU.mult)
            SH2 = self.mod(3)
            GT1 = self.mod(2)
            RW = sb("RW", [128, 8, NE], F32)
            self.dma("sp", RW.v, self.router_w.v.rearrange("(k p) e -> p k e", p=128))
            MT = [sb(f"MT{i}", [128, 8, 512], BF16) for i in range(2)]
            XT = [sb(f"X3{i}", [128, D], F32) for i in range(2)]
            X1 = [sb(f"X1{i}", [128, D], F32) for i in range(2)]
            TMs = [sb(f"TM3{i}", [128, D], F32) for i in range(2)]
            junks = [sb(f"junk3{i}", [128, D], BF16) for i in range(2)]
            ssqs = [sb(f"ssq3{i}", [128, 1], F32) for i in range(2)]
            H2Fs = [sb(f"H2F{i}", [128, D], F32) for i in range(2)]
            H2B = [sb(f"H2B{i}", [128, D], BF16) for i in range(2)]
            H2Ts = [sb(f"H2T{i}", [128, D], F32) for i in range(2)]
            POs = [ps(f"PO{i}", [128, 512], F32) for i in range(2)]
            PTRs = [ps(f"PTR{i}", [128, 512], F32) for i in range(2)]
            PLs = [ps(f"PL{i}", [128, NE], F32) for i in range(2)]
            identf = self.cst("ident")
            mview = self.mixT_s.v.rearrange("(k p) t -> p k t", p=128)

            def tile3(i):
                s_ = i % 2
                TM, junk, ssq, H2F, H2T, PO, PTR, PL = TMs[s_], junks[s_], ssqs[s_], H2Fs[s_], H2Ts[s_], POs[s_], PTRs[s_], PLs[s_]
                mt = MT[(i // 4) % 2]
                if i % 4 == 0:
                    self.dma("sp", mt.v, mview[:, :, i * 128:i * 128 + 512])
                xt = XT[i % 2]
                x1 = X1[i % 2]
                self.dma("act", xt.v, x_src[i * 128:(i + 1) * 128, :] if l == 0 else self.out_tile(i))
                yield
                tcol = (i % 4) * 128
                for hf in range(2):
                    for k in range(8):
                        self.mm(PO.v, mt[:, k, tcol:tcol + 128], WO[:, k, hf * 512:(hf + 1) * 512], start=(k == 0), stop=(k == 7))
                    self.tt("dve", TM[:, hf * 512:(hf + 1) * 512], PO.v, GT1[:, hf * 512:(hf + 1) * 512], ALU.mult)
                    yield
                self.tt("pool", x1.v, TM.v, xt.v, ALU.add)
                yield
                self.dma("sp", self.out_tile(i), x1.v)
                self.act(junk.v, x1.v, AF.Square, accum=ssq.v)
                yield
                self.rsqrt(ssq.v, ssq.v, 1.0 / D, EPS)
                yield
                self.stt("dve", H2F.v, x1.v, ssq[:, 0:1], G2.v, ALU.mult, ALU.mult)
                yield
                self.tt("pool", H2F.v, H2F.v, SH2, ALU.add)
                yield
                hb = H2B[i % 2]
                self.cp("act", hb.v, H2F.v)
                self.dma("act", self.h2_s[i * 128:(i + 1) * 128, :], hb.v)
                yield
                for hf in range(2):
                    for k in range(4):
                        kk = hf * 4 + k
                        self.tr(PTR[:, k * 128:(k + 1) * 128], H2F[:, kk * 128:(kk + 1) * 128], identf)
                    self.cp("dve" if hf == 0 else "act", H2T[:, hf * 512:(hf + 1) * 512], PTR.v)
                    yield
                for k in range(8):
                    self.mm(PL.v, H2T[:, k * 128:(k + 1) * 128], RW[:, k, :], start=(k == 0), stop=(k == 7))
                self.cp("dve", self.LOG[:, i, :], PL.v)
                yield

            for pi in range(NT // 2):
                gens = [tile3(2 * pi), tile3(2 * pi + 1)]
                while gens:
                    for g in list(gens):
                        try:
                            next(g)
                        except StopIteration:
                            gens.remove(g)
            self.dump(f"log{l}", self.LOG.v, [128, NT, NE])

    def phase_moe(self, l):
        import os
        BIG = 1.0e30
        NTE = NT * NE
        with contextlib.ExitStack() as rst:
            rsb = lambda n, s_, d: self.sb(rst, n, s_, d)
            RG1 = rsb("RG1", [128, NT], F32)
            RG2 = rsb("RG2", [128, NT], F32)
            D1I = rsb("D1I", [128, NT], I32)
            D2I = rsb("D2I", [128, NT], I32)
            IDXW = rsb("IDXW", [128, NB, 12], I32)
            with contextlib.ExitStack() as st:
                sb = lambda n, s_, d: self.sb(st, n, s_, d)
                ps = lambda n, s_, d: self.ps(st, n, s_, d)
                RB = sb("RB", [128, NE], F32)
                self.dma("sp", RB.v, self.router_b.v.pbc(128))
                SCO = sb("SCO", [128, NT, NE], F32)
                BIA = sb("BIA", [128, NT, NE], F32)
                TA = sb("TA", [128, NT, NE], F32)
                TB = sb("TB", [128, NT, NE], F32)
                OH1 = sb("OH1", [128, NT, NE], F32)
                OH2 = sb("OH2", [128, NT, NE], F32)
                M1 = sb("M1", [128, NT * 8], F32)
                M2 = sb("M2", [128, NT * 8], F32)
                GM = sb("GM", [128, NT], F32)
                V1 = sb("V1", [128, NT], F32)
                W1 = sb("W1", [128, NT], F32)
                W2 = sb("W2", [128, NT], F32)
                self.act(SCO.v, self.LOG.v, AF.Sigmoid)
                self.tt("dve", BIA.v, SCO.v, RB.v.unsqueeze(1).bc([128, NT, NE]), ALU.add)
                b4 = BIA.v.rearrange("p i (g k) -> p (i g) k", k=4)
                self.red("dve", M1.v, b4, op=ALU.max)
                ta4 = TA.v.rearrange("p i (g k) -> p (i g) k", k=4)
                self.tt("dve", ta4, b4, M1.v.unsqueeze(2).bc([128, NT * 8, 4]), ALU.is_equal)
                self.stt("dve", TA.v, TA.v, -BIG, BIA.v, ALU.mult, ALU.add)
                self.red("dve", M2.v, ta4, op=ALU.max)
                self.tt("dve", M1.v, M1.v, M2.v, ALU.add)
                gs3 = M1.v.rearrange("p (i g) -> p i g", g=8)
                self.red("dve", GM.v, gs3, op=ALU.max)
                self.tt("dve", M2.v.rearrange("p (i g) -> p i g", g=8), gs3, GM.v.unsqueeze(2).bc([128, NT, 8]), ALU.is_equal)
                self.ts("dve", ta4, M2.v.unsqueeze(2).bc([128, NT * 8, 4]), BIG, -BIG, op0=ALU.mult, op1=ALU.add)
                self.tt("dve", TA.v, TA.v, BIA.v, ALU.add)
                self.red("dve", V1.v, TA.v, op=ALU.max)
                self.tt("dve", OH1.v, TA.v, V1.v.unsqueeze(2).bc([128, NT, NE]), ALU.is_equal)
                self.stt("dve", TB.v, OH1.v, -BIG, TA.v, ALU.mult, ALU.add)
                self.red("dve", V1.v, TB.v, op=ALU.max)
                self.tt("dve", OH2.v, TB.v, V1.v.unsqueeze(2).bc([128, NT, NE]), ALU.is_equal)
                self.tt("dve", TA.v, SCO.v, OH1.v, ALU.mult)
                self.red("dve", W1.v, TA.v)
                self.tt("dve", TA.v, SCO.v, OH2.v, ALU.mult)
                self.red("dve", W2.v, TA.v)
                self.tt("dve", V1.v, W1.v, W2.v, ALU.add)
                self.recip(V1.v, V1.v)
                self.tt("dve", RG1.v, W1.v, V1.v, ALU.mult)
                self.tt("dve", RG2.v, W2.v, V1.v, ALU.mult)
                MS_ = sb("MSEL", [128, NT, NE], BF16)
                self.tt("dve", MS_.v, OH1.v, OH2.v, ALU.add)
                LTSb = sb("LTSb", [128, 128], BF16)
                self.cp("dve", LTSb.v, self.cst("lts"))
                PRE = sb("PRE", [128, NT, NE], F32)
                TOT = sb("TOT", [128, NT, NE], F32)
                PP = [ps(f"PP{i}", [128, 512], F32) for i in range(2)]
                msf = MS_.v.rearrange("p i e -> p (i e)")
                pre_f = PRE.v.rearrange("p i e -> p (i e)")
                tot_f = TOT.v.rearrange("p i e -> p (i e)")
                for c in range(NTE // 512):
                    self.mm(PP[0].v, LTSb.v, msf[:, c * 512:(c + 1) * 512])
                    self.cp("dve", pre_f[:, c * 512:(c + 1) * 512], PP[0].v)
                    self.mm(PP[1].v, self.onesb.v, msf[:, c * 512:(c + 1) * 512])
                    self.cp("act", tot_f[:, c * 512:(c + 1) * 512], PP[1].v)
                A_, B_ = TA, TB
                self.cp("dve", A_.v, TOT.v)
                k = 1
                while k < NT:
                    self.tt("dve", B_[:, k:, :], A_[:, k:, :], A_[:, :NT - k, :], ALU.add)
                    self.cp("dve", B_[:, :k, :], A_[:, :k, :])
                    A_, B_ = B_, A_
                    k *= 2
                INC = A_
                OFF = B_
                self.tt("dve", OFF.v, INC.v, TOT.v, ALU.subtract)
                CNT = INC[:, NT - 1, :]
                CMP = sb("CMP", [128, NE, 32], F32)
                self.tt("dve", CMP.v, CNT.unsqueeze(2).bc([128, NE, 32]), self.cst("jb")[:, 0:32].unsqueeze(1).bc([128, NE, 32]), ALU.is_gt)
                PADE = sb("PADE", [128, NE], F32)
                self.red("dve", PADE.v, CMP.v)
                self.ts("dve", PADE.v, PADE.v, float(BLK))
                EA = sb("EA", [128, NE], F32)
                EB = sb("EB", [128, NE], F32)
                self.cp("dve", EA.v, PADE.v)
                a_, b_ = EA, EB
                k = 1
                while k < NE:
                    self.tt("dve", b_[:, k:], a_[:, k:], a_[:, :NE - k], ALU.add)
                    self.cp("dve", b_[:, :k], a_[:, :k])
                    a_, b_ = b_, a_
                    k *= 2
                PEND = a_
                PST = b_
                self.tt("dve", PST.v, PEND.v, PADE.v, ALU.subtract)
                self.tt("dve", PRE.v, PRE.v, OFF.v, ALU.add)
                self.tt("dve", PRE.v, PRE.v, PST.v.unsqueeze(1).bc([128, NT, NE]), ALU.add)
                self.tt("dve", TOT.v, PRE.v, OH1.v, ALU.mult)
                self.red("dve", W1.v, TOT.v)
                self.tt("dve", TOT.v, PRE.v, OH2.v, ALU.mult)
                self.red("dve", W2.v, TOT.v)
                self.cp("dve", D1I.v, W1.v)
                self.cp("dve", D2I.v, W2.v)
                CM2 = sb("CM2", [128, NB, NE], F32)
                self.tt("dve", CM2.v, PEND.v.unsqueeze(1).bc([128, NB, NE]), self.cst("jb").unsqueeze(2).bc([128, NB, NE]), ALU.is_le)
                BE = sb("BE", [128, NB], F32)
                self.red("dve", BE.v, CM2.v)
                self.ts("dve", BE.v, BE.v, float(NE - 1), None, op0=ALU.min)
                IDXF = sb("IDXF", [128, NB, 12], F32)
                coff = self.cst("coff")
                self.stt("dve", IDXF[:, :, 0:8], BE.v.unsqueeze(2).bc([128, NB, 8]), float(D), coff[:, 0:8].unsqueeze(1).bc([128, NB, 8]), ALU.mult, ALU.add)
                self.stt("dve", IDXF[:, :, 8:12], BE.v.unsqueeze(2).bc([128, NB, 4]), float(DE), coff[:, 8:12].unsqueeze(1).bc([128, NB, 4]), ALU.mult, ALU.add)
                self.cp("dve", IDXW.v, IDXF.v)
                if self.debug:
                    self.dump(f"d1_{l}", W1.v, [128, NT])
                    self.dump(f"d2_{l}", W2.v, [128, NT])
                    self.dump(f"g1_{l}", RG1.v, [128, NT])
                    self.dump(f"g2_{l}", RG2.v, [128, NT])
                    self.dump(f"be_{l}", BE.v, [128, NB])
            self.fw.barrier()
            if os.environ.get("MOECUT", "9") == "0":
                return
            with contextlib.ExitStack() as st:
                sb = lambda n, s_, d: self.sb(st, n, s_, d)
                HB = [sb(f"HBm{i}", [128, D], BF16) for i in range(3)]
                for i in range(NT):
                    hb = HB[i % 3]
                    self.dma("sp", hb.v, self.h2_s[i * 128:(i + 1) * 128, :])
                    self.scatter(self.xs_s.v, hb.v, D1I[:, i:i + 1])
                    self.scatter(self.xs_s.v, hb.v, D2I[:, i:i + 1])
            self.fw.barrier()
            if os.environ.get("MOECUT", "9") == "1":
                return
            with contextlib.ExitStack() as st:
                sb = lambda n, s_, d: self.sb(st, n, s_, d)
                ps = lambda n, s_, d: self.ps(st, n, s_, d)
                WGU = [sb(f"WGU{i}", [128, 8, 2 * DE], BF16) for i in range(2)]
                WD = [sb(f"WD{i}", [128, 4, D], BF16) for i in range(2)]
                for t_ in WGU + WD:
                    t_.b.nowaw = True
                XB = [sb(f"XB{i}", [128, NSUB, D], BF16) for i in range(2)]
                XTB = [sb(f"XTB{i}", [128, 8, BLK], BF16) for i in range(2)]
                SGt = sb("SGt", [128, BLK], F32)
                UT = sb("UT", [128, 4, BLK], BF16)
                YB = [sb(f"YB{i}", [128, D], F32) for i in range(2)]
                TPX = ps("TPX", [128, 1024], BF16)
                PG = [ps(f"PG{i}", [128, BLK], F32) for i in range(2)]
                PU = [ps(f"PU{i}", [128, BLK], F32) for i in range(2)]
                PD = [ps(f"PD{i}", [128, 512], F32) for i in range(2)]
                wgu_t = self.wgub[l].v
                wd_t = self.wdb[l].v
                nblk = int(os.environ.get("MOEBLK", str(NB)))
                yi = 0
                for j in range(nblk):
                    wgu, wd, xb, xtb = WGU[j % 2], WD[j % 2], XB[j % 2], XTB[j % 2]
                    for c in range(8):
                        self.gather(wgu[:, c, :], wgu_t, IDXW[:, j, c:c + 1])
                    for c in range(4):
                        self.gather(wd[:, c, :], wd_t, IDXW[:, j, 8 + c:9 + c])
                    self.dma("sp", xb.v, self.xs_s[j * BLK:(j + 1) * BLK, :].rearrange("(s p) d -> p s d", p=128))
                    for sub in range(NSUB):
                        for k in range(8):
                            self.tr(TPX[:, k * 128:(k + 1) * 128], xb[:, sub, k * 128:(k + 1) * 128], self.identb.v)
                        self.cp("dve" if sub % 2 == 0 else "act", xtb[:, :, sub * 128:(sub + 1) * 128], TPX.v.rearrange("p (k t) -> p k t", k=8))
                    for fc in range(4):
                        pg, pu = PG[fc % 2], PU[fc % 2]
                        for k in range(8):
                            self.mm(pg.v, wgu[:, k, fc * 128:(fc + 1) * 128], xtb[:, k, :], start=(k == 0), stop=(k == 7))
                        for k in range(8):
                            self.mm(pu.v, wgu[:, k, DE + fc * 128:DE + (fc + 1) * 128], xtb[:, k, :], start=(k == 0), stop=(k == 7))
                        self.act(SGt.v, pg.v, AF.Silu)
                        self.tt("dve", UT[:, fc, :], SGt.v, pu.v, ALU.mult)
                    for sub in range(NSUB):
                        yb = YB[yi % 2]
                        yi += 1
                        for hf in range(2):
                            pd = PD[hf]
                            for fc in range(4):
                                self.mm(pd.v, UT[:, fc, sub * 128:(sub + 1) * 128], wd[:, fc, hf * 512:(hf + 1) * 512], start=(fc == 0), stop=(fc == 3))
                            self.cp("act" if hf == 0 else "dve", yb[:, hf * 512:(hf + 1) * 512], pd.v)
                        self.dma("act", self.ys_s[j * BLK + sub * 128:j * BLK + (sub + 1) * 128, :], yb.v)
            self.fw.barrier()
            if os.environ.get("MOECUT", "9") == "2":
                return
            with contextlib.ExitStack() as st:
                sb = lambda n, s_, d: self.sb(st, n, s_, d)
                GT2 = self.mod(5)
                Y1 = [sb(f"Y1{i}", [128, D], F32) for i in range(2)]
                Y2 = [sb(f"Y2{i}", [128, D], F32) for i in range(2)]
                XX = [sb(f"XX{i}", [128, D], F32) for i in range(2)]
                TT = [sb(f"TT{i}", [128, D], F32) for i in range(2)]
                def loads(i):
                    self.gather(Y1[i % 2].v, self.ys_s.v, D1I[:, i:i + 1])
                    self.gather(Y2[i % 2].v, self.ys_s.v, D2I[:, i:i + 1])
                    self.dma("sp", XX[i % 2].v, self.out_tile(i))

                loads(0)
                for i in range(NT):
                    y1, y2, xx, tt_ = Y1[i % 2], Y2[i % 2], XX[i % 2], TT[i % 2]
                    if i + 1 < NT:
                        loads(i + 1)
                    self.ts("dve", tt_.v, y1.v, RG1[:, i:i + 1], None, op0=ALU.mult)
                    self.stt("dve", tt_.v, y2.v, RG2[:, i:i + 1], tt_.v, ALU.mult, ALU.add)
                    if self.debug and l == 0:
                        self.dma("act", self.ydbg[i * 128:(i + 1) * 128, :], tt_.v)
                    self.tt("pool", tt_.v, tt_.v, GT2, ALU.mult)
                    self.tt("pool", xx.v, xx.v, tt_.v, ALU.add)
                    self.dma("sp", self.out_tile(i), xx.v)


_CACHE = {}


def _get_prog(stop_after=None, debug=False):
    key = (stop_after, debug)
    if key not in _CACHE:
        p = Prog(stop_after=stop_after, debug=debug)
        p.build()
        _CACHE[key] = p
    return _CACHE[key]


def make_in_map(inputs, b):
    f = lambda a: np.ascontiguousarray(a, dtype=np.float32)
    m = {
        "x": f(inputs["x"][b]),
        "c": f(np.asarray(inputs["c"][b]).reshape(8, 128).T),
        "pos": np.ascontiguousarray(np.asarray(inputs["positions"][b]).reshape(NT, 128).T.astype(np.int32)),
        "consts": CONST_NP,
    }
    for k in ("w_mod", "b_mod", "norm1_g", "norm2_g", "w_in", "ret_norm_g", "diff_q_g", "diff_k_g", "lam_q1", "lam_k1",
              "lam_q2", "lam_k2", "diff_sub_g", "gla_w_a2", "gla_b_a", "gla_norm_g", "w_out", "router_w", "router_b"):
        m[k] = f(inputs[k])
    for l in range(DEPTH):
        m[f"w_gate{l}"] = f(inputs["w_gate"][l]).reshape(NE * D, DE)
        m[f"w_up{l}"] = f(inputs["w_up"][l]).reshape(NE * D, DE)
        m[f"w_down{l}"] = f(inputs["w_down"][l]).reshape(NE * DE, D)
    return m


def kernel(**inputs):
    prog = _get_prog()
    shared = make_in_map(inputs, 0)
    in_maps = []
    for b in range(8):
        m = dict(shared)
        m["x"] = np.ascontiguousarray(inputs["x"][b], dtype=np.float32)
        m["c"] = np.ascontiguousarray(np.asarray(inputs["c"][b], dtype=np.float32).reshape(8, 128).T)
        m["pos"] = np.ascontiguousarray(np.asarray(inputs["positions"][b]).reshape(NT, 128).T.astype(np.int32))
        in_maps.append(m)
    res = run_bass_kernel_spmd(prog.nc, in_maps, core_ids=list(range(8)))
    return np.stack([np.asarray(r["out"], dtype=np.float32) for r in res.results], axis=0)
```
